# Optimizing a Trainium2 kernel written in Bass

```python
import jax, jax.numpy as jnp
from jax import lax
import numpy as np

D_MODEL = 1024
BATCH = 32
SEQ = 2048
DEPTH = 1

CHUNK = 64
EPS = 1e-6
RWKV_HEAD = 64
RWKV_HEADS = 8
RWKV_WIDTH = RWKV_HEADS * RWKV_HEAD
DECAY_LORA = 64
ICLR_LORA = 64
GATE_LORA = 128
GN_EPS = 64e-5
MLSTM_HEADS = 4
MLSTM_DK = 64
MLSTM_DV = 128
MLSTM_QK = MLSTM_HEADS * MLSTM_DK
MLSTM_WIDTH = MLSTM_HEADS * MLSTM_DV
CONV_WIDTH = 4
GATE_SOFTCAP = 15.0
MIX_WIDTH = RWKV_WIDTH + MLSTM_WIDTH
RWKV_COLS = 3 * RWKV_WIDTH + DECAY_LORA + ICLR_LORA + GATE_LORA
MLSTM_COLS = 2 * MLSTM_QK + 2 * MLSTM_WIDTH + 2 * MLSTM_HEADS
IN_COLS = RWKV_COLS + MLSTM_COLS
N_GROUPS = 4
EXPERTS_PER_GROUP = 8
N_EXPERTS = N_GROUPS * EXPERTS_PER_GROUP
TOP_K = 2
D_EXPERT = 256

kernel_name = 'hybrid_rwkv7_mlstm_hmoe_block'


def rms_norm(x, w):
    xf = x.astype(jnp.float32)
    y = xf * lax.rsqrt(jnp.mean(xf * xf, axis=-1, keepdims=True) + EPS)
    return (y * w.astype(jnp.float32)).astype(x.dtype)


def modulate(xn, shift, scale):
    return xn * (1.0 + scale[:, None, :]) + shift[:, None, :]


def token_shift(u, mu):
    prev = jnp.pad(u, ((0, 0), (1, 0), (0, 0)))[:, :-1]
    return u + (prev - u) * mu


def causal_dwconv(u, w, b):
    ch = u.shape[-1]
    out = lax.conv_general_dilated(
        u, w[:, None, :].astype(u.dtype), window_strides=(1,),
        padding=[(w.shape[0] - 1, 0)], dimension_numbers=('NWC', 'WIO', 'NWC'),
        feature_group_count=ch)
    return out + b


def softcap(z):
    return GATE_SOFTCAP * jnp.tanh(z / GATE_SOFTCAP)


def rwkv7_group(u, mu, w0, w_up, a0, a_up, g_up, k_k, k_a, r_k, gn_w, gn_b):
    bsz, seq, _ = u.shape
    f32 = jnp.float32
    u = token_shift(u, mu)
    o1 = RWKV_WIDTH
    o2 = 2 * RWKV_WIDTH
    o3 = 3 * RWKV_WIDTH
    o4 = o3 + DECAY_LORA
    o5 = o4 + ICLR_LORA
    r, k, v, wd, ad, gd = jnp.split(u, [o1, o2, o3, o4, o5], axis=-1)
    w_log = -jax.nn.softplus(-(w0 + jnp.tanh(wd) @ w_up)) - 0.5
    decay = jnp.exp(-jnp.exp(w_log.astype(f32)))
    a = jax.nn.sigmoid(a0 + ad @ a_up)
    g = jax.nn.sigmoid(gd) @ g_up
    heads = lambda t: t.reshape(bsz, seq, RWKV_HEADS, RWKV_HEAD).astype(f32)
    kk = heads(k * k_k)
    kk = kk / jnp.maximum(jnp.sqrt(jnp.sum(kk * kk, axis=-1, keepdims=True)), 1e-12)
    k = k * (1.0 + (a - 1.0) * k_a)
    r_h, k_h, v_h, w_h, a_h = heads(r), heads(k), heads(v), heads(decay), heads(a)
    b_h = kk * a_h

    def step(state, inp):
        r_t, w_t, k_t, v_t, kk_t, b_t = inp
        sa = jnp.einsum('bhij,bhj->bhi', state, -kk_t)
        state = (state * w_t[:, :, None, :] + sa[..., None] * b_t[:, :, None, :]
                 + v_t[..., None] * k_t[:, :, None, :])
        return state, jnp.einsum('bhij,bhj->bhi', state, r_t)

    xs = tuple(jnp.swapaxes(t, 0, 1) for t in (r_h, w_h, k_h, v_h, kk, b_h))
    state0 = jnp.zeros((bsz, RWKV_HEADS, RWKV_HEAD, RWKV_HEAD), f32)
    _, y = lax.scan(step, state0, xs)
    y = jnp.swapaxes(y, 0, 1)
    mean = jnp.mean(y, axis=-1, keepdims=True)
    var = jnp.mean(jnp.square(y - mean), axis=-1, keepdims=True)
    y = (y - mean) * lax.rsqrt(var + GN_EPS)
    y = y.reshape(bsz, seq, RWKV_WIDTH) * gn_w + gn_b
    bonus = jnp.sum(r_h * k_h * r_k.astype(f32), axis=-1, keepdims=True) * v_h
    y = (y + bonus.reshape(bsz, seq, RWKV_WIDTH)) * g
    return y.astype(u.dtype)


def to_chunks(t):
    bsz, seq, nh = t.shape[:3]
    t = t.reshape((bsz, seq // CHUNK, CHUNK, nh) + t.shape[3:])
    return t.transpose((1, 0, 3, 2) + tuple(range(4, t.ndim)))


def mlstm_chunkwise(q, k, v, log_i, log_f):
    bsz, seq = q.shape[:2]
    f32 = jnp.float32
    causal = jnp.tril(jnp.ones((CHUNK, CHUNK), dtype=bool))

    def step(carry, inp):
        c_mat, n_vec, m = carry
        qc, kc, vc, li, lf = inp
        b = jnp.cumsum(lf, axis=-1)
        d_mat = jnp.where(causal, b[..., :, None] - b[..., None, :] + li[..., None, :], -jnp.inf)
        inter = b + m[..., None]
        m_t = jnp.maximum(inter, jnp.max(d_mat, axis=-1))
        scores = jnp.einsum('bhtd,bhsd->bhts', qc, kc) * jnp.exp(d_mat - m_t[..., None])
        w_inter = jnp.exp(inter - m_t)
        num = (jnp.einsum('bhts,bhsv->bhtv', scores, vc)
               + w_inter[..., None] * jnp.einsum('bhtd,bhdv->bhtv', qc, c_mat))
        den = jnp.sum(scores, axis=-1) + w_inter * jnp.einsum('bhtd,bhd->bht', qc, n_vec)
        h = num / jnp.maximum(jnp.abs(den), jnp.exp(-m_t))[..., None]
        end_log = b[..., -1:] - b + li
        m_new = jnp.maximum(b[..., -1] + m, jnp.max(end_log, axis=-1))
        carry_w = jnp.exp(b[..., -1] + m - m_new)
        kw = kc * jnp.exp(end_log - m_new[..., None])[..., None]
        c_mat = carry_w[..., None, None] * c_mat + jnp.einsum('bhsd,bhsv->bhdv', kw, vc)
        n_vec = carry_w[..., None] * n_vec + jnp.sum(kw, axis=-2)
        return (c_mat, n_vec, m_new), h

    carry0 = (jnp.zeros((bsz, MLSTM_HEADS, MLSTM_DK, MLSTM_DV), f32),
              jnp.zeros((bsz, MLSTM_HEADS, MLSTM_DK), f32),
              jnp.zeros((bsz, MLSTM_HEADS), f32))
    xs = (to_chunks(q * (MLSTM_DK ** -0.5)), to_chunks(k), to_chunks(v),
          to_chunks(log_i), to_chunks(log_f))
    _, h = lax.scan(step, carry0, xs)
    return h.transpose(1, 0, 3, 2, 4).reshape(bsz, seq, MLSTM_HEADS, MLSTM_DV)


def mlstm_group(u, conv_w, conv_b, i_b, f_b, hn_w):
    bsz, seq, _ = u.shape
    f32 = jnp.float32
    o1 = 2 * MLSTM_QK
    o2 = o1 + MLSTM_WIDTH
    o3 = o2 + MLSTM_WIDTH
    o4 = o3 + MLSTM_HEADS
    qk, v, o, ig, fg = jnp.split(u, [o1, o2, o3, o4], axis=-1)
    qk = jax.nn.silu(causal_dwconv(qk, conv_w, conv_b))
    q, k = jnp.split(qk, 2, axis=-1)
    log_i = softcap((ig + i_b).astype(f32))
    log_f = jax.nn.log_sigmoid(softcap((fg + f_b).astype(f32)))
    heads_k = lambda t: t.reshape(bsz, seq, MLSTM_HEADS, MLSTM_DK).astype(f32)
    h = mlstm_chunkwise(heads_k(q), heads_k(k),
                        v.reshape(bsz, seq, MLSTM_HEADS, MLSTM_DV).astype(f32), log_i, log_f)
    h = h * lax.rsqrt(jnp.mean(h * h, axis=-1, keepdims=True) + EPS)
    h = h.reshape(bsz, seq, MLSTM_WIDTH) * hn_w
    return (h * jax.nn.sigmoid(o.astype(f32))).astype(u.dtype)


def hier_moe(xn, w_group, b_group, w_router, b_router, w_gate, w_up, w_down):
    bsz, seq, _ = xn.shape
    f32 = jnp.float32
    g_logits = (xn @ w_group + b_group).astype(f32)
    g_prob = jax.nn.softmax(g_logits, axis=-1)
    g_idx = jnp.argmax(g_logits, axis=-1)
    g_onehot = jax.nn.one_hot(g_idx, N_GROUPS, dtype=f32)
    g_w = jnp.max(g_prob, axis=-1)
    e_logits = (xn @ w_router + b_router).astype(f32).reshape(bsz, seq, N_GROUPS, EXPERTS_PER_GROUP)
    sel = jnp.sum(e_logits * g_onehot[..., None], axis=2)
    top_v, top_i = lax.top_k(sel, TOP_K)
    top_w = jax.nn.softmax(top_v, axis=-1)
    within = jnp.sum(jax.nn.one_hot(top_i, EXPERTS_PER_GROUP, dtype=f32) * top_w[..., None], axis=-2)
    combine = (g_onehot[..., None] * within[..., None, :] * g_w[..., None, None])
    combine = combine.reshape(bsz, seq, N_EXPERTS).astype(xn.dtype)

    def per_sequence(args):
        xs, cw = args
        hg = jnp.einsum('sd,edf->sef', xs, w_gate)
        hu = jnp.einsum('sd,edf->sef', xs, w_up)
        act = jax.nn.silu(hg) * hu * cw[..., None]
        return jnp.einsum('sef,efd->sd', act, w_down)

    return lax.map(per_sequence, (xn, combine))


def setup_inputs(seed: int = 0) -> dict:
    key = jax.random.key(seed)
    ks = jax.random.split(key, 32)
    L, D = DEPTH, D_MODEL

    def nrm(i, shape, scale):
        return scale * jax.random.normal(ks[i], shape, jnp.float32)

    def uni(i, shape, lo, hi):
        return jax.random.uniform(ks[i], shape, jnp.float32, lo, hi)

    return {
        'x': nrm(0, (BATCH, SEQ, D), 1.0),
        'c': nrm(1, (BATCH, D), 1.0),
        'ada_w': nrm(2, (L, D, 6 * D), 0.5 * D ** -0.5),
        'ada_b': nrm(3, (L, 6 * D), 0.01),
        'mix_norm_w': 1.0 + nrm(4, (L, D), 0.02),
        'w_in': nrm(5, (L, D, IN_COLS), D ** -0.5),
        'rwkv_mu': uni(6, (L, RWKV_COLS), 0.0, 1.0),
        'rwkv_w0': uni(7, (L, RWKV_WIDTH), -4.0, 0.0),
        'rwkv_w_up': nrm(8, (L, DECAY_LORA, RWKV_WIDTH), DECAY_LORA ** -0.5),
        'rwkv_a0': nrm(9, (L, RWKV_WIDTH), 0.5),
        'rwkv_a_up': nrm(10, (L, ICLR_LORA, RWKV_WIDTH), ICLR_LORA ** -0.5),
        'rwkv_g_up': nrm(11, (L, GATE_LORA, RWKV_WIDTH), GATE_LORA ** -0.5),
        'rwkv_k_k': 0.85 + nrm(12, (L, RWKV_WIDTH), 0.05),
        'rwkv_k_a': 1.0 + nrm(13, (L, RWKV_WIDTH), 0.05),
        'rwkv_r_k': nrm(14, (L, RWKV_HEADS, RWKV_HEAD), 0.1),
        'rwkv_gn_w': 1.0 + nrm(15, (L, RWKV_WIDTH), 0.02),
        'rwkv_gn_b': nrm(16, (L, RWKV_WIDTH), 0.01),
        'mlstm_conv_w': nrm(17, (L, CONV_WIDTH, 2 * MLSTM_QK), CONV_WIDTH ** -0.5),
        'mlstm_conv_b': nrm(18, (L, 2 * MLSTM_QK), 0.01),
        'mlstm_i_b': -3.0 + nrm(19, (L, MLSTM_HEADS), 0.1),
        'mlstm_f_b': jnp.linspace(3.0, 6.0, MLSTM_HEADS, dtype=jnp.float32)[None, :] + nrm(20, (L, MLSTM_HEADS), 0.1),
        'mlstm_hn_w': 1.0 + nrm(21, (L, MLSTM_WIDTH), 0.02),
        'w_out': nrm(22, (L, MIX_WIDTH, D), MIX_WIDTH ** -0.5),
        'ffn_norm_w': 1.0 + nrm(23, (L, D), 0.02),
        'moe_w_group': nrm(24, (L, D, N_GROUPS), D ** -0.5),
        'moe_b_group': nrm(25, (L, N_GROUPS), 0.01),
        'moe_w_router': nrm(26, (L, D, N_EXPERTS), D ** -0.5),
        'moe_b_router': nrm(27, (L, N_EXPERTS), 0.01),
        'moe_w_gate': nrm(28, (L, N_EXPERTS, D, D_EXPERT), D ** -0.5),
        'moe_w_up': nrm(29, (L, N_EXPERTS, D, D_EXPERT), D ** -0.5),
        'moe_w_down': nrm(30, (L, N_EXPERTS, D_EXPERT, D), D_EXPERT ** -0.5),
        'final_norm_w': 1.0 + nrm(31, (D,), 0.02),
    }


def reference(x, c, ada_w, ada_b, mix_norm_w, w_in, rwkv_mu, rwkv_w0, rwkv_w_up, rwkv_a0,
              rwkv_a_up, rwkv_g_up, rwkv_k_k, rwkv_k_a, rwkv_r_k, rwkv_gn_w, rwkv_gn_b,
              mlstm_conv_w, mlstm_conv_b, mlstm_i_b, mlstm_f_b, mlstm_hn_w, w_out, ffn_norm_w,
              moe_w_group, moe_b_group, moe_w_router, moe_b_router, moe_w_gate, moe_w_up,
              moe_w_down, final_norm_w):
    for l in range(DEPTH):
        mod = jax.nn.silu(c) @ ada_w[l] + ada_b[l]
        sh_m, sc_m, g_m, sh_f, sc_f, g_f = jnp.split(mod, 6, axis=-1)
        h = modulate(rms_norm(x, mix_norm_w[l]), sh_m, sc_m)
        proj = h @ w_in[l]
        y_r = rwkv7_group(proj[..., :RWKV_COLS], rwkv_mu[l], rwkv_w0[l], rwkv_w_up[l], rwkv_a0[l],
                          rwkv_a_up[l], rwkv_g_up[l], rwkv_k_k[l], rwkv_k_a[l], rwkv_r_k[l],
                          rwkv_gn_w[l], rwkv_gn_b[l])
        y_m = mlstm_group(proj[..., RWKV_COLS:], mlstm_conv_w[l], mlstm_conv_b[l], mlstm_i_b[l],
                          mlstm_f_b[l], mlstm_hn_w[l])
        y = jnp.concatenate([y_r, y_m], axis=-1) @ w_out[l]
        x = x + g_m[:, None, :] * y
        h = modulate(rms_norm(x, ffn_norm_w[l]), sh_f, sc_f)
        y = hier_moe(h, moe_w_group[l], moe_b_group[l], moe_w_router[l], moe_b_router[l],
                     moe_w_gate[l], moe_w_up[l], moe_w_down[l])
        x = x + g_f[:, None, :] * y
    return rms_norm(x, final_norm_w)
```

```python
import numpy as np
from concourse.bass_utils import run_bass_kernel_spmd
import numpy as np
from contextlib import ExitStack
import concourse.bass as bass
import concourse.mybir as mybir

F32 = mybir.dt.float32
BF16 = mybir.dt.bfloat16
AF = mybir.ActivationFunctionType
ALU = mybir.AluOpType
AX = mybir.AxisListType

ENGS = ("pe", "act", "dve", "pool", "sp")
SEM_CAP = 30000


class Buf:
    __slots__ = ("name", "writers", "readers", "psum")

    def __init__(self, name, psum=False):
        self.name = name
        self.writers = {}
        self.readers = []
        self.psum = psum


class V:
    __slots__ = ("ap", "buf")

    def __init__(self, ap, buf):
        self.ap = ap
        self.buf = buf

    def __getitem__(self, k):
        return V(self.ap[k], self.buf)

    def re(self, pat, **kw):
        return V(self.ap.rearrange(pat, **kw), self.buf)

    def bc(self, axis, n):
        a = self.ap.unsqueeze(axis)
        shp = list(a.shape)
        shp[axis] = n
        return V(a.to_broadcast(shp), self.buf)

    def sub(self, name):
        return V(self.ap, Buf(name))


class Op:
    __slots__ = ("eng", "fn", "idx", "eidx", "dma_key", "deps", "signals", "sigval", "semi", "extra", "phase")

    def __init__(self, eng, fn, dma_key):
        self.eng = eng
        self.fn = fn
        self.dma_key = dma_key
        self.deps = []
        self.signals = False
        self.sigval = 0
        self.semi = 0
        self.extra = []


def _bufs(vs):
    out = []
    for v in vs:
        if isinstance(v, V):
            out.append(v.buf)
        elif isinstance(v, Buf):
            out.append(v)
    return out


class Prog:
    def __init__(self, nc):
        self.nc = nc
        self.ops = []
        self.eops = {e: [] for e in ENGS}
        self.gstack = ExitStack()
        self.pstack = None
        self.cnt = {e: 0 for e in ENGS}
        self.dcnt = {}
        self.esems = {e: [] for e in ENGS}
        self.dsems = {}
        self.free_dsems = []
        self.waited = {e: {} for e in ENGS}
        self.carry = []
        self.nops_total = 0
        self.phase = 0

    def _stack(self):
        return self.pstack if self.pstack is not None else self.gstack

    def tile(self, name, shape, dt=F32):
        t = self._stack().enter_context(self.nc.sbuf_tensor(name, list(shape), dt))
        return V(t[:], Buf(name))

    def psum(self, name, shape, dt=F32):
        t = self._stack().enter_context(self.nc.psum_tensor(name, list(shape), dt))
        return V(t[:], Buf(name, psum=True))

    def begin_phase(self):
        self.pstack = ExitStack()
        self.ops = []
        self.eops = {e: [] for e in ENGS}

    def add(self, eng, fn, reads=(), writes=(), dma_key=None):
        op = Op(eng, fn, dma_key)
        op.idx = len(self.ops)
        op.phase = self.phase
        op.eidx = len(self.eops[eng])
        deps = {}
        wkey = ("dma", dma_key) if dma_key is not None else eng
        rb = _bufs(reads)
        wb = _bufs(writes)
        for b in rb:
            for w in b.writers.values():
                deps[id(w)] = w
            if b.psum:
                for r in b.readers:
                    if r.eng != eng:
                        deps[id(r)] = r
        for b in wb:
            for w in b.writers.values():
                deps[id(w)] = w
            for r in b.readers:
                deps[id(r)] = r
        deps.pop(id(op), None)
        op.deps = list(deps.values())
        for b in rb:
            b.readers.append(op)
        for b in wb:
            b.writers[wkey] = op
            b.readers = []
        self.ops.append(op)
        self.eops[eng].append(op)
        return op

    def mm(self, out, lhsT, rhs, start=True, stop=True, extra_r=()):
        return self.add("pe", lambda e: e.matmul(out.ap, lhsT=lhsT.ap, rhs=rhs.ap, start=start, stop=stop),
                        [lhsT, rhs] + list(extra_r), [out])

    def tr(self, out, in_, ident):
        return self.add("pe", lambda e: e.transpose(out.ap, in_.ap, ident.ap), [in_, ident], [out])

    def act(self, out, in_, func, bias=None, scale=None, accum=None, eng="act"):
        kw = {}
        r = [in_]
        if bias is not None:
            kw["bias"] = bias.ap if isinstance(bias, V) else bias
            if isinstance(bias, V):
                r.append(bias)
        if scale is not None:
            kw["scale"] = scale.ap if isinstance(scale, V) else scale
            if isinstance(scale, V):
                r.append(scale)
        w = [out]
        if accum is not None:
            kw["accum_out"] = accum.ap
            w.append(accum)
        return self.add(eng, lambda e: e.activation(out=out.ap, in_=in_.ap, func=func, **kw), r, w)

    def tt(self, eng, out, in0, in1, op):
        return self.add(eng, lambda e: e.tensor_tensor(out=out.ap, in0=in0.ap, in1=in1.ap, op=op), [in0, in1], [out])

    def ts(self, eng, out, in0, s1, op0, s2=None, op1=None, accum=None):
        r = [in0]
        a1 = s1.ap if isinstance(s1, V) else s1
        a2 = s2.ap if isinstance(s2, V) else s2
        if isinstance(s1, V):
            r.append(s1)
        if isinstance(s2, V):
            r.append(s2)
        w = [out]
        kw = {}
        if op1 is not None:
            kw["op1"] = op1
        if accum is not None:
            kw["accum_out"] = accum.ap
            w.append(accum)
        return self.add(eng, lambda e: e.tensor_scalar(out=out.ap, in0=in0.ap, scalar1=a1, scalar2=a2, op0=op0, **kw), r, w)

    def stt(self, eng, out, in0, scalar, in1, op0, op1):
        eng = "dve"
        r = [in0, in1]
        a = scalar.ap if isinstance(scalar, V) else scalar
        if isinstance(scalar, V):
            r.append(scalar)
        return self.add(eng, lambda e: e.scalar_tensor_tensor(out=out.ap, in0=in0.ap, scalar=a, in1=in1.ap, op0=op0, op1=op1), r, [out])

    def copy(self, eng, out, in_):
        if eng == "act":
            return self.add(eng, lambda e: e.copy(out=out.ap, in_=in_.ap), [in_], [out])
        return self.add(eng, lambda e: e.tensor_copy(out=out.ap, in_=in_.ap), [in_], [out])

    def red(self, eng, out, in_, op, axis=AX.X):
        return self.add(eng, lambda e: e.tensor_reduce(out=out.ap, in_=in_.ap, axis=axis, op=op), [in_], [out])

    def memset(self, eng, out, val):
        return self.add(eng, lambda e: e.memset(out.ap, val), [], [out])

    def dma(self, eng, out, in_, key, **kw):
        r = [in_] if isinstance(in_, V) else []
        w = [out] if isinstance(out, V) else []
        oa = out.ap if isinstance(out, V) else out
        ia = in_.ap if isinstance(in_, V) else in_
        return self.add(eng, lambda e: e.dma_start(out=oa, in_=ia, **kw), r, w, dma_key=key)

    def end_phase(self):
        nc = self.nc
        ops = self.ops
        need = {}
        for op in ops:
            wl = []
            for p in op.deps:
                if p.phase != self.phase:
                    continue
                if p.dma_key is not None:
                    wl.append(p)
                elif p.eng != op.eng:
                    p.signals = True
                    wl.append(p)
                else:
                    if op.eng == "pe" and op.dma_key is None:
                        continue
                    if op.dma_key is not None or (op.eidx - p.eidx) <= 4:
                        p.signals = True
                        wl.append(p)
            need[id(op)] = wl
        lastc = {}
        for e in ENGS:
            for op in reversed(self.eops[e]):
                if op.dma_key is None:
                    op.signals = True
                    lastc[e] = op
                    break
        for op in ops:
            if op.dma_key is not None:
                if op.dma_key not in self.dsems:
                    if self.free_dsems:
                        sem_, c0_ = self.free_dsems.pop()
                        self.dsems[op.dma_key] = sem_
                        self.dcnt[op.dma_key] = c0_
                    else:
                        self.dsems[op.dma_key] = self.gstack.enter_context(nc.semaphore("d_%s" % (op.dma_key,)))
                        self.dcnt[op.dma_key] = 0
                self.dcnt[op.dma_key] += 16
                op.sigval = self.dcnt[op.dma_key]
            elif op.signals:
                c = self.cnt[op.eng]
                op.semi = c // SEM_CAP
                op.sigval = c % SEM_CAP + 1
                self.cnt[op.eng] = c + 1
                while len(self.esems[op.eng]) <= op.semi:
                    i = len(self.esems[op.eng])
                    self.esems[op.eng].append(self.gstack.enter_context(nc.semaphore("s_%s_%d" % (op.eng, i))))
        plans = {e: [] for e in ENGS}
        first = {e: True for e in ENGS}
        for op in ops:
            ws = {}
            if first[op.eng]:
                first[op.eng] = False
                for key, sem, v in self.carry:
                    if self.waited[op.eng].get(key, 0) < v:
                        ws[key] = (sem, v)
            for p in need[id(op)]:
                if p.dma_key is not None:
                    sem = self.dsems[p.dma_key]
                    key = ("dsem", id(sem))
                else:
                    key = (p.eng, p.semi)
                    sem = self.esems[p.eng][p.semi]
                v = p.sigval
                if self.waited[op.eng].get(key, 0) >= v:
                    continue
                if key not in ws or ws[key][1] < v:
                    ws[key] = (sem, v)
            for key, (sem, v) in ws.items():
                self.waited[op.eng][key] = v
            if op.dma_key is not None:
                inc = (self.dsems[op.dma_key], 16)
            elif op.signals:
                inc = (self.esems[op.eng][op.semi], 1)
            else:
                inc = None
            plans[op.eng].append((op, list(ws.values()), inc))
        carry = [(("dsem", id(self.dsems[k])), self.dsems[k], v) for k, v in self.dcnt.items()]
        for e, op in lastc.items():
            carry.append(((e, op.semi), self.esems[e][op.semi], op.sigval))
        self.carry = carry + [c for c in self.carry if c[0] not in {x[0] for x in carry}]
        final_waits = [(sem, v) for (_, sem, v) in self.carry]

        def run(engobj, plan, final=None):
            for op, ws, inc in plan:
                for sem, v in ws:
                    engobj.wait_ge(sem, v)
                ins = op.fn(engobj)
                if inc is not None:
                    ins.then_inc(inc[0], inc[1])
            if final:
                for sem, v in final:
                    engobj.wait_ge(sem, v)

        with nc.Block() as block:
            @block.tensor
            def _(e):
                run(e, plans["pe"])

            @block.scalar
            def _(e):
                run(e, plans["act"])

            @block.vector
            def _(e):
                run(e, plans["dve"])

            @block.gpsimd
            def _(e):
                run(e, plans["pool"])

            @block.sync
            def _(e):
                run(e, plans["sp"], final_waits)
        self.nops_total += len(ops)
        self.phase += 1
        for k_ in list(self.dsems):
            self.free_dsems.append((self.dsems.pop(k_), self.dcnt.pop(k_)))
        self.ops = []
        self.eops = {e: [] for e in ENGS}
        if self.pstack is not None:
            self.pstack.close()
            self.pstack = None

    def finish(self):
        self.gstack.close()

D = 1024
INC = 3336
RW = 1792
NE = 32
DE = 256
EPS = 1e-6
GN_EPS = 64e-5
DEC = 0.6065306597126334

PARAM_SHAPES = {
    "ada_w": [1024, 6144], "ada_b": [1, 6144], "mix_norm_w": [1, 1024], "w_in": [1024, 3336],
    "rwkv_mu": [1, 1792], "rwkv_w0": [1, 512], "rwkv_w_up": [64, 512], "rwkv_a0": [1, 512],
    "rwkv_a_up": [64, 512], "rwkv_g_up": [128, 512], "rwkv_k_k": [1, 512], "rwkv_k_a": [1, 512],
    "rwkv_r_k": [1, 512], "rwkv_gn_w": [1, 512], "rwkv_gn_b": [1, 512], "mlstm_conv_w": [4, 512],
    "mlstm_conv_b": [1, 512], "mlstm_i_b": [1, 4], "mlstm_f_b": [1, 4], "mlstm_hn_w": [1, 512],
    "w_out": [1024, 1024], "ffn_norm_w": [1, 1024], "moe_w_group": [1024, 4], "moe_b_group": [1, 4],
    "moe_w_router": [1024, 32], "moe_b_router": [1, 32], "moe_w_gate": [32, 1024, 256],
    "moe_w_up": [32, 1024, 256], "moe_w_down": [32, 256, 1024], "final_norm_w": [1, 1024],
}


def build(NSEQ, SEQ, taps=None, stop_after=99):
    nc = bass.Bass("TRN2", target_bir_lowering=False)
    NT = SEQ // 128
    NTOK = NSEQ * SEQ
    NTILES = NSEQ * NT
    PADR = SEQ + 3
    dr = {}
    dr["x"] = nc.dram_tensor("x", [NTOK, D], F32, kind="ExternalInput").ap()
    dr["c"] = nc.dram_tensor("c", [NSEQ, D], F32, kind="ExternalInput").ap()
    for k, shp in PARAM_SHAPES.items():
        dr[k] = nc.dram_tensor(k, shp, F32, kind="ExternalInput").ap()
    out_d = nc.dram_tensor("out", [NTOK, D], F32, kind="ExternalOutput").ap()
    proj_s = nc.dram_tensor("proj_s", [NSEQ * PADR, INC], F32).ap()
    x1_s = nc.dram_tensor("x1_s", [NTOK, D], F32).ap()
    h2T_s = nc.dram_tensor("h2T_s", [128, 8, NTOK], BF16).ap()
    cw_s = nc.dram_tensor("cw_s", [NTOK, NE], F32).ap()
    mod_s = nc.dram_tensor("mod_s", [NSEQ, 6144], F32).ap()
    wg_s = nc.dram_tensor("wg_s", [NE, 128, 2048], BF16).ap()
    wu_s = nc.dram_tensor("wu_s", [NE, 128, 2048], BF16).ap()
    wd_s = nc.dram_tensor("wd_s", [NE, 128, 2048], BF16).ap()
    tapd = {}

    P = Prog(nc)

    def tap(name, v, shape):
        if taps is None or name not in taps:
            return
        t = nc.dram_tensor("tap_" + name, list(shape), v.ap.dtype, kind="ExternalOutput").ap()
        tapd[name] = t
        P.dma("sp", t, v, "tap_" + name)

    identf = P.tile("identf", [128, 128])
    ident = P.tile("ident", [128, 128], BF16)
    tri = P.tile("tri", [128, 128])
    onesf = P.tile("onesf", [128, 128])
    trimid = P.tile("trimid", [128, 128])
    indmid = P.tile("indmid", [128, 2])
    masknegf = P.tile("masknegf", [128, 128])
    maskA = P.tile("maskA", [128, 4, 128], BF16)
    mSI = P.tile("mSI", [128, 2, 2, 128], BF16)
    epsM = P.tile("epsM", [128, 1])
    epsG = P.tile("epsG", [128, 1])
    gamM = P.tile("gamM", [128, NSEQ, 8])
    shM = P.tile("shM", [128, NSEQ, 8])
    gamF = P.tile("gamF", [128, NSEQ, 8])
    shF = P.tile("shF", [128, NSEQ, 8])

    P.begin_phase()
    P.memset("pool", identf, 1.0)
    P.add("pool", lambda e: e.affine_select(identf.ap, identf.ap, [[-1, 128]], ALU.is_equal, 0.0, base=0, channel_multiplier=1), [identf], [identf])
    P.copy("pool", ident, identf)
    P.memset("pool", tri, 1.0)
    P.add("pool", lambda e: e.affine_select(tri.ap, tri.ap, [[1, 128]], ALU.is_ge, 0.0, base=0, channel_multiplier=-1), [tri], [tri])
    P.memset("pool", onesf, 1.0)
    colm = P.tile("colm", [128, 128])
    P.memset("pool", colm, 1.0)
    P.add("pool", lambda e: e.affine_select(colm.ap, colm.ap, [[0, 128]], ALU.is_ge, 0.0, base=63, channel_multiplier=-1), [colm], [colm])
    P.tt("pool", trimid, tri, colm, ALU.subtract)
    P.ts("pool", trimid, trimid, -DEC, ALU.mult)
    P.ts("pool", indmid[:, 0:1], colm[:, 0:1], -DEC, ALU.mult)
    P.ts("pool", indmid[:, 1:2], colm[:, 0:1], DEC, ALU.mult, -DEC, ALU.add)
    P.memset("pool", masknegf, 0.0)
    P.add("pool", lambda e: e.affine_select(masknegf.ap, masknegf.ap, [[1, 128]], ALU.is_ge, -30000.0, base=0, channel_multiplier=-1), [masknegf], [masknegf])
    mstr = P.tile("mstr", [128, 128])
    P.memset("pool", mstr, 1.0)
    P.add("pool", lambda e: e.affine_select(mstr.ap, mstr.ap, [[1, 128]], ALU.is_ge, 0.0, base=-1, channel_multiplier=-1), [mstr], [mstr])
    mlow = P.tile("mlow", [128, 128])
    P.memset("pool", mlow, 1.0)
    P.add("pool", lambda e: e.affine_select(mlow.ap, mlow.ap, [[-1, 128]], ALU.is_ge, 0.0, base=-1, channel_multiplier=1), [mlow], [mlow])
    for hh in range(4):
        P.copy("pool", maskA[:, hh, :], mlow)
    for hh in range(2):
        P.copy("pool", mSI[:, hh, 0, :], mstr)
        P.copy("pool", mSI[:, hh, 1, :], tri)
    P.memset("pool", epsM, EPS)
    P.memset("pool", epsG, GN_EPS)

    def bcload(dst, src, n, key):
        P.dma("sp", dst, src.partition_broadcast(n), key)

    ct = P.tile("ct", [128, D])
    P.dma("sp", ct[0:NSEQ, :], dr["c"][:, :], "c12")
    sct = P.tile("sct", [128, D])
    P.act(sct[0:NSEQ, :], ct[0:NSEQ, :], AF.Silu)
    psA = P.psum("psA", [128, 512])
    psB = P.psum("psB", [128, 512])
    psC = P.psum("psC", [128, 512])
    scT = P.tile("scT", [128, 8, NSEQ])
    for k in range(8):
        P.mm(psA[:, k * NSEQ:(k + 1) * NSEQ], sct[0:NSEQ, k * 128:(k + 1) * 128], identf[0:NSEQ, 0:NSEQ])
    P.copy("dve", scT, psA[:, 0:8 * NSEQ].re("p (k b) -> p k b", k=8))
    adab = P.tile("adab", [128, 6144])
    P.dma("sp", adab[0:NSEQ, :], dr["ada_b"][0:1, :].partition_broadcast(NSEQ), "c13")
    nrm = P.tile("nrm", [128, 2 * D])
    P.dma("sp", nrm[0:1, 0:D], dr["mix_norm_w"][0:1, :], "c14")
    P.dma("sp", nrm[0:1, D:2 * D], dr["ffn_norm_w"][0:1, :], "c14")
    nrmT = P.tile("nrmT", [128, 16])
    for k in range(16):
        P.mm(psC[:, k:k + 1], nrm[0:1, k * 128:(k + 1) * 128], onesf[0:1, 0:1])
    P.copy("dve", nrmT, psC[:, 0:16])
    adw = [P.tile("adw%d" % i, [128, 8, 512]) for i in range(2)]
    modb = [P.tile("modb%d" % i, [128, 512]) for i in range(2)]
    modT = P.tile("modT", [128, 48, NSEQ])
    for cb in range(12):
        a = adw[cb % 2]
        P.dma("sp", a, dr["ada_w"][:, cb * 512:(cb + 1) * 512].rearrange("(k p) f -> p k f", p=128), "adw%d" % (cb % 2))
        pm = psA if cb % 2 == 0 else psB
        for k in range(8):
            P.mm(pm[0:NSEQ, :], scT[:, k, :], a[:, k, :], start=(k == 0), stop=(k == 7))
        mb = modb[cb % 2]
        P.tt("dve", mb[0:NSEQ, :], pm[0:NSEQ, :], adab[0:NSEQ, cb * 512:(cb + 1) * 512], ALU.add)
        P.dma("sp", mod_s[:, cb * 512:(cb + 1) * 512], mb[0:NSEQ, :], "mods")
        for q in range(4):
            P.mm(psC[:, 64 + q * NSEQ:64 + (q + 1) * NSEQ], mb[0:NSEQ, q * 128:(q + 1) * 128], identf[0:NSEQ, 0:NSEQ])
        P.copy("act", modT[:, cb * 4:(cb + 1) * 4, :], psC[:, 64:64 + 4 * NSEQ].re("p (q b) -> p q b", q=4))
    for b in range(NSEQ):
        P.stt("dve", gamM[:, b, :], modT[:, 8:16, b], 1.0, nrmT[:, 0:8], ALU.add, ALU.mult)
        P.copy("dve", shM[:, b, :], modT[:, 0:8, b])
        P.stt("dve", gamF[:, b, :], modT[:, 32:40, b], 1.0, nrmT[:, 8:16], ALU.add, ALU.mult)
        P.copy("dve", shF[:, b, :], modT[:, 24:32, b])
    zt = P.tile("zt", [128, 512])
    P.memset("pool", zt, 0.0)
    for b in range(NSEQ):
        P.dma("sp", proj_s[b * PADR:b * PADR + 3, :].rearrange("r (a f) -> (r a) f", f=417), zt[0:24, 0:417], "zpad")
    tap("gamM", gamM, [128, NSEQ, 8])
    tap("shM", shM, [128, NSEQ, 8])
    P.end_phase()
    if stop_after <= 0:
        P.finish()
        return nc, tapd

    ycat_s = nc.dram_tensor("ycat_s", [NTOK, D], BF16).ap()

    def recip(eng, o, a):
        P.add(eng, lambda e: e.reciprocal(out=o.ap, in_=a.ap), [a], [o])

    P.begin_phase()
    Win = P.tile("Win", [128, 8, INC], BF16)
    stg = [P.tile("stg%d" % i, [128, INC]) for i in range(2)]
    ceng = ["act", "dve", "pool"]
    for k in range(8):
        s = stg[k % 2]
        P.dma("sp", s, dr["w_in"][k * 128:(k + 1) * 128, :], "stg%d" % (k % 2))
        P.copy(ceng[k % 3], Win[:, k, :], s)
    xt = [P.tile("xt%d" % i, [128, D]) for i in range(3)]
    junk = P.tile("junkA", [128, D], BF16)
    xn = [P.tile("xn%d" % i, [128, D], BF16) for i in range(2)]
    hT = [P.tile("hT%d" % i, [128, 8, 128], BF16) for i in range(2)]
    hTk = [[hT[i][:, k, :].sub("hT%d_%d" % (i, k)) for k in range(8)] for i in range(2)]
    pj = [P.tile("pj%d" % i, [128, INC]) for i in range(3)]
    pjc = [[pj[i][:, cb * 512:min(INC, (cb + 1) * 512)].sub("pj%d_%d" % (i, cb)) for cb in range(7)] for i in range(3)]
    st = [P.tile("stA%d" % i, [128, 4]) for i in range(2)]
    ptb = [P.psum("ptbA%d" % i, [128, 8, 128], BF16) for i in range(2)]
    pp = [P.psum("ppA%d" % i, [128, 512]) for i in range(5)]
    wst = [P.tile("wst%d" % i, [128, 2048]) for i in range(4)]
    wbt = [P.tile("wbt%d" % i, [128, 2048], BF16) for i in range(4)]
    pc_list = []
    for e in range(NE):
        pc_list.append((dr["moe_w_gate"][e].rearrange("(k p) f -> p k f", p=128), wg_s[e], 8))
        pc_list.append((dr["moe_w_up"][e].rearrange("(k p) f -> p k f", p=128), wu_s[e], 8))
        pc_list.append((dr["moe_w_down"][e].rearrange("(c p) d -> p c d", p=128), wd_s[e], 2))

    def pc_load(n):
        src, dst, a_ = pc_list[n]
        P.dma("pool", wst[n % 4].re("p (a b) -> p a b", a=a_), src, "wst%d" % (n % 4))

    def precast_gen():
        for n in range(min(3, len(pc_list))):
            pc_load(n)
        for n in range(len(pc_list)):
            if n + 3 < len(pc_list):
                pc_load(n + 3)
            P.copy("pool" if n % 2 == 0 else "act", wbt[n % 4], wst[n % 4])
            P.dma("pool", pc_list[n][1], wbt[n % 4], "wbt%d" % (n % 4))
            yield

    pcg = precast_gen()

    def a_load(i):
        P.dma("sp", xt[i % 3], dr["x"][i * 128:(i + 1) * 128, :], "xA%d" % (i % 3))

    def a_front(i):
        b = i // NT
        sl = i % 2
        s4 = st[sl]
        P.act(junk, xt[i % 3], AF.Square, accum=s4[:, 0:1])
        P.ts("dve", s4[:, 1:2], s4[:, 0:1], 1.0 / D, ALU.mult)
        P.act(s4[:, 2:3], s4[:, 1:2], AF.Sqrt, bias=epsM[:, 0:1], scale=1.0)
        recip("dve", s4[:, 3:4], s4[:, 2:3])
        P.act(xn[sl], xt[i % 3], AF.Copy, scale=s4[:, 3:4])
        for k in range(8):
            P.tr(ptb[k // 4][:, k % 4, :], xn[sl][:, k * 128:(k + 1) * 128], ident)
        for k in range(8):
            if k < 4:
                P.ts("dve", hTk[sl][k], ptb[0][:, k % 4, :], gamM[:, b, k:k + 1], ALU.mult, shM[:, b, k:k + 1], ALU.add)
            else:
                P.act(hTk[sl][k], ptb[1][:, k % 4, :], AF.Identity, bias=shM[:, b, k:k + 1], scale=gamM[:, b, k:k + 1])

    ppi = [0]

    def a_back(i):
        b = i // NT
        it = i % NT
        sl = i % 2
        for cb in range(7):
            c0 = cb * 512
            cw_ = min(512, INC - c0)
            pq = pp[ppi[0] % 5]; ppi[0] += 1
            for k in range(8):
                P.mm(pq[:, 0:cw_], hTk[sl][k], Win[:, k, c0:c0 + cw_], start=(k == 0), stop=(k == 7))
            P.copy("act" if cb % 2 == 0 else "dve", pjc[i % 3][cb], pq[:, 0:cw_])
        r0 = b * PADR + 3 + it * 128
        P.add("sp", (lambda o_, i_: (lambda e: e.dma_start(out=o_, in_=i_)))(proj_s[r0:r0 + 128, :], pj[i % 3].ap), pjc[i % 3], [], dma_key="pjA%d" % (i % 3))

    a_load(0)
    if NTILES > 1:
        a_load(1)
    a_front(0)
    for i in range(NTILES):
        if i + 2 < NTILES:
            a_load(i + 2)
        if i + 1 < NTILES:
            a_front(i + 1)
        a_back(i)
        for _ in range(2):
            next(pcg, None)
    for _ in pcg:
        pass
    P.end_phase()
    if stop_after <= 1:
        P.finish()
        return nc, tapd

    P.begin_phase()
    Wup = P.tile("Wup", [128, 512], BF16)
    Aup = P.tile("Aup", [128, 512], BF16)
    Gup = P.tile("Gup", [128, 512], BF16)
    mu_bc = P.tile("mu_bc", [128, RW])
    kk_bc = P.tile("kk_bc", [128, 512])
    ka_bc = P.tile("ka_bc", [128, 512])
    rk_bc = P.tile("rk_bc", [128, 512])
    gnw_bc = P.tile("gnw_bc", [128, 512])
    gnb_bc = P.tile("gnb_bc", [128, 512])
    bcload(mu_bc, dr["rwkv_mu"][0:1, :], 128, "c0")
    bcload(kk_bc, dr["rwkv_k_k"][0:1, :], 128, "c1")
    bcload(ka_bc, dr["rwkv_k_a"][0:1, :], 128, "c2")
    bcload(rk_bc, dr["rwkv_r_k"][0:1, :], 128, "c3")
    bcload(gnw_bc, dr["rwkv_gn_w"][0:1, :], 128, "c4")
    bcload(gnb_bc, dr["rwkv_gn_b"][0:1, :], 128, "c5")
    s = P.tile("stgB1", [128, 1536])
    P.dma("sp", s[0:64, 0:512], dr["rwkv_w_up"][:, :], "stgb1")
    P.dma("sp", s[64:65, 0:512], dr["rwkv_w0"][0:1, :], "stgb1")
    P.dma("sp", s[0:64, 512:1024], dr["rwkv_a_up"][:, :], "stgb1")
    P.dma("sp", s[64:65, 512:1024], dr["rwkv_a0"][0:1, :], "stgb1")
    P.dma("sp", s[:, 1024:1536], dr["rwkv_g_up"][:, :], "stgb1")
    P.copy("act", Wup[0:64, :], s[0:64, 0:512])
    P.copy("act", Wup[64:65, :], s[64:65, 0:512])
    P.copy("dve", Aup[0:64, :], s[0:64, 512:1024])
    P.copy("dve", Aup[64:65, :], s[64:65, 512:1024])
    P.copy("pool", Gup, s[:, 1024:1536])
    NSTR = 2 if NSEQ % 2 == 0 else 1

    def alloc_b1(q):
        sx = "_s%d" % q
        T = {"id": q}
        T["rw"] = P.tile("rw" + sx, [128, RW])
        T["rwp"] = P.tile("rwp" + sx, [128, RW])
        T["li"] = P.tile("li" + sx, [128, 256], BF16)
        T["liT"] = P.tile("liT" + sx, [128, 384], BF16)
        for n in ("sgz", "av", "gv", "Ep", "Em", "Epv", "kkk", "tmpR", "tmpR2", "kkv", "knew", "y_sb"):
            T[n] = P.tile(n + sx, [128, 512])
        T["gmL"] = P.tile("gmL" + sx, [128, 4, 2])
        T["sm8"] = P.tile("sm8" + sx, [128, 64])
        T["tok4"] = P.tile("tok4" + sx, [128, 4, 512], BF16)
        T["v_bf"] = P.tile("v_bf" + sx, [128, 512], BF16)
        T["FT"] = P.tile("FT" + sx, [128, 4, 4, 128], BF16)
        T["A_sb"] = [[P.tile("A_sb%d_%d%s" % (g, i, sx), [128, 4, 128], BF16) for i in range(2)] for g in range(2)]
        T["MT"] = [[P.tile("MT%d_%d%s" % (g, i, sx), [128, 4, 2, 128], BF16) for i in range(2)] for g in range(2)]
        T["MRB"] = P.tile("MRB" + sx, [128, 8, 2, 128], BF16)
        T["AKRK"] = P.tile("AKRK" + sx, [128, 8, 2, 128], BF16)
        T["TT"] = P.tile("TT" + sx, [128, 8, 128], BF16)
        T["X_bf"] = P.tile("X_bf" + sx, [128, 512], BF16)
        T["U_bf"] = P.tile("U_bf" + sx, [128, 512], BF16)
        T["Hst"] = P.tile("Hst" + sx, [128, 4, 64])
        T["Ht"] = P.tile("Ht" + sx, [128, 4, 64])
        T["Hbd"] = P.tile("Hbd" + sx, [128, 4, 128], BF16)
        T["yr"] = [P.tile("yr%d%s" % (i, sx), [128, 512], BF16) for i in range(2)]
        P.memset("pool", T["liT"], 1.0)
        P.memset("pool", T["Hbd"], 0.0)
        return T

    B1S = [alloc_b1(q) for q in range(NSTR)]
    ptbB = [P.psum("ptbB%d" % i, [128, 1024], BF16) for i in range(2)]
    pf = [P.psum("pfB%d" % i, [128, 512]) for i in range(5)]
    pfi = [0]

    def bank():
        b_ = pf[pfi[0] % 5]
        pfi[0] += 1
        return b_

    H8 = "p (h c) -> p h c"
    def b1_tile(i, T):
        rw = T["rw"]
        rwp = T["rwp"]
        li = T["li"]
        liT = T["liT"]
        sgz = T["sgz"]
        av = T["av"]
        gv = T["gv"]
        Ep = T["Ep"]
        Em = T["Em"]
        Epv = T["Epv"]
        gmL = T["gmL"]
        kkk = T["kkk"]
        tmpR = T["tmpR"]
        tmpR2 = T["tmpR2"]
        kkv = T["kkv"]
        knew = T["knew"]
        sm8 = T["sm8"]
        tok4 = T["tok4"]
        v_bf = T["v_bf"]
        FT = T["FT"]
        A_sb = T["A_sb"]
        MT = T["MT"]
        MRB = T["MRB"]
        AKRK = T["AKRK"]
        TT = T["TT"]
        X_bf = T["X_bf"]
        U_bf = T["U_bf"]
        Hst = T["Hst"]
        Ht = T["Ht"]
        Hbd = T["Hbd"]
        y_sb = T["y_sb"]
        yr = T["yr"]
        b = i // NT
        it = i % NT
        r0 = b * PADR + 3 + it * 128
        P.dma("sp", rw, proj_s[r0:r0 + 128, 0:RW], "rw%d" % T["id"])
        P.dma("sp", rwp, proj_s[r0 - 1:r0 + 127, 0:RW], "rwp%d" % T["id"])
        if it == 0:
            P.memset("pool", Hst, 0.0)
        u = rwp
        P.tt("dve", u, u, rw, ALU.subtract)
        P.tt("dve", u, u, mu_bc, ALU.mult)
        P.tt("dve", u, u, rw, ALU.add)
        r_ = u[:, 0:512]; k_ = u[:, 512:1024]; v_ = u[:, 1024:1536]
        if i == 0:
            tap("u0", u, [128, RW])
        yield
        P.act(li[:, 0:64], u[:, 1536:1600], AF.Tanh)
        P.copy("pool", li[:, 64:128], u[:, 1600:1664])
        P.act(li[:, 128:256], u[:, 1664:1792], AF.Sigmoid)
        pb0 = ptbB[0]
        P.tr(pb0[0:64, 0:128], li[:, 0:64], ident)
        P.tr(pb0[0:64, 128:256], li[:, 64:128], ident)
        P.tr(pb0[:, 256:384], li[:, 128:256], ident)
        P.copy("dve", liT[0:64, 0:256], pb0[0:64, 0:256])
        P.copy("act", liT[:, 256:384], pb0[:, 256:384])
        pz = bank(); pa = bank(); pg = bank()
        P.mm(pz, liT[0:65, 0:128], Wup[0:65, :])
        P.mm(pa, liT[0:65, 128:256], Aup[0:65, :])
        P.mm(pg, liT[:, 256:384], Gup)
        P.act(sgz, pz, AF.Sigmoid)
        P.act(av, pa, AF.Sigmoid)
        P.copy("dve", gv, pg)
        yield
        pc = bank()
        P.mm(pc, trimid, sgz)
        P.act(Ep, pc, AF.Exp)
        P.act(Em, pc, AF.Exp, scale=-1.0)
        P.stt("dve", Epv, sgz, DEC, pc, ALU.mult, ALU.add)
        P.act(Epv, Epv, AF.Exp)
        psm = bank()
        for j in range(4):
            P.mm(psm[:, 2 * j:2 * j + 2], sgz[:, j * 128:(j + 1) * 128], indmid)
        P.act(gmL, psm[:, 0:8].re("p (j t) -> p j t", j=4), AF.Exp)
        yield
        P.tt("dve", kkk, k_, kk_bc, ALU.mult)
        P.tt("dve", tmpR, kkk, kkk, ALU.mult)
        P.red("dve", sm8[:, 0:8], tmpR.re(H8, h=8), ALU.add)
        P.act(sm8[:, 8:16], sm8[:, 0:8], AF.Sqrt)
        P.ts("dve", sm8[:, 8:16], sm8[:, 8:16], 1e-12, ALU.max)
        recip("dve", sm8[:, 16:24], sm8[:, 8:16])
        P.tt("dve", kkv.re(H8, h=8), kkk.re(H8, h=8), sm8[:, 16:24].bc(2, 64), ALU.mult)
        P.stt("pool", tmpR, av, -1.0, ka_bc, ALU.add, ALU.mult)
        P.stt("pool", knew, tmpR, 1.0, k_, ALU.add, ALU.mult)
        P.tt("pool", tmpR, r_, knew, ALU.mult)
        P.tt("pool", tmpR, tmpR, rk_bc, ALU.mult)
        P.red("dve", sm8[:, 24:32], tmpR.re(H8, h=8), ALU.add)
        yield
        P.stt("dve", tok4[:, 0, :], kkv, -1.0, Epv, ALU.mult, ALU.mult)
        P.tt("dve", tok4[:, 1, :], r_, Ep, ALU.mult)
        P.tt("dve", tok4[:, 2, :], knew, Em, ALU.mult)
        P.tt("dve", tmpR2, kkv, av, ALU.mult)
        P.tt("dve", tok4[:, 3, :], tmpR2, Em, ALU.mult)
        P.copy("act", v_bf, v_)
        if i == 0:
            tap("tok4", tok4, [128, 4, 512])
            tap("sgz", sgz, [128, 512])
        yield
        for half in range(2):
            pb_ = ptbB[half]
            for jj in range(2):
                j = half * 2 + jj
                for w in range(4):
                    n = jj * 4 + w
                    P.tr(pb_[:, n * 128:(n + 1) * 128], tok4[:, w, j * 128:(j + 1) * 128], ident)
            P.copy("act" if half == 0 else "dve",
                   FT[:, half * 2:half * 2 + 2, :, :], pb_[:, :].re("p (j w t) -> p j w t", j=2, w=4))
        yield
        for g in range(2):
            pA = bank()
            pMR = [bank(), bank()]
            pKR = [bank(), bank()]
            for hh in range(4):
                j = hh
                po = 64 * g
                aT = FT[po:po + 64, j, 0, :]
                arT = FT[po:po + 64, j, 0:2, :]
                kT = FT[po:po + 64, j, 2, :]
                bT = FT[po:po + 64, j, 3, :]
                P.mm(pA[:, hh * 128:(hh + 1) * 128], aT, bT)
                P.mm(pMR[hh // 2][:, (hh % 2) * 256:(hh % 2 + 1) * 256], bT, arT)
                P.mm(pKR[hh // 2][:, (hh % 2) * 256:(hh % 2 + 1) * 256], kT, arT)
            P.tt("dve", A_sb[g][0], pA[:, :].re("p (h t) -> p h t", h=4), maskA, ALU.mult)
            for q in range(2):
                P.tt("dve", MRB[:, g * 4 + q * 2:g * 4 + q * 2 + 2, :, :], pMR[q][:, :].re("p (h w t) -> p h w t", h=2, w=2), mSI, ALU.mult)
                P.tt("dve", AKRK[:, g * 4 + q * 2:g * 4 + q * 2 + 2, :, :], pKR[q][:, :].re("p (h w t) -> p h w t", h=2, w=2), mSI, ALU.mult)
            P.tt("pool", MT[g][0][:, :, 1, :], MRB[:, g * 4:g * 4 + 4, 0, :], ident.bc(1, 4), ALU.add)
            if g == 0:
                yield
        yield
        for g in range(2):
            pM = bank(); pA2 = bank()
            for hh in range(4):
                h = g * 4 + hh
                P.mm(pM[:, hh * 128:(hh + 1) * 128], A_sb[g][0][:, hh, :], MRB[:, h, 0, :])
                P.mm(pA2[:, hh * 128:(hh + 1) * 128], MRB[:, h, 0, :], A_sb[g][0][:, hh, :])
            P.copy("act", MT[g][0][:, :, 0, :], pM[:, :].re("p (h t) -> p h t", h=4))
            P.copy("dve", A_sb[g][1], pA2[:, :].re("p (h t) -> p h t", h=4))
            if g == 0:
                yield
        yield
        cur = 0
        for lev in range(1, 6):
            yield
            for g in range(2):
                Ak = A_sb[g][lev % 2]
                An = A_sb[g][(lev + 1) % 2]
                Mc = MT[g][cur]
                Mn = MT[g][1 - cur]
                pS = [bank(), bank()]
                pA2 = bank()
                for hh in range(4):
                    reg = pS[hh // 2][:, (hh % 2) * 256:(hh % 2 + 1) * 256]
                    P.mm(reg, Ak[:, hh, :], Mc[:, hh, :, :], start=True, stop=False)
                    P.mm(reg[:, 128:256], ident, Mc[:, hh, 1, :], start=False, stop=True)
                    P.mm(pA2[:, hh * 128:(hh + 1) * 128], Mc[:, hh, 0, :], Ak[:, hh, :])
                for q in range(2):
                    P.copy("act" if q == 0 else "dve", Mn[:, q * 2:q * 2 + 2, :, :], pS[q][:, :].re("p (h w t) -> p h w t", h=2, w=2))
                P.copy("act", An, pA2[:, :].re("p (h t) -> p h t", h=4))
                if g == 0:
                    yield
            cur = 1 - cur
        for g in range(2):
            Ak = A_sb[g][0]
            Mc = MT[g][cur]
            pT = bank()
            for hh in range(4):
                P.mm(pT[:, hh * 128:(hh + 1) * 128], Ak[:, hh, :], Mc[:, hh, 1, :], start=True, stop=False)
                P.mm(pT[:, hh * 128:(hh + 1) * 128], ident, Mc[:, hh, 1, :], start=False, stop=True)
            P.copy("act" if g == 0 else "dve", TT[:, g * 4:g * 4 + 4, :], pT[:, :].re("p (h t) -> p h t", h=4))
            if g == 0:
                yield
        if i == 0:
            tap("TT", TT, [128, 8, 128])
            tap("MRB", MRB, [128, 8, 2, 128])
        yield
        P.tt("pool", Ht, Hst, gmL[:, :, 0].bc(2, 64), ALU.mult)
        P.copy("pool", Hbd[0:64, :, 0:64], Ht[0:64, :, :])
        P.copy("pool", Hbd[64:128, :, 64:128], Ht[64:128, :, :])
        pX = bank()
        SI = [(h % 2) * 4 + h // 2 for h in range(8)]
        for j in range(4):
            P.mm(pX[:, j * 128:(j + 1) * 128], FT[:, j, 0, :], Hbd[:, j, :], start=True, stop=False)
            for h in (2 * j, 2 * j + 1):
                P.mm(pX[:, h * 64:(h + 1) * 64], AKRK[:, SI[h], 0, :], v_bf[:, h * 64:(h + 1) * 64], start=False, stop=(h == 2 * j + 1))
        P.copy("act", X_bf, pX)
        yield
        pU = bank()
        for h in range(8):
            P.mm(pU[:, h * 64:(h + 1) * 64], TT[:, SI[h], :], X_bf[:, h * 64:(h + 1) * 64])
        P.copy("dve", U_bf, pU)
        yield
        pY = bank()
        for j in range(4):
            P.mm(pY[:, j * 128:(j + 1) * 128], FT[:, j, 1, :], Hbd[:, j, :], start=True, stop=False)
            for h in (2 * j, 2 * j + 1):
                P.mm(pY[:, h * 64:(h + 1) * 64], AKRK[:, SI[h], 1, :], v_bf[:, h * 64:(h + 1) * 64], start=False, stop=False)
                P.mm(pY[:, h * 64:(h + 1) * 64], MRB[:, SI[h], 1, :], U_bf[:, h * 64:(h + 1) * 64], start=False, stop=(h == 2 * j + 1))
        P.copy("act", y_sb, pY)
        yield
        pH = bank()
        for j in range(4):
            P.mm(pH[:, j * 128:(j + 1) * 128], tok4[:, 2, j * 128:(j + 1) * 128], v_bf[:, j * 128:(j + 1) * 128], start=True, stop=False)
            P.mm(pH[:, j * 128:(j + 1) * 128], tok4[:, 3, j * 128:(j + 1) * 128], U_bf[:, j * 128:(j + 1) * 128], start=False, stop=True)
        pHv = pH[:, :].re("p (j v) -> p j v", j=4)
        P.tt("dve", Hst[0:64, :, :], pHv[0:64, :, 0:64], Ht[0:64, :, :], ALU.add)
        P.tt("dve", Hst[64:128, :, :], pHv[64:128, :, 64:128], Ht[64:128, :, :], ALU.add)
        P.tt("pool", Hst, Hst, gmL[:, :, 1].bc(2, 64), ALU.mult)
        yield
        if i <= 1:
            tap("y_rwkv%d" % i, y_sb, [128, 512])
        y3 = y_sb.re(H8, h=8)
        P.red("dve", sm8[:, 32:40], y3, ALU.add)
        P.tt("pool", tmpR, y_sb, y_sb, ALU.mult)
        P.red("dve", sm8[:, 40:48], tmpR.re(H8, h=8), ALU.add)
        P.ts("dve", sm8[:, 32:40], sm8[:, 32:40], 1.0 / 64, ALU.mult)
        P.tt("dve", sm8[:, 48:56], sm8[:, 32:40], sm8[:, 32:40], ALU.mult)
        P.stt("dve", sm8[:, 40:48], sm8[:, 40:48], 1.0 / 64, sm8[:, 48:56], ALU.mult, ALU.subtract)
        P.act(sm8[:, 48:56], sm8[:, 40:48], AF.Sqrt, bias=epsG[:, 0:1], scale=1.0)
        recip("dve", sm8[:, 56:64], sm8[:, 48:56])
        P.tt("dve", tmpR.re(H8, h=8), y3, sm8[:, 32:40].bc(2, 64), ALU.subtract)
        P.tt("dve", tmpR.re(H8, h=8), tmpR.re(H8, h=8), sm8[:, 56:64].bc(2, 64), ALU.mult)
        P.tt("pool", tmpR, tmpR, gnw_bc, ALU.mult)
        P.tt("dve", tmpR, tmpR, gnb_bc, ALU.add)
        P.tt("pool", tmpR2.re(H8, h=8), v_.re(H8, h=8), sm8[:, 24:32].bc(2, 64), ALU.mult)
        P.tt("dve", tmpR, tmpR, tmpR2, ALU.add)
        P.tt("pool", yr[i % 2], tmpR, gv, ALU.mult)
        P.dma("sp", ycat_s[i * 128:(i + 1) * 128, 0:512], yr[i % 2], "yrs%d_%d" % (T["id"], i % 2))
        if i == 0:
            tap("yr", yr[0], [128, 512])
        if i == 1:
            tap("yr1", yr[1], [128, 512])

    def run_streams(tile_fn, streams, offset=0):
        ns = min(len(streams), NSEQ)

        def chain(q):
            for b_ in range(q, NSEQ, ns):
                for it_ in range(NT):
                    yield from tile_fn(b_ * NT + it_, streams[q])
                    yield

        gens = [chain(q) for q in range(ns)]
        for q, g_ in enumerate(gens):
            for _ in range(q * offset):
                next(g_, None)
        live = list(gens)
        while live:
            nxt = []
            for g_ in live:
                try:
                    next(g_)
                    nxt.append(g_)
                except StopIteration:
                    pass
            live = nxt

    run_streams(b1_tile, B1S, offset=0)
    P.end_phase()
    if stop_after <= 2:
        P.finish()
        return nc, tapd

    P.begin_phase()
    hnw_bc = P.tile("hnw_bc", [128, 512])
    cvw_bc = P.tile("cvw_bc", [128, 4, 512])
    cvb_bc = P.tile("cvb_bc", [128, 512])
    gb_bc = P.tile("gb_bc", [128, 8])
    bcload(hnw_bc, dr["mlstm_hn_w"][0:1, :], 128, "c6")
    for j in range(4):
        bcload(cvw_bc[:, j, :], dr["mlstm_conv_w"][j:j + 1, :], 128, "c7")
    bcload(cvb_bc, dr["mlstm_conv_b"][0:1, :], 128, "c8")
    bcload(gb_bc[:, 0:4], dr["mlstm_i_b"][0:1, :], 128, "c9")
    bcload(gb_bc[:, 4:8], dr["mlstm_f_b"][0:1, :], 128, "c9")
    ones_bf = P.tile("ones_bf", [128, 1], BF16)

    def alloc_b2(q):
        sx = "_m%d" % q
        T = {"id": q}
        T["qk4"] = P.tile("qk4" + sx, [128, 4, 512])
        T["mr"] = P.tile("mr" + sx, [128, 1032])
        for n in ("cacc", "ctmp", "slu", "og"):
            T[n] = P.tile(n + sx, [128, 512])
        T["qq"] = P.tile("qq" + sx, [128, 768], BF16)
        T["qkT"] = P.tile("qkT" + sx, [128, 12, 128], BF16)
        T["g8"] = P.tile("g8" + sx, [128, 64])
        T["sm4"] = P.tile("sm4" + sx, [128, 16])
        T["lfb"] = P.tile("lfb" + sx, [128, 4, 128])
        T["DTm"] = P.tile("DTm" + sx, [128, 4, 128])
        T["PTm"] = P.tile("PTm" + sx, [128, 4, 128], BF16)
        T["Vb"] = P.tile("Vb" + sx, [128, 4, 128], BF16)
        T["Kw"] = P.tile("Kw" + sx, [128, 4, 64], BF16)
        T["Cst"] = P.tile("Cst" + sx, [128, 4, 128])
        T["nst"] = P.tile("nst" + sx, [128, 4])
        T["C_bf"] = P.tile("C_bf" + sx, [128, 4, 128], BF16)
        T["n_bf"] = P.tile("n_bf" + sx, [128, 4], BF16)
        T["hm"] = P.tile("hm" + sx, [128, 4, 128])
        T["ym"] = [P.tile("ym%d%s" % (i, sx), [128, 512], BF16) for i in range(2)]
        return T

    NSTR2 = 4 if NSEQ % 4 == 0 else NSTR
    B2S = [alloc_b2(q) for q in range(NSTR2)]
    ptbM = [P.psum("ptbM%d" % i, [128, 1024], BF16) for i in range(2)]
    pfm = [P.psum("pfM%d" % i, [128, 512]) for i in range(5)]
    pmi = [0]

    def bankm():
        b_ = pfm[pmi[0] % 5]
        pmi[0] += 1
        return b_

    P.memset("pool", ones_bf, 1.0)
    H4 = "p (h c) -> p h c"
    def b2_tile(i, T):
        qk4 = T["qk4"]
        mr = T["mr"]
        cacc = T["cacc"]
        ctmp = T["ctmp"]
        slu = T["slu"]
        qq = T["qq"]
        qkT = T["qkT"]
        g8 = T["g8"]
        sm4 = T["sm4"]
        lfb = T["lfb"]
        DTm = T["DTm"]
        PTm = T["PTm"]
        Vb = T["Vb"]
        Kw = T["Kw"]
        Cst = T["Cst"]
        nst = T["nst"]
        C_bf = T["C_bf"]
        n_bf = T["n_bf"]
        hm = T["hm"]
        og = T["og"]
        ym = T["ym"]
        b = i // NT
        it = i % NT
        r0 = b * PADR + 3 + it * 128
        for j in range(4):
            P.dma("sp", qk4[:, j, :], proj_s[r0 - 3 + j:r0 + 125 + j, RW:RW + 512], "qk4%d" % T["id"])
        P.dma("sp", mr, proj_s[r0:r0 + 128, RW + 512:INC], "mr%d" % T["id"])
        if it == 0:
            P.memset("pool", Cst, 0.0)
            P.memset("pool", nst, 0.0)
            P.memset("pool", C_bf, 0.0)
            P.memset("pool", n_bf, 0.0)
        q4 = qk4
        cparts = [cacc, ctmp, og, hm.re("p h v -> p (h v)")]
        for j in range(4):
            P.tt("pool", cparts[j], q4[:, j, :], cvw_bc[:, j, :], ALU.mult)
        for j in range(1, 4):
            P.tt("dve", cacc, cacc, cparts[j], ALU.add)
        P.tt("dve", cacc, cacc, cvb_bc, ALU.add)
        P.act(slu, cacc, AF.Silu)
        m = mr
        P.act(og, m[:, 512:1024], AF.Tanh, scale=0.5)
        yield
        P.tt("dve", g8[:, 0:8], m[:, 1024:1032], gb_bc, ALU.add)
        P.act(g8[:, 8:16], g8[:, 0:8], AF.Exp, scale=2.0 / 15.0)
        P.ts("dve", g8[:, 8:16], g8[:, 8:16], 1.0, ALU.add)
        recip("dve", g8[:, 8:16], g8[:, 8:16])
        P.ts("dve", g8[:, 8:16], g8[:, 8:16], -30.0, ALU.mult, 15.0, ALU.add)
        P.act(g8[:, 16:20], g8[:, 12:16], AF.Exp, scale=-1.0)
        P.act(g8[:, 20:24], g8[:, 16:20], AF.Ln, bias=1.0, scale=1.0)
        P.ts("dve", g8[:, 24:28], g8[:, 20:24], -1.0, ALU.mult)
        lf = g8[:, 24:28]
        yield
        psg = bankm()
        P.mm(psg[:, 0:4], tri, lf)
        P.mm(psg[:, 4:8], onesf, lf)
        P.copy("dve", g8[:, 28:36], psg[:, 0:8])
        bt = g8[:, 28:32]; bL = g8[:, 32:36]
        P.tt("dve", g8[:, 36:40], g8[:, 8:12], bt, ALU.subtract)
        P.act(g8[:, 40:44], bt, AF.Exp)
        P.tt("dve", g8[:, 44:48], g8[:, 36:40], bL, ALU.add)
        P.act(g8[:, 44:48], g8[:, 44:48], AF.Exp)
        P.act(g8[:, 48:52], bL, AF.Exp)
        yield
        P.copy("pool", lfb, lf.bc(2, 128))
        P.ts("dve", qq[:, 0:256], slu[:, 0:256], 0.125, ALU.mult)
        P.copy("pool", qq[:, 256:512], slu[:, 256:512])
        P.stt("dve", qq[:, 512:768].re(H4, h=4), slu[:, 0:256].re(H4, h=4), 0.125, g8[:, 40:44].bc(2, 64), ALU.mult, ALU.mult)
        yield
        for n in range(12):
            pb_ = ptbM[0] if n < 8 else ptbM[1]
            nn = n % 8
            P.tr(pb_[0:64, nn * 128:(nn + 1) * 128], qq[:, n * 64:(n + 1) * 64], ident)
        P.copy("act", qkT[0:64, 0:8, :], ptbM[0][0:64, :].re("p (n t) -> p n t", n=8))
        P.copy("dve", qkT[0:64, 8:12, :], ptbM[1][0:64, 0:512].re("p (n t) -> p n t", n=4))
        P.copy("act", Vb, m[:, 0:512].re("p (h v) -> p h v", h=4))
        P.tt("pool", Kw, slu[:, 256:512].re(H4, h=4), g8[:, 44:48].bc(2, 64), ALU.mult)
        yield
        pE = bankm(); pS_ = bankm(); pN = bankm(); pCm = bankm(); pdn = bankm()
        for h in range(4):
            P.mm(pE[:, h * 128:(h + 1) * 128], lfb[:, h, :], tri, start=True, stop=False)
            P.mm(pE[:, h * 128:(h + 1) * 128], identf, masknegf, start=False, stop=True)
        for h in range(4):
            P.mm(pS_[:, h * 128:(h + 1) * 128], qkT[0:64, 4 + h, :], qkT[0:64, h, :])
        for h in range(4):
            P.act(DTm[:, h, :], pE[:, h * 128:(h + 1) * 128], AF.Exp, bias=g8[:, 36 + h:37 + h], scale=1.0)
        P.tt("dve", PTm, pS_[:, :].re("p (h t) -> p h t", h=4), DTm, ALU.mult)
        for h in range(4):
            P.mm(pN[:, h * 128:(h + 1) * 128], PTm[:, h, :], Vb[:, h, :], start=True, stop=False)
            P.mm(pN[:, h * 128:(h + 1) * 128], qkT[0:64, 8 + h, :], C_bf[0:64, h, :], start=False, stop=True)
            P.mm(pdn[:, h:h + 1], PTm[:, h, :], ones_bf[:, 0:1], start=True, stop=False)
            P.mm(pdn[:, h:h + 1], qkT[0:64, 8 + h, :], n_bf[0:64, h:h + 1], start=False, stop=True)
            P.mm(pCm[0:64, h * 128:(h + 1) * 128], Kw[:, h, :], Vb[:, h, :])
            P.mm(pdn[0:64, 8 + h:9 + h], Kw[:, h, :], ones_bf[:, 0:1])
        P.copy("dve", g8[:, 52:56], pdn[:, 0:4])
        P.stt("dve", g8[:, 56:60], g8[:, 52:56], -1.0, g8[:, 52:56], ALU.mult, ALU.max)
        P.ts("dve", g8[:, 56:60], g8[:, 56:60], 1.0, ALU.max)
        recip("dve", g8[:, 60:64], g8[:, 56:60])
        P.tt("dve", hm, pN[:, :].re("p (h v) -> p h v", h=4), g8[:, 60:64].bc(2, 128), ALU.mult)
        if i <= 1:
            tap("h_mlstm%d" % i, hm, [128, 4, 128])
        P.tt("pool", Cst[0:64], Cst[0:64], g8[0:64, 48:52].bc(2, 128), ALU.mult)
        P.tt("dve", Cst[0:64], Cst[0:64], pCm[0:64, :].re("p (h v) -> p h v", h=4), ALU.add)
        P.tt("pool", nst[0:64], nst[0:64], g8[0:64, 48:52], ALU.mult)
        P.tt("dve", nst[0:64], nst[0:64], pdn[0:64, 8:12], ALU.add)
        P.copy("pool", C_bf[0:64], Cst[0:64])
        P.copy("pool", n_bf[0:64], nst[0:64])
        yield
        hflat = hm.re("p h v -> p (h v)")
        P.tt("pool", ctmp.re("p (h v) -> p h v", h=4), hm, hm, ALU.mult)
        P.red("dve", sm4[:, 0:4], ctmp.re("p (h v) -> p h v", h=4), ALU.add)
        P.ts("dve", sm4[:, 0:4], sm4[:, 0:4], 1.0 / 128, ALU.mult)
        P.act(sm4[:, 4:8], sm4[:, 0:4], AF.Ln, bias=epsM[:, 0:1], scale=1.0)
        P.act(sm4[:, 8:12], sm4[:, 4:8], AF.Exp, scale=-0.5)
        P.ts("dve", sm4[:, 8:12], sm4[:, 8:12], 0.5, ALU.mult)
        P.tt("dve", hm, hm, sm4[:, 8:12].bc(2, 128), ALU.mult)
        P.tt("pool", hflat, hflat, hnw_bc, ALU.mult)
        P.stt("dve", ym[i % 2], og, 1.0, hflat, ALU.add, ALU.mult)
        P.dma("sp", ycat_s[i * 128:(i + 1) * 128, 512:1024], ym[i % 2], "yms%d_%d" % (T["id"], i % 2))
        if i == 0:
            tap("ym", ym[0], [128, 512])
        if i == 1:
            tap("ym1", ym[1], [128, 512])

    run_streams(b2_tile, B2S, offset=2)
    P.end_phase()
    if stop_after <= 3:
        P.finish()
        return nc, tapd

    P.begin_phase()
    Wout = P.tile("Wout", [128, 8, D], BF16)
    Wgr = P.tile("Wgr", [128, 8, 36], BF16)
    brt_bc = P.tile("brt_bc", [128, 36])
    stg3 = [P.tile("stg3_%d" % i, [128, D]) for i in range(2)]
    for k in range(8):
        s = stg3[k % 2]
        P.dma("sp", s, dr["w_out"][k * 128:(k + 1) * 128, :], "stg3_%d" % (k % 2))
        P.copy(ceng[k % 3], Wout[:, k, :], s)
    s = P.tile("stg3r", [128, 288])
    P.dma("sp", s[:, 0:32].re("p (k g) -> p k g", k=8), dr["moe_w_group"].rearrange("(k p) g -> p k g", p=128), "stg3r")
    P.dma("sp", s[:, 32:288].re("p (k g) -> p k g", k=8), dr["moe_w_router"].rearrange("(k p) g -> p k g", p=128), "stg3r")
    P.copy("act", Wgr[:, :, 0:4], s[:, 0:32].re("p (k g) -> p k g", k=8))
    P.copy("act", Wgr[:, :, 4:36], s[:, 32:288].re("p (k g) -> p k g", k=8))
    bcload(brt_bc[:, 0:4], dr["moe_b_group"][0:1, :], 128, "c10")
    bcload(brt_bc[:, 4:36], dr["moe_b_router"][0:1, :], 128, "c10")
    def alloc_b3(q):
        sx = "_o%d" % q
        T = {"id": q}
        T["ycat"] = P.tile("ycat" + sx, [128, D], BF16)
        T["xB"] = P.tile("xB" + sx, [128, D])
        T["ycT"] = P.tile("ycT" + sx, [128, 8, 128], BF16)
        T["x1"] = P.tile("x1" + sx, [128, D])
        T["xn2"] = P.tile("xn2" + sx, [128, D], BF16)
        T["junkB"] = P.tile("junkB" + sx, [128, D], BF16)
        T["h2T"] = P.tile("h2T" + sx, [128, 8, 128], BF16)
        T["h2Tk"] = [T["h2T"][:, k, :].sub("h2T%s_%d" % (sx, k)) for k in range(8)]
        T["lg"] = P.tile("lg" + sx, [128, 36])
        T["r8"] = P.tile("r8" + sx, [128, 96])
        T["s16"] = P.tile("s16" + sx, [128, 16])
        T["cwt"] = P.tile("cwt" + sx, [128, 4, 8])
        T["gm_bc"] = P.tile("gm_bc" + sx, [128, D])
        return T

    B3S = [alloc_b3(q) for q in range(NSTR2)]
    ptb3 = [P.psum("ptb3_%d" % i, [128, 1024], BF16) for i in range(2)]
    pf3 = [P.psum("pf3_%d" % i, [128, 512]) for i in range(5)]
    p3i = [0]

    def bank3():
        b_ = pf3[p3i[0] % 5]
        p3i[0] += 1
        return b_

    def b3_tile(i, T):
        ycat = T["ycat"]
        xB = T["xB"]
        ycT = T["ycT"]
        x1 = T["x1"]
        xn2 = T["xn2"]
        junkB = T["junkB"]
        h2T = T["h2T"]
        h2Tk = T["h2Tk"]
        lg = T["lg"]
        r8 = T["r8"]
        s16 = T["s16"]
        cwt = T["cwt"]
        gm_bc = T["gm_bc"]
        b = i // NT
        it = i % NT
        P.dma("sp", ycat, ycat_s[i * 128:(i + 1) * 128, :], "ycl%d" % T["id"])
        P.dma("sp", xB, dr["x"][i * 128:(i + 1) * 128, :], "xB%d" % T["id"])
        if it == 0:
            P.dma("sp", gm_bc, mod_s[b:b + 1, 2048:3072].partition_broadcast(128), "gmbc%d" % T["id"])
        for k in range(8):
            P.tr(ptb3[0][:, k * 128:(k + 1) * 128], ycat[:, k * 128:(k + 1) * 128], ident)
        P.copy("dve", ycT, ptb3[0][:, :].re("p (k t) -> p k t", k=8))
        yield
        x1t = x1
        for cb in range(2):
            po_ = bank3()
            for k in range(8):
                P.mm(po_, ycT[:, k, :], Wout[:, k, cb * 512:(cb + 1) * 512], start=(k == 0), stop=(k == 7))
            P.tt("dve", x1t[:, cb * 512:(cb + 1) * 512], po_, gm_bc[:, cb * 512:(cb + 1) * 512], ALU.mult)
        P.tt("pool", x1t, x1t, xB, ALU.add)
        P.dma("sp", x1_s[i * 128:(i + 1) * 128, :], x1t, "x1s%d" % T["id"])
        if i == 0:
            tap("x1", x1t, [128, D])
        if i == 1:
            tap("x1b", x1t, [128, D])
        yield
        P.act(junkB, x1t, AF.Square, accum=s16[:, 0:1])
        P.ts("dve", s16[:, 1:2], s16[:, 0:1], 1.0 / D, ALU.mult)
        P.act(s16[:, 2:3], s16[:, 1:2], AF.Ln, bias=epsM[:, 0:1], scale=1.0)
        P.act(s16[:, 3:4], s16[:, 2:3], AF.Exp, scale=-0.5)
        P.act(xn2, x1t, AF.Copy, scale=s16[:, 3:4])
        for k in range(8):
            P.tr(ptb3[1][:, k * 128:(k + 1) * 128], xn2[:, k * 128:(k + 1) * 128], ident)
        h2 = h2T
        h2k = h2Tk
        for k in range(8):
            P.act(h2k[k], ptb3[1][:, k * 128:(k + 1) * 128], AF.Identity, bias=shF[:, b, k:k + 1], scale=gamF[:, b, k:k + 1])
        P.add("sp", (lambda o_, i_: (lambda e: e.dma_start(out=o_, in_=i_)))(h2T_s[:, :, i * 128:(i + 1) * 128], h2.ap), h2k, [], dma_key="h2s%d" % T["id"])
        yield
        pr = bank3()
        for k in range(8):
            P.mm(pr[:, 0:36], h2k[k], Wgr[:, k, :], start=(k == 0), stop=(k == 7))
        P.tt("dve", lg, pr[:, 0:36], brt_bc, ALU.add)
        yield
        P.red("dve", r8[:, 0:1], lg[:, 0:4], ALU.max)
        P.ts("dve", r8[:, 1:5], lg[:, 0:4], r8[:, 0:1], ALU.is_equal)
        P.ts("dve", r8[:, 5:6], r8[:, 0:1], -1.0, ALU.mult)
        P.act(r8[:, 6:10], lg[:, 0:4], AF.Exp, bias=r8[:, 5:6], scale=1.0, accum=r8[:, 10:11])
        recip("dve", r8[:, 11:12], r8[:, 10:11])
        P.tt("dve", r8[:, 16:48].re("p (g e) -> p g e", g=4), lg[:, 4:36].re("p (g e) -> p g e", g=4), r8[:, 1:5].bc(2, 8), ALU.mult)
        P.red("dve", r8[:, 48:56], r8[:, 16:48].re("p (g e) -> p e g", g=4), ALU.add)
        P.red("dve", r8[:, 56:57], r8[:, 48:56], ALU.max)
        P.ts("dve", r8[:, 64:72], r8[:, 48:56], r8[:, 56:57], ALU.is_equal)
        P.stt("dve", r8[:, 72:80], r8[:, 64:72], -1e30, r8[:, 48:56], ALU.mult, ALU.add)
        P.red("dve", r8[:, 57:58], r8[:, 72:80], ALU.max)
        P.ts("dve", r8[:, 80:88], r8[:, 72:80], r8[:, 57:58], ALU.is_equal)
        P.tt("dve", r8[:, 58:59], r8[:, 56:57], r8[:, 57:58], ALU.subtract)
        P.act(r8[:, 60:61], r8[:, 58:59], AF.Exp, scale=-1.0)
        P.ts("dve", r8[:, 59:60], r8[:, 60:61], 1.0, ALU.add)
        recip("dve", r8[:, 59:60], r8[:, 59:60])
        P.tt("dve", r8[:, 60:61], r8[:, 60:61], r8[:, 59:60], ALU.mult)
        P.ts("dve", r8[:, 64:72], r8[:, 64:72], r8[:, 59:60], ALU.mult)
        P.stt("dve", r8[:, 64:72], r8[:, 80:88], r8[:, 60:61], r8[:, 64:72], ALU.mult, ALU.add)
        P.ts("dve", r8[:, 64:72], r8[:, 64:72], r8[:, 11:12], ALU.mult)
        cw_t = cwt
        P.tt("dve", cw_t, r8[:, 1:5].bc(2, 8), r8[:, 64:72].bc(1, 4), ALU.mult)
        P.dma("sp", cw_s[i * 128:(i + 1) * 128, :], cw_t.re("p g e -> p (g e)"), "cws%d" % T["id"])
        if i == 0:
            tap("cw", cw_t, [128, 4, 8])

    run_streams(b3_tile, B3S, offset=0)
    P.end_phase()
    if stop_after <= 4:
        P.finish()
        return nc, tapd

    P.begin_phase()
    BLK = min(1024, SEQ)
    SUB = min(512, BLK)
    NB = NTOK // BLK
    TPB = BLK // 128
    fnw_bc = P.tile("fnw_bc", [128, D])
    bcload(fnw_bc, dr["final_norm_w"][0:1, :], 128, "c11")
    h2bs = [P.tile("h2b%d" % i, [128, 8, BLK], BF16) for i in range(2)]
    cwbs = [P.tile("cwb%d" % i, [128, TPB, NE]) for i in range(2)]
    yaccss = [[P.tile("yacc%d_%d" % (j, i), [128, TPB, 512]) for i in range(2)] for j in range(2)]
    gf_bcs = [P.tile("gf_bc%d" % i, [128, D]) for i in range(2)]
    wgb = [P.tile("wgb%d" % i, [128, 8, DE], BF16) for i in range(2)]
    wub = [P.tile("wub%d" % i, [128, 8, DE], BF16) for i in range(2)]
    wdb = [P.tile("wdb%d" % i, [128, 2, D], BF16) for i in range(2)]
    sg = [P.tile("sg%d" % i, [128, SUB]) for i in range(2)]
    actT = [[P.tile("actT%d_%d" % (s_, f), [128, SUB], BF16) for f in range(2)] for s_ in range(2)]
    x1c = [P.tile("x1c%d" % i, [128, D]) for i in range(2)]
    junkC = P.tile("junkC", [128, D], BF16)
    pG = [P.psum("pG%d" % i, [128, 512]) for i in range(2)]
    pUu = [P.psum("pU%d" % i, [128, 512]) for i in range(2)]
    pD = [P.psum("pD%d" % i, [128, 512]) for i in range(3)]
    pdi = [0]
    wcnt = [0]

    obig = P.tile("obig", [128, TPB, D])
    obt = [obig[:, ti, :].sub("obig_%d" % ti) for ti in range(TPB)]
    ssq = P.tile("ssqC", [128, 4, TPB])

    def epilogue(blk, slot):
        for ti in range(TPB):
            gi = blk * TPB + ti
            sl = gi % 2
            P.dma("sp", x1c[sl], x1_s[gi * 128:(gi + 1) * 128, :], "x1c%d" % sl)
            o = obt[ti]
            for cb in range(2):
                P.tt("dve", o[:, cb * 512:(cb + 1) * 512], yaccss[slot][cb][:, ti, :], gf_bcs[slot][:, cb * 512:(cb + 1) * 512], ALU.mult)
            P.tt("dve", o, o, x1c[sl], ALU.add)
            P.act(junkC, o, AF.Square, accum=ssq[:, 0, ti:ti + 1])
        P.ts("dve", ssq[:, 1, :], ssq[:, 0, :], 1.0 / D, ALU.mult)
        P.act(ssq[:, 2, :], ssq[:, 1, :], AF.Sqrt, bias=epsM[:, 0:1], scale=1.0)
        recip("dve", ssq[:, 3, :], ssq[:, 2, :])
        for ti in range(TPB):
            gi = blk * TPB + ti
            o = obt[ti]
            P.stt("dve", o, o, ssq[:, 3, ti:ti + 1], fnw_bc, ALU.mult, ALU.mult)
            P.dma("sp", out_d[gi * 128:(gi + 1) * 128, :], o, "outC%d" % (ti % 4))

    for blk in range(NB):
        t0 = blk * BLK
        b = t0 // SEQ
        slot = blk % 2
        h2b = h2bs[slot]
        cwb = cwbs[slot]
        yaccs = yaccss[slot]
        P.dma("sp", h2b, h2T_s[:, :, t0:t0 + BLK], "h2b%d" % slot)
        P.dma("sp", cwb, cw_s[t0:t0 + BLK, :].rearrange("(n p) e -> p n e", p=128), "cwb%d" % slot)
        P.dma("sp", gf_bcs[slot], mod_s[b:b + 1, 5120:6144].partition_broadcast(128), "gfbc%d" % slot)
        NSB = BLK // SUB
        units = [(e, sb, (e * NSB + sb) % 2) for e in range(NE) for sb in range(NSB)]

        def c_loads(e):
            ws = wcnt[0] % 2
            wcnt[0] += 1
            P.dma("sp", wgb[ws], wg_s[e].rearrange("p (k f) -> p k f", k=8), "wgb%d" % ws)
            P.dma("sp", wub[ws], wu_s[e].rearrange("p (k f) -> p k f", k=8), "wub%d" % ws)
            P.dma("sp", wdb[ws], wd_s[e].rearrange("p (c d) -> p c d", c=2), "wdb%d" % ws)
            return ws

        wslot = {}

        def c_gu(u, fc):
            e, sb, asl = u
            ws = wslot[e]
            for k in range(8):
                P.mm(pG[fc][:, 0:SUB], wgb[ws][:, k, fc * 128:(fc + 1) * 128], h2b[:, k, sb * SUB:(sb + 1) * SUB], start=(k == 0), stop=(k == 7))
            for k in range(8):
                P.mm(pUu[fc][:, 0:SUB], wub[ws][:, k, fc * 128:(fc + 1) * 128], h2b[:, k, sb * SUB:(sb + 1) * SUB], start=(k == 0), stop=(k == 7))
            P.act(sg[fc], pG[fc][:, 0:SUB], AF.Silu)
            P.tt("dve", actT[asl][fc], pUu[fc][:, 0:SUB], sg[fc], ALU.mult)

        def c_down(u):
            e, sb, asl = u
            ws = wslot[e]
            for tt_ in range(SUB // 128):
                ti = sb * (SUB // 128) + tt_
                for cb in range(2):
                    pd_ = pD[pdi[0] % 3]; pdi[0] += 1
                    for fc in range(2):
                        P.mm(pd_, actT[asl][fc][:, tt_ * 128:(tt_ + 1) * 128], wdb[ws][:, fc, cb * 512:(cb + 1) * 512], start=(fc == 0), stop=(fc == 1))
                    ysl = yaccs[cb][:, ti, :]
                    if e == 0:
                        P.ts("dve", ysl, pd_, cwb[:, ti, e:e + 1], ALU.mult)
                    else:
                        P.stt("dve", ysl, pd_, cwb[:, ti, e:e + 1], ysl, ALU.mult, ALU.add)

        wslot[0] = c_loads(0)
        c_gu(units[0], 0)
        c_gu(units[0], 1)
        for n in range(len(units)):
            if n + 1 < len(units):
                if units[n + 1][1] == 0:
                    wslot[units[n + 1][0]] = c_loads(units[n + 1][0])
                c_gu(units[n + 1], 0)
            c_down(units[n])
            if n + 1 < len(units):
                c_gu(units[n + 1], 1)
        epilogue(blk, slot)
    P.end_phase()
    P.finish()
    return nc, tapd


_NC_CACHE = {}


def kernel(**inputs):
    NCORES = 8
    x = np.asarray(inputs["x"], dtype=np.float32)
    B, S, _ = x.shape
    NSEQ = B // NCORES
    key = (NSEQ, S)
    if key not in _NC_CACHE:
        _NC_CACHE[key] = build(NSEQ, S)[0]
    nc = _NC_CACHE[key]
    shared = {}
    for k, shp in PARAM_SHAPES.items():
        shared[k] = np.ascontiguousarray(np.asarray(inputs[k], dtype=np.float32).reshape(shp))
    c = np.asarray(inputs["c"], dtype=np.float32)
    in_maps = []
    for i in range(NCORES):
        m = dict(shared)
        m["x"] = np.ascontiguousarray(x[i * NSEQ:(i + 1) * NSEQ].reshape(NSEQ * S, D))
        m["c"] = np.ascontiguousarray(c[i * NSEQ:(i + 1) * NSEQ])
        in_maps.append(m)
    res = run_bass_kernel_spmd(nc, in_maps, core_ids=list(range(NCORES)))
    outs = [np.asarray(r["out"]).reshape(NSEQ, S, D) for r in res.results]
    return np.concatenate(outs, axis=0).astype(np.float32)
```

```python
import numpy as np
from concourse.bass_utils import run_bass_kernel_spmd
import numpy as np
from contextlib import ExitStack
import concourse.bass as bass
import concourse.mybir as mybir

F32 = mybir.dt.float32
BF16 = mybir.dt.bfloat16
AF = mybir.ActivationFunctionType
ALU = mybir.AluOpType
AX = mybir.AxisListType

ENGS = ("pe", "act", "dve", "pool", "sp")
SEM_CAP = 30000


class Buf:
    __slots__ = ("name", "writers", "readers", "psum")

    def __init__(self, name, psum=False):
        self.name = name
        self.writers = {}
        self.readers = []
        self.psum = psum


class V:
    __slots__ = ("ap", "buf")

    def __init__(self, ap, buf):
        self.ap = ap
        self.buf = buf

    def __getitem__(self, k):
        return V(self.ap[k], self.buf)

    def re(self, pat, **kw):
        return V(self.ap.rearrange(pat, **kw), self.buf)

    def bc(self, axis, n):
        a = self.ap.unsqueeze(axis)
        shp = list(a.shape)
        shp[axis] = n
        return V(a.to_broadcast(shp), self.buf)

    def sub(self, name):
        return V(self.ap, Buf(name))


class Op:
    __slots__ = ("eng", "fn", "idx", "eidx", "dma_key", "deps", "signals", "sigval", "semi", "extra", "phase")

    def __init__(self, eng, fn, dma_key):
        self.eng = eng
        self.fn = fn
        self.dma_key = dma_key
        self.deps = []
        self.signals = False
        self.sigval = 0
        self.semi = 0
        self.extra = []


def _bufs(vs):
    out = []
    for v in vs:
        if isinstance(v, V):
            out.append(v.buf)
        elif isinstance(v, Buf):
            out.append(v)
    return out


class Prog:
    def __init__(self, nc):
        self.nc = nc
        self.ops = []
        self.eops = {e: [] for e in ENGS}
        self.gstack = ExitStack()
        self.pstack = None
        self.cnt = {e: 0 for e in ENGS}
        self.dcnt = {}
        self.esems = {e: [] for e in ENGS}
        self.dsems = {}
        self.free_dsems = []
        self.waited = {e: {} for e in ENGS}
        self.carry = []
        self.nops_total = 0
        self.phase = 0

    def _stack(self):
        return self.pstack if self.pstack is not None else self.gstack

    def tile(self, name, shape, dt=F32):
        t = self._stack().enter_context(self.nc.sbuf_tensor(name, list(shape), dt))
        return V(t[:], Buf(name))

    def psum(self, name, shape, dt=F32):
        t = self._stack().enter_context(self.nc.psum_tensor(name, list(shape), dt))
        return V(t[:], Buf(name, psum=True))

    def begin_phase(self):
        self.pstack = ExitStack()
        self.ops = []
        self.eops = {e: [] for e in ENGS}

    def add(self, eng, fn, reads=(), writes=(), dma_key=None):
        op = Op(eng, fn, dma_key)
        op.idx = len(self.ops)
        op.phase = self.phase
        op.eidx = len(self.eops[eng])
        deps = {}
        wkey = ("dma", dma_key) if dma_key is not None else eng
        rb = _bufs(reads)
        wb = _bufs(writes)
        for b in rb:
            for w in b.writers.values():
                deps[id(w)] = w
            if b.psum:
                for r in b.readers:
                    if r.eng != eng:
                        deps[id(r)] = r
        for b in wb:
            for w in b.writers.values():
                deps[id(w)] = w
            for r in b.readers:
                deps[id(r)] = r
        deps.pop(id(op), None)
        op.deps = list(deps.values())
        for b in rb:
            b.readers.append(op)
        for b in wb:
            b.writers[wkey] = op
            b.readers = []
        self.ops.append(op)
        self.eops[eng].append(op)
        return op

    def mm(self, out, lhsT, rhs, start=True, stop=True, extra_r=()):
        return self.add("pe", lambda e: e.matmul(out.ap, lhsT=lhsT.ap, rhs=rhs.ap, start=start, stop=stop),
                        [lhsT, rhs] + list(extra_r), [out])

    def tr(self, out, in_, ident):
        return self.add("pe", lambda e: e.transpose(out.ap, in_.ap, ident.ap), [in_, ident], [out])

    def act(self, out, in_, func, bias=None, scale=None, accum=None, eng="act"):
        kw = {}
        r = [in_]
        if bias is not None:
            kw["bias"] = bias.ap if isinstance(bias, V) else bias
            if isinstance(bias, V):
                r.append(bias)
        if scale is not None:
            kw["scale"] = scale.ap if isinstance(scale, V) else scale
            if isinstance(scale, V):
                r.append(scale)
        w = [out]
        if accum is not None:
            kw["accum_out"] = accum.ap
            w.append(accum)
        return self.add(eng, lambda e: e.activation(out=out.ap, in_=in_.ap, func=func, **kw), r, w)

    def tt(self, eng, out, in0, in1, op):
        return self.add(eng, lambda e: e.tensor_tensor(out=out.ap, in0=in0.ap, in1=in1.ap, op=op), [in0, in1], [out])

    def ts(self, eng, out, in0, s1, op0, s2=None, op1=None, accum=None):
        r = [in0]
        a1 = s1.ap if isinstance(s1, V) else s1
        a2 = s2.ap if isinstance(s2, V) else s2
        if isinstance(s1, V):
            r.append(s1)
        if isinstance(s2, V):
            r.append(s2)
        w = [out]
        kw = {}
        if op1 is not None:
            kw["op1"] = op1
        if accum is not None:
            kw["accum_out"] = accum.ap
            w.append(accum)
        return self.add(eng, lambda e: e.tensor_scalar(out=out.ap, in0=in0.ap, scalar1=a1, scalar2=a2, op0=op0, **kw), r, w)

    def stt(self, eng, out, in0, scalar, in1, op0, op1):
        eng = "dve"
        r = [in0, in1]
        a = scalar.ap if isinstance(scalar, V) else scalar
        if isinstance(scalar, V):
            r.append(scalar)
        return self.add(eng, lambda e: e.scalar_tensor_tensor(out=out.ap, in0=in0.ap, scalar=a, in1=in1.ap, op0=op0, op1=op1), r, [out])

    def copy(self, eng, out, in_):
        if eng == "act":
            return self.add(eng, lambda e: e.copy(out=out.ap, in_=in_.ap), [in_], [out])
        return self.add(eng, lambda e: e.tensor_copy(out=out.ap, in_=in_.ap), [in_], [out])

    def red(self, eng, out, in_, op, axis=AX.X):
        return self.add(eng, lambda e: e.tensor_reduce(out=out.ap, in_=in_.ap, axis=axis, op=op), [in_], [out])

    def memset(self, eng, out, val):
        return self.add(eng, lambda e: e.memset(out.ap, val), [], [out])

    def dma(self, eng, out, in_, key, **kw):
        r = [in_] if isinstance(in_, V) else []
        w = [out] if isinstance(out, V) else []
        oa = out.ap if isinstance(out, V) else out
        ia = in_.ap if isinstance(in_, V) else in_
        return self.add(eng, lambda e: e.dma_start(out=oa, in_=ia, **kw), r, w, dma_key=key)

    def end_phase(self):
        nc = self.nc
        ops = self.ops
        need = {}
        for op in ops:
            wl = []
            for p in op.deps:
                if p.phase != self.phase:
                    continue
                if p.dma_key is not None:
                    wl.append(p)
                elif p.eng != op.eng:
                    p.signals = True
                    wl.append(p)
                else:
                    if op.eng == "pe" and op.dma_key is None:
                        continue
                    if op.dma_key is not None or (op.eidx - p.eidx) <= 4:
                        p.signals = True
                        wl.append(p)
            need[id(op)] = wl
        lastc = {}
        for e in ENGS:
            for op in reversed(self.eops[e]):
                if op.dma_key is None:
                    op.signals = True
                    lastc[e] = op
                    break
        for op in ops:
            if op.dma_key is not None:
                if op.dma_key not in self.dsems:
                    if self.free_dsems:
                        sem_, c0_ = self.free_dsems.pop()
                        self.dsems[op.dma_key] = sem_
                        self.dcnt[op.dma_key] = c0_
                    else:
                        self.dsems[op.dma_key] = self.gstack.enter_context(nc.semaphore("d_%s" % (op.dma_key,)))
                        self.dcnt[op.dma_key] = 0
                self.dcnt[op.dma_key] += 16
                op.sigval = self.dcnt[op.dma_key]
            elif op.signals:
                c = self.cnt[op.eng]
                op.semi = c // SEM_CAP
                op.sigval = c % SEM_CAP + 1
                self.cnt[op.eng] = c + 1
                while len(self.esems[op.eng]) <= op.semi:
                    i = len(self.esems[op.eng])
                    self.esems[op.eng].append(self.gstack.enter_context(nc.semaphore("s_%s_%d" % (op.eng, i))))
        plans = {e: [] for e in ENGS}
        first = {e: True for e in ENGS}
        for op in ops:
            ws = {}
            if first[op.eng]:
                first[op.eng] = False
                for key, sem, v in self.carry:
                    if self.waited[op.eng].get(key, 0) < v:
                        ws[key] = (sem, v)
            for p in need[id(op)]:
                if p.dma_key is not None:
                    sem = self.dsems[p.dma_key]
                    key = ("dsem", id(sem))
                else:
                    key = (p.eng, p.semi)
                    sem = self.esems[p.eng][p.semi]
                v = p.sigval
                if self.waited[op.eng].get(key, 0) >= v:
                    continue
                if key not in ws or ws[key][1] < v:
                    ws[key] = (sem, v)
            for key, (sem, v) in ws.items():
                self.waited[op.eng][key] = v
            if op.dma_key is not None:
                inc = (self.dsems[op.dma_key], 16)
            elif op.signals:
                inc = (self.esems[op.eng][op.semi], 1)
            else:
                inc = None
            plans[op.eng].append((op, list(ws.values()), inc))
        carry = [(("dsem", id(self.dsems[k])), self.dsems[k], v) for k, v in self.dcnt.items()]
        for e, op in lastc.items():
            carry.append(((e, op.semi), self.esems[e][op.semi], op.sigval))
        self.carry = carry + [c for c in self.carry if c[0] not in {x[0] for x in carry}]
        final_waits = [(sem, v) for (_, sem, v) in self.carry]

        def run(engobj, plan, final=None):
            for op, ws, inc in plan:
                for sem, v in ws:
                    engobj.wait_ge(sem, v)
                ins = op.fn(engobj)
                if inc is not None:
                    ins.then_inc(inc[0], inc[1])
            if final:
                for sem, v in final:
                    engobj.wait_ge(sem, v)

        with nc.Block() as block:
            @block.tensor
            def _(e):
                run(e, plans["pe"])

            @block.scalar
            def _(e):
                run(e, plans["act"])

            @block.vector
            def _(e):
                run(e, plans["dve"])

            @block.gpsimd
            def _(e):
                run(e, plans["pool"])

            @block.sync
            def _(e):
                run(e, plans["sp"], final_waits)
        self.nops_total += len(ops)
        self.phase += 1
        for k_ in list(self.dsems):
            self.free_dsems.append((self.dsems.pop(k_), self.dcnt.pop(k_)))
        self.ops = []
        self.eops = {e: [] for e in ENGS}
        if self.pstack is not None:
            self.pstack.close()
            self.pstack = None

    def finish(self):
        self.gstack.close()

D = 1024
INC = 3336
RW = 1792
NE = 32
DE = 256
EPS = 1e-6
GN_EPS = 64e-5
DEC = 0.6065306597126334

PARAM_SHAPES = {
    "ada_w": [1024, 6144], "ada_b": [1, 6144], "mix_norm_w": [1, 1024], "w_in": [1024, 3336],
    "rwkv_mu": [1, 1792], "rwkv_w0": [1, 512], "rwkv_w_up": [64, 512], "rwkv_a0": [1, 512],
    "rwkv_a_up": [64, 512], "rwkv_g_up": [128, 512], "rwkv_k_k": [1, 512], "rwkv_k_a": [1, 512],
    "rwkv_r_k": [1, 512], "rwkv_gn_w": [1, 512], "rwkv_gn_b": [1, 512], "mlstm_conv_w": [4, 512],
    "mlstm_conv_b": [1, 512], "mlstm_i_b": [1, 4], "mlstm_f_b": [1, 4], "mlstm_hn_w": [1, 512],
    "w_out": [1024, 1024], "ffn_norm_w": [1, 1024], "moe_w_group": [1024, 4], "moe_b_group": [1, 4],
    "moe_w_router": [1024, 32], "moe_b_router": [1, 32], "moe_w_gate": [32, 1024, 256],
    "moe_w_up": [32, 1024, 256], "moe_w_down": [32, 256, 1024], "final_norm_w": [1, 1024],
}


def build(NSEQ, SEQ, taps=None, stop_after=99):
    nc = bass.Bass("TRN2", target_bir_lowering=False)
    NT = SEQ // 128
    NTOK = NSEQ * SEQ
    NTILES = NSEQ * NT
    PADR = SEQ + 3
    dr = {}
    dr["x"] = nc.dram_tensor("x", [NTOK, D], F32, kind="ExternalInput").ap()
    dr["c"] = nc.dram_tensor("c", [NSEQ, D], F32, kind="ExternalInput").ap()
    for k, shp in PARAM_SHAPES.items():
        dr[k] = nc.dram_tensor(k, shp, F32, kind="ExternalInput").ap()
    out_d = nc.dram_tensor("out", [NTOK, D], F32, kind="ExternalOutput").ap()
    proj_s = nc.dram_tensor("proj_s", [NSEQ * PADR, INC], F32).ap()
    x1_s = nc.dram_tensor("x1_s", [NTOK, D], F32).ap()
    h2T_s = nc.dram_tensor("h2T_s", [128, 8, NTOK], BF16).ap()
    cw_s = nc.dram_tensor("cw_s", [NTOK, NE], F32).ap()
    mod_s = nc.dram_tensor("mod_s", [NSEQ, 6144], F32).ap()
    wg_s = nc.dram_tensor("wg_s", [NE, 128, 2048], BF16).ap()
    wu_s = nc.dram_tensor("wu_s", [NE, 128, 2048], BF16).ap()
    wd_s = nc.dram_tensor("wd_s", [NE, 128, 2048], BF16).ap()
    tapd = {}

    P = Prog(nc)

    def tap(name, v, shape):
        if taps is None or name not in taps:
            return
        t = nc.dram_tensor("tap_" + name, list(shape), v.ap.dtype, kind="ExternalOutput").ap()
        tapd[name] = t
        P.dma("sp", t, v, "tap_" + name)

    identf = P.tile("identf", [128, 128])
    ident = P.tile("ident", [128, 128], BF16)
    tri = P.tile("tri", [128, 128])
    onesf = P.tile("onesf", [128, 128])
    trimid = P.tile("trimid", [128, 128])
    indmid = P.tile("indmid", [128, 2])
    masknegf = P.tile("masknegf", [128, 128])
    maskA = P.tile("maskA", [128, 4, 128], BF16)
    mSI = P.tile("mSI", [128, 2, 2, 128], BF16)
    epsM = P.tile("epsM", [128, 1])
    epsG = P.tile("epsG", [128, 1])
    gamM = P.tile("gamM", [128, NSEQ, 8])
    shM = P.tile("shM", [128, NSEQ, 8])
    gamF = P.tile("gamF", [128, NSEQ, 8])
    shF = P.tile("shF", [128, NSEQ, 8])

    P.begin_phase()
    P.memset("pool", identf, 1.0)
    P.add("pool", lambda e: e.affine_select(identf.ap, identf.ap, [[-1, 128]], ALU.is_equal, 0.0, base=0, channel_multiplier=1), [identf], [identf])
    P.copy("pool", ident, identf)
    P.memset("pool", tri, 1.0)
    P.add("pool", lambda e: e.affine_select(tri.ap, tri.ap, [[1, 128]], ALU.is_ge, 0.0, base=0, channel_multiplier=-1), [tri], [tri])
    P.memset("pool", onesf, 1.0)
    colm = P.tile("colm", [128, 128])
    P.memset("pool", colm, 1.0)
    P.add("pool", lambda e: e.affine_select(colm.ap, colm.ap, [[0, 128]], ALU.is_ge, 0.0, base=63, channel_multiplier=-1), [colm], [colm])
    P.tt("pool", trimid, tri, colm, ALU.subtract)
    P.ts("pool", trimid, trimid, -DEC, ALU.mult)
    P.ts("pool", indmid[:, 0:1], colm[:, 0:1], -DEC, ALU.mult)
    P.ts("pool", indmid[:, 1:2], colm[:, 0:1], DEC, ALU.mult, -DEC, ALU.add)
    P.memset("pool", masknegf, 0.0)
    P.add("pool", lambda e: e.affine_select(masknegf.ap, masknegf.ap, [[1, 128]], ALU.is_ge, -30000.0, base=0, channel_multiplier=-1), [masknegf], [masknegf])
    mstr = P.tile("mstr", [128, 128])
    P.memset("pool", mstr, 1.0)
    P.add("pool", lambda e: e.affine_select(mstr.ap, mstr.ap, [[1, 128]], ALU.is_ge, 0.0, base=-1, channel_multiplier=-1), [mstr], [mstr])
    mlow = P.tile("mlow", [128, 128])
    P.memset("pool", mlow, 1.0)
    P.add("pool", lambda e: e.affine_select(mlow.ap, mlow.ap, [[-1, 128]], ALU.is_ge, 0.0, base=-1, channel_multiplier=1), [mlow], [mlow])
    for hh in range(4):
        P.copy("pool", maskA[:, hh, :], mlow)
    for hh in range(2):
        P.copy("pool", mSI[:, hh, 0, :], mstr)
        P.copy("pool", mSI[:, hh, 1, :], tri)
    P.memset("pool", epsM, EPS)
    P.memset("pool", epsG, GN_EPS)

    def bcload(dst, src, n, key):
        P.dma("sp", dst, src.partition_broadcast(n), key)

    ct = P.tile("ct", [128, D])
    P.dma("sp", ct[0:NSEQ, :], dr["c"][:, :], "c12")
    sct = P.tile("sct", [128, D])
    P.act(sct[0:NSEQ, :], ct[0:NSEQ, :], AF.Silu)
    psA = P.psum("psA", [128, 512])
    psB = P.psum("psB", [128, 512])
    psC = P.psum("psC", [128, 512])
    scT = P.tile("scT", [128, 8, NSEQ])
    for k in range(8):
        P.mm(psA[:, k * NSEQ:(k + 1) * NSEQ], sct[0:NSEQ, k * 128:(k + 1) * 128], identf[0:NSEQ, 0:NSEQ])
    P.copy("dve", scT, psA[:, 0:8 * NSEQ].re("p (k b) -> p k b", k=8))
    adab = P.tile("adab", [128, 6144])
    P.dma("sp", adab[0:NSEQ, :], dr["ada_b"][0:1, :].partition_broadcast(NSEQ), "c13")
    nrm = P.tile("nrm", [128, 2 * D])
    P.dma("sp", nrm[0:1, 0:D], dr["mix_norm_w"][0:1, :], "c14")
    P.dma("sp", nrm[0:1, D:2 * D], dr["ffn_norm_w"][0:1, :], "c14")
    nrmT = P.tile("nrmT", [128, 16])
    for k in range(16):
        P.mm(psC[:, k:k + 1], nrm[0:1, k * 128:(k + 1) * 128], onesf[0:1, 0:1])
    P.copy("dve", nrmT, psC[:, 0:16])
    adw = [P.tile("adw%d" % i, [128, 8, 512]) for i in range(2)]
    modb = [P.tile("modb%d" % i, [128, 512]) for i in range(2)]
    modT = P.tile("modT", [128, 48, NSEQ])
    for cb in range(12):
        a = adw[cb % 2]
        P.dma("sp", a, dr["ada_w"][:, cb * 512:(cb + 1) * 512].rearrange("(k p) f -> p k f", p=128), "adw%d" % (cb % 2))
        pm = psA if cb % 2 == 0 else psB
        for k in range(8):
            P.mm(pm[0:NSEQ, :], scT[:, k, :], a[:, k, :], start=(k == 0), stop=(k == 7))
        mb = modb[cb % 2]
        P.tt("dve", mb[0:NSEQ, :], pm[0:NSEQ, :], adab[0:NSEQ, cb * 512:(cb + 1) * 512], ALU.add)
        P.dma("sp", mod_s[:, cb * 512:(cb + 1) * 512], mb[0:NSEQ, :], "mods")
        for q in range(4):
            P.mm(psC[:, 64 + q * NSEQ:64 + (q + 1) * NSEQ], mb[0:NSEQ, q * 128:(q + 1) * 128], identf[0:NSEQ, 0:NSEQ])
        P.copy("act", modT[:, cb * 4:(cb + 1) * 4, :], psC[:, 64:64 + 4 * NSEQ].re("p (q b) -> p q b", q=4))
    for b in range(NSEQ):
        P.stt("dve", gamM[:, b, :], modT[:, 8:16, b], 1.0, nrmT[:, 0:8], ALU.add, ALU.mult)
        P.copy("dve", shM[:, b, :], modT[:, 0:8, b])
        P.stt("dve", gamF[:, b, :], modT[:, 32:40, b], 1.0, nrmT[:, 8:16], ALU.add, ALU.mult)
        P.copy("dve", shF[:, b, :], modT[:, 24:32, b])
    zt = P.tile("zt", [128, 512])
    P.memset("pool", zt, 0.0)
    for b in range(NSEQ):
        P.dma("sp", proj_s[b * PADR:b * PADR + 3, :].rearrange("r (a f) -> (r a) f", f=417), zt[0:24, 0:417], "zpad")
    tap("gamM", gamM, [128, NSEQ, 8])
    tap("shM", shM, [128, NSEQ, 8])
    P.end_phase()
    if stop_after <= 0:
        P.finish()
        return nc, tapd

    ycat_s = nc.dram_tensor("ycat_s", [NTOK, D], BF16).ap()

    def recip(eng, o, a):
        P.add(eng, lambda e: e.reciprocal(out=o.ap, in_=a.ap), [a], [o])

    P.begin_phase()
    Win = P.tile("Win", [128, 8, INC], BF16)
    stg = [P.tile("stg%d" % i, [128, INC]) for i in range(2)]
    ceng = ["act", "dve", "pool"]
    for k in range(8):
        s = stg[k % 2]
        P.dma("sp", s, dr["w_in"][k * 128:(k + 1) * 128, :], "stg%d" % (k % 2))
        P.copy(ceng[k % 3], Win[:, k, :], s)
    xt = [P.tile("xt%d" % i, [128, D]) for i in range(3)]
    junk = P.tile("junkA", [128, D], BF16)
    xn = [P.tile("xn%d" % i, [128, D], BF16) for i in range(2)]
    hT = [P.tile("hT%d" % i, [128, 8, 128], BF16) for i in range(2)]
    hTk = [[hT[i][:, k, :].sub("hT%d_%d" % (i, k)) for k in range(8)] for i in range(2)]
    pj = [P.tile("pj%d" % i, [128, INC]) for i in range(3)]
    pjc = [[pj[i][:, cb * 512:min(INC, (cb + 1) * 512)].sub("pj%d_%d" % (i, cb)) for cb in range(7)] for i in range(3)]
    st = [P.tile("stA%d" % i, [128, 4]) for i in range(2)]
    ptb = [P.psum("ptbA%d" % i, [128, 8, 128], BF16) for i in range(2)]
    pp = [P.psum("ppA%d" % i, [128, 512]) for i in range(5)]
    wst = [P.tile("wst%d" % i, [128, 2048]) for i in range(4)]
    wbt = [P.tile("wbt%d" % i, [128, 2048], BF16) for i in range(4)]
    pc_list = []
    for e in range(NE):
        pc_list.append((dr["moe_w_gate"][e].rearrange("(k p) f -> p k f", p=128), wg_s[e], 8))
        pc_list.append((dr["moe_w_up"][e].rearrange("(k p) f -> p k f", p=128), wu_s[e], 8))
        pc_list.append((dr["moe_w_down"][e].rearrange("(c p) d -> p c d", p=128), wd_s[e], 2))

    def pc_load(n):
        src, dst, a_ = pc_list[n]
        P.dma("pool", wst[n % 4].re("p (a b) -> p a b", a=a_), src, "wst%d" % (n % 4))

    def precast_gen():
        for n in range(min(3, len(pc_list))):
            pc_load(n)
        for n in range(len(pc_list)):
            if n + 3 < len(pc_list):
                pc_load(n + 3)
            P.copy("pool" if n % 2 == 0 else "act", wbt[n % 4], wst[n % 4])
            P.dma("pool", pc_list[n][1], wbt[n % 4], "wbt%d" % (n % 4))
            yield

    pcg = precast_gen()

    def a_load(i):
        P.dma("sp", xt[i % 3], dr["x"][i * 128:(i + 1) * 128, :], "xA%d" % (i % 3))

    def a_front(i):
        b = i // NT
        sl = i % 2
        s4 = st[sl]
        P.act(junk, xt[i % 3], AF.Square, accum=s4[:, 0:1])
        P.ts("dve", s4[:, 1:2], s4[:, 0:1], 1.0 / D, ALU.mult)
        P.act(s4[:, 2:3], s4[:, 1:2], AF.Sqrt, bias=epsM[:, 0:1], scale=1.0)
        recip("dve", s4[:, 3:4], s4[:, 2:3])
        P.act(xn[sl], xt[i % 3], AF.Copy, scale=s4[:, 3:4])
        for k in range(8):
            P.tr(ptb[k // 4][:, k % 4, :], xn[sl][:, k * 128:(k + 1) * 128], ident)
        for k in range(8):
            if k < 4:
                P.ts("dve", hTk[sl][k], ptb[0][:, k % 4, :], gamM[:, b, k:k + 1], ALU.mult, shM[:, b, k:k + 1], ALU.add)
            else:
                P.act(hTk[sl][k], ptb[1][:, k % 4, :], AF.Identity, bias=shM[:, b, k:k + 1], scale=gamM[:, b, k:k + 1])

    ppi = [0]

    def a_back(i):
        b = i // NT
        it = i % NT
        sl = i % 2
        for cb in range(7):
            c0 = cb * 512
            cw_ = min(512, INC - c0)
            pq = pp[ppi[0] % 5]; ppi[0] += 1
            for k in range(8):
                P.mm(pq[:, 0:cw_], hTk[sl][k], Win[:, k, c0:c0 + cw_], start=(k == 0), stop=(k == 7))
            P.copy("act" if cb % 2 == 0 else "dve", pjc[i % 3][cb], pq[:, 0:cw_])
        r0 = b * PADR + 3 + it * 128
        P.add("sp", (lambda o_, i_: (lambda e: e.dma_start(out=o_, in_=i_)))(proj_s[r0:r0 + 128, :], pj[i % 3].ap), pjc[i % 3], [], dma_key="pjA%d" % (i % 3))

    a_load(0)
    if NTILES > 1:
        a_load(1)
    a_front(0)
    for i in range(NTILES):
        if i + 2 < NTILES:
            a_load(i + 2)
        if i + 1 < NTILES:
            a_front(i + 1)
        a_back(i)
        for _ in range(2):
            next(pcg, None)
    for _ in pcg:
        pass
    P.end_phase()
    if stop_after <= 1:
        P.finish()
        return nc, tapd

    P.begin_phase()
    Wup = P.tile("Wup", [128, 512], BF16)
    Aup = P.tile("Aup", [128, 512], BF16)
    Gup = P.tile("Gup", [128, 512], BF16)
    mu_bc = P.tile("mu_bc", [128, RW])
    kk_bc = P.tile("kk_bc", [128, 512])
    ka_bc = P.tile("ka_bc", [128, 512])
    rk_bc = P.tile("rk_bc", [128, 512])
    gnw_bc = P.tile("gnw_bc", [128, 512])
    gnb_bc = P.tile("gnb_bc", [128, 512])
    bcload(mu_bc, dr["rwkv_mu"][0:1, :], 128, "c0")
    bcload(kk_bc, dr["rwkv_k_k"][0:1, :], 128, "c1")
    bcload(ka_bc, dr["rwkv_k_a"][0:1, :], 128, "c2")
    bcload(rk_bc, dr["rwkv_r_k"][0:1, :], 128, "c3")
    bcload(gnw_bc, dr["rwkv_gn_w"][0:1, :], 128, "c4")
    bcload(gnb_bc, dr["rwkv_gn_b"][0:1, :], 128, "c5")
    s = P.tile("stgB1", [128, 1536])
    P.dma("sp", s[0:64, 0:512], dr["rwkv_w_up"][:, :], "stgb1")
    P.dma("sp", s[64:65, 0:512], dr["rwkv_w0"][0:1, :], "stgb1")
    P.dma("sp", s[0:64, 512:1024], dr["rwkv_a_up"][:, :], "stgb1")
    P.dma("sp", s[64:65, 512:1024], dr["rwkv_a0"][0:1, :], "stgb1")
    P.dma("sp", s[:, 1024:1536], dr["rwkv_g_up"][:, :], "stgb1")
    P.copy("act", Wup[0:64, :], s[0:64, 0:512])
    P.copy("act", Wup[64:65, :], s[64:65, 0:512])
    P.copy("dve", Aup[0:64, :], s[0:64, 512:1024])
    P.copy("dve", Aup[64:65, :], s[64:65, 512:1024])
    P.copy("pool", Gup, s[:, 1024:1536])
    NSTR = 2 if NSEQ % 2 == 0 else 1

    def alloc_b1(q):
        sx = "_s%d" % q
        T = {"id": q}
        T["rw"] = P.tile("rw" + sx, [128, RW])
        T["rwp"] = P.tile("rwp" + sx, [128, RW])
        T["li"] = P.tile("li" + sx, [128, 256], BF16)
        T["liT"] = P.tile("liT" + sx, [128, 384], BF16)
        for n in ("sgz", "av", "gv", "Ep", "Em", "Epv", "kkk", "tmpR", "tmpR2", "kkv", "knew", "y_sb"):
            T[n] = P.tile(n + sx, [128, 512])
        T["gmL"] = P.tile("gmL" + sx, [128, 4, 2])
        T["sm8"] = P.tile("sm8" + sx, [128, 64])
        T["tok4"] = P.tile("tok4" + sx, [128, 4, 512], BF16)
        T["v_bf"] = P.tile("v_bf" + sx, [128, 512], BF16)
        T["FT"] = P.tile("FT" + sx, [128, 4, 4, 128], BF16)
        T["A_sb"] = [[P.tile("A_sb%d_%d%s" % (g, i, sx), [128, 4, 128], BF16) for i in range(2)] for g in range(2)]
        T["MT"] = [[P.tile("MT%d_%d%s" % (g, i, sx), [128, 4, 2, 128], BF16) for i in range(2)] for g in range(2)]
        T["MRB"] = P.tile("MRB" + sx, [128, 8, 2, 128], BF16)
        T["AKRK"] = P.tile("AKRK" + sx, [128, 8, 2, 128], BF16)
        T["TT"] = P.tile("TT" + sx, [128, 8, 128], BF16)
        T["X_bf"] = P.tile("X_bf" + sx, [128, 512], BF16)
        T["U_bf"] = P.tile("U_bf" + sx, [128, 512], BF16)
        T["Hst"] = P.tile("Hst" + sx, [128, 4, 64])
        T["Ht"] = P.tile("Ht" + sx, [128, 4, 64])
        T["Hbd"] = P.tile("Hbd" + sx, [128, 4, 128], BF16)
        T["yr"] = [P.tile("yr%d%s" % (i, sx), [128, 512], BF16) for i in range(2)]
        P.memset("pool", T["liT"], 1.0)
        P.memset("pool", T["Hbd"], 0.0)
        return T

    B1S = [alloc_b1(q) for q in range(NSTR)]
    ptbB = [P.psum("ptbB%d" % i, [128, 1024], BF16) for i in range(2)]
    pf = [P.psum("pfB%d" % i, [128, 512]) for i in range(5)]
    pfi = [0]

    def bank():
        b_ = pf[pfi[0] % 5]
        pfi[0] += 1
        return b_

    H8 = "p (h c) -> p h c"
    def b1_tile(i, T):
        rw = T["rw"]
        rwp = T["rwp"]
        li = T["li"]
        liT = T["liT"]
        sgz = T["sgz"]
        av = T["av"]
        gv = T["gv"]
        Ep = T["Ep"]
        Em = T["Em"]
        Epv = T["Epv"]
        gmL = T["gmL"]
        kkk = T["kkk"]
        tmpR = T["tmpR"]
        tmpR2 = T["tmpR2"]
        kkv = T["kkv"]
        knew = T["knew"]
        sm8 = T["sm8"]
        tok4 = T["tok4"]
        v_bf = T["v_bf"]
        FT = T["FT"]
        A_sb = T["A_sb"]
        MT = T["MT"]
        MRB = T["MRB"]
        AKRK = T["AKRK"]
        TT = T["TT"]
        X_bf = T["X_bf"]
        U_bf = T["U_bf"]
        Hst = T["Hst"]
        Ht = T["Ht"]
        Hbd = T["Hbd"]
        y_sb = T["y_sb"]
        yr = T["yr"]
        b = i // NT
        it = i % NT
        r0 = b * PADR + 3 + it * 128
        P.dma("sp", rw, proj_s[r0:r0 + 128, 0:RW], "rw%d" % T["id"])
        P.dma("sp", rwp, proj_s[r0 - 1:r0 + 127, 0:RW], "rwp%d" % T["id"])
        if it == 0:
            P.memset("pool", Hst, 0.0)
        u = rwp
        P.tt("dve", u, u, rw, ALU.subtract)
        P.tt("dve", u, u, mu_bc, ALU.mult)
        P.tt("dve", u, u, rw, ALU.add)
        r_ = u[:, 0:512]; k_ = u[:, 512:1024]; v_ = u[:, 1024:1536]
        if i == 0:
            tap("u0", u, [128, RW])
        yield
        P.act(li[:, 0:64], u[:, 1536:1600], AF.Tanh)
        P.copy("pool", li[:, 64:128], u[:, 1600:1664])
        P.act(li[:, 128:256], u[:, 1664:1792], AF.Sigmoid)
        pb0 = ptbB[0]
        P.tr(pb0[0:64, 0:128], li[:, 0:64], ident)
        P.tr(pb0[0:64, 128:256], li[:, 64:128], ident)
        P.tr(pb0[:, 256:384], li[:, 128:256], ident)
        P.copy("dve", liT[0:64, 0:256], pb0[0:64, 0:256])
        P.copy("act", liT[:, 256:384], pb0[:, 256:384])
        pz = bank(); pa = bank(); pg = bank()
        P.mm(pz, liT[0:65, 0:128], Wup[0:65, :])
        P.mm(pa, liT[0:65, 128:256], Aup[0:65, :])
        P.mm(pg, liT[:, 256:384], Gup)
        P.act(sgz, pz, AF.Sigmoid)
        P.act(av, pa, AF.Sigmoid)
        P.copy("dve", gv, pg)
        yield
        pc = bank()
        P.mm(pc, trimid, sgz)
        P.act(Ep, pc, AF.Exp)
        P.act(Em, pc, AF.Exp, scale=-1.0)
        P.stt("dve", Epv, sgz, DEC, pc, ALU.mult, ALU.add)
        P.act(Epv, Epv, AF.Exp)
        psm = bank()
        for j in range(4):
            P.mm(psm[:, 2 * j:2 * j + 2], sgz[:, j * 128:(j + 1) * 128], indmid)
        P.act(gmL, psm[:, 0:8].re("p (j t) -> p j t", j=4), AF.Exp)
        yield
        P.tt("dve", kkk, k_, kk_bc, ALU.mult)
        P.tt("dve", tmpR, kkk, kkk, ALU.mult)
        P.red("dve", sm8[:, 0:8], tmpR.re(H8, h=8), ALU.add)
        P.act(sm8[:, 8:16], sm8[:, 0:8], AF.Sqrt)
        P.ts("dve", sm8[:, 8:16], sm8[:, 8:16], 1e-12, ALU.max)
        recip("dve", sm8[:, 16:24], sm8[:, 8:16])
        P.tt("dve", kkv.re(H8, h=8), kkk.re(H8, h=8), sm8[:, 16:24].bc(2, 64), ALU.mult)
        P.stt("pool", tmpR, av, -1.0, ka_bc, ALU.add, ALU.mult)
        P.stt("pool", knew, tmpR, 1.0, k_, ALU.add, ALU.mult)
        P.tt("pool", tmpR, r_, knew, ALU.mult)
        P.tt("pool", tmpR, tmpR, rk_bc, ALU.mult)
        P.red("dve", sm8[:, 24:32], tmpR.re(H8, h=8), ALU.add)
        yield
        P.stt("dve", tok4[:, 0, :], kkv, -1.0, Epv, ALU.mult, ALU.mult)
        P.tt("dve", tok4[:, 1, :], r_, Ep, ALU.mult)
        P.tt("dve", tok4[:, 2, :], knew, Em, ALU.mult)
        P.tt("dve", tmpR2, kkv, av, ALU.mult)
        P.tt("dve", tok4[:, 3, :], tmpR2, Em, ALU.mult)
        P.copy("act", v_bf, v_)
        if i == 0:
            tap("tok4", tok4, [128, 4, 512])
            tap("sgz", sgz, [128, 512])
        yield
        for half in range(2):
            pb_ = ptbB[half]
            for jj in range(2):
                j = half * 2 + jj
                for w in range(4):
                    n = jj * 4 + w
                    P.tr(pb_[:, n * 128:(n + 1) * 128], tok4[:, w, j * 128:(j + 1) * 128], ident)
            P.copy("act" if half == 0 else "dve",
                   FT[:, half * 2:half * 2 + 2, :, :], pb_[:, :].re("p (j w t) -> p j w t", j=2, w=4))
        yield
        for g in range(2):
            pA = bank()
            pMR = [bank(), bank()]
            pKR = [bank(), bank()]
            for hh in range(4):
                j = hh
                po = 64 * g
                aT = FT[po:po + 64, j, 0, :]
                arT = FT[po:po + 64, j, 0:2, :]
                kT = FT[po:po + 64, j, 2, :]
                bT = FT[po:po + 64, j, 3, :]
                P.mm(pA[:, hh * 128:(hh + 1) * 128], aT, bT)
                P.mm(pMR[hh // 2][:, (hh % 2) * 256:(hh % 2 + 1) * 256], bT, arT)
                P.mm(pKR[hh // 2][:, (hh % 2) * 256:(hh % 2 + 1) * 256], kT, arT)
            P.tt("dve", A_sb[g][0], pA[:, :].re("p (h t) -> p h t", h=4), maskA, ALU.mult)
            for q in range(2):
                P.tt("dve", MRB[:, g * 4 + q * 2:g * 4 + q * 2 + 2, :, :], pMR[q][:, :].re("p (h w t) -> p h w t", h=2, w=2), mSI, ALU.mult)
                P.tt("dve", AKRK[:, g * 4 + q * 2:g * 4 + q * 2 + 2, :, :], pKR[q][:, :].re("p (h w t) -> p h w t", h=2, w=2), mSI, ALU.mult)
            P.tt("pool", MT[g][0][:, :, 1, :], MRB[:, g * 4:g * 4 + 4, 0, :], ident.bc(1, 4), ALU.add)
            if g == 0:
                yield
        yield
        for g in range(2):
            pM = bank(); pA2 = bank()
            for hh in range(4):
                h = g * 4 + hh
                P.mm(pM[:, hh * 128:(hh + 1) * 128], A_sb[g][0][:, hh, :], MRB[:, h, 0, :])
                P.mm(pA2[:, hh * 128:(hh + 1) * 128], MRB[:, h, 0, :], A_sb[g][0][:, hh, :])
            P.copy("act", MT[g][0][:, :, 0, :], pM[:, :].re("p (h t) -> p h t", h=4))
            P.copy("act", A_sb[g][1], pA2[:, :].re("p (h t) -> p h t", h=4))
            if g == 0:
                yield
        yield
        cur = 0
        for lev in range(1, 6):
            yield
            for g in range(2):
                Ak = A_sb[g][lev % 2]
                An = A_sb[g][(lev + 1) % 2]
                Mc = MT[g][cur]
                Mn = MT[g][1 - cur]
                pS = [bank(), bank()]
                pA2 = bank()
                for hh in range(4):
                    reg = pS[hh // 2][:, (hh % 2) * 256:(hh % 2 + 1) * 256]
                    P.mm(reg, Ak[:, hh, :], Mc[:, hh, :, :], start=True, stop=False)
                    P.mm(reg[:, 128:256], ident, Mc[:, hh, 1, :], start=False, stop=True)
                    P.mm(pA2[:, hh * 128:(hh + 1) * 128], Mc[:, hh, 0, :], Ak[:, hh, :])
                for q in range(2):
                    P.copy("act", Mn[:, q * 2:q * 2 + 2, :, :], pS[q][:, :].re("p (h w t) -> p h w t", h=2, w=2))
                P.copy("act", An, pA2[:, :].re("p (h t) -> p h t", h=4))
                if g == 0:
                    yield
            cur = 1 - cur
        for g in range(2):
            Ak = A_sb[g][0]
            Mc = MT[g][cur]
            pT = bank()
            for hh in range(4):
                P.mm(pT[:, hh * 128:(hh + 1) * 128], Ak[:, hh, :], Mc[:, hh, 1, :], start=True, stop=False)
                P.mm(pT[:, hh * 128:(hh + 1) * 128], ident, Mc[:, hh, 1, :], start=False, stop=True)
            P.copy("act", TT[:, g * 4:g * 4 + 4, :], pT[:, :].re("p (h t) -> p h t", h=4))
            if g == 0:
                yield
        if i == 0:
            tap("TT", TT, [128, 8, 128])
            tap("MRB", MRB, [128, 8, 2, 128])
        yield
        P.tt("pool", Ht, Hst, gmL[:, :, 0].bc(2, 64), ALU.mult)
        P.copy("pool", Hbd[0:64, :, 0:64], Ht[0:64, :, :])
        P.copy("pool", Hbd[64:128, :, 64:128], Ht[64:128, :, :])
        pX = bank()
        SI = [(h % 2) * 4 + h // 2 for h in range(8)]
        for j in range(4):
            P.mm(pX[:, j * 128:(j + 1) * 128], FT[:, j, 0, :], Hbd[:, j, :], start=True, stop=False)
            for h in (2 * j, 2 * j + 1):
                P.mm(pX[:, h * 64:(h + 1) * 64], AKRK[:, SI[h], 0, :], v_bf[:, h * 64:(h + 1) * 64], start=False, stop=(h == 2 * j + 1))
        P.copy("act", X_bf, pX)
        yield
        pU = bank()
        for h in range(8):
            P.mm(pU[:, h * 64:(h + 1) * 64], TT[:, SI[h], :], X_bf[:, h * 64:(h + 1) * 64])
        P.copy("dve", U_bf, pU)
        yield
        pY = bank()
        for j in range(4):
            P.mm(pY[:, j * 128:(j + 1) * 128], FT[:, j, 1, :], Hbd[:, j, :], start=True, stop=False)
            for h in (2 * j, 2 * j + 1):
                P.mm(pY[:, h * 64:(h + 1) * 64], AKRK[:, SI[h], 1, :], v_bf[:, h * 64:(h + 1) * 64], start=False, stop=False)
                P.mm(pY[:, h * 64:(h + 1) * 64], MRB[:, SI[h], 1, :], U_bf[:, h * 64:(h + 1) * 64], start=False, stop=(h == 2 * j + 1))
        P.copy("act", y_sb, pY)
        yield
        pH = bank()
        for j in range(4):
            P.mm(pH[:, j * 128:(j + 1) * 128], tok4[:, 2, j * 128:(j + 1) * 128], v_bf[:, j * 128:(j + 1) * 128], start=True, stop=False)
            P.mm(pH[:, j * 128:(j + 1) * 128], tok4[:, 3, j * 128:(j + 1) * 128], U_bf[:, j * 128:(j + 1) * 128], start=False, stop=True)
        pHv = pH[:, :].re("p (j v) -> p j v", j=4)
        P.tt("dve", Hst[0:64, :, :], pHv[0:64, :, 0:64], Ht[0:64, :, :], ALU.add)
        P.tt("dve", Hst[64:128, :, :], pHv[64:128, :, 64:128], Ht[64:128, :, :], ALU.add)
        P.tt("pool", Hst, Hst, gmL[:, :, 1].bc(2, 64), ALU.mult)
        yield
        if i <= 1:
            tap("y_rwkv%d" % i, y_sb, [128, 512])
        y3 = y_sb.re(H8, h=8)
        P.red("dve", sm8[:, 32:40], y3, ALU.add)
        P.tt("pool", tmpR, y_sb, y_sb, ALU.mult)
        P.red("dve", sm8[:, 40:48], tmpR.re(H8, h=8), ALU.add)
        P.ts("dve", sm8[:, 32:40], sm8[:, 32:40], 1.0 / 64, ALU.mult)
        P.tt("dve", sm8[:, 48:56], sm8[:, 32:40], sm8[:, 32:40], ALU.mult)
        P.stt("dve", sm8[:, 40:48], sm8[:, 40:48], 1.0 / 64, sm8[:, 48:56], ALU.mult, ALU.subtract)
        P.act(sm8[:, 48:56], sm8[:, 40:48], AF.Sqrt, bias=epsG[:, 0:1], scale=1.0)
        recip("dve", sm8[:, 56:64], sm8[:, 48:56])
        P.tt("dve", tmpR.re(H8, h=8), y3, sm8[:, 32:40].bc(2, 64), ALU.subtract)
        P.tt("dve", tmpR.re(H8, h=8), tmpR.re(H8, h=8), sm8[:, 56:64].bc(2, 64), ALU.mult)
        P.tt("pool", tmpR, tmpR, gnw_bc, ALU.mult)
        P.tt("dve", tmpR, tmpR, gnb_bc, ALU.add)
        P.tt("pool", tmpR2.re(H8, h=8), v_.re(H8, h=8), sm8[:, 24:32].bc(2, 64), ALU.mult)
        P.tt("dve", tmpR, tmpR, tmpR2, ALU.add)
        P.tt("pool", yr[i % 2], tmpR, gv, ALU.mult)
        P.dma("sp", ycat_s[i * 128:(i + 1) * 128, 0:512], yr[i % 2], "yrs%d_%d" % (T["id"], i % 2))
        if i == 0:
            tap("yr", yr[0], [128, 512])
        if i == 1:
            tap("yr1", yr[1], [128, 512])

    def run_streams(tile_fn, streams, offset=0):
        ns = min(len(streams), NSEQ)

        def chain(q):
            for b_ in range(q, NSEQ, ns):
                for it_ in range(NT):
                    yield from tile_fn(b_ * NT + it_, streams[q])
                    yield

        gens = [chain(q) for q in range(ns)]
        for q, g_ in enumerate(gens):
            for _ in range(q * offset):
                next(g_, None)
        live = list(gens)
        while live:
            nxt = []
            for g_ in live:
                try:
                    next(g_)
                    nxt.append(g_)
                except StopIteration:
                    pass
            live = nxt

    run_streams(b1_tile, B1S, offset=16)
    P.end_phase()
    if stop_after <= 2:
        P.finish()
        return nc, tapd

    P.begin_phase()
    hnw_bc = P.tile("hnw_bc", [128, 512])
    cvw_bc = P.tile("cvw_bc", [128, 4, 512])
    cvb_bc = P.tile("cvb_bc", [128, 512])
    gb_bc = P.tile("gb_bc", [128, 8])
    bcload(hnw_bc, dr["mlstm_hn_w"][0:1, :], 128, "c6")
    for j in range(4):
        bcload(cvw_bc[:, j, :], dr["mlstm_conv_w"][j:j + 1, :], 128, "c7")
    bcload(cvb_bc, dr["mlstm_conv_b"][0:1, :], 128, "c8")
    bcload(gb_bc[:, 0:4], dr["mlstm_i_b"][0:1, :], 128, "c9")
    bcload(gb_bc[:, 4:8], dr["mlstm_f_b"][0:1, :], 128, "c9")
    ones_bf = P.tile("ones_bf", [128, 1], BF16)

    def alloc_b2(q):
        sx = "_m%d" % q
        T = {"id": q}
        T["qk4"] = P.tile("qk4" + sx, [128, 4, 512])
        T["mr"] = P.tile("mr" + sx, [128, 1032])
        for n in ("cacc", "ctmp", "slu", "og"):
            T[n] = P.tile(n + sx, [128, 512])
        T["qq"] = P.tile("qq" + sx, [128, 768], BF16)
        T["qkT"] = P.tile("qkT" + sx, [128, 12, 128], BF16)
        T["g8"] = P.tile("g8" + sx, [128, 64])
        T["sm4"] = P.tile("sm4" + sx, [128, 16])
        T["lfb"] = P.tile("lfb" + sx, [128, 4, 128])
        T["DTm"] = P.tile("DTm" + sx, [128, 4, 128])
        T["PTm"] = P.tile("PTm" + sx, [128, 4, 128], BF16)
        T["Vb"] = P.tile("Vb" + sx, [128, 4, 128], BF16)
        T["Kw"] = P.tile("Kw" + sx, [128, 4, 64], BF16)
        T["Cst"] = P.tile("Cst" + sx, [128, 4, 128])
        T["nst"] = P.tile("nst" + sx, [128, 4])
        T["C_bf"] = P.tile("C_bf" + sx, [128, 4, 128], BF16)
        T["n_bf"] = P.tile("n_bf" + sx, [128, 4], BF16)
        T["hm"] = P.tile("hm" + sx, [128, 4, 128])
        T["ym"] = [P.tile("ym%d%s" % (i, sx), [128, 512], BF16) for i in range(2)]
        return T

    NSTR2 = 4 if NSEQ % 4 == 0 else NSTR
    B2S = [alloc_b2(q) for q in range(NSTR2)]
    ptbM = [P.psum("ptbM%d" % i, [128, 1024], BF16) for i in range(2)]
    pfm = [P.psum("pfM%d" % i, [128, 512]) for i in range(5)]
    pmi = [0]

    def bankm():
        b_ = pfm[pmi[0] % 5]
        pmi[0] += 1
        return b_

    P.memset("pool", ones_bf, 1.0)
    H4 = "p (h c) -> p h c"
    def b2_tile(i, T):
        qk4 = T["qk4"]
        mr = T["mr"]
        cacc = T["cacc"]
        ctmp = T["ctmp"]
        slu = T["slu"]
        qq = T["qq"]
        qkT = T["qkT"]
        g8 = T["g8"]
        sm4 = T["sm4"]
        lfb = T["lfb"]
        DTm = T["DTm"]
        PTm = T["PTm"]
        Vb = T["Vb"]
        Kw = T["Kw"]
        Cst = T["Cst"]
        nst = T["nst"]
        C_bf = T["C_bf"]
        n_bf = T["n_bf"]
        hm = T["hm"]
        og = T["og"]
        ym = T["ym"]
        b = i // NT
        it = i % NT
        r0 = b * PADR + 3 + it * 128
        for j in range(4):
            P.dma("sp", qk4[:, j, :], proj_s[r0 - 3 + j:r0 + 125 + j, RW:RW + 512], "qk4%d" % T["id"])
        P.dma("sp", mr, proj_s[r0:r0 + 128, RW + 512:INC], "mr%d" % T["id"])
        if it == 0:
            P.memset("pool", Cst, 0.0)
            P.memset("pool", nst, 0.0)
            P.memset("pool", C_bf, 0.0)
            P.memset("pool", n_bf, 0.0)
        q4 = qk4
        cparts = [cacc, ctmp, og, hm.re("p h v -> p (h v)")]
        for j in range(4):
            P.tt("pool", cparts[j], q4[:, j, :], cvw_bc[:, j, :], ALU.mult)
        for j in range(1, 4):
            P.tt("dve", cacc, cacc, cparts[j], ALU.add)
        P.tt("dve", cacc, cacc, cvb_bc, ALU.add)
        P.act(slu, cacc, AF.Silu)
        m = mr
        P.act(og, m[:, 512:1024], AF.Tanh, scale=0.5)
        yield
        P.tt("dve", g8[:, 0:8], m[:, 1024:1032], gb_bc, ALU.add)
        P.act(g8[:, 8:16], g8[:, 0:8], AF.Exp, scale=2.0 / 15.0)
        P.ts("dve", g8[:, 8:16], g8[:, 8:16], 1.0, ALU.add)
        recip("dve", g8[:, 8:16], g8[:, 8:16])
        P.ts("dve", g8[:, 8:16], g8[:, 8:16], -30.0, ALU.mult, 15.0, ALU.add)
        P.act(g8[:, 16:20], g8[:, 12:16], AF.Exp, scale=-1.0)
        P.act(g8[:, 20:24], g8[:, 16:20], AF.Ln, bias=1.0, scale=1.0)
        P.ts("dve", g8[:, 24:28], g8[:, 20:24], -1.0, ALU.mult)
        lf = g8[:, 24:28]
        yield
        psg = bankm()
        P.mm(psg[:, 0:4], tri, lf)
        P.mm(psg[:, 4:8], onesf, lf)
        P.copy("dve", g8[:, 28:36], psg[:, 0:8])
        bt = g8[:, 28:32]; bL = g8[:, 32:36]
        P.tt("dve", g8[:, 36:40], g8[:, 8:12], bt, ALU.subtract)
        P.act(g8[:, 40:44], bt, AF.Exp)
        P.tt("dve", g8[:, 44:48], g8[:, 36:40], bL, ALU.add)
        P.act(g8[:, 44:48], g8[:, 44:48], AF.Exp)
        P.act(g8[:, 48:52], bL, AF.Exp)
        yield
        P.copy("pool", lfb, lf.bc(2, 128))
        P.ts("dve", qq[:, 0:256], slu[:, 0:256], 0.125, ALU.mult)
        P.copy("pool", qq[:, 256:512], slu[:, 256:512])
        P.stt("dve", qq[:, 512:768].re(H4, h=4), slu[:, 0:256].re(H4, h=4), 0.125, g8[:, 40:44].bc(2, 64), ALU.mult, ALU.mult)
        yield
        for n in range(12):
            pb_ = ptbM[0] if n < 8 else ptbM[1]
            nn = n % 8
            P.tr(pb_[0:64, nn * 128:(nn + 1) * 128], qq[:, n * 64:(n + 1) * 64], ident)
        P.copy("act", qkT[0:64, 0:8, :], ptbM[0][0:64, :].re("p (n t) -> p n t", n=8))
        P.copy("dve", qkT[0:64, 8:12, :], ptbM[1][0:64, 0:512].re("p (n t) -> p n t", n=4))
        P.copy("act", Vb, m[:, 0:512].re("p (h v) -> p h v", h=4))
        P.tt("pool", Kw, slu[:, 256:512].re(H4, h=4), g8[:, 44:48].bc(2, 64), ALU.mult)
        yield
        pE = bankm(); pS_ = bankm(); pN = bankm(); pCm = bankm(); pdn = bankm()
        for h in range(4):
            P.mm(pE[:, h * 128:(h + 1) * 128], lfb[:, h, :], tri, start=True, stop=False)
            P.mm(pE[:, h * 128:(h + 1) * 128], identf, masknegf, start=False, stop=True)
        for h in range(4):
            P.mm(pS_[:, h * 128:(h + 1) * 128], qkT[0:64, 4 + h, :], qkT[0:64, h, :])
        for h in range(4):
            P.act(DTm[:, h, :], pE[:, h * 128:(h + 1) * 128], AF.Exp, bias=g8[:, 36 + h:37 + h], scale=1.0)
        P.tt("dve", PTm, pS_[:, :].re("p (h t) -> p h t", h=4), DTm, ALU.mult)
        for h in range(4):
            P.mm(pN[:, h * 128:(h + 1) * 128], PTm[:, h, :], Vb[:, h, :], start=True, stop=False)
            P.mm(pN[:, h * 128:(h + 1) * 128], qkT[0:64, 8 + h, :], C_bf[0:64, h, :], start=False, stop=True)
            P.mm(pdn[:, h:h + 1], PTm[:, h, :], ones_bf[:, 0:1], start=True, stop=False)
            P.mm(pdn[:, h:h + 1], qkT[0:64, 8 + h, :], n_bf[0:64, h:h + 1], start=False, stop=True)
            P.mm(pCm[0:64, h * 128:(h + 1) * 128], Kw[:, h, :], Vb[:, h, :])
            P.mm(pdn[0:64, 8 + h:9 + h], Kw[:, h, :], ones_bf[:, 0:1])
        P.copy("dve", g8[:, 52:56], pdn[:, 0:4])
        P.stt("dve", g8[:, 56:60], g8[:, 52:56], -1.0, g8[:, 52:56], ALU.mult, ALU.max)
        P.ts("dve", g8[:, 56:60], g8[:, 56:60], 1.0, ALU.max)
        recip("dve", g8[:, 60:64], g8[:, 56:60])
        P.tt("dve", hm, pN[:, :].re("p (h v) -> p h v", h=4), g8[:, 60:64].bc(2, 128), ALU.mult)
        if i <= 1:
            tap("h_mlstm%d" % i, hm, [128, 4, 128])
        P.tt("pool", Cst[0:64], Cst[0:64], g8[0:64, 48:52].bc(2, 128), ALU.mult)
        P.tt("dve", Cst[0:64], Cst[0:64], pCm[0:64, :].re("p (h v) -> p h v", h=4), ALU.add)
        P.tt("pool", nst[0:64], nst[0:64], g8[0:64, 48:52], ALU.mult)
        P.tt("dve", nst[0:64], nst[0:64], pdn[0:64, 8:12], ALU.add)
        P.copy("pool", C_bf[0:64], Cst[0:64])
        P.copy("pool", n_bf[0:64], nst[0:64])
        yield
        hflat = hm.re("p h v -> p (h v)")
        P.tt("pool", ctmp.re("p (h v) -> p h v", h=4), hm, hm, ALU.mult)
        P.red("dve", sm4[:, 0:4], ctmp.re("p (h v) -> p h v", h=4), ALU.add)
        P.ts("dve", sm4[:, 0:4], sm4[:, 0:4], 1.0 / 128, ALU.mult)
        P.act(sm4[:, 4:8], sm4[:, 0:4], AF.Ln, bias=epsM[:, 0:1], scale=1.0)
        P.act(sm4[:, 8:12], sm4[:, 4:8], AF.Exp, scale=-0.5)
        P.ts("dve", sm4[:, 8:12], sm4[:, 8:12], 0.5, ALU.mult)
        P.tt("dve", hm, hm, sm4[:, 8:12].bc(2, 128), ALU.mult)
        P.tt("pool", hflat, hflat, hnw_bc, ALU.mult)
        P.stt("dve", ym[i % 2], og, 1.0, hflat, ALU.add, ALU.mult)
        P.dma("sp", ycat_s[i * 128:(i + 1) * 128, 512:1024], ym[i % 2], "yms%d_%d" % (T["id"], i % 2))
        if i == 0:
            tap("ym", ym[0], [128, 512])
        if i == 1:
            tap("ym1", ym[1], [128, 512])

    run_streams(b2_tile, B2S, offset=2)
    P.end_phase()
    if stop_after <= 3:
        P.finish()
        return nc, tapd

    P.begin_phase()
    Wout = P.tile("Wout", [128, 8, D], BF16)
    Wgr = P.tile("Wgr", [128, 8, 36], BF16)
    brt_bc = P.tile("brt_bc", [128, 36])
    stg3 = [P.tile("stg3_%d" % i, [128, D]) for i in range(2)]
    for k in range(8):
        s = stg3[k % 2]
        P.dma("sp", s, dr["w_out"][k * 128:(k + 1) * 128, :], "stg3_%d" % (k % 2))
        P.copy(ceng[k % 3], Wout[:, k, :], s)
    s = P.tile("stg3r", [128, 288])
    P.dma("sp", s[:, 0:32].re("p (k g) -> p k g", k=8), dr["moe_w_group"].rearrange("(k p) g -> p k g", p=128), "stg3r")
    P.dma("sp", s[:, 32:288].re("p (k g) -> p k g", k=8), dr["moe_w_router"].rearrange("(k p) g -> p k g", p=128), "stg3r")
    P.copy("act", Wgr[:, :, 0:4], s[:, 0:32].re("p (k g) -> p k g", k=8))
    P.copy("act", Wgr[:, :, 4:36], s[:, 32:288].re("p (k g) -> p k g", k=8))
    bcload(brt_bc[:, 0:4], dr["moe_b_group"][0:1, :], 128, "c10")
    bcload(brt_bc[:, 4:36], dr["moe_b_router"][0:1, :], 128, "c10")
    def alloc_b3(q):
        sx = "_o%d" % q
        T = {"id": q}
        T["ycat"] = P.tile("ycat" + sx, [128, D], BF16)
        T["xB"] = P.tile("xB" + sx, [128, D])
        T["ycT"] = P.tile("ycT" + sx, [128, 8, 128], BF16)
        T["x1"] = P.tile("x1" + sx, [128, D])
        T["xn2"] = P.tile("xn2" + sx, [128, D], BF16)
        T["junkB"] = P.tile("junkB" + sx, [128, D], BF16)
        T["h2T"] = P.tile("h2T" + sx, [128, 8, 128], BF16)
        T["h2Tk"] = [T["h2T"][:, k, :].sub("h2T%s_%d" % (sx, k)) for k in range(8)]
        T["lg"] = P.tile("lg" + sx, [128, 36])
        T["r8"] = P.tile("r8" + sx, [128, 96])
        T["s16"] = P.tile("s16" + sx, [128, 16])
        T["cwt"] = P.tile("cwt" + sx, [128, 4, 8])
        T["gm_bc"] = P.tile("gm_bc" + sx, [128, D])
        return T

    B3S = [alloc_b3(q) for q in range(NSTR2)]
    ptb3 = [P.psum("ptb3_%d" % i, [128, 1024], BF16) for i in range(2)]
    pf3 = [P.psum("pf3_%d" % i, [128, 512]) for i in range(5)]
    p3i = [0]

    def bank3():
        b_ = pf3[p3i[0] % 5]
        p3i[0] += 1
        return b_

    def b3_tile(i, T):
        ycat = T["ycat"]
        xB = T["xB"]
        ycT = T["ycT"]
        x1 = T["x1"]
        xn2 = T["xn2"]
        junkB = T["junkB"]
        h2T = T["h2T"]
        h2Tk = T["h2Tk"]
        lg = T["lg"]
        r8 = T["r8"]
        s16 = T["s16"]
        cwt = T["cwt"]
        gm_bc = T["gm_bc"]
        b = i // NT
        it = i % NT
        P.dma("sp", ycat, ycat_s[i * 128:(i + 1) * 128, :], "ycl%d" % T["id"])
        P.dma("sp", xB, dr["x"][i * 128:(i + 1) * 128, :], "xB%d" % T["id"])
        if it == 0:
            P.dma("sp", gm_bc, mod_s[b:b + 1, 2048:3072].partition_broadcast(128), "gmbc%d" % T["id"])
        for k in range(8):
            P.tr(ptb3[0][:, k * 128:(k + 1) * 128], ycat[:, k * 128:(k + 1) * 128], ident)
        P.copy("dve", ycT, ptb3[0][:, :].re("p (k t) -> p k t", k=8))
        yield
        x1t = x1
        for cb in range(2):
            po_ = bank3()
            for k in range(8):
                P.mm(po_, ycT[:, k, :], Wout[:, k, cb * 512:(cb + 1) * 512], start=(k == 0), stop=(k == 7))
            P.tt("dve", x1t[:, cb * 512:(cb + 1) * 512], po_, gm_bc[:, cb * 512:(cb + 1) * 512], ALU.mult)
        P.tt("pool", x1t, x1t, xB, ALU.add)
        P.dma("sp", x1_s[i * 128:(i + 1) * 128, :], x1t, "x1s%d" % T["id"])
        if i == 0:
            tap("x1", x1t, [128, D])
        if i == 1:
            tap("x1b", x1t, [128, D])
        yield
        P.act(junkB, x1t, AF.Square, accum=s16[:, 0:1])
        P.ts("dve", s16[:, 1:2], s16[:, 0:1], 1.0 / D, ALU.mult)
        P.act(s16[:, 2:3], s16[:, 1:2], AF.Ln, bias=epsM[:, 0:1], scale=1.0)
        P.act(s16[:, 3:4], s16[:, 2:3], AF.Exp, scale=-0.5)
        P.act(xn2, x1t, AF.Copy, scale=s16[:, 3:4])
        for k in range(8):
            P.tr(ptb3[1][:, k * 128:(k + 1) * 128], xn2[:, k * 128:(k + 1) * 128], ident)
        h2 = h2T
        h2k = h2Tk
        for k in range(8):
            P.act(h2k[k], ptb3[1][:, k * 128:(k + 1) * 128], AF.Identity, bias=shF[:, b, k:k + 1], scale=gamF[:, b, k:k + 1])
        P.add("sp", (lambda o_, i_: (lambda e: e.dma_start(out=o_, in_=i_)))(h2T_s[:, :, i * 128:(i + 1) * 128], h2.ap), h2k, [], dma_key="h2s%d" % T["id"])
        yield
        pr = bank3()
        for k in range(8):
            P.mm(pr[:, 0:36], h2k[k], Wgr[:, k, :], start=(k == 0), stop=(k == 7))
        P.tt("dve", lg, pr[:, 0:36], brt_bc, ALU.add)
        yield
        P.red("dve", r8[:, 0:1], lg[:, 0:4], ALU.max)
        P.ts("dve", r8[:, 1:5], lg[:, 0:4], r8[:, 0:1], ALU.is_equal)
        P.ts("dve", r8[:, 5:6], r8[:, 0:1], -1.0, ALU.mult)
        P.act(r8[:, 6:10], lg[:, 0:4], AF.Exp, bias=r8[:, 5:6], scale=1.0, accum=r8[:, 10:11])
        recip("dve", r8[:, 11:12], r8[:, 10:11])
        P.tt("dve", r8[:, 16:48].re("p (g e) -> p g e", g=4), lg[:, 4:36].re("p (g e) -> p g e", g=4), r8[:, 1:5].bc(2, 8), ALU.mult)
        P.red("dve", r8[:, 48:56], r8[:, 16:48].re("p (g e) -> p e g", g=4), ALU.add)
        P.red("dve", r8[:, 56:57], r8[:, 48:56], ALU.max)
        P.ts("dve", r8[:, 64:72], r8[:, 48:56], r8[:, 56:57], ALU.is_equal)
        P.stt("dve", r8[:, 72:80], r8[:, 64:72], -1e30, r8[:, 48:56], ALU.mult, ALU.add)
        P.red("dve", r8[:, 57:58], r8[:, 72:80], ALU.max)
        P.ts("dve", r8[:, 80:88], r8[:, 72:80], r8[:, 57:58], ALU.is_equal)
        P.tt("dve", r8[:, 58:59], r8[:, 56:57], r8[:, 57:58], ALU.subtract)
        P.act(r8[:, 60:61], r8[:, 58:59], AF.Exp, scale=-1.0)
        P.ts("dve", r8[:, 59:60], r8[:, 60:61], 1.0, ALU.add)
        recip("dve", r8[:, 59:60], r8[:, 59:60])
        P.tt("dve", r8[:, 60:61], r8[:, 60:61], r8[:, 59:60], ALU.mult)
        P.ts("dve", r8[:, 64:72], r8[:, 64:72], r8[:, 59:60], ALU.mult)
        P.stt("dve", r8[:, 64:72], r8[:, 80:88], r8[:, 60:61], r8[:, 64:72], ALU.mult, ALU.add)
        P.ts("dve", r8[:, 64:72], r8[:, 64:72], r8[:, 11:12], ALU.mult)
        cw_t = cwt
        P.tt("dve", cw_t, r8[:, 1:5].bc(2, 8), r8[:, 64:72].bc(1, 4), ALU.mult)
        P.dma("sp", cw_s[i * 128:(i + 1) * 128, :], cw_t.re("p g e -> p (g e)"), "cws%d" % T["id"])
        if i == 0:
            tap("cw", cw_t, [128, 4, 8])

    run_streams(b3_tile, B3S, offset=0)
    P.end_phase()
    if stop_after <= 4:
        P.finish()
        return nc, tapd

    P.begin_phase()
    BLK = min(1024, SEQ)
    SUB = min(512, BLK)
    NB = NTOK // BLK
    TPB = BLK // 128
    fnw_bc = P.tile("fnw_bc", [128, D])
    bcload(fnw_bc, dr["final_norm_w"][0:1, :], 128, "c11")
    h2bs = [P.tile("h2b%d" % i, [128, 8, BLK], BF16) for i in range(2)]
    cwbs = [P.tile("cwb%d" % i, [128, TPB, NE]) for i in range(2)]
    yaccss = [[P.tile("yacc%d_%d" % (j, i), [128, TPB, 512]) for i in range(2)] for j in range(2)]
    gf_bcs = [P.tile("gf_bc%d" % i, [128, D]) for i in range(2)]
    wgb = [P.tile("wgb%d" % i, [128, 8, DE], BF16) for i in range(2)]
    wub = [P.tile("wub%d" % i, [128, 8, DE], BF16) for i in range(2)]
    wdb = [P.tile("wdb%d" % i, [128, 2, D], BF16) for i in range(2)]
    sg = [P.tile("sg%d" % i, [128, SUB]) for i in range(2)]
    actT = [[P.tile("actT%d_%d" % (s_, f), [128, SUB], BF16) for f in range(2)] for s_ in range(2)]
    x1c = [P.tile("x1c%d" % i, [128, D]) for i in range(2)]
    junkC = P.tile("junkC", [128, D], BF16)
    pG = [P.psum("pG%d" % i, [128, 512]) for i in range(2)]
    pUu = [P.psum("pU%d" % i, [128, 512]) for i in range(2)]
    pD = [P.psum("pD%d" % i, [128, 512]) for i in range(3)]
    pdi = [0]
    wcnt = [0]

    obig = P.tile("obig", [128, TPB, D])
    obt = [obig[:, ti, :].sub("obig_%d" % ti) for ti in range(TPB)]
    ssq = P.tile("ssqC", [128, 4, TPB])

    def epilogue(blk, slot):
        for ti in range(TPB):
            gi = blk * TPB + ti
            sl = gi % 2
            P.dma("sp", x1c[sl], x1_s[gi * 128:(gi + 1) * 128, :], "x1c%d" % sl)
            o = obt[ti]
            for cb in range(2):
                P.tt("dve", o[:, cb * 512:(cb + 1) * 512], yaccss[slot][cb][:, ti, :], gf_bcs[slot][:, cb * 512:(cb + 1) * 512], ALU.mult)
            P.tt("dve", o, o, x1c[sl], ALU.add)
            P.act(junkC, o, AF.Square, accum=ssq[:, 0, ti:ti + 1])
        P.ts("dve", ssq[:, 1, :], ssq[:, 0, :], 1.0 / D, ALU.mult)
        P.act(ssq[:, 2, :], ssq[:, 1, :], AF.Sqrt, bias=epsM[:, 0:1], scale=1.0)
        recip("dve", ssq[:, 3, :], ssq[:, 2, :])
        for ti in range(TPB):
            gi = blk * TPB + ti
            o = obt[ti]
            P.stt("dve", o, o, ssq[:, 3, ti:ti + 1], fnw_bc, ALU.mult, ALU.mult)
            P.dma("sp", out_d[gi * 128:(gi + 1) * 128, :], o, "outC%d" % (ti % 4))

    for blk in range(NB):
        t0 = blk * BLK
        b = t0 // SEQ
        slot = blk % 2
        h2b = h2bs[slot]
        cwb = cwbs[slot]
        yaccs = yaccss[slot]
        P.dma("sp", h2b, h2T_s[:, :, t0:t0 + BLK], "h2b%d" % slot)
        P.dma("sp", cwb, cw_s[t0:t0 + BLK, :].rearrange("(n p) e -> p n e", p=128), "cwb%d" % slot)
        P.dma("sp", gf_bcs[slot], mod_s[b:b + 1, 5120:6144].partition_broadcast(128), "gfbc%d" % slot)
        NSB = BLK // SUB
        units = [(e, sb, (e * NSB + sb) % 2) for e in range(NE) for sb in range(NSB)]

        def c_loads(e):
            ws = wcnt[0] % 2
            wcnt[0] += 1
            P.dma("sp", wgb[ws], wg_s[e].rearrange("p (k f) -> p k f", k=8), "wgb%d" % ws)
            P.dma("sp", wub[ws], wu_s[e].rearrange("p (k f) -> p k f", k=8), "wub%d" % ws)
            P.dma("sp", wdb[ws], wd_s[e].rearrange("p (c d) -> p c d", c=2), "wdb%d" % ws)
            return ws

        wslot = {}

        def c_gu(u, fc):
            e, sb, asl = u
            ws = wslot[e]
            for k in range(8):
                P.mm(pG[fc][:, 0:SUB], wgb[ws][:, k, fc * 128:(fc + 1) * 128], h2b[:, k, sb * SUB:(sb + 1) * SUB], start=(k == 0), stop=(k == 7))
            for k in range(8):
                P.mm(pUu[fc][:, 0:SUB], wub[ws][:, k, fc * 128:(fc + 1) * 128], h2b[:, k, sb * SUB:(sb + 1) * SUB], start=(k == 0), stop=(k == 7))
            P.act(sg[fc], pG[fc][:, 0:SUB], AF.Silu)
            P.tt("dve", actT[asl][fc], pUu[fc][:, 0:SUB], sg[fc], ALU.mult)

        def c_down(u):
            e, sb, asl = u
            ws = wslot[e]
            for tt_ in range(SUB // 128):
                ti = sb * (SUB // 128) + tt_
                for cb in range(2):
                    pd_ = pD[pdi[0] % 3]; pdi[0] += 1
                    for fc in range(2):
                        P.mm(pd_, actT[asl][fc][:, tt_ * 128:(tt_ + 1) * 128], wdb[ws][:, fc, cb * 512:(cb + 1) * 512], start=(fc == 0), stop=(fc == 1))
                    ysl = yaccs[cb][:, ti, :]
                    if e == 0:
                        P.ts("dve", ysl, pd_, cwb[:, ti, e:e + 1], ALU.mult)
                    else:
                        P.stt("dve", ysl, pd_, cwb[:, ti, e:e + 1], ysl, ALU.mult, ALU.add)

        wslot[0] = c_loads(0)
        c_gu(units[0], 0)
        c_gu(units[0], 1)
        for n in range(len(units)):
            if n + 1 < len(units):
                if units[n + 1][1] == 0:
                    wslot[units[n + 1][0]] = c_loads(units[n + 1][0])
                c_gu(units[n + 1], 0)
            c_down(units[n])
            if n + 1 < len(units):
                c_gu(units[n + 1], 1)
        epilogue(blk, slot)
    P.end_phase()
    P.finish()
    return nc, tapd


_NC_CACHE = {}


def kernel(**inputs):
    NCORES = 8
    x = np.asarray(inputs["x"], dtype=np.float32)
    B, S, _ = x.shape
    NSEQ = B // NCORES
    key = (NSEQ, S)
    if key not in _NC_CACHE:
        _NC_CACHE[key] = build(NSEQ, S)[0]
    nc = _NC_CACHE[key]
    shared = {}
    for k, shp in PARAM_SHAPES.items():
        shared[k] = np.ascontiguousarray(np.asarray(inputs[k], dtype=np.float32).reshape(shp))
    c = np.asarray(inputs["c"], dtype=np.float32)
    in_maps = []
    for i in range(NCORES):
        m = dict(shared)
        m["x"] = np.ascontiguousarray(x[i * NSEQ:(i + 1) * NSEQ].reshape(NSEQ * S, D))
        m["c"] = np.ascontiguousarray(c[i * NSEQ:(i + 1) * NSEQ])
        in_maps.append(m)
    res = run_bass_kernel_spmd(nc, in_maps, core_ids=list(range(NCORES)))
    outs = [np.asarray(r["out"]).reshape(NSEQ, S, D) for r in res.results]
    return np.concatenate(outs, axis=0).astype(np.float32)
```

```python
import numpy as np
from concourse.bass_utils import run_bass_kernel_spmd
import numpy as np
from contextlib import ExitStack
import concourse.bass as bass
import concourse.mybir as mybir

F32 = mybir.dt.float32
BF16 = mybir.dt.bfloat16
AF = mybir.ActivationFunctionType
ALU = mybir.AluOpType
AX = mybir.AxisListType

ENGS = ("pe", "act", "dve", "pool", "sp")
SEM_CAP = 30000


class Buf:
    __slots__ = ("name", "writers", "readers", "psum")

    def __init__(self, name, psum=False):
        self.name = name
        self.writers = {}
        self.readers = []
        self.psum = psum


class V:
    __slots__ = ("ap", "buf")

    def __init__(self, ap, buf):
        self.ap = ap
        self.buf = buf

    def __getitem__(self, k):
        return V(self.ap[k], self.buf)

    def re(self, pat, **kw):
        return V(self.ap.rearrange(pat, **kw), self.buf)

    def bc(self, axis, n):
        a = self.ap.unsqueeze(axis)
        shp = list(a.shape)
        shp[axis] = n
        return V(a.to_broadcast(shp), self.buf)

    def sub(self, name):
        return V(self.ap, Buf(name))


class Op:
    __slots__ = ("eng", "fn", "idx", "eidx", "dma_key", "deps", "signals", "sigval", "semi", "extra", "phase")

    def __init__(self, eng, fn, dma_key):
        self.eng = eng
        self.fn = fn
        self.dma_key = dma_key
        self.deps = []
        self.signals = False
        self.sigval = 0
        self.semi = 0
        self.extra = []


def _bufs(vs):
    out = []
    for v in vs:
        if isinstance(v, V):
            out.append(v.buf)
        elif isinstance(v, Buf):
            out.append(v)
    return out


class Prog:
    def __init__(self, nc):
        self.nc = nc
        self.ops = []
        self.eops = {e: [] for e in ENGS}
        self.gstack = ExitStack()
        self.pstack = None
        self.cnt = {e: 0 for e in ENGS}
        self.dcnt = {}
        self.esems = {e: [] for e in ENGS}
        self.dsems = {}
        self.free_dsems = []
        self.waited = {e: {} for e in ENGS}
        self.carry = []
        self.nops_total = 0
        self.phase = 0

    def _stack(self):
        return self.pstack if self.pstack is not None else self.gstack

    def tile(self, name, shape, dt=F32):
        t = self._stack().enter_context(self.nc.sbuf_tensor(name, list(shape), dt))
        return V(t[:], Buf(name))

    def psum(self, name, shape, dt=F32):
        t = self._stack().enter_context(self.nc.psum_tensor(name, list(shape), dt))
        return V(t[:], Buf(name, psum=True))

    def begin_phase(self):
        self.pstack = ExitStack()
        self.ops = []
        self.eops = {e: [] for e in ENGS}

    def add(self, eng, fn, reads=(), writes=(), dma_key=None):
        op = Op(eng, fn, dma_key)
        op.idx = len(self.ops)
        op.phase = self.phase
        op.eidx = len(self.eops[eng])
        deps = {}
        wkey = ("dma", dma_key) if dma_key is not None else eng
        rb = _bufs(reads)
        wb = _bufs(writes)
        for b in rb:
            for w in b.writers.values():
                deps[id(w)] = w
            if b.psum:
                for r in b.readers:
                    if r.eng != eng:
                        deps[id(r)] = r
        for b in wb:
            for w in b.writers.values():
                deps[id(w)] = w
            for r in b.readers:
                deps[id(r)] = r
        deps.pop(id(op), None)
        op.deps = list(deps.values())
        for b in rb:
            b.readers.append(op)
        for b in wb:
            b.writers[wkey] = op
            b.readers = []
        self.ops.append(op)
        self.eops[eng].append(op)
        return op

    def mm(self, out, lhsT, rhs, start=True, stop=True, extra_r=()):
        return self.add("pe", lambda e: e.matmul(out.ap, lhsT=lhsT.ap, rhs=rhs.ap, start=start, stop=stop),
                        [lhsT, rhs] + list(extra_r), [out])

    def tr(self, out, in_, ident):
        return self.add("pe", lambda e: e.transpose(out.ap, in_.ap, ident.ap), [in_, ident], [out])

    def act(self, out, in_, func, bias=None, scale=None, accum=None, eng="act"):
        kw = {}
        r = [in_]
        if bias is not None:
            kw["bias"] = bias.ap if isinstance(bias, V) else bias
            if isinstance(bias, V):
                r.append(bias)
        if scale is not None:
            kw["scale"] = scale.ap if isinstance(scale, V) else scale
            if isinstance(scale, V):
                r.append(scale)
        w = [out]
        if accum is not None:
            kw["accum_out"] = accum.ap
            w.append(accum)
        return self.add(eng, lambda e: e.activation(out=out.ap, in_=in_.ap, func=func, **kw), r, w)

    def tt(self, eng, out, in0, in1, op):
        return self.add(eng, lambda e: e.tensor_tensor(out=out.ap, in0=in0.ap, in1=in1.ap, op=op), [in0, in1], [out])

    def ts(self, eng, out, in0, s1, op0, s2=None, op1=None, accum=None):
        r = [in0]
        a1 = s1.ap if isinstance(s1, V) else s1
        a2 = s2.ap if isinstance(s2, V) else s2
        if isinstance(s1, V):
            r.append(s1)
        if isinstance(s2, V):
            r.append(s2)
        w = [out]
        kw = {}
        if op1 is not None:
            kw["op1"] = op1
        if accum is not None:
            kw["accum_out"] = accum.ap
            w.append(accum)
        return self.add(eng, lambda e: e.tensor_scalar(out=out.ap, in0=in0.ap, scalar1=a1, scalar2=a2, op0=op0, **kw), r, w)

    def stt(self, eng, out, in0, scalar, in1, op0, op1):
        eng = "dve"
        r = [in0, in1]
        a = scalar.ap if isinstance(scalar, V) else scalar
        if isinstance(scalar, V):
            r.append(scalar)
        return self.add(eng, lambda e: e.scalar_tensor_tensor(out=out.ap, in0=in0.ap, scalar=a, in1=in1.ap, op0=op0, op1=op1), r, [out])

    def copy(self, eng, out, in_):
        if eng == "act":
            return self.add(eng, lambda e: e.copy(out=out.ap, in_=in_.ap), [in_], [out])
        return self.add(eng, lambda e: e.tensor_copy(out=out.ap, in_=in_.ap), [in_], [out])

    def red(self, eng, out, in_, op, axis=AX.X):
        return self.add(eng, lambda e: e.tensor_reduce(out=out.ap, in_=in_.ap, axis=axis, op=op), [in_], [out])

    def memset(self, eng, out, val):
        return self.add(eng, lambda e: e.memset(out.ap, val), [], [out])

    def dma(self, eng, out, in_, key, **kw):
        r = [in_] if isinstance(in_, V) else []
        w = [out] if isinstance(out, V) else []
        oa = out.ap if isinstance(out, V) else out
        ia = in_.ap if isinstance(in_, V) else in_
        return self.add(eng, lambda e: e.dma_start(out=oa, in_=ia, **kw), r, w, dma_key=key)

    def end_phase(self):
        nc = self.nc
        ops = self.ops
        need = {}
        for op in ops:
            wl = []
            for p in op.deps:
                if p.phase != self.phase:
                    continue
                if p.dma_key is not None:
                    wl.append(p)
                elif p.eng != op.eng:
                    p.signals = True
                    wl.append(p)
                else:
                    if op.eng == "pe" and op.dma_key is None:
                        continue
                    if op.dma_key is not None or (op.eidx - p.eidx) <= 4:
                        p.signals = True
                        wl.append(p)
            need[id(op)] = wl
        lastc = {}
        for e in ENGS:
            for op in reversed(self.eops[e]):
                if op.dma_key is None:
                    op.signals = True
                    lastc[e] = op
                    break
        for op in ops:
            if op.dma_key is not None:
                if op.dma_key not in self.dsems:
                    if self.free_dsems:
                        sem_, c0_ = self.free_dsems.pop()
                        self.dsems[op.dma_key] = sem_
                        self.dcnt[op.dma_key] = c0_
                    else:
                        self.dsems[op.dma_key] = self.gstack.enter_context(nc.semaphore("d_%s" % (op.dma_key,)))
                        self.dcnt[op.dma_key] = 0
                self.dcnt[op.dma_key] += 16
                op.sigval = self.dcnt[op.dma_key]
            elif op.signals:
                c = self.cnt[op.eng]
                op.semi = c // SEM_CAP
                op.sigval = c % SEM_CAP + 1
                self.cnt[op.eng] = c + 1
                while len(self.esems[op.eng]) <= op.semi:
                    i = len(self.esems[op.eng])
                    self.esems[op.eng].append(self.gstack.enter_context(nc.semaphore("s_%s_%d" % (op.eng, i))))
        plans = {e: [] for e in ENGS}
        first = {e: True for e in ENGS}
        for op in ops:
            ws = {}
            if first[op.eng]:
                first[op.eng] = False
                for key, sem, v in self.carry:
                    if self.waited[op.eng].get(key, 0) < v:
                        ws[key] = (sem, v)
            for p in need[id(op)]:
                if p.dma_key is not None:
                    sem = self.dsems[p.dma_key]
                    key = ("dsem", id(sem))
                else:
                    key = (p.eng, p.semi)
                    sem = self.esems[p.eng][p.semi]
                v = p.sigval
                if self.waited[op.eng].get(key, 0) >= v:
                    continue
                if key not in ws or ws[key][1] < v:
                    ws[key] = (sem, v)
            for key, (sem, v) in ws.items():
                self.waited[op.eng][key] = v
            if op.dma_key is not None:
                inc = (self.dsems[op.dma_key], 16)
            elif op.signals:
                inc = (self.esems[op.eng][op.semi], 1)
            else:
                inc = None
            plans[op.eng].append((op, list(ws.values()), inc))
        carry = [(("dsem", id(self.dsems[k])), self.dsems[k], v) for k, v in self.dcnt.items()]
        for e, op in lastc.items():
            carry.append(((e, op.semi), self.esems[e][op.semi], op.sigval))
        self.carry = carry + [c for c in self.carry if c[0] not in {x[0] for x in carry}]
        final_waits = [(sem, v) for (_, sem, v) in self.carry]

        def run(engobj, plan, final=None):
            for op, ws, inc in plan:
                for sem, v in ws:
                    engobj.wait_ge(sem, v)
                ins = op.fn(engobj)
                if inc is not None:
                    ins.then_inc(inc[0], inc[1])
            if final:
                for sem, v in final:
                    engobj.wait_ge(sem, v)

        with nc.Block() as block:
            @block.tensor
            def _(e):
                run(e, plans["pe"])

            @block.scalar
            def _(e):
                run(e, plans["act"])

            @block.vector
            def _(e):
                run(e, plans["dve"])

            @block.gpsimd
            def _(e):
                run(e, plans["pool"])

            @block.sync
            def _(e):
                run(e, plans["sp"], final_waits)
        self.nops_total += len(ops)
        self.phase += 1
        for k_ in list(self.dsems):
            self.free_dsems.append((self.dsems.pop(k_), self.dcnt.pop(k_)))
        self.ops = []
        self.eops = {e: [] for e in ENGS}
        if self.pstack is not None:
            self.pstack.close()
            self.pstack = None

    def finish(self):
        self.gstack.close()

D = 1024
INC = 3336
RW = 1792
NE = 32
DE = 256
EPS = 1e-6
GN_EPS = 64e-5
DEC = 0.6065306597126334

PARAM_SHAPES = {
    "ada_w": [1024, 6144], "ada_b": [1, 6144], "mix_norm_w": [1, 1024], "w_in": [1024, 3336],
    "rwkv_mu": [1, 1792], "rwkv_w0": [1, 512], "rwkv_w_up": [64, 512], "rwkv_a0": [1, 512],
    "rwkv_a_up": [64, 512], "rwkv_g_up": [128, 512], "rwkv_k_k": [1, 512], "rwkv_k_a": [1, 512],
    "rwkv_r_k": [1, 512], "rwkv_gn_w": [1, 512], "rwkv_gn_b": [1, 512], "mlstm_conv_w": [4, 512],
    "mlstm_conv_b": [1, 512], "mlstm_i_b": [1, 4], "mlstm_f_b": [1, 4], "mlstm_hn_w": [1, 512],
    "w_out": [1024, 1024], "ffn_norm_w": [1, 1024], "moe_w_group": [1024, 4], "moe_b_group": [1, 4],
    "moe_w_router": [1024, 32], "moe_b_router": [1, 32], "moe_w_gate": [32, 1024, 256],
    "moe_w_up": [32, 1024, 256], "moe_w_down": [32, 256, 1024], "final_norm_w": [1, 1024],
}


def build(NSEQ, SEQ, taps=None, stop_after=99):
    nc = bass.Bass("TRN2", target_bir_lowering=False)
    NT = SEQ // 128
    NTOK = NSEQ * SEQ
    NTILES = NSEQ * NT
    PADR = SEQ + 3
    dr = {}
    dr["x"] = nc.dram_tensor("x", [NTOK, D], F32, kind="ExternalInput").ap()
    dr["c"] = nc.dram_tensor("c", [NSEQ, D], F32, kind="ExternalInput").ap()
    for k, shp in PARAM_SHAPES.items():
        dr[k] = nc.dram_tensor(k, shp, F32, kind="ExternalInput").ap()
    out_d = nc.dram_tensor("out", [NTOK, D], F32, kind="ExternalOutput").ap()
    proj_s = nc.dram_tensor("proj_s", [NSEQ * PADR, INC], F32).ap()
    x1_s = nc.dram_tensor("x1_s", [NTOK, D], F32).ap()
    h2T_s = nc.dram_tensor("h2T_s", [128, 8, NTOK], BF16).ap()
    cw_s = nc.dram_tensor("cw_s", [NTOK, NE], F32).ap()
    mod_s = nc.dram_tensor("mod_s", [NSEQ, 6144], F32).ap()
    wg_s = nc.dram_tensor("wg_s", [NE, 128, 2048], BF16).ap()
    wu_s = nc.dram_tensor("wu_s", [NE, 128, 2048], BF16).ap()
    wd_s = nc.dram_tensor("wd_s", [NE, 128, 2048], BF16).ap()
    tapd = {}

    P = Prog(nc)

    def tap(name, v, shape):
        if taps is None or name not in taps:
            return
        t = nc.dram_tensor("tap_" + name, list(shape), v.ap.dtype, kind="ExternalOutput").ap()
        tapd[name] = t
        P.dma("sp", t, v, "tap_" + name)

    identf = P.tile("identf", [128, 128])
    ident = P.tile("ident", [128, 128], BF16)
    tri = P.tile("tri", [128, 128])
    onesf = P.tile("onesf", [128, 128])
    trimid = P.tile("trimid", [128, 128])
    indmid = P.tile("indmid", [128, 2])
    masknegf = P.tile("masknegf", [128, 128])
    maskA = P.tile("maskA", [128, 4, 128], BF16)
    mSI = P.tile("mSI", [128, 2, 2, 128], BF16)
    epsM = P.tile("epsM", [128, 1])
    epsG = P.tile("epsG", [128, 1])
    gamM = P.tile("gamM", [128, NSEQ, 8])
    shM = P.tile("shM", [128, NSEQ, 8])
    gamF = P.tile("gamF", [128, NSEQ, 8])
    shF = P.tile("shF", [128, NSEQ, 8])

    P.begin_phase()
    P.memset("pool", identf, 1.0)
    P.add("pool", lambda e: e.affine_select(identf.ap, identf.ap, [[-1, 128]], ALU.is_equal, 0.0, base=0, channel_multiplier=1), [identf], [identf])
    P.copy("pool", ident, identf)
    P.memset("pool", tri, 1.0)
    P.add("pool", lambda e: e.affine_select(tri.ap, tri.ap, [[1, 128]], ALU.is_ge, 0.0, base=0, channel_multiplier=-1), [tri], [tri])
    P.memset("pool", onesf, 1.0)
    colm = P.tile("colm", [128, 128])
    P.memset("pool", colm, 1.0)
    P.add("pool", lambda e: e.affine_select(colm.ap, colm.ap, [[0, 128]], ALU.is_ge, 0.0, base=63, channel_multiplier=-1), [colm], [colm])
    P.tt("pool", trimid, tri, colm, ALU.subtract)
    P.ts("pool", trimid, trimid, -DEC, ALU.mult)
    P.ts("pool", indmid[:, 0:1], colm[:, 0:1], -DEC, ALU.mult)
    P.ts("pool", indmid[:, 1:2], colm[:, 0:1], DEC, ALU.mult, -DEC, ALU.add)
    P.memset("pool", masknegf, 0.0)
    P.add("pool", lambda e: e.affine_select(masknegf.ap, masknegf.ap, [[1, 128]], ALU.is_ge, -30000.0, base=0, channel_multiplier=-1), [masknegf], [masknegf])
    mstr = P.tile("mstr", [128, 128])
    P.memset("pool", mstr, 1.0)
    P.add("pool", lambda e: e.affine_select(mstr.ap, mstr.ap, [[1, 128]], ALU.is_ge, 0.0, base=-1, channel_multiplier=-1), [mstr], [mstr])
    mlow = P.tile("mlow", [128, 128])
    P.memset("pool", mlow, 1.0)
    P.add("pool", lambda e: e.affine_select(mlow.ap, mlow.ap, [[-1, 128]], ALU.is_ge, 0.0, base=-1, channel_multiplier=1), [mlow], [mlow])
    for hh in range(4):
        P.copy("pool", maskA[:, hh, :], mlow)
    for hh in range(2):
        P.copy("pool", mSI[:, hh, 0, :], mstr)
        P.copy("pool", mSI[:, hh, 1, :], tri)
    P.memset("pool", epsM, EPS)
    P.memset("pool", epsG, GN_EPS)

    def bcload(dst, src, n, key):
        P.dma("sp", dst, src.partition_broadcast(n), key)

    ct = P.tile("ct", [128, D])
    P.dma("sp", ct[0:NSEQ, :], dr["c"][:, :], "c12")
    sct = P.tile("sct", [128, D])
    P.act(sct[0:NSEQ, :], ct[0:NSEQ, :], AF.Silu)
    psA = P.psum("psA", [128, 512])
    psB = P.psum("psB", [128, 512])
    psC = P.psum("psC", [128, 512])
    scT = P.tile("scT", [128, 8, NSEQ])
    for k in range(8):
        P.mm(psA[:, k * NSEQ:(k + 1) * NSEQ], sct[0:NSEQ, k * 128:(k + 1) * 128], identf[0:NSEQ, 0:NSEQ])
    P.copy("dve", scT, psA[:, 0:8 * NSEQ].re("p (k b) -> p k b", k=8))
    adab = P.tile("adab", [128, 6144])
    P.dma("sp", adab[0:NSEQ, :], dr["ada_b"][0:1, :].partition_broadcast(NSEQ), "c13")
    nrm = P.tile("nrm", [128, 2 * D])
    P.dma("sp", nrm[0:1, 0:D], dr["mix_norm_w"][0:1, :], "c14")
    P.dma("sp", nrm[0:1, D:2 * D], dr["ffn_norm_w"][0:1, :], "c14")
    nrmT = P.tile("nrmT", [128, 16])
    for k in range(16):
        P.mm(psC[:, k:k + 1], nrm[0:1, k * 128:(k + 1) * 128], onesf[0:1, 0:1])
    P.copy("dve", nrmT, psC[:, 0:16])
    adw = [P.tile("adw%d" % i, [128, 8, 512]) for i in range(2)]
    modb = [P.tile("modb%d" % i, [128, 512]) for i in range(2)]
    modT = P.tile("modT", [128, 48, NSEQ])
    for cb in range(12):
        a = adw[cb % 2]
        P.dma("sp", a, dr["ada_w"][:, cb * 512:(cb + 1) * 512].rearrange("(k p) f -> p k f", p=128), "adw%d" % (cb % 2))
        pm = psA if cb % 2 == 0 else psB
        for k in range(8):
            P.mm(pm[0:NSEQ, :], scT[:, k, :], a[:, k, :], start=(k == 0), stop=(k == 7))
        mb = modb[cb % 2]
        P.tt("dve", mb[0:NSEQ, :], pm[0:NSEQ, :], adab[0:NSEQ, cb * 512:(cb + 1) * 512], ALU.add)
        P.dma("sp", mod_s[:, cb * 512:(cb + 1) * 512], mb[0:NSEQ, :], "mods")
        for q in range(4):
            P.mm(psC[:, 64 + q * NSEQ:64 + (q + 1) * NSEQ], mb[0:NSEQ, q * 128:(q + 1) * 128], identf[0:NSEQ, 0:NSEQ])
        P.copy("act", modT[:, cb * 4:(cb + 1) * 4, :], psC[:, 64:64 + 4 * NSEQ].re("p (q b) -> p q b", q=4))
    for b in range(NSEQ):
        P.stt("dve", gamM[:, b, :], modT[:, 8:16, b], 1.0, nrmT[:, 0:8], ALU.add, ALU.mult)
        P.copy("dve", shM[:, b, :], modT[:, 0:8, b])
        P.stt("dve", gamF[:, b, :], modT[:, 32:40, b], 1.0, nrmT[:, 8:16], ALU.add, ALU.mult)
        P.copy("dve", shF[:, b, :], modT[:, 24:32, b])
    zt = P.tile("zt", [128, 512])
    P.memset("pool", zt, 0.0)
    for b in range(NSEQ):
        P.dma("sp", proj_s[b * PADR:b * PADR + 3, :].rearrange("r (a f) -> (r a) f", f=417), zt[0:24, 0:417], "zpad")
    tap("gamM", gamM, [128, NSEQ, 8])
    tap("shM", shM, [128, NSEQ, 8])
    P.end_phase()
    if stop_after <= 0:
        P.finish()
        return nc, tapd

    ycat_s = nc.dram_tensor("ycat_s", [NTOK, D], BF16).ap()

    def recip(eng, o, a):
        P.add(eng, lambda e: e.reciprocal(out=o.ap, in_=a.ap), [a], [o])

    P.begin_phase()
    Win = P.tile("Win", [128, 8, INC], BF16)
    stg = [P.tile("stg%d" % i, [128, INC]) for i in range(2)]
    ceng = ["act", "dve", "pool"]
    for k in range(8):
        s = stg[k % 2]
        P.dma("sp", s, dr["w_in"][k * 128:(k + 1) * 128, :], "stg%d" % (k % 2))
        P.copy(ceng[k % 3], Win[:, k, :], s)
    xt = [P.tile("xt%d" % i, [128, D]) for i in range(3)]
    junk = P.tile("junkA", [128, D], BF16)
    xn = [P.tile("xn%d" % i, [128, D], BF16) for i in range(2)]
    hT = [P.tile("hT%d" % i, [128, 8, 128], BF16) for i in range(2)]
    hTk = [[hT[i][:, k, :].sub("hT%d_%d" % (i, k)) for k in range(8)] for i in range(2)]
    pj = [P.tile("pj%d" % i, [128, INC]) for i in range(3)]
    pjc = [[pj[i][:, cb * 512:min(INC, (cb + 1) * 512)].sub("pj%d_%d" % (i, cb)) for cb in range(7)] for i in range(3)]
    st = [P.tile("stA%d" % i, [128, 4]) for i in range(2)]
    ptb = [P.psum("ptbA%d" % i, [128, 8, 128], BF16) for i in range(2)]
    pp = [P.psum("ppA%d" % i, [128, 512]) for i in range(5)]
    wst = [P.tile("wst%d" % i, [128, 2048]) for i in range(4)]
    wbt = [P.tile("wbt%d" % i, [128, 2048], BF16) for i in range(4)]
    pc_list = []
    for e in range(NE):
        pc_list.append((dr["moe_w_gate"][e].rearrange("(k p) f -> p k f", p=128), wg_s[e], 8))
        pc_list.append((dr["moe_w_up"][e].rearrange("(k p) f -> p k f", p=128), wu_s[e], 8))
        pc_list.append((dr["moe_w_down"][e].rearrange("(c p) d -> p c d", p=128), wd_s[e], 2))

    def pc_load(n):
        src, dst, a_ = pc_list[n]
        P.dma("pool", wst[n % 4].re("p (a b) -> p a b", a=a_), src, "wst%d" % (n % 4))

    def precast_gen():
        for n in range(min(3, len(pc_list))):
            pc_load(n)
        for n in range(len(pc_list)):
            if n + 3 < len(pc_list):
                pc_load(n + 3)
            P.copy("pool" if n % 2 == 0 else "act", wbt[n % 4], wst[n % 4])
            P.dma("pool", pc_list[n][1], wbt[n % 4], "wbt%d" % (n % 4))
            yield

    pcg = precast_gen()

    def a_load(i):
        P.dma("sp", xt[i % 3], dr["x"][i * 128:(i + 1) * 128, :], "xA%d" % (i % 3))

    def a_front(i):
        b = i // NT
        sl = i % 2
        s4 = st[sl]
        P.act(junk, xt[i % 3], AF.Square, accum=s4[:, 0:1])
        P.ts("dve", s4[:, 1:2], s4[:, 0:1], 1.0 / D, ALU.mult)
        P.act(s4[:, 2:3], s4[:, 1:2], AF.Sqrt, bias=epsM[:, 0:1], scale=1.0)
        recip("dve", s4[:, 3:4], s4[:, 2:3])
        P.act(xn[sl], xt[i % 3], AF.Copy, scale=s4[:, 3:4])
        for k in range(8):
            P.tr(ptb[k // 4][:, k % 4, :], xn[sl][:, k * 128:(k + 1) * 128], ident)
        for k in range(8):
            if k < 4:
                P.ts("dve", hTk[sl][k], ptb[0][:, k % 4, :], gamM[:, b, k:k + 1], ALU.mult, shM[:, b, k:k + 1], ALU.add)
            else:
                P.act(hTk[sl][k], ptb[1][:, k % 4, :], AF.Identity, bias=shM[:, b, k:k + 1], scale=gamM[:, b, k:k + 1])

    ppi = [0]

    def a_back(i):
        b = i // NT
        it = i % NT
        sl = i % 2
        for cb in range(7):
            c0 = cb * 512
            cw_ = min(512, INC - c0)
            pq = pp[ppi[0] % 5]; ppi[0] += 1
            for k in range(8):
                P.mm(pq[:, 0:cw_], hTk[sl][k], Win[:, k, c0:c0 + cw_], start=(k == 0), stop=(k == 7))
            P.copy("act" if cb % 2 == 0 else "dve", pjc[i % 3][cb], pq[:, 0:cw_])
        r0 = b * PADR + 3 + it * 128
        P.add("sp", (lambda o_, i_: (lambda e: e.dma_start(out=o_, in_=i_)))(proj_s[r0:r0 + 128, :], pj[i % 3].ap), pjc[i % 3], [], dma_key="pjA%d" % (i % 3))

    a_load(0)
    if NTILES > 1:
        a_load(1)
    a_front(0)
    for i in range(NTILES):
        if i + 2 < NTILES:
            a_load(i + 2)
        if i + 1 < NTILES:
            a_front(i + 1)
        a_back(i)
        for _ in range(2):
            next(pcg, None)
    for _ in pcg:
        pass
    P.end_phase()
    if stop_after <= 1:
        P.finish()
        return nc, tapd

    P.begin_phase()
    Wup = P.tile("Wup", [128, 512], BF16)
    Aup = P.tile("Aup", [128, 512], BF16)
    Gup = P.tile("Gup", [128, 512], BF16)
    mu_bc = P.tile("mu_bc", [128, RW])
    kk_bc = P.tile("kk_bc", [128, 512])
    ka_bc = P.tile("ka_bc", [128, 512])
    rk_bc = P.tile("rk_bc", [128, 512])
    gnw_bc = P.tile("gnw_bc", [128, 512])
    gnb_bc = P.tile("gnb_bc", [128, 512])
    bcload(mu_bc, dr["rwkv_mu"][0:1, :], 128, "c0")
    bcload(kk_bc, dr["rwkv_k_k"][0:1, :], 128, "c1")
    bcload(ka_bc, dr["rwkv_k_a"][0:1, :], 128, "c2")
    bcload(rk_bc, dr["rwkv_r_k"][0:1, :], 128, "c3")
    bcload(gnw_bc, dr["rwkv_gn_w"][0:1, :], 128, "c4")
    bcload(gnb_bc, dr["rwkv_gn_b"][0:1, :], 128, "c5")
    s = P.tile("stgB1", [128, 1536])
    P.dma("sp", s[0:64, 0:512], dr["rwkv_w_up"][:, :], "stgb1")
    P.dma("sp", s[64:65, 0:512], dr["rwkv_w0"][0:1, :], "stgb1")
    P.dma("sp", s[0:64, 512:1024], dr["rwkv_a_up"][:, :], "stgb1")
    P.dma("sp", s[64:65, 512:1024], dr["rwkv_a0"][0:1, :], "stgb1")
    P.dma("sp", s[:, 1024:1536], dr["rwkv_g_up"][:, :], "stgb1")
    P.copy("act", Wup[0:64, :], s[0:64, 0:512])
    P.copy("act", Wup[64:65, :], s[64:65, 0:512])
    P.copy("dve", Aup[0:64, :], s[0:64, 512:1024])
    P.copy("dve", Aup[64:65, :], s[64:65, 512:1024])
    P.copy("pool", Gup, s[:, 1024:1536])
    NSTR = 2 if NSEQ % 2 == 0 else 1

    def alloc_b1(q):
        sx = "_s%d" % q
        T = {"id": q}
        T["rw"] = P.tile("rw" + sx, [128, RW])
        T["rwp"] = P.tile("rwp" + sx, [128, RW])
        T["li"] = P.tile("li" + sx, [128, 256], BF16)
        T["liT"] = P.tile("liT" + sx, [128, 384], BF16)
        for n in ("sgz", "av", "gv", "Ep", "Em", "Epv", "kkk", "tmpR", "tmpR2", "kkv", "knew", "y_sb"):
            T[n] = P.tile(n + sx, [128, 512])
        T["gmL"] = P.tile("gmL" + sx, [128, 4, 2])
        T["sm8"] = P.tile("sm8" + sx, [128, 64])
        T["tok4"] = P.tile("tok4" + sx, [128, 4, 512], BF16)
        T["v_bf"] = P.tile("v_bf" + sx, [128, 512], BF16)
        T["FT"] = P.tile("FT" + sx, [128, 4, 4, 128], BF16)
        T["A_sb"] = [[P.tile("A_sb%d_%d%s" % (g, i, sx), [128, 4, 128], BF16) for i in range(2)] for g in range(2)]
        T["MT"] = [[P.tile("MT%d_%d%s" % (g, i, sx), [128, 4, 2, 128], BF16) for i in range(2)] for g in range(2)]
        T["MRB"] = P.tile("MRB" + sx, [128, 8, 2, 128], BF16)
        T["AKRK"] = P.tile("AKRK" + sx, [128, 8, 2, 128], BF16)
        T["TT"] = P.tile("TT" + sx, [128, 8, 128], BF16)
        T["X_bf"] = P.tile("X_bf" + sx, [128, 512], BF16)
        T["U_bf"] = P.tile("U_bf" + sx, [128, 512], BF16)
        T["Hst"] = P.tile("Hst" + sx, [128, 4, 64])
        T["Ht"] = P.tile("Ht" + sx, [128, 4, 64])
        T["Hbd"] = P.tile("Hbd" + sx, [128, 4, 128], BF16)
        T["yr"] = [P.tile("yr%d%s" % (i, sx), [128, 512], BF16) for i in range(2)]
        P.memset("pool", T["liT"], 1.0)
        P.memset("pool", T["Hbd"], 0.0)
        return T

    B1S = [alloc_b1(q) for q in range(NSTR)]
    ptbB = [P.psum("ptbB%d" % i, [128, 1024], BF16) for i in range(2)]
    pf = [P.psum("pfB%d" % i, [128, 512]) for i in range(5)]
    pfi = [0]

    def bank():
        b_ = pf[pfi[0] % 5]
        pfi[0] += 1
        return b_

    H8 = "p (h c) -> p h c"
    def b1_tile(i, T):
        rw = T["rw"]
        rwp = T["rwp"]
        li = T["li"]
        liT = T["liT"]
        sgz = T["sgz"]
        av = T["av"]
        gv = T["gv"]
        Ep = T["Ep"]
        Em = T["Em"]
        Epv = T["Epv"]
        gmL = T["gmL"]
        kkk = T["kkk"]
        tmpR = T["tmpR"]
        tmpR2 = T["tmpR2"]
        kkv = T["kkv"]
        knew = T["knew"]
        sm8 = T["sm8"]
        tok4 = T["tok4"]
        v_bf = T["v_bf"]
        FT = T["FT"]
        A_sb = T["A_sb"]
        MT = T["MT"]
        MRB = T["MRB"]
        AKRK = T["AKRK"]
        TT = T["TT"]
        X_bf = T["X_bf"]
        U_bf = T["U_bf"]
        Hst = T["Hst"]
        Ht = T["Ht"]
        Hbd = T["Hbd"]
        y_sb = T["y_sb"]
        yr = T["yr"]
        b = i // NT
        it = i % NT
        r0 = b * PADR + 3 + it * 128
        P.dma("sp", rw, proj_s[r0:r0 + 128, 0:RW], "rw%d" % T["id"])
        P.dma("sp", rwp, proj_s[r0 - 1:r0 + 127, 0:RW], "rwp%d" % T["id"])
        if it == 0:
            P.memset("pool", Hst, 0.0)
        u = rwp
        P.tt("dve", u, u, rw, ALU.subtract)
        P.tt("dve", u, u, mu_bc, ALU.mult)
        P.tt("dve", u, u, rw, ALU.add)
        r_ = u[:, 0:512]; k_ = u[:, 512:1024]; v_ = u[:, 1024:1536]
        if i == 0:
            tap("u0", u, [128, RW])
        yield
        P.act(li[:, 0:64], u[:, 1536:1600], AF.Tanh)
        P.copy("pool", li[:, 64:128], u[:, 1600:1664])
        P.act(li[:, 128:256], u[:, 1664:1792], AF.Sigmoid)
        pb0 = ptbB[0]
        P.tr(pb0[0:64, 0:128], li[:, 0:64], ident)
        P.tr(pb0[0:64, 128:256], li[:, 64:128], ident)
        P.tr(pb0[:, 256:384], li[:, 128:256], ident)
        P.copy("dve", liT[0:64, 0:256], pb0[0:64, 0:256])
        P.copy("act", liT[:, 256:384], pb0[:, 256:384])
        pz = bank(); pa = bank(); pg = bank()
        P.mm(pz, liT[0:65, 0:128], Wup[0:65, :])
        P.mm(pa, liT[0:65, 128:256], Aup[0:65, :])
        P.mm(pg, liT[:, 256:384], Gup)
        P.act(sgz, pz, AF.Sigmoid)
        P.act(av, pa, AF.Sigmoid)
        P.copy("dve", gv, pg)
        yield
        pc = bank()
        P.mm(pc, trimid, sgz)
        P.act(Ep, pc, AF.Exp)
        P.act(Em, pc, AF.Exp, scale=-1.0)
        P.stt("dve", Epv, sgz, DEC, pc, ALU.mult, ALU.add)
        P.act(Epv, Epv, AF.Exp)
        psm = bank()
        for j in range(4):
            P.mm(psm[:, 2 * j:2 * j + 2], sgz[:, j * 128:(j + 1) * 128], indmid)
        P.act(gmL, psm[:, 0:8].re("p (j t) -> p j t", j=4), AF.Exp)
        yield
        P.tt("dve", kkk, k_, kk_bc, ALU.mult)
        P.tt("dve", tmpR, kkk, kkk, ALU.mult)
        P.red("dve", sm8[:, 0:8], tmpR.re(H8, h=8), ALU.add)
        P.act(sm8[:, 8:16], sm8[:, 0:8], AF.Sqrt)
        P.ts("dve", sm8[:, 8:16], sm8[:, 8:16], 1e-12, ALU.max)
        recip("dve", sm8[:, 16:24], sm8[:, 8:16])
        P.tt("dve", kkv.re(H8, h=8), kkk.re(H8, h=8), sm8[:, 16:24].bc(2, 64), ALU.mult)
        P.stt("pool", tmpR, av, -1.0, ka_bc, ALU.add, ALU.mult)
        P.stt("pool", knew, tmpR, 1.0, k_, ALU.add, ALU.mult)
        P.tt("pool", tmpR, r_, knew, ALU.mult)
        P.tt("pool", tmpR, tmpR, rk_bc, ALU.mult)
        P.red("dve", sm8[:, 24:32], tmpR.re(H8, h=8), ALU.add)
        yield
        P.stt("dve", tok4[:, 0, :], kkv, -1.0, Epv, ALU.mult, ALU.mult)
        P.tt("dve", tok4[:, 1, :], r_, Ep, ALU.mult)
        P.tt("dve", tok4[:, 2, :], knew, Em, ALU.mult)
        P.tt("dve", tmpR2, kkv, av, ALU.mult)
        P.tt("dve", tok4[:, 3, :], tmpR2, Em, ALU.mult)
        P.copy("act", v_bf, v_)
        if i == 0:
            tap("tok4", tok4, [128, 4, 512])
            tap("sgz", sgz, [128, 512])
        yield
        for half in range(2):
            pb_ = ptbB[half]
            for jj in range(2):
                j = half * 2 + jj
                for w in range(4):
                    n = jj * 4 + w
                    P.tr(pb_[:, n * 128:(n + 1) * 128], tok4[:, w, j * 128:(j + 1) * 128], ident)
            P.copy("act" if half == 0 else "dve",
                   FT[:, half * 2:half * 2 + 2, :, :], pb_[:, :].re("p (j w t) -> p j w t", j=2, w=4))
        yield
        for g in range(2):
            pA = bank()
            pMR = [bank(), bank()]
            pKR = [bank(), bank()]
            for hh in range(4):
                j = hh
                po = 64 * g
                aT = FT[po:po + 64, j, 0, :]
                arT = FT[po:po + 64, j, 0:2, :]
                kT = FT[po:po + 64, j, 2, :]
                bT = FT[po:po + 64, j, 3, :]
                P.mm(pA[:, hh * 128:(hh + 1) * 128], aT, bT)
                P.mm(pMR[hh // 2][:, (hh % 2) * 256:(hh % 2 + 1) * 256], bT, arT)
                P.mm(pKR[hh // 2][:, (hh % 2) * 256:(hh % 2 + 1) * 256], kT, arT)
            P.tt("dve", A_sb[g][0], pA[:, :].re("p (h t) -> p h t", h=4), maskA, ALU.mult)
            for q in range(2):
                P.tt("dve", MRB[:, g * 4 + q * 2:g * 4 + q * 2 + 2, :, :], pMR[q][:, :].re("p (h w t) -> p h w t", h=2, w=2), mSI, ALU.mult)
                P.tt("dve", AKRK[:, g * 4 + q * 2:g * 4 + q * 2 + 2, :, :], pKR[q][:, :].re("p (h w t) -> p h w t", h=2, w=2), mSI, ALU.mult)
            P.tt("pool", MT[g][0][:, :, 1, :], MRB[:, g * 4:g * 4 + 4, 0, :], ident.bc(1, 4), ALU.add)
            if g == 0:
                yield
        yield
        for g in range(2):
            pM = bank(); pA2 = bank()
            for hh in range(4):
                h = g * 4 + hh
                P.mm(pM[:, hh * 128:(hh + 1) * 128], A_sb[g][0][:, hh, :], MRB[:, h, 0, :])
                P.mm(pA2[:, hh * 128:(hh + 1) * 128], MRB[:, h, 0, :], A_sb[g][0][:, hh, :])
            P.copy("act", MT[g][0][:, :, 0, :], pM[:, :].re("p (h t) -> p h t", h=4))
            P.copy("dve", A_sb[g][1], pA2[:, :].re("p (h t) -> p h t", h=4))
            if g == 0:
                yield
        yield
        cur = 0
        for lev in range(1, 6):
            yield
            for g in range(2):
                Ak = A_sb[g][lev % 2]
                An = A_sb[g][(lev + 1) % 2]
                Mc = MT[g][cur]
                Mn = MT[g][1 - cur]
                pS = [bank(), bank()]
                pA2 = bank()
                for hh in range(4):
                    reg = pS[hh // 2][:, (hh % 2) * 256:(hh % 2 + 1) * 256]
                    P.mm(reg, Ak[:, hh, :], Mc[:, hh, :, :], start=True, stop=False)
                    P.mm(reg[:, 128:256], ident, Mc[:, hh, 1, :], start=False, stop=True)
                    P.mm(pA2[:, hh * 128:(hh + 1) * 128], Mc[:, hh, 0, :], Ak[:, hh, :])
                for q in range(2):
                    P.copy("act" if q == 0 else "dve", Mn[:, q * 2:q * 2 + 2, :, :], pS[q][:, :].re("p (h w t) -> p h w t", h=2, w=2))
                P.copy("act", An, pA2[:, :].re("p (h t) -> p h t", h=4))
                if g == 0:
                    yield
            cur = 1 - cur
        for g in range(2):
            Ak = A_sb[g][0]
            Mc = MT[g][cur]
            pT = bank()
            for hh in range(4):
                P.mm(pT[:, hh * 128:(hh + 1) * 128], Ak[:, hh, :], Mc[:, hh, 1, :], start=True, stop=False)
                P.mm(pT[:, hh * 128:(hh + 1) * 128], ident, Mc[:, hh, 1, :], start=False, stop=True)
            P.copy("act" if g == 0 else "dve", TT[:, g * 4:g * 4 + 4, :], pT[:, :].re("p (h t) -> p h t", h=4))
            if g == 0:
                yield
        if i == 0:
            tap("TT", TT, [128, 8, 128])
            tap("MRB", MRB, [128, 8, 2, 128])
        yield
        P.tt("pool", Ht, Hst, gmL[:, :, 0].bc(2, 64), ALU.mult)
        P.copy("pool", Hbd[0:64, :, 0:64], Ht[0:64, :, :])
        P.copy("pool", Hbd[64:128, :, 64:128], Ht[64:128, :, :])
        pX = bank()
        SI = [(h % 2) * 4 + h // 2 for h in range(8)]
        for j in range(4):
            P.mm(pX[:, j * 128:(j + 1) * 128], FT[:, j, 0, :], Hbd[:, j, :], start=True, stop=False)
            for h in (2 * j, 2 * j + 1):
                P.mm(pX[:, h * 64:(h + 1) * 64], AKRK[:, SI[h], 0, :], v_bf[:, h * 64:(h + 1) * 64], start=False, stop=(h == 2 * j + 1))
        P.copy("act", X_bf, pX)
        yield
        pU = bank()
        for h in range(8):
            P.mm(pU[:, h * 64:(h + 1) * 64], TT[:, SI[h], :], X_bf[:, h * 64:(h + 1) * 64])
        P.copy("dve", U_bf, pU)
        yield
        pY = bank()
        for j in range(4):
            P.mm(pY[:, j * 128:(j + 1) * 128], FT[:, j, 1, :], Hbd[:, j, :], start=True, stop=False)
            for h in (2 * j, 2 * j + 1):
                P.mm(pY[:, h * 64:(h + 1) * 64], AKRK[:, SI[h], 1, :], v_bf[:, h * 64:(h + 1) * 64], start=False, stop=False)
                P.mm(pY[:, h * 64:(h + 1) * 64], MRB[:, SI[h], 1, :], U_bf[:, h * 64:(h + 1) * 64], start=False, stop=(h == 2 * j + 1))
        P.copy("act", y_sb, pY)
        yield
        pH = bank()
        for j in range(4):
            P.mm(pH[:, j * 128:(j + 1) * 128], tok4[:, 2, j * 128:(j + 1) * 128], v_bf[:, j * 128:(j + 1) * 128], start=True, stop=False)
            P.mm(pH[:, j * 128:(j + 1) * 128], tok4[:, 3, j * 128:(j + 1) * 128], U_bf[:, j * 128:(j + 1) * 128], start=False, stop=True)
        pHv = pH[:, :].re("p (j v) -> p j v", j=4)
        P.tt("dve", Hst[0:64, :, :], pHv[0:64, :, 0:64], Ht[0:64, :, :], ALU.add)
        P.tt("dve", Hst[64:128, :, :], pHv[64:128, :, 64:128], Ht[64:128, :, :], ALU.add)
        P.tt("pool", Hst, Hst, gmL[:, :, 1].bc(2, 64), ALU.mult)
        yield
        if i <= 1:
            tap("y_rwkv%d" % i, y_sb, [128, 512])
        y3 = y_sb.re(H8, h=8)
        P.red("dve", sm8[:, 32:40], y3, ALU.add)
        P.tt("pool", tmpR, y_sb, y_sb, ALU.mult)
        P.red("dve", sm8[:, 40:48], tmpR.re(H8, h=8), ALU.add)
        P.ts("dve", sm8[:, 32:40], sm8[:, 32:40], 1.0 / 64, ALU.mult)
        P.tt("dve", sm8[:, 48:56], sm8[:, 32:40], sm8[:, 32:40], ALU.mult)
        P.stt("dve", sm8[:, 40:48], sm8[:, 40:48], 1.0 / 64, sm8[:, 48:56], ALU.mult, ALU.subtract)
        P.act(sm8[:, 48:56], sm8[:, 40:48], AF.Sqrt, bias=epsG[:, 0:1], scale=1.0)
        recip("dve", sm8[:, 56:64], sm8[:, 48:56])
        P.tt("dve", tmpR.re(H8, h=8), y3, sm8[:, 32:40].bc(2, 64), ALU.subtract)
        P.tt("dve", tmpR.re(H8, h=8), tmpR.re(H8, h=8), sm8[:, 56:64].bc(2, 64), ALU.mult)
        P.tt("pool", tmpR, tmpR, gnw_bc, ALU.mult)
        P.tt("dve", tmpR, tmpR, gnb_bc, ALU.add)
        P.tt("pool", tmpR2.re(H8, h=8), v_.re(H8, h=8), sm8[:, 24:32].bc(2, 64), ALU.mult)
        P.tt("dve", tmpR, tmpR, tmpR2, ALU.add)
        P.tt("pool", yr[i % 2], tmpR, gv, ALU.mult)
        P.dma("sp", ycat_s[i * 128:(i + 1) * 128, 0:512], yr[i % 2], "yrs%d_%d" % (T["id"], i % 2))
        if i == 0:
            tap("yr", yr[0], [128, 512])
        if i == 1:
            tap("yr1", yr[1], [128, 512])

    def run_streams(tile_fn, streams, offset=0):
        ns = min(len(streams), NSEQ)

        def chain(q):
            for b_ in range(q, NSEQ, ns):
                for it_ in range(NT):
                    yield from tile_fn(b_ * NT + it_, streams[q])
                    yield

        gens = [chain(q) for q in range(ns)]
        for q, g_ in enumerate(gens):
            for _ in range(q * offset):
                next(g_, None)
        live = list(gens)
        while live:
            nxt = []
            for g_ in live:
                try:
                    next(g_)
                    nxt.append(g_)
                except StopIteration:
                    pass
            live = nxt

    run_streams(b1_tile, B1S, offset=0)
    P.end_phase()
    if stop_after <= 2:
        P.finish()
        return nc, tapd

    P.begin_phase()
    hnw_bc = P.tile("hnw_bc", [128, 512])
    cvw_bc = P.tile("cvw_bc", [128, 4, 512])
    cvb_bc = P.tile("cvb_bc", [128, 512])
    gb_bc = P.tile("gb_bc", [128, 8])
    bcload(hnw_bc, dr["mlstm_hn_w"][0:1, :], 128, "c6")
    for j in range(4):
        bcload(cvw_bc[:, j, :], dr["mlstm_conv_w"][j:j + 1, :], 128, "c7")
    bcload(cvb_bc, dr["mlstm_conv_b"][0:1, :], 128, "c8")
    bcload(gb_bc[:, 0:4], dr["mlstm_i_b"][0:1, :], 128, "c9")
    bcload(gb_bc[:, 4:8], dr["mlstm_f_b"][0:1, :], 128, "c9")
    ones_bf = P.tile("ones_bf", [128, 1], BF16)

    def alloc_b2(q):
        sx = "_m%d" % q
        T = {"id": q}
        T["qk4"] = P.tile("qk4" + sx, [128, 4, 512])
        T["mr"] = P.tile("mr" + sx, [128, 1032])
        for n in ("cacc", "ctmp", "slu", "og"):
            T[n] = P.tile(n + sx, [128, 512])
        T["qq"] = P.tile("qq" + sx, [128, 768], BF16)
        T["qkT"] = P.tile("qkT" + sx, [128, 12, 128], BF16)
        T["g8"] = P.tile("g8" + sx, [128, 64])
        T["sm4"] = P.tile("sm4" + sx, [128, 16])
        T["lfb"] = P.tile("lfb" + sx, [128, 4, 128])
        T["DTm"] = P.tile("DTm" + sx, [128, 4, 128])
        T["PTm"] = P.tile("PTm" + sx, [128, 4, 128], BF16)
        T["Vb"] = P.tile("Vb" + sx, [128, 4, 128], BF16)
        T["Kw"] = P.tile("Kw" + sx, [128, 4, 64], BF16)
        T["Cst"] = P.tile("Cst" + sx, [128, 4, 128])
        T["nst"] = P.tile("nst" + sx, [128, 4])
        T["C_bf"] = P.tile("C_bf" + sx, [128, 4, 128], BF16)
        T["n_bf"] = P.tile("n_bf" + sx, [128, 4], BF16)
        T["hm"] = P.tile("hm" + sx, [128, 4, 128])
        T["ym"] = [P.tile("ym%d%s" % (i, sx), [128, 512], BF16) for i in range(2)]
        return T

    NSTR2 = 4 if NSEQ % 4 == 0 else NSTR
    B2S = [alloc_b2(q) for q in range(NSTR2)]
    ptbM = [P.psum("ptbM%d" % i, [128, 1024], BF16) for i in range(2)]
    pfm = [P.psum("pfM%d" % i, [128, 512]) for i in range(5)]
    pmi = [0]

    def bankm():
        b_ = pfm[pmi[0] % 5]
        pmi[0] += 1
        return b_

    P.memset("pool", ones_bf, 1.0)
    H4 = "p (h c) -> p h c"
    def b2_tile(i, T):
        qk4 = T["qk4"]
        mr = T["mr"]
        cacc = T["cacc"]
        ctmp = T["ctmp"]
        slu = T["slu"]
        qq = T["qq"]
        qkT = T["qkT"]
        g8 = T["g8"]
        sm4 = T["sm4"]
        lfb = T["lfb"]
        DTm = T["DTm"]
        PTm = T["PTm"]
        Vb = T["Vb"]
        Kw = T["Kw"]
        Cst = T["Cst"]
        nst = T["nst"]
        C_bf = T["C_bf"]
        n_bf = T["n_bf"]
        hm = T["hm"]
        og = T["og"]
        ym = T["ym"]
        b = i // NT
        it = i % NT
        r0 = b * PADR + 3 + it * 128
        for j in range(4):
            P.dma("sp", qk4[:, j, :], proj_s[r0 - 3 + j:r0 + 125 + j, RW:RW + 512], "qk4%d" % T["id"])
        P.dma("sp", mr, proj_s[r0:r0 + 128, RW + 512:INC], "mr%d" % T["id"])
        if it == 0:
            P.memset("pool", Cst, 0.0)
            P.memset("pool", nst, 0.0)
            P.memset("pool", C_bf, 0.0)
            P.memset("pool", n_bf, 0.0)
        q4 = qk4
        cparts = [cacc, ctmp, og, hm.re("p h v -> p (h v)")]
        for j in range(4):
            P.tt("dve" if j % 2 == 0 else "pool", cparts[j], q4[:, j, :], cvw_bc[:, j, :], ALU.mult)
        for j in range(1, 4):
            P.tt("dve", cacc, cacc, cparts[j], ALU.add)
        P.tt("dve", cacc, cacc, cvb_bc, ALU.add)
        P.act(slu, cacc, AF.Silu)
        m = mr
        P.act(og, m[:, 512:1024], AF.Tanh, scale=0.5)
        yield
        P.tt("dve", g8[:, 0:8], m[:, 1024:1032], gb_bc, ALU.add)
        P.act(g8[:, 8:16], g8[:, 0:8], AF.Exp, scale=2.0 / 15.0)
        P.ts("dve", g8[:, 8:16], g8[:, 8:16], 1.0, ALU.add)
        recip("dve", g8[:, 8:16], g8[:, 8:16])
        P.ts("dve", g8[:, 8:16], g8[:, 8:16], -30.0, ALU.mult, 15.0, ALU.add)
        P.act(g8[:, 16:20], g8[:, 12:16], AF.Exp, scale=-1.0)
        P.act(g8[:, 20:24], g8[:, 16:20], AF.Ln, bias=1.0, scale=1.0)
        P.ts("dve", g8[:, 24:28], g8[:, 20:24], -1.0, ALU.mult)
        lf = g8[:, 24:28]
        yield
        psg = bankm()
        P.mm(psg[:, 0:4], tri, lf)
        P.mm(psg[:, 4:8], onesf, lf)
        P.copy("dve", g8[:, 28:36], psg[:, 0:8])
        bt = g8[:, 28:32]; bL = g8[:, 32:36]
        P.tt("dve", g8[:, 36:40], g8[:, 8:12], bt, ALU.subtract)
        P.act(g8[:, 40:44], bt, AF.Exp)
        P.tt("dve", g8[:, 44:48], g8[:, 36:40], bL, ALU.add)
        P.act(g8[:, 44:48], g8[:, 44:48], AF.Exp)
        P.act(g8[:, 48:52], bL, AF.Exp)
        yield
        P.copy("pool", lfb, lf.bc(2, 128))
        P.ts("dve", qq[:, 0:256], slu[:, 0:256], 0.125, ALU.mult)
        P.copy("pool", qq[:, 256:512], slu[:, 256:512])
        P.stt("dve", qq[:, 512:768].re(H4, h=4), slu[:, 0:256].re(H4, h=4), 0.125, g8[:, 40:44].bc(2, 64), ALU.mult, ALU.mult)
        yield
        for n in range(12):
            pb_ = ptbM[0] if n < 8 else ptbM[1]
            nn = n % 8
            P.tr(pb_[0:64, nn * 128:(nn + 1) * 128], qq[:, n * 64:(n + 1) * 64], ident)
        P.copy("act", qkT[0:64, 0:8, :], ptbM[0][0:64, :].re("p (n t) -> p n t", n=8))
        P.copy("dve", qkT[0:64, 8:12, :], ptbM[1][0:64, 0:512].re("p (n t) -> p n t", n=4))
        P.copy("act", Vb, m[:, 0:512].re("p (h v) -> p h v", h=4))
        P.tt("pool", Kw, slu[:, 256:512].re(H4, h=4), g8[:, 44:48].bc(2, 64), ALU.mult)
        yield
        pE = bankm(); pS_ = bankm(); pN = bankm(); pCm = bankm(); pdn = bankm()
        for h in range(4):
            P.mm(pE[:, h * 128:(h + 1) * 128], lfb[:, h, :], tri, start=True, stop=False)
            P.mm(pE[:, h * 128:(h + 1) * 128], identf, masknegf, start=False, stop=True)
        for h in range(4):
            P.mm(pS_[:, h * 128:(h + 1) * 128], qkT[0:64, 4 + h, :], qkT[0:64, h, :])
        for h in range(4):
            P.act(DTm[:, h, :], pE[:, h * 128:(h + 1) * 128], AF.Exp, bias=g8[:, 36 + h:37 + h], scale=1.0)
        P.tt("dve", PTm, pS_[:, :].re("p (h t) -> p h t", h=4), DTm, ALU.mult)
        for h in range(4):
            P.mm(pN[:, h * 128:(h + 1) * 128], PTm[:, h, :], Vb[:, h, :], start=True, stop=False)
            P.mm(pN[:, h * 128:(h + 1) * 128], qkT[0:64, 8 + h, :], C_bf[0:64, h, :], start=False, stop=True)
            P.mm(pdn[:, h:h + 1], PTm[:, h, :], ones_bf[:, 0:1], start=True, stop=False)
            P.mm(pdn[:, h:h + 1], qkT[0:64, 8 + h, :], n_bf[0:64, h:h + 1], start=False, stop=True)
            P.mm(pCm[0:64, h * 128:(h + 1) * 128], Kw[:, h, :], Vb[:, h, :])
            P.mm(pdn[0:64, 8 + h:9 + h], Kw[:, h, :], ones_bf[:, 0:1])
        P.copy("dve", g8[:, 52:56], pdn[:, 0:4])
        P.stt("dve", g8[:, 56:60], g8[:, 52:56], -1.0, g8[:, 52:56], ALU.mult, ALU.max)
        P.ts("dve", g8[:, 56:60], g8[:, 56:60], 1.0, ALU.max)
        recip("dve", g8[:, 60:64], g8[:, 56:60])
        P.tt("dve", hm, pN[:, :].re("p (h v) -> p h v", h=4), g8[:, 60:64].bc(2, 128), ALU.mult)
        if i <= 1:
            tap("h_mlstm%d" % i, hm, [128, 4, 128])
        P.tt("pool", Cst[0:64], Cst[0:64], g8[0:64, 48:52].bc(2, 128), ALU.mult)
        P.tt("dve", Cst[0:64], Cst[0:64], pCm[0:64, :].re("p (h v) -> p h v", h=4), ALU.add)
        P.tt("pool", nst[0:64], nst[0:64], g8[0:64, 48:52], ALU.mult)
        P.tt("dve", nst[0:64], nst[0:64], pdn[0:64, 8:12], ALU.add)
        P.copy("pool", C_bf[0:64], Cst[0:64])
        P.copy("pool", n_bf[0:64], nst[0:64])
        yield
        hflat = hm.re("p h v -> p (h v)")
        P.tt("pool", ctmp.re("p (h v) -> p h v", h=4), hm, hm, ALU.mult)
        P.red("dve", sm4[:, 0:4], ctmp.re("p (h v) -> p h v", h=4), ALU.add)
        P.ts("dve", sm4[:, 0:4], sm4[:, 0:4], 1.0 / 128, ALU.mult)
        P.act(sm4[:, 4:8], sm4[:, 0:4], AF.Ln, bias=epsM[:, 0:1], scale=1.0)
        P.act(sm4[:, 8:12], sm4[:, 4:8], AF.Exp, scale=-0.5)
        P.ts("dve", sm4[:, 8:12], sm4[:, 8:12], 0.5, ALU.mult)
        P.tt("dve", hm, hm, sm4[:, 8:12].bc(2, 128), ALU.mult)
        P.tt("pool", hflat, hflat, hnw_bc, ALU.mult)
        P.stt("dve", ym[i % 2], og, 1.0, hflat, ALU.add, ALU.mult)
        P.dma("sp", ycat_s[i * 128:(i + 1) * 128, 512:1024], ym[i % 2], "yms%d_%d" % (T["id"], i % 2))
        if i == 0:
            tap("ym", ym[0], [128, 512])
        if i == 1:
            tap("ym1", ym[1], [128, 512])

    run_streams(b2_tile, B2S, offset=2)
    P.end_phase()
    if stop_after <= 3:
        P.finish()
        return nc, tapd

    P.begin_phase()
    Wout = P.tile("Wout", [128, 8, D], BF16)
    Wgr = P.tile("Wgr", [128, 8, 36], BF16)
    brt_bc = P.tile("brt_bc", [128, 36])
    stg3 = [P.tile("stg3_%d" % i, [128, D]) for i in range(2)]
    for k in range(8):
        s = stg3[k % 2]
        P.dma("sp", s, dr["w_out"][k * 128:(k + 1) * 128, :], "stg3_%d" % (k % 2))
        P.copy(ceng[k % 3], Wout[:, k, :], s)
    s = P.tile("stg3r", [128, 288])
    P.dma("sp", s[:, 0:32].re("p (k g) -> p k g", k=8), dr["moe_w_group"].rearrange("(k p) g -> p k g", p=128), "stg3r")
    P.dma("sp", s[:, 32:288].re("p (k g) -> p k g", k=8), dr["moe_w_router"].rearrange("(k p) g -> p k g", p=128), "stg3r")
    P.copy("act", Wgr[:, :, 0:4], s[:, 0:32].re("p (k g) -> p k g", k=8))
    P.copy("act", Wgr[:, :, 4:36], s[:, 32:288].re("p (k g) -> p k g", k=8))
    bcload(brt_bc[:, 0:4], dr["moe_b_group"][0:1, :], 128, "c10")
    bcload(brt_bc[:, 4:36], dr["moe_b_router"][0:1, :], 128, "c10")
    def alloc_b3(q):
        sx = "_o%d" % q
        T = {"id": q}
        T["ycat"] = P.tile("ycat" + sx, [128, D], BF16)
        T["xB"] = P.tile("xB" + sx, [128, D])
        T["ycT"] = P.tile("ycT" + sx, [128, 8, 128], BF16)
        T["x1"] = P.tile("x1" + sx, [128, D])
        T["xn2"] = P.tile("xn2" + sx, [128, D], BF16)
        T["junkB"] = P.tile("junkB" + sx, [128, D], BF16)
        T["h2T"] = P.tile("h2T" + sx, [128, 8, 128], BF16)
        T["h2Tk"] = [T["h2T"][:, k, :].sub("h2T%s_%d" % (sx, k)) for k in range(8)]
        T["lg"] = P.tile("lg" + sx, [128, 36])
        T["r8"] = P.tile("r8" + sx, [128, 96])
        T["s16"] = P.tile("s16" + sx, [128, 16])
        T["cwt"] = P.tile("cwt" + sx, [128, 4, 8])
        T["gm_bc"] = P.tile("gm_bc" + sx, [128, D])
        return T

    B3S = [alloc_b3(q) for q in range(NSTR2)]
    ptb3 = [P.psum("ptb3_%d" % i, [128, 1024], BF16) for i in range(2)]
    pf3 = [P.psum("pf3_%d" % i, [128, 512]) for i in range(5)]
    p3i = [0]

    def bank3():
        b_ = pf3[p3i[0] % 5]
        p3i[0] += 1
        return b_

    def b3_tile(i, T):
        ycat = T["ycat"]
        xB = T["xB"]
        ycT = T["ycT"]
        x1 = T["x1"]
        xn2 = T["xn2"]
        junkB = T["junkB"]
        h2T = T["h2T"]
        h2Tk = T["h2Tk"]
        lg = T["lg"]
        r8 = T["r8"]
        s16 = T["s16"]
        cwt = T["cwt"]
        gm_bc = T["gm_bc"]
        b = i // NT
        it = i % NT
        P.dma("sp", ycat, ycat_s[i * 128:(i + 1) * 128, :], "ycl%d" % T["id"])
        P.dma("sp", xB, dr["x"][i * 128:(i + 1) * 128, :], "xB%d" % T["id"])
        if it == 0:
            P.dma("sp", gm_bc, mod_s[b:b + 1, 2048:3072].partition_broadcast(128), "gmbc%d" % T["id"])
        for k in range(8):
            P.tr(ptb3[0][:, k * 128:(k + 1) * 128], ycat[:, k * 128:(k + 1) * 128], ident)
        P.copy("dve", ycT, ptb3[0][:, :].re("p (k t) -> p k t", k=8))
        yield
        x1t = x1
        for cb in range(2):
            po_ = bank3()
            for k in range(8):
                P.mm(po_, ycT[:, k, :], Wout[:, k, cb * 512:(cb + 1) * 512], start=(k == 0), stop=(k == 7))
            P.tt("dve", x1t[:, cb * 512:(cb + 1) * 512], po_, gm_bc[:, cb * 512:(cb + 1) * 512], ALU.mult)
        P.tt("pool", x1t, x1t, xB, ALU.add)
        P.dma("sp", x1_s[i * 128:(i + 1) * 128, :], x1t, "x1s%d" % T["id"])
        if i == 0:
            tap("x1", x1t, [128, D])
        if i == 1:
            tap("x1b", x1t, [128, D])
        yield
        P.act(junkB, x1t, AF.Square, accum=s16[:, 0:1])
        P.ts("dve", s16[:, 1:2], s16[:, 0:1], 1.0 / D, ALU.mult)
        P.act(s16[:, 2:3], s16[:, 1:2], AF.Ln, bias=epsM[:, 0:1], scale=1.0)
        P.act(s16[:, 3:4], s16[:, 2:3], AF.Exp, scale=-0.5)
        P.act(xn2, x1t, AF.Copy, scale=s16[:, 3:4])
        for k in range(8):
            P.tr(ptb3[1][:, k * 128:(k + 1) * 128], xn2[:, k * 128:(k + 1) * 128], ident)
        h2 = h2T
        h2k = h2Tk
        for k in range(8):
            P.act(h2k[k], ptb3[1][:, k * 128:(k + 1) * 128], AF.Identity, bias=shF[:, b, k:k + 1], scale=gamF[:, b, k:k + 1])
        P.add("sp", (lambda o_, i_: (lambda e: e.dma_start(out=o_, in_=i_)))(h2T_s[:, :, i * 128:(i + 1) * 128], h2.ap), h2k, [], dma_key="h2s%d" % T["id"])
        yield
        pr = bank3()
        for k in range(8):
            P.mm(pr[:, 0:36], h2k[k], Wgr[:, k, :], start=(k == 0), stop=(k == 7))
        P.tt("dve", lg, pr[:, 0:36], brt_bc, ALU.add)
        yield
        P.red("dve", r8[:, 0:1], lg[:, 0:4], ALU.max)
        P.ts("dve", r8[:, 1:5], lg[:, 0:4], r8[:, 0:1], ALU.is_equal)
        P.ts("dve", r8[:, 5:6], r8[:, 0:1], -1.0, ALU.mult)
        P.act(r8[:, 6:10], lg[:, 0:4], AF.Exp, bias=r8[:, 5:6], scale=1.0, accum=r8[:, 10:11])
        recip("dve", r8[:, 11:12], r8[:, 10:11])
        P.tt("dve", r8[:, 16:48].re("p (g e) -> p g e", g=4), lg[:, 4:36].re("p (g e) -> p g e", g=4), r8[:, 1:5].bc(2, 8), ALU.mult)
        P.red("dve", r8[:, 48:56], r8[:, 16:48].re("p (g e) -> p e g", g=4), ALU.add)
        P.red("dve", r8[:, 56:57], r8[:, 48:56], ALU.max)
        P.ts("dve", r8[:, 64:72], r8[:, 48:56], r8[:, 56:57], ALU.is_equal)
        P.stt("dve", r8[:, 72:80], r8[:, 64:72], -1e30, r8[:, 48:56], ALU.mult, ALU.add)
        P.red("dve", r8[:, 57:58], r8[:, 72:80], ALU.max)
        P.ts("dve", r8[:, 80:88], r8[:, 72:80], r8[:, 57:58], ALU.is_equal)
        P.tt("dve", r8[:, 58:59], r8[:, 56:57], r8[:, 57:58], ALU.subtract)
        P.act(r8[:, 60:61], r8[:, 58:59], AF.Exp, scale=-1.0)
        P.ts("dve", r8[:, 59:60], r8[:, 60:61], 1.0, ALU.add)
        recip("dve", r8[:, 59:60], r8[:, 59:60])
        P.tt("dve", r8[:, 60:61], r8[:, 60:61], r8[:, 59:60], ALU.mult)
        P.ts("dve", r8[:, 64:72], r8[:, 64:72], r8[:, 59:60], ALU.mult)
        P.stt("dve", r8[:, 64:72], r8[:, 80:88], r8[:, 60:61], r8[:, 64:72], ALU.mult, ALU.add)
        P.ts("dve", r8[:, 64:72], r8[:, 64:72], r8[:, 11:12], ALU.mult)
        cw_t = cwt
        P.tt("dve", cw_t, r8[:, 1:5].bc(2, 8), r8[:, 64:72].bc(1, 4), ALU.mult)
        P.dma("sp", cw_s[i * 128:(i + 1) * 128, :], cw_t.re("p g e -> p (g e)"), "cws%d" % T["id"])
        if i == 0:
            tap("cw", cw_t, [128, 4, 8])

    run_streams(b3_tile, B3S, offset=0)
    P.end_phase()
    if stop_after <= 4:
        P.finish()
        return nc, tapd

    P.begin_phase()
    BLK = min(1024, SEQ)
    SUB = min(512, BLK)
    NB = NTOK // BLK
    TPB = BLK // 128
    fnw_bc = P.tile("fnw_bc", [128, D])
    bcload(fnw_bc, dr["final_norm_w"][0:1, :], 128, "c11")
    h2bs = [P.tile("h2b%d" % i, [128, 8, BLK], BF16) for i in range(2)]
    cwbs = [P.tile("cwb%d" % i, [128, TPB, NE]) for i in range(2)]
    yaccss = [[P.tile("yacc%d_%d" % (j, i), [128, TPB, 512]) for i in range(2)] for j in range(2)]
    gf_bcs = [P.tile("gf_bc%d" % i, [128, D]) for i in range(2)]
    wgb = [P.tile("wgb%d" % i, [128, 8, DE], BF16) for i in range(2)]
    wub = [P.tile("wub%d" % i, [128, 8, DE], BF16) for i in range(2)]
    wdb = [P.tile("wdb%d" % i, [128, 2, D], BF16) for i in range(2)]
    sg = [P.tile("sg%d" % i, [128, SUB]) for i in range(2)]
    actT = [[P.tile("actT%d_%d" % (s_, f), [128, SUB], BF16) for f in range(2)] for s_ in range(2)]
    x1c = [P.tile("x1c%d" % i, [128, D]) for i in range(2)]
    junkC = P.tile("junkC", [128, D], BF16)
    pG = [P.psum("pG%d" % i, [128, 512]) for i in range(2)]
    pUu = [P.psum("pU%d" % i, [128, 512]) for i in range(2)]
    pD = [P.psum("pD%d" % i, [128, 512]) for i in range(3)]
    pdi = [0]
    wcnt = [0]

    obig = P.tile("obig", [128, TPB, D])
    obt = [obig[:, ti, :].sub("obig_%d" % ti) for ti in range(TPB)]
    ssq = P.tile("ssqC", [128, 4, TPB])

    def epilogue(blk, slot):
        for ti in range(TPB):
            gi = blk * TPB + ti
            sl = gi % 2
            P.dma("sp", x1c[sl], x1_s[gi * 128:(gi + 1) * 128, :], "x1c%d" % sl)
            o = obt[ti]
            for cb in range(2):
                P.tt("dve", o[:, cb * 512:(cb + 1) * 512], yaccss[slot][cb][:, ti, :], gf_bcs[slot][:, cb * 512:(cb + 1) * 512], ALU.mult)
            P.tt("dve", o, o, x1c[sl], ALU.add)
            P.act(junkC, o, AF.Square, accum=ssq[:, 0, ti:ti + 1])
            yield
        P.ts("dve", ssq[:, 1, :], ssq[:, 0, :], 1.0 / D, ALU.mult)
        P.act(ssq[:, 2, :], ssq[:, 1, :], AF.Sqrt, bias=epsM[:, 0:1], scale=1.0)
        recip("dve", ssq[:, 3, :], ssq[:, 2, :])
        for ti in range(TPB):
            gi = blk * TPB + ti
            o = obt[ti]
            P.stt("dve", o, o, ssq[:, 3, ti:ti + 1], fnw_bc, ALU.mult, ALU.mult)
            P.dma("sp", out_d[gi * 128:(gi + 1) * 128, :], o, "outC%d" % (ti % 4))
            yield

    fin_prev = None
    for blk in range(NB):
        t0 = blk * BLK
        b = t0 // SEQ
        slot = blk % 2
        h2b = h2bs[slot]
        cwb = cwbs[slot]
        yaccs = yaccss[slot]
        P.dma("sp", h2b, h2T_s[:, :, t0:t0 + BLK], "h2b%d" % slot)
        P.dma("sp", cwb, cw_s[t0:t0 + BLK, :].rearrange("(n p) e -> p n e", p=128), "cwb%d" % slot)
        P.dma("sp", gf_bcs[slot], mod_s[b:b + 1, 5120:6144].partition_broadcast(128), "gfbc%d" % slot)
        NSB = BLK // SUB
        units = [(e, sb, (e * NSB + sb) % 2) for e in range(NE) for sb in range(NSB)]

        def c_loads(e):
            ws = wcnt[0] % 2
            wcnt[0] += 1
            P.dma("sp", wgb[ws], wg_s[e].rearrange("p (k f) -> p k f", k=8), "wgb%d" % ws)
            P.dma("sp", wub[ws], wu_s[e].rearrange("p (k f) -> p k f", k=8), "wub%d" % ws)
            P.dma("sp", wdb[ws], wd_s[e].rearrange("p (c d) -> p c d", c=2), "wdb%d" % ws)
            return ws

        wslot = {}

        def c_gu(u, fc):
            e, sb, asl = u
            ws = wslot[e]
            for k in range(8):
                P.mm(pG[fc][:, 0:SUB], wgb[ws][:, k, fc * 128:(fc + 1) * 128], h2b[:, k, sb * SUB:(sb + 1) * SUB], start=(k == 0), stop=(k == 7))
            for k in range(8):
                P.mm(pUu[fc][:, 0:SUB], wub[ws][:, k, fc * 128:(fc + 1) * 128], h2b[:, k, sb * SUB:(sb + 1) * SUB], start=(k == 0), stop=(k == 7))
            P.act(sg[fc], pG[fc][:, 0:SUB], AF.Silu)
            P.tt("dve", actT[asl][fc], pUu[fc][:, 0:SUB], sg[fc], ALU.mult)

        def c_down(u):
            e, sb, asl = u
            ws = wslot[e]
            for tt_ in range(SUB // 128):
                ti = sb * (SUB // 128) + tt_
                for cb in range(2):
                    pd_ = pD[pdi[0] % 3]; pdi[0] += 1
                    for fc in range(2):
                        P.mm(pd_, actT[asl][fc][:, tt_ * 128:(tt_ + 1) * 128], wdb[ws][:, fc, cb * 512:(cb + 1) * 512], start=(fc == 0), stop=(fc == 1))
                    ysl = yaccs[cb][:, ti, :]
                    if e == 0:
                        P.ts("dve", ysl, pd_, cwb[:, ti, e:e + 1], ALU.mult)
                    else:
                        P.stt("dve", ysl, pd_, cwb[:, ti, e:e + 1], ysl, ALU.mult, ALU.add)

        wslot[0] = c_loads(0)
        c_gu(units[0], 0)
        c_gu(units[0], 1)
        for n in range(len(units)):
            if n + 1 < len(units):
                if units[n + 1][1] == 0:
                    wslot[units[n + 1][0]] = c_loads(units[n + 1][0])
                c_gu(units[n + 1], 0)
            c_down(units[n])
            if n + 1 < len(units):
                c_gu(units[n + 1], 1)
            if fin_prev is not None and n >= 1:
                next(fin_prev, None)
        if fin_prev is not None:
            for _ in fin_prev:
                pass
        fin_prev = epilogue(blk, slot)
    for _ in fin_prev:
        pass
    P.end_phase()
    P.finish()
    return nc, tapd


_NC_CACHE = {}


def kernel(**inputs):
    NCORES = 8
    x = np.asarray(inputs["x"], dtype=np.float32)
    B, S, _ = x.shape
    NSEQ = B // NCORES
    key = (NSEQ, S)
    if key not in _NC_CACHE:
        _NC_CACHE[key] = build(NSEQ, S)[0]
    nc = _NC_CACHE[key]
    shared = {}
    for k, shp in PARAM_SHAPES.items():
        shared[k] = np.ascontiguousarray(np.asarray(inputs[k], dtype=np.float32).reshape(shp))
    c = np.asarray(inputs["c"], dtype=np.float32)
    in_maps = []
    for i in range(NCORES):
        m = dict(shared)
        m["x"] = np.ascontiguousarray(x[i * NSEQ:(i + 1) * NSEQ].reshape(NSEQ * S, D))
        m["c"] = np.ascontiguousarray(c[i * NSEQ:(i + 1) * NSEQ])
        in_maps.append(m)
    res = run_bass_kernel_spmd(nc, in_maps, core_ids=list(range(NCORES)))
    outs = [np.asarray(r["out"]).reshape(NSEQ, S, D) for r in res.results]
    return np.concatenate(outs, axis=0).astype(np.float32)
```

```python
import numpy as np
from concourse.bass_utils import run_bass_kernel_spmd
import numpy as np
from contextlib import ExitStack
import concourse.bass as bass
import concourse.mybir as mybir

F32 = mybir.dt.float32
BF16 = mybir.dt.bfloat16
AF = mybir.ActivationFunctionType
ALU = mybir.AluOpType
AX = mybir.AxisListType

ENGS = ("pe", "act", "dve", "pool", "sp")
SEM_CAP = 30000


class Buf:
    __slots__ = ("name", "writers", "readers", "psum")

    def __init__(self, name, psum=False):
        self.name = name
        self.writers = {}
        self.readers = []
        self.psum = psum


class V:
    __slots__ = ("ap", "buf")

    def __init__(self, ap, buf):
        self.ap = ap
        self.buf = buf

    def __getitem__(self, k):
        return V(self.ap[k], self.buf)

    def re(self, pat, **kw):
        return V(self.ap.rearrange(pat, **kw), self.buf)

    def bc(self, axis, n):
        a = self.ap.unsqueeze(axis)
        shp = list(a.shape)
        shp[axis] = n
        return V(a.to_broadcast(shp), self.buf)

    def sub(self, name):
        return V(self.ap, Buf(name))


class Op:
    __slots__ = ("eng", "fn", "idx", "eidx", "dma_key", "deps", "signals", "sigval", "semi", "extra", "phase")

    def __init__(self, eng, fn, dma_key):
        self.eng = eng
        self.fn = fn
        self.dma_key = dma_key
        self.deps = []
        self.signals = False
        self.sigval = 0
        self.semi = 0
        self.extra = []


def _bufs(vs):
    out = []
    for v in vs:
        if isinstance(v, V):
            out.append(v.buf)
        elif isinstance(v, Buf):
            out.append(v)
    return out


class Prog:
    def __init__(self, nc):
        self.nc = nc
        self.ops = []
        self.eops = {e: [] for e in ENGS}
        self.gstack = ExitStack()
        self.pstack = None
        self.cnt = {e: 0 for e in ENGS}
        self.dcnt = {}
        self.esems = {e: [] for e in ENGS}
        self.dsems = {}
        self.free_dsems = []
        self.waited = {e: {} for e in ENGS}
        self.carry = []
        self.nops_total = 0
        self.phase = 0

    def _stack(self):
        return self.pstack if self.pstack is not None else self.gstack

    def tile(self, name, shape, dt=F32):
        t = self._stack().enter_context(self.nc.sbuf_tensor(name, list(shape), dt))
        return V(t[:], Buf(name))

    def psum(self, name, shape, dt=F32):
        t = self._stack().enter_context(self.nc.psum_tensor(name, list(shape), dt))
        return V(t[:], Buf(name, psum=True))

    def begin_phase(self):
        self.pstack = ExitStack()
        self.ops = []
        self.eops = {e: [] for e in ENGS}

    def add(self, eng, fn, reads=(), writes=(), dma_key=None):
        op = Op(eng, fn, dma_key)
        op.idx = len(self.ops)
        op.phase = self.phase
        op.eidx = len(self.eops[eng])
        deps = {}
        wkey = ("dma", dma_key) if dma_key is not None else eng
        rb = _bufs(reads)
        wb = _bufs(writes)
        for b in rb:
            for w in b.writers.values():
                deps[id(w)] = w
            if b.psum:
                for r in b.readers:
                    if r.eng != eng:
                        deps[id(r)] = r
        for b in wb:
            for w in b.writers.values():
                deps[id(w)] = w
            for r in b.readers:
                deps[id(r)] = r
        deps.pop(id(op), None)
        op.deps = list(deps.values())
        for b in rb:
            b.readers.append(op)
        for b in wb:
            b.writers[wkey] = op
            b.readers = []
        self.ops.append(op)
        self.eops[eng].append(op)
        return op

    def mm(self, out, lhsT, rhs, start=True, stop=True, extra_r=()):
        return self.add("pe", lambda e: e.matmul(out.ap, lhsT=lhsT.ap, rhs=rhs.ap, start=start, stop=stop),
                        [lhsT, rhs] + list(extra_r), [out])

    def tr(self, out, in_, ident):
        return self.add("pe", lambda e: e.transpose(out.ap, in_.ap, ident.ap), [in_, ident], [out])

    def act(self, out, in_, func, bias=None, scale=None, accum=None, eng="act"):
        kw = {}
        r = [in_]
        if bias is not None:
            kw["bias"] = bias.ap if isinstance(bias, V) else bias
            if isinstance(bias, V):
                r.append(bias)
        if scale is not None:
            kw["scale"] = scale.ap if isinstance(scale, V) else scale
            if isinstance(scale, V):
                r.append(scale)
        w = [out]
        if accum is not None:
            kw["accum_out"] = accum.ap
            w.append(accum)
        return self.add(eng, lambda e: e.activation(out=out.ap, in_=in_.ap, func=func, **kw), r, w)

    def tt(self, eng, out, in0, in1, op):
        return self.add(eng, lambda e: e.tensor_tensor(out=out.ap, in0=in0.ap, in1=in1.ap, op=op), [in0, in1], [out])

    def ts(self, eng, out, in0, s1, op0, s2=None, op1=None, accum=None):
        r = [in0]
        a1 = s1.ap if isinstance(s1, V) else s1
        a2 = s2.ap if isinstance(s2, V) else s2
        if isinstance(s1, V):
            r.append(s1)
        if isinstance(s2, V):
            r.append(s2)
        w = [out]
        kw = {}
        if op1 is not None:
            kw["op1"] = op1
        if accum is not None:
            kw["accum_out"] = accum.ap
            w.append(accum)
        return self.add(eng, lambda e: e.tensor_scalar(out=out.ap, in0=in0.ap, scalar1=a1, scalar2=a2, op0=op0, **kw), r, w)

    def stt(self, eng, out, in0, scalar, in1, op0, op1):
        eng = "dve"
        r = [in0, in1]
        a = scalar.ap if isinstance(scalar, V) else scalar
        if isinstance(scalar, V):
            r.append(scalar)
        return self.add(eng, lambda e: e.scalar_tensor_tensor(out=out.ap, in0=in0.ap, scalar=a, in1=in1.ap, op0=op0, op1=op1), r, [out])

    def copy(self, eng, out, in_):
        if eng == "act":
            return self.add(eng, lambda e: e.copy(out=out.ap, in_=in_.ap), [in_], [out])
        return self.add(eng, lambda e: e.tensor_copy(out=out.ap, in_=in_.ap), [in_], [out])

    def red(self, eng, out, in_, op, axis=AX.X):
        return self.add(eng, lambda e: e.tensor_reduce(out=out.ap, in_=in_.ap, axis=axis, op=op), [in_], [out])

    def memset(self, eng, out, val):
        return self.add(eng, lambda e: e.memset(out.ap, val), [], [out])

    def dma(self, eng, out, in_, key, **kw):
        r = [in_] if isinstance(in_, V) else []
        w = [out] if isinstance(out, V) else []
        oa = out.ap if isinstance(out, V) else out
        ia = in_.ap if isinstance(in_, V) else in_
        return self.add(eng, lambda e: e.dma_start(out=oa, in_=ia, **kw), r, w, dma_key=key)

    def end_phase(self):
        nc = self.nc
        ops = self.ops
        need = {}
        for op in ops:
            wl = []
            for p in op.deps:
                if p.phase != self.phase:
                    continue
                if p.dma_key is not None:
                    wl.append(p)
                elif p.eng != op.eng:
                    p.signals = True
                    wl.append(p)
                else:
                    if op.eng == "pe" and op.dma_key is None:
                        continue
                    if op.dma_key is not None or (op.eidx - p.eidx) <= 4:
                        p.signals = True
                        wl.append(p)
            need[id(op)] = wl
        lastc = {}
        for e in ENGS:
            for op in reversed(self.eops[e]):
                if op.dma_key is None:
                    op.signals = True
                    lastc[e] = op
                    break
        for op in ops:
            if op.dma_key is not None:
                if op.dma_key not in self.dsems:
                    if self.free_dsems:
                        sem_, c0_ = self.free_dsems.pop()
                        self.dsems[op.dma_key] = sem_
                        self.dcnt[op.dma_key] = c0_
                    else:
                        self.dsems[op.dma_key] = self.gstack.enter_context(nc.semaphore("d_%s" % (op.dma_key,)))
                        self.dcnt[op.dma_key] = 0
                self.dcnt[op.dma_key] += 16
                op.sigval = self.dcnt[op.dma_key]
            elif op.signals:
                c = self.cnt[op.eng]
                op.semi = c // SEM_CAP
                op.sigval = c % SEM_CAP + 1
                self.cnt[op.eng] = c + 1
                while len(self.esems[op.eng]) <= op.semi:
                    i = len(self.esems[op.eng])
                    self.esems[op.eng].append(self.gstack.enter_context(nc.semaphore("s_%s_%d" % (op.eng, i))))
        plans = {e: [] for e in ENGS}
        first = {e: True for e in ENGS}
        for op in ops:
            ws = {}
            if first[op.eng]:
                first[op.eng] = False
                for key, sem, v in self.carry:
                    if self.waited[op.eng].get(key, 0) < v:
                        ws[key] = (sem, v)
            for p in need[id(op)]:
                if p.dma_key is not None:
                    sem = self.dsems[p.dma_key]
                    key = ("dsem", id(sem))
                else:
                    key = (p.eng, p.semi)
                    sem = self.esems[p.eng][p.semi]
                v = p.sigval
                if self.waited[op.eng].get(key, 0) >= v:
                    continue
                if key not in ws or ws[key][1] < v:
                    ws[key] = (sem, v)
            for key, (sem, v) in ws.items():
                self.waited[op.eng][key] = v
            if op.dma_key is not None:
                inc = (self.dsems[op.dma_key], 16)
            elif op.signals:
                inc = (self.esems[op.eng][op.semi], 1)
            else:
                inc = None
            plans[op.eng].append((op, list(ws.values()), inc))
        carry = [(("dsem", id(self.dsems[k])), self.dsems[k], v) for k, v in self.dcnt.items()]
        for e, op in lastc.items():
            carry.append(((e, op.semi), self.esems[e][op.semi], op.sigval))
        self.carry = carry + [c for c in self.carry if c[0] not in {x[0] for x in carry}]
        final_waits = [(sem, v) for (_, sem, v) in self.carry]

        def run(engobj, plan, final=None):
            for op, ws, inc in plan:
                for sem, v in ws:
                    engobj.wait_ge(sem, v)
                ins = op.fn(engobj)
                if inc is not None:
                    ins.then_inc(inc[0], inc[1])
            if final:
                for sem, v in final:
                    engobj.wait_ge(sem, v)

        with nc.Block() as block:
            @block.tensor
            def _(e):
                run(e, plans["pe"])

            @block.scalar
            def _(e):
                run(e, plans["act"])

            @block.vector
            def _(e):
                run(e, plans["dve"])

            @block.gpsimd
            def _(e):
                run(e, plans["pool"])

            @block.sync
            def _(e):
                run(e, plans["sp"], final_waits)
        self.nops_total += len(ops)
        self.phase += 1
        for k_ in list(self.dsems):
            self.free_dsems.append((self.dsems.pop(k_), self.dcnt.pop(k_)))
        self.ops = []
        self.eops = {e: [] for e in ENGS}
        if self.pstack is not None:
            self.pstack.close()
            self.pstack = None

    def finish(self):
        self.gstack.close()

D = 1024
INC = 3336
RW = 1792
NE = 32
DE = 256
EPS = 1e-6
GN_EPS = 64e-5
DEC = 0.6065306597126334

PARAM_SHAPES = {
    "ada_w": [1024, 6144], "ada_b": [1, 6144], "mix_norm_w": [1, 1024], "w_in": [1024, 3336],
    "rwkv_mu": [1, 1792], "rwkv_w0": [1, 512], "rwkv_w_up": [64, 512], "rwkv_a0": [1, 512],
    "rwkv_a_up": [64, 512], "rwkv_g_up": [128, 512], "rwkv_k_k": [1, 512], "rwkv_k_a": [1, 512],
    "rwkv_r_k": [1, 512], "rwkv_gn_w": [1, 512], "rwkv_gn_b": [1, 512], "mlstm_conv_w": [4, 512],
    "mlstm_conv_b": [1, 512], "mlstm_i_b": [1, 4], "mlstm_f_b": [1, 4], "mlstm_hn_w": [1, 512],
    "w_out": [1024, 1024], "ffn_norm_w": [1, 1024], "moe_w_group": [1024, 4], "moe_b_group": [1, 4],
    "moe_w_router": [1024, 32], "moe_b_router": [1, 32], "moe_w_gate": [32, 1024, 256],
    "moe_w_up": [32, 1024, 256], "moe_w_down": [32, 256, 1024], "final_norm_w": [1, 1024],
}


def build(NSEQ, SEQ, taps=None, stop_after=99):
    nc = bass.Bass("TRN2", target_bir_lowering=False)
    NT = SEQ // 128
    NTOK = NSEQ * SEQ
    NTILES = NSEQ * NT
    PADR = SEQ + 3
    dr = {}
    dr["x"] = nc.dram_tensor("x", [NTOK, D], F32, kind="ExternalInput").ap()
    dr["c"] = nc.dram_tensor("c", [NSEQ, D], F32, kind="ExternalInput").ap()
    for k, shp in PARAM_SHAPES.items():
        dr[k] = nc.dram_tensor(k, shp, F32, kind="ExternalInput").ap()
    out_d = nc.dram_tensor("out", [NTOK, D], F32, kind="ExternalOutput").ap()
    proj_s = nc.dram_tensor("proj_s", [NSEQ * PADR, INC], F32).ap()
    x1_s = nc.dram_tensor("x1_s", [NTOK, D], F32).ap()
    h2T_s = nc.dram_tensor("h2T_s", [128, 8, NTOK], BF16).ap()
    cw_s = nc.dram_tensor("cw_s", [NTOK, NE], F32).ap()
    mod_s = nc.dram_tensor("mod_s", [NSEQ, 6144], F32).ap()
    wg_s = nc.dram_tensor("wg_s", [NE, 128, 2048], BF16).ap()
    wu_s = nc.dram_tensor("wu_s", [NE, 128, 2048], BF16).ap()
    wd_s = nc.dram_tensor("wd_s", [NE, 128, 2048], BF16).ap()
    tapd = {}

    P = Prog(nc)

    def tap(name, v, shape):
        if taps is None or name not in taps:
            return
        t = nc.dram_tensor("tap_" + name, list(shape), v.ap.dtype, kind="ExternalOutput").ap()
        tapd[name] = t
        P.dma("sp", t, v, "tap_" + name)

    identf = P.tile("identf", [128, 128])
    ident = P.tile("ident", [128, 128], BF16)
    tri = P.tile("tri", [128, 128])
    onesf = P.tile("onesf", [128, 128])
    trimid = P.tile("trimid", [128, 128])
    indmid = P.tile("indmid", [128, 2])
    masknegf = P.tile("masknegf", [128, 128])
    maskA = P.tile("maskA", [128, 4, 128], BF16)
    mSI = P.tile("mSI", [128, 2, 2, 128], BF16)
    epsM = P.tile("epsM", [128, 1])
    epsG = P.tile("epsG", [128, 1])
    gamM = P.tile("gamM", [128, NSEQ, 8])
    shM = P.tile("shM", [128, NSEQ, 8])
    gamF = P.tile("gamF", [128, NSEQ, 8])
    shF = P.tile("shF", [128, NSEQ, 8])

    P.begin_phase()
    P.memset("pool", identf, 1.0)
    P.add("pool", lambda e: e.affine_select(identf.ap, identf.ap, [[-1, 128]], ALU.is_equal, 0.0, base=0, channel_multiplier=1), [identf], [identf])
    P.copy("pool", ident, identf)
    P.memset("pool", tri, 1.0)
    P.add("pool", lambda e: e.affine_select(tri.ap, tri.ap, [[1, 128]], ALU.is_ge, 0.0, base=0, channel_multiplier=-1), [tri], [tri])
    P.memset("pool", onesf, 1.0)
    colm = P.tile("colm", [128, 128])
    P.memset("pool", colm, 1.0)
    P.add("pool", lambda e: e.affine_select(colm.ap, colm.ap, [[0, 128]], ALU.is_ge, 0.0, base=63, channel_multiplier=-1), [colm], [colm])
    P.tt("pool", trimid, tri, colm, ALU.subtract)
    P.ts("pool", trimid, trimid, -DEC, ALU.mult)
    P.ts("pool", indmid[:, 0:1], colm[:, 0:1], -DEC, ALU.mult)
    P.ts("pool", indmid[:, 1:2], colm[:, 0:1], DEC, ALU.mult, -DEC, ALU.add)
    P.memset("pool", masknegf, 0.0)
    P.add("pool", lambda e: e.affine_select(masknegf.ap, masknegf.ap, [[1, 128]], ALU.is_ge, -30000.0, base=0, channel_multiplier=-1), [masknegf], [masknegf])
    mstr = P.tile("mstr", [128, 128])
    P.memset("pool", mstr, 1.0)
    P.add("pool", lambda e: e.affine_select(mstr.ap, mstr.ap, [[1, 128]], ALU.is_ge, 0.0, base=-1, channel_multiplier=-1), [mstr], [mstr])
    mlow = P.tile("mlow", [128, 128])
    P.memset("pool", mlow, 1.0)
    P.add("pool", lambda e: e.affine_select(mlow.ap, mlow.ap, [[-1, 128]], ALU.is_ge, 0.0, base=-1, channel_multiplier=1), [mlow], [mlow])
    for hh in range(4):
        P.copy("pool", maskA[:, hh, :], mlow)
    for hh in range(2):
        P.copy("pool", mSI[:, hh, 0, :], mstr)
        P.copy("pool", mSI[:, hh, 1, :], tri)
    P.memset("pool", epsM, EPS)
    P.memset("pool", epsG, GN_EPS)

    def bcload(dst, src, n, key):
        P.dma("sp", dst, src.partition_broadcast(n), key)

    ct = P.tile("ct", [128, D])
    P.dma("sp", ct[0:NSEQ, :], dr["c"][:, :], "c12")
    sct = P.tile("sct", [128, D])
    P.act(sct[0:NSEQ, :], ct[0:NSEQ, :], AF.Silu)
    psA = P.psum("psA", [128, 512])
    psB = P.psum("psB", [128, 512])
    psC = P.psum("psC", [128, 512])
    scT = P.tile("scT", [128, 8, NSEQ])
    for k in range(8):
        P.mm(psA[:, k * NSEQ:(k + 1) * NSEQ], sct[0:NSEQ, k * 128:(k + 1) * 128], identf[0:NSEQ, 0:NSEQ])
    P.copy("dve", scT, psA[:, 0:8 * NSEQ].re("p (k b) -> p k b", k=8))
    adab = P.tile("adab", [128, 6144])
    P.dma("sp", adab[0:NSEQ, :], dr["ada_b"][0:1, :].partition_broadcast(NSEQ), "c13")
    nrm = P.tile("nrm", [128, 2 * D])
    P.dma("sp", nrm[0:1, 0:D], dr["mix_norm_w"][0:1, :], "c14")
    P.dma("sp", nrm[0:1, D:2 * D], dr["ffn_norm_w"][0:1, :], "c14")
    nrmT = P.tile("nrmT", [128, 16])
    for k in range(16):
        P.mm(psC[:, k:k + 1], nrm[0:1, k * 128:(k + 1) * 128], onesf[0:1, 0:1])
    P.copy("dve", nrmT, psC[:, 0:16])
    adw = [P.tile("adw%d" % i, [128, 8, 512]) for i in range(2)]
    modb = [P.tile("modb%d" % i, [128, 512]) for i in range(2)]
    modT = P.tile("modT", [128, 48, NSEQ])
    for cb in range(12):
        a = adw[cb % 2]
        P.dma("sp", a, dr["ada_w"][:, cb * 512:(cb + 1) * 512].rearrange("(k p) f -> p k f", p=128), "adw%d" % (cb % 2))
        pm = psA if cb % 2 == 0 else psB
        for k in range(8):
            P.mm(pm[0:NSEQ, :], scT[:, k, :], a[:, k, :], start=(k == 0), stop=(k == 7))
        mb = modb[cb % 2]
        P.tt("dve", mb[0:NSEQ, :], pm[0:NSEQ, :], adab[0:NSEQ, cb * 512:(cb + 1) * 512], ALU.add)
        P.dma("sp", mod_s[:, cb * 512:(cb + 1) * 512], mb[0:NSEQ, :], "mods")
        for q in range(4):
            P.mm(psC[:, 64 + q * NSEQ:64 + (q + 1) * NSEQ], mb[0:NSEQ, q * 128:(q + 1) * 128], identf[0:NSEQ, 0:NSEQ])
        P.copy("act", modT[:, cb * 4:(cb + 1) * 4, :], psC[:, 64:64 + 4 * NSEQ].re("p (q b) -> p q b", q=4))
    for b in range(NSEQ):
        P.stt("dve", gamM[:, b, :], modT[:, 8:16, b], 1.0, nrmT[:, 0:8], ALU.add, ALU.mult)
        P.copy("dve", shM[:, b, :], modT[:, 0:8, b])
        P.stt("dve", gamF[:, b, :], modT[:, 32:40, b], 1.0, nrmT[:, 8:16], ALU.add, ALU.mult)
        P.copy("dve", shF[:, b, :], modT[:, 24:32, b])
    zt = P.tile("zt", [128, 512])
    P.memset("pool", zt, 0.0)
    for b in range(NSEQ):
        P.dma("sp", proj_s[b * PADR:b * PADR + 3, :].rearrange("r (a f) -> (r a) f", f=417), zt[0:24, 0:417], "zpad")
    tap("gamM", gamM, [128, NSEQ, 8])
    tap("shM", shM, [128, NSEQ, 8])
    P.end_phase()
    if stop_after <= 0:
        P.finish()
        return nc, tapd

    ycat_s = nc.dram_tensor("ycat_s", [NTOK, D], BF16).ap()

    def recip(eng, o, a):
        P.add(eng, lambda e: e.reciprocal(out=o.ap, in_=a.ap), [a], [o])

    P.begin_phase()
    Win = P.tile("Win", [128, 8, INC], BF16)
    stg = [P.tile("stg%d" % i, [128, INC]) for i in range(2)]
    ceng = ["act", "dve", "pool"]
    for k in range(8):
        s = stg[k % 2]
        P.dma("sp", s, dr["w_in"][k * 128:(k + 1) * 128, :], "stg%d" % (k % 2))
        P.copy(ceng[k % 3], Win[:, k, :], s)
    xt = [P.tile("xt%d" % i, [128, D]) for i in range(3)]
    junk = P.tile("junkA", [128, D], BF16)
    xn = [P.tile("xn%d" % i, [128, D], BF16) for i in range(2)]
    hT = [P.tile("hT%d" % i, [128, 8, 128], BF16) for i in range(2)]
    hTk = [[hT[i][:, k, :].sub("hT%d_%d" % (i, k)) for k in range(8)] for i in range(2)]
    pj = [P.tile("pj%d" % i, [128, INC]) for i in range(3)]
    pjc = [[pj[i][:, cb * 512:min(INC, (cb + 1) * 512)].sub("pj%d_%d" % (i, cb)) for cb in range(7)] for i in range(3)]
    st = [P.tile("stA%d" % i, [128, 4]) for i in range(2)]
    ptb = [P.psum("ptbA%d" % i, [128, 8, 128], BF16) for i in range(2)]
    pp = [P.psum("ppA%d" % i, [128, 512]) for i in range(5)]
    wst = [P.tile("wst%d" % i, [128, 2048]) for i in range(4)]
    wbt = [P.tile("wbt%d" % i, [128, 2048], BF16) for i in range(4)]
    pc_list = []
    for e in range(NE):
        pc_list.append((dr["moe_w_gate"][e].rearrange("(k p) f -> p k f", p=128), wg_s[e], 8))
        pc_list.append((dr["moe_w_up"][e].rearrange("(k p) f -> p k f", p=128), wu_s[e], 8))
        pc_list.append((dr["moe_w_down"][e].rearrange("(c p) d -> p c d", p=128), wd_s[e], 2))

    def pc_load(n):
        src, dst, a_ = pc_list[n]
        P.dma("pool", wst[n % 4].re("p (a b) -> p a b", a=a_), src, "wst%d" % (n % 4))

    def precast_gen():
        for n in range(min(3, len(pc_list))):
            pc_load(n)
        for n in range(len(pc_list)):
            if n + 3 < len(pc_list):
                pc_load(n + 3)
            P.copy("pool" if n % 2 == 0 else "act", wbt[n % 4], wst[n % 4])
            P.dma("pool", pc_list[n][1], wbt[n % 4], "wbt%d" % (n % 4))
            yield

    pcg = precast_gen()

    def a_load(i):
        P.dma("sp", xt[i % 3], dr["x"][i * 128:(i + 1) * 128, :], "xA%d" % (i % 3))

    def a_front(i):
        b = i // NT
        sl = i % 2
        s4 = st[sl]
        P.act(junk, xt[i % 3], AF.Square, accum=s4[:, 0:1])
        P.ts("dve", s4[:, 1:2], s4[:, 0:1], 1.0 / D, ALU.mult)
        P.act(s4[:, 2:3], s4[:, 1:2], AF.Sqrt, bias=epsM[:, 0:1], scale=1.0)
        recip("dve", s4[:, 3:4], s4[:, 2:3])
        P.act(xn[sl], xt[i % 3], AF.Copy, scale=s4[:, 3:4])
        for k in range(8):
            P.tr(ptb[k // 4][:, k % 4, :], xn[sl][:, k * 128:(k + 1) * 128], ident)
        for k in range(8):
            if k < 4:
                P.ts("dve", hTk[sl][k], ptb[0][:, k % 4, :], gamM[:, b, k:k + 1], ALU.mult, shM[:, b, k:k + 1], ALU.add)
            else:
                P.act(hTk[sl][k], ptb[1][:, k % 4, :], AF.Identity, bias=shM[:, b, k:k + 1], scale=gamM[:, b, k:k + 1])

    ppi = [0]

    def a_back(i):
        b = i // NT
        it = i % NT
        sl = i % 2
        for cb in range(7):
            c0 = cb * 512
            cw_ = min(512, INC - c0)
            pq = pp[ppi[0] % 5]; ppi[0] += 1
            for k in range(8):
                P.mm(pq[:, 0:cw_], hTk[sl][k], Win[:, k, c0:c0 + cw_], start=(k == 0), stop=(k == 7))
            P.copy("act" if cb % 2 == 0 else "dve", pjc[i % 3][cb], pq[:, 0:cw_])
        r0 = b * PADR + 3 + it * 128
        P.add("sp", (lambda o_, i_: (lambda e: e.dma_start(out=o_, in_=i_)))(proj_s[r0:r0 + 128, :], pj[i % 3].ap), pjc[i % 3], [], dma_key="pjA%d" % (i % 3))

    a_load(0)
    if NTILES > 1:
        a_load(1)
    a_front(0)
    for i in range(NTILES):
        if i + 2 < NTILES:
            a_load(i + 2)
        if i + 1 < NTILES:
            a_front(i + 1)
        a_back(i)
        for _ in range(2):
            next(pcg, None)
    for _ in pcg:
        pass
    P.end_phase()
    if stop_after <= 1:
        P.finish()
        return nc, tapd

    P.begin_phase()
    Wup = P.tile("Wup", [128, 512], BF16)
    Aup = P.tile("Aup", [128, 512], BF16)
    Gup = P.tile("Gup", [128, 512], BF16)
    mu_bc = P.tile("mu_bc", [128, RW])
    kk_bc = P.tile("kk_bc", [128, 512])
    ka_bc = P.tile("ka_bc", [128, 512])
    rk_bc = P.tile("rk_bc", [128, 512])
    gnw_bc = P.tile("gnw_bc", [128, 512])
    gnb_bc = P.tile("gnb_bc", [128, 512])
    bcload(mu_bc, dr["rwkv_mu"][0:1, :], 128, "c0")
    bcload(kk_bc, dr["rwkv_k_k"][0:1, :], 128, "c1")
    bcload(ka_bc, dr["rwkv_k_a"][0:1, :], 128, "c2")
    bcload(rk_bc, dr["rwkv_r_k"][0:1, :], 128, "c3")
    bcload(gnw_bc, dr["rwkv_gn_w"][0:1, :], 128, "c4")
    bcload(gnb_bc, dr["rwkv_gn_b"][0:1, :], 128, "c5")
    s = P.tile("stgB1", [128, 1536])
    P.dma("sp", s[0:64, 0:512], dr["rwkv_w_up"][:, :], "stgb1")
    P.dma("sp", s[64:65, 0:512], dr["rwkv_w0"][0:1, :], "stgb1")
    P.dma("sp", s[0:64, 512:1024], dr["rwkv_a_up"][:, :], "stgb1")
    P.dma("sp", s[64:65, 512:1024], dr["rwkv_a0"][0:1, :], "stgb1")
    P.dma("sp", s[:, 1024:1536], dr["rwkv_g_up"][:, :], "stgb1")
    P.copy("act", Wup[0:64, :], s[0:64, 0:512])
    P.copy("act", Wup[64:65, :], s[64:65, 0:512])
    P.copy("dve", Aup[0:64, :], s[0:64, 512:1024])
    P.copy("dve", Aup[64:65, :], s[64:65, 512:1024])
    P.copy("pool", Gup, s[:, 1024:1536])
    NSTR = 2 if NSEQ % 2 == 0 else 1

    def alloc_b1(q):
        sx = "_s%d" % q
        T = {"id": q}
        T["rw"] = P.tile("rw" + sx, [128, RW])
        T["rwp"] = P.tile("rwp" + sx, [128, RW])
        T["li"] = P.tile("li" + sx, [128, 256], BF16)
        T["liT"] = P.tile("liT" + sx, [128, 384], BF16)
        for n in ("sgz", "av", "gv", "Ep", "Em", "Epv", "kkk", "tmpR", "tmpR2", "kkv", "knew", "y_sb"):
            T[n] = P.tile(n + sx, [128, 512])
        T["gmL"] = P.tile("gmL" + sx, [128, 4, 2])
        T["sm8"] = P.tile("sm8" + sx, [128, 64])
        T["tok4"] = P.tile("tok4" + sx, [128, 4, 512], BF16)
        T["v_bf"] = P.tile("v_bf" + sx, [128, 512], BF16)
        T["FT"] = P.tile("FT" + sx, [128, 4, 4, 128], BF16)
        T["A_sb"] = [[P.tile("A_sb%d_%d%s" % (g, i, sx), [128, 4, 128], BF16) for i in range(2)] for g in range(2)]
        T["MT"] = [[P.tile("MT%d_%d%s" % (g, i, sx), [128, 4, 2, 128], BF16) for i in range(2)] for g in range(2)]
        T["MRB"] = P.tile("MRB" + sx, [128, 8, 2, 128], BF16)
        T["AKRK"] = P.tile("AKRK" + sx, [128, 8, 2, 128], BF16)
        T["TT"] = P.tile("TT" + sx, [128, 8, 128], BF16)
        T["X_bf"] = P.tile("X_bf" + sx, [128, 512], BF16)
        T["U_bf"] = P.tile("U_bf" + sx, [128, 512], BF16)
        T["Hst"] = P.tile("Hst" + sx, [128, 4, 64])
        T["Ht"] = P.tile("Ht" + sx, [128, 4, 64])
        T["Hbd"] = P.tile("Hbd" + sx, [128, 4, 128], BF16)
        T["yr"] = [P.tile("yr%d%s" % (i, sx), [128, 512], BF16) for i in range(2)]
        P.memset("pool", T["liT"], 1.0)
        P.memset("pool", T["Hbd"], 0.0)
        return T

    B1S = [alloc_b1(q) for q in range(NSTR)]
    ptbB = [P.psum("ptbB%d" % i, [128, 1024], BF16) for i in range(2)]
    pf = [P.psum("pfB%d" % i, [128, 512]) for i in range(5)]
    pfi = [0]

    def bank():
        b_ = pf[pfi[0] % 5]
        pfi[0] += 1
        return b_

    H8 = "p (h c) -> p h c"
    def b1_tile(i, T):
        rw = T["rw"]
        rwp = T["rwp"]
        li = T["li"]
        liT = T["liT"]
        sgz = T["sgz"]
        av = T["av"]
        gv = T["gv"]
        Ep = T["Ep"]
        Em = T["Em"]
        Epv = T["Epv"]
        gmL = T["gmL"]
        kkk = T["kkk"]
        tmpR = T["tmpR"]
        tmpR2 = T["tmpR2"]
        kkv = T["kkv"]
        knew = T["knew"]
        sm8 = T["sm8"]
        tok4 = T["tok4"]
        v_bf = T["v_bf"]
        FT = T["FT"]
        A_sb = T["A_sb"]
        MT = T["MT"]
        MRB = T["MRB"]
        AKRK = T["AKRK"]
        TT = T["TT"]
        X_bf = T["X_bf"]
        U_bf = T["U_bf"]
        Hst = T["Hst"]
        Ht = T["Ht"]
        Hbd = T["Hbd"]
        y_sb = T["y_sb"]
        yr = T["yr"]
        b = i // NT
        it = i % NT
        r0 = b * PADR + 3 + it * 128
        P.dma("sp", rw, proj_s[r0:r0 + 128, 0:RW], "rw%d" % T["id"])
        P.dma("sp", rwp, proj_s[r0 - 1:r0 + 127, 0:RW], "rwp%d" % T["id"])
        if it == 0:
            P.memset("pool", Hst, 0.0)
        u = rwp
        P.tt("dve", u, u, rw, ALU.subtract)
        P.tt("dve", u, u, mu_bc, ALU.mult)
        P.tt("dve", u, u, rw, ALU.add)
        r_ = u[:, 0:512]; k_ = u[:, 512:1024]; v_ = u[:, 1024:1536]
        if i == 0:
            tap("u0", u, [128, RW])
        yield
        P.act(li[:, 0:64], u[:, 1536:1600], AF.Tanh)
        P.copy("pool", li[:, 64:128], u[:, 1600:1664])
        P.act(li[:, 128:256], u[:, 1664:1792], AF.Sigmoid)
        pb0 = ptbB[0]
        P.tr(pb0[0:64, 0:128], li[:, 0:64], ident)
        P.tr(pb0[0:64, 128:256], li[:, 64:128], ident)
        P.tr(pb0[:, 256:384], li[:, 128:256], ident)
        P.copy("act", liT[0:64, 0:256], pb0[0:64, 0:256])
        P.copy("act", liT[:, 256:384], pb0[:, 256:384])
        pz = bank(); pa = bank(); pg = bank()
        P.mm(pz, liT[0:65, 0:128], Wup[0:65, :])
        P.mm(pa, liT[0:65, 128:256], Aup[0:65, :])
        P.mm(pg, liT[:, 256:384], Gup)
        P.act(sgz, pz, AF.Sigmoid)
        P.act(av, pa, AF.Sigmoid)
        P.copy("act", gv, pg)
        yield
        pc = bank()
        P.mm(pc, trimid, sgz)
        P.act(Ep, pc, AF.Exp)
        P.act(Em, pc, AF.Exp, scale=-1.0)
        P.stt("dve", Epv, sgz, DEC, pc, ALU.mult, ALU.add)
        P.act(Epv, Epv, AF.Exp)
        psm = bank()
        for j in range(4):
            P.mm(psm[:, 2 * j:2 * j + 2], sgz[:, j * 128:(j + 1) * 128], indmid)
        P.act(gmL, psm[:, 0:8].re("p (j t) -> p j t", j=4), AF.Exp)
        yield
        P.tt("dve", kkk, k_, kk_bc, ALU.mult)
        P.tt("dve", tmpR, kkk, kkk, ALU.mult)
        P.red("dve", sm8[:, 0:8], tmpR.re(H8, h=8), ALU.add)
        P.act(sm8[:, 8:16], sm8[:, 0:8], AF.Sqrt)
        P.ts("dve", sm8[:, 8:16], sm8[:, 8:16], 1e-12, ALU.max)
        recip("dve", sm8[:, 16:24], sm8[:, 8:16])
        P.tt("dve", kkv.re(H8, h=8), kkk.re(H8, h=8), sm8[:, 16:24].bc(2, 64), ALU.mult)
        P.stt("pool", tmpR, av, -1.0, ka_bc, ALU.add, ALU.mult)
        P.stt("pool", knew, tmpR, 1.0, k_, ALU.add, ALU.mult)
        P.tt("pool", tmpR, r_, knew, ALU.mult)
        P.tt("pool", tmpR, tmpR, rk_bc, ALU.mult)
        P.red("dve", sm8[:, 24:32], tmpR.re(H8, h=8), ALU.add)
        yield
        P.stt("dve", tok4[:, 0, :], kkv, -1.0, Epv, ALU.mult, ALU.mult)
        P.tt("dve", tok4[:, 1, :], r_, Ep, ALU.mult)
        P.tt("dve", tok4[:, 2, :], knew, Em, ALU.mult)
        P.tt("dve", tmpR2, kkv, av, ALU.mult)
        P.tt("dve", tok4[:, 3, :], tmpR2, Em, ALU.mult)
        P.copy("act", v_bf, v_)
        if i == 0:
            tap("tok4", tok4, [128, 4, 512])
            tap("sgz", sgz, [128, 512])
        yield
        for half in range(2):
            pb_ = ptbB[half]
            for jj in range(2):
                j = half * 2 + jj
                for w in range(4):
                    n = jj * 4 + w
                    P.tr(pb_[:, n * 128:(n + 1) * 128], tok4[:, w, j * 128:(j + 1) * 128], ident)
            P.copy("act" if half == 0 else "dve",
                   FT[:, half * 2:half * 2 + 2, :, :], pb_[:, :].re("p (j w t) -> p j w t", j=2, w=4))
        yield
        for g in range(2):
            pA = bank()
            pMR = [bank(), bank()]
            pKR = [bank(), bank()]
            for hh in range(4):
                j = hh
                po = 64 * g
                aT = FT[po:po + 64, j, 0, :]
                arT = FT[po:po + 64, j, 0:2, :]
                kT = FT[po:po + 64, j, 2, :]
                bT = FT[po:po + 64, j, 3, :]
                P.mm(pA[:, hh * 128:(hh + 1) * 128], aT, bT)
                P.mm(pMR[hh // 2][:, (hh % 2) * 256:(hh % 2 + 1) * 256], bT, arT)
                P.mm(pKR[hh // 2][:, (hh % 2) * 256:(hh % 2 + 1) * 256], kT, arT)
            P.tt("dve", A_sb[g][0], pA[:, :].re("p (h t) -> p h t", h=4), maskA, ALU.mult)
            for q in range(2):
                P.tt("dve", MRB[:, g * 4 + q * 2:g * 4 + q * 2 + 2, :, :], pMR[q][:, :].re("p (h w t) -> p h w t", h=2, w=2), mSI, ALU.mult)
                P.tt("dve", AKRK[:, g * 4 + q * 2:g * 4 + q * 2 + 2, :, :], pKR[q][:, :].re("p (h w t) -> p h w t", h=2, w=2), mSI, ALU.mult)
            P.tt("pool", MT[g][0][:, :, 1, :], MRB[:, g * 4:g * 4 + 4, 0, :], ident.bc(1, 4), ALU.add)
            if g == 0:
                yield
        yield
        for g in range(2):
            pM = bank(); pA2 = bank()
            for hh in range(4):
                h = g * 4 + hh
                P.mm(pM[:, hh * 128:(hh + 1) * 128], A_sb[g][0][:, hh, :], MRB[:, h, 0, :])
                P.mm(pA2[:, hh * 128:(hh + 1) * 128], MRB[:, h, 0, :], A_sb[g][0][:, hh, :])
            P.copy("act", MT[g][0][:, :, 0, :], pM[:, :].re("p (h t) -> p h t", h=4))
            P.copy("dve", A_sb[g][1], pA2[:, :].re("p (h t) -> p h t", h=4))
            if g == 0:
                yield
        yield
        cur = 0
        for lev in range(1, 6):
            yield
            for g in range(2):
                Ak = A_sb[g][lev % 2]
                An = A_sb[g][(lev + 1) % 2]
                Mc = MT[g][cur]
                Mn = MT[g][1 - cur]
                pS = [bank(), bank()]
                pA2 = bank()
                for hh in range(4):
                    reg = pS[hh // 2][:, (hh % 2) * 256:(hh % 2 + 1) * 256]
                    P.mm(reg, Ak[:, hh, :], Mc[:, hh, :, :], start=True, stop=False)
                    P.mm(reg[:, 128:256], ident, Mc[:, hh, 1, :], start=False, stop=True)
                    P.mm(pA2[:, hh * 128:(hh + 1) * 128], Mc[:, hh, 0, :], Ak[:, hh, :])
                for q in range(2):
                    P.copy("act" if q == 0 else "dve", Mn[:, q * 2:q * 2 + 2, :, :], pS[q][:, :].re("p (h w t) -> p h w t", h=2, w=2))
                P.copy("act", An, pA2[:, :].re("p (h t) -> p h t", h=4))
                if g == 0:
                    yield
            cur = 1 - cur
        for g in range(2):
            Ak = A_sb[g][0]
            Mc = MT[g][cur]
            pT = bank()
            for hh in range(4):
                P.mm(pT[:, hh * 128:(hh + 1) * 128], Ak[:, hh, :], Mc[:, hh, 1, :], start=True, stop=False)
                P.mm(pT[:, hh * 128:(hh + 1) * 128], ident, Mc[:, hh, 1, :], start=False, stop=True)
            P.copy("act" if g == 0 else "dve", TT[:, g * 4:g * 4 + 4, :], pT[:, :].re("p (h t) -> p h t", h=4))
            if g == 0:
                yield
        if i == 0:
            tap("TT", TT, [128, 8, 128])
            tap("MRB", MRB, [128, 8, 2, 128])
        yield
        P.tt("pool", Ht, Hst, gmL[:, :, 0].bc(2, 64), ALU.mult)
        P.copy("pool", Hbd[0:64, :, 0:64], Ht[0:64, :, :])
        P.copy("pool", Hbd[64:128, :, 64:128], Ht[64:128, :, :])
        pX = bank()
        SI = [(h % 2) * 4 + h // 2 for h in range(8)]
        for j in range(4):
            P.mm(pX[:, j * 128:(j + 1) * 128], FT[:, j, 0, :], Hbd[:, j, :], start=True, stop=False)
            for h in (2 * j, 2 * j + 1):
                P.mm(pX[:, h * 64:(h + 1) * 64], AKRK[:, SI[h], 0, :], v_bf[:, h * 64:(h + 1) * 64], start=False, stop=(h == 2 * j + 1))
        P.copy("act", X_bf, pX)
        yield
        pU = bank()
        for h in range(8):
            P.mm(pU[:, h * 64:(h + 1) * 64], TT[:, SI[h], :], X_bf[:, h * 64:(h + 1) * 64])
        P.copy("act", U_bf, pU)
        yield
        pY = bank()
        for j in range(4):
            P.mm(pY[:, j * 128:(j + 1) * 128], FT[:, j, 1, :], Hbd[:, j, :], start=True, stop=False)
            for h in (2 * j, 2 * j + 1):
                P.mm(pY[:, h * 64:(h + 1) * 64], AKRK[:, SI[h], 1, :], v_bf[:, h * 64:(h + 1) * 64], start=False, stop=False)
                P.mm(pY[:, h * 64:(h + 1) * 64], MRB[:, SI[h], 1, :], U_bf[:, h * 64:(h + 1) * 64], start=False, stop=(h == 2 * j + 1))
        P.copy("act", y_sb, pY)
        yield
        pH = bank()
        for j in range(4):
            P.mm(pH[:, j * 128:(j + 1) * 128], tok4[:, 2, j * 128:(j + 1) * 128], v_bf[:, j * 128:(j + 1) * 128], start=True, stop=False)
            P.mm(pH[:, j * 128:(j + 1) * 128], tok4[:, 3, j * 128:(j + 1) * 128], U_bf[:, j * 128:(j + 1) * 128], start=False, stop=True)
        pHv = pH[:, :].re("p (j v) -> p j v", j=4)
        P.tt("dve", Hst[0:64, :, :], pHv[0:64, :, 0:64], Ht[0:64, :, :], ALU.add)
        P.tt("dve", Hst[64:128, :, :], pHv[64:128, :, 64:128], Ht[64:128, :, :], ALU.add)
        P.tt("pool", Hst, Hst, gmL[:, :, 1].bc(2, 64), ALU.mult)
        yield
        if i <= 1:
            tap("y_rwkv%d" % i, y_sb, [128, 512])
        y3 = y_sb.re(H8, h=8)
        P.red("dve", sm8[:, 32:40], y3, ALU.add)
        P.tt("pool", tmpR, y_sb, y_sb, ALU.mult)
        P.red("dve", sm8[:, 40:48], tmpR.re(H8, h=8), ALU.add)
        P.ts("dve", sm8[:, 32:40], sm8[:, 32:40], 1.0 / 64, ALU.mult)
        P.tt("dve", sm8[:, 48:56], sm8[:, 32:40], sm8[:, 32:40], ALU.mult)
        P.stt("dve", sm8[:, 40:48], sm8[:, 40:48], 1.0 / 64, sm8[:, 48:56], ALU.mult, ALU.subtract)
        P.act(sm8[:, 48:56], sm8[:, 40:48], AF.Sqrt, bias=epsG[:, 0:1], scale=1.0)
        recip("dve", sm8[:, 56:64], sm8[:, 48:56])
        P.tt("dve", tmpR.re(H8, h=8), y3, sm8[:, 32:40].bc(2, 64), ALU.subtract)
        P.tt("dve", tmpR.re(H8, h=8), tmpR.re(H8, h=8), sm8[:, 56:64].bc(2, 64), ALU.mult)
        P.tt("pool", tmpR, tmpR, gnw_bc, ALU.mult)
        P.tt("dve", tmpR, tmpR, gnb_bc, ALU.add)
        P.tt("pool", tmpR2.re(H8, h=8), v_.re(H8, h=8), sm8[:, 24:32].bc(2, 64), ALU.mult)
        P.tt("dve", tmpR, tmpR, tmpR2, ALU.add)
        P.tt("pool", yr[i % 2], tmpR, gv, ALU.mult)
        P.dma("sp", ycat_s[i * 128:(i + 1) * 128, 0:512], yr[i % 2], "yrs%d_%d" % (T["id"], i % 2))
        if i == 0:
            tap("yr", yr[0], [128, 512])
        if i == 1:
            tap("yr1", yr[1], [128, 512])

    def run_streams(tile_fn, streams, offset=0):
        ns = min(len(streams), NSEQ)

        def chain(q):
            for b_ in range(q, NSEQ, ns):
                for it_ in range(NT):
                    yield from tile_fn(b_ * NT + it_, streams[q])
                    yield

        gens = [chain(q) for q in range(ns)]
        for q, g_ in enumerate(gens):
            for _ in range(q * offset):
                next(g_, None)
        live = list(gens)
        while live:
            nxt = []
            for g_ in live:
                try:
                    next(g_)
                    nxt.append(g_)
                except StopIteration:
                    pass
            live = nxt

    run_streams(b1_tile, B1S, offset=0)
    P.end_phase()
    if stop_after <= 2:
        P.finish()
        return nc, tapd

    P.begin_phase()
    hnw_bc = P.tile("hnw_bc", [128, 512])
    cvw_bc = P.tile("cvw_bc", [128, 4, 512])
    cvb_bc = P.tile("cvb_bc", [128, 512])
    gb_bc = P.tile("gb_bc", [128, 8])
    bcload(hnw_bc, dr["mlstm_hn_w"][0:1, :], 128, "c6")
    for j in range(4):
        bcload(cvw_bc[:, j, :], dr["mlstm_conv_w"][j:j + 1, :], 128, "c7")
    bcload(cvb_bc, dr["mlstm_conv_b"][0:1, :], 128, "c8")
    bcload(gb_bc[:, 0:4], dr["mlstm_i_b"][0:1, :], 128, "c9")
    bcload(gb_bc[:, 4:8], dr["mlstm_f_b"][0:1, :], 128, "c9")
    ones_bf = P.tile("ones_bf", [128, 1], BF16)

    def alloc_b2(q):
        sx = "_m%d" % q
        T = {"id": q}
        T["qk4"] = P.tile("qk4" + sx, [128, 4, 512])
        T["mr"] = P.tile("mr" + sx, [128, 1032])
        for n in ("cacc", "ctmp", "slu", "og"):
            T[n] = P.tile(n + sx, [128, 512])
        T["qq"] = P.tile("qq" + sx, [128, 768], BF16)
        T["qkT"] = P.tile("qkT" + sx, [128, 12, 128], BF16)
        T["g8"] = P.tile("g8" + sx, [128, 64])
        T["sm4"] = P.tile("sm4" + sx, [128, 16])
        T["lfb"] = P.tile("lfb" + sx, [128, 4, 128])
        T["DTm"] = P.tile("DTm" + sx, [128, 4, 128])
        T["PTm"] = P.tile("PTm" + sx, [128, 4, 128], BF16)
        T["Vb"] = P.tile("Vb" + sx, [128, 4, 128], BF16)
        T["Kw"] = P.tile("Kw" + sx, [128, 4, 64], BF16)
        T["Cst"] = P.tile("Cst" + sx, [128, 4, 128])
        T["nst"] = P.tile("nst" + sx, [128, 4])
        T["C_bf"] = P.tile("C_bf" + sx, [128, 4, 128], BF16)
        T["n_bf"] = P.tile("n_bf" + sx, [128, 4], BF16)
        T["hm"] = P.tile("hm" + sx, [128, 4, 128])
        T["ym"] = [P.tile("ym%d%s" % (i, sx), [128, 512], BF16) for i in range(2)]
        return T

    NSTR2 = 4 if NSEQ % 4 == 0 else NSTR
    B2S = [alloc_b2(q) for q in range(NSTR2)]
    ptbM = [P.psum("ptbM%d" % i, [128, 1024], BF16) for i in range(2)]
    pfm = [P.psum("pfM%d" % i, [128, 512]) for i in range(5)]
    pmi = [0]

    def bankm():
        b_ = pfm[pmi[0] % 5]
        pmi[0] += 1
        return b_

    P.memset("pool", ones_bf, 1.0)
    H4 = "p (h c) -> p h c"
    def b2_tile(i, T):
        qk4 = T["qk4"]
        mr = T["mr"]
        cacc = T["cacc"]
        ctmp = T["ctmp"]
        slu = T["slu"]
        qq = T["qq"]
        qkT = T["qkT"]
        g8 = T["g8"]
        sm4 = T["sm4"]
        lfb = T["lfb"]
        DTm = T["DTm"]
        PTm = T["PTm"]
        Vb = T["Vb"]
        Kw = T["Kw"]
        Cst = T["Cst"]
        nst = T["nst"]
        C_bf = T["C_bf"]
        n_bf = T["n_bf"]
        hm = T["hm"]
        og = T["og"]
        ym = T["ym"]
        b = i // NT
        it = i % NT
        r0 = b * PADR + 3 + it * 128
        for j in range(4):
            P.dma("sp", qk4[:, j, :], proj_s[r0 - 3 + j:r0 + 125 + j, RW:RW + 512], "qk4%d" % T["id"])
        P.dma("sp", mr, proj_s[r0:r0 + 128, RW + 512:INC], "mr%d" % T["id"])
        if it == 0:
            P.memset("pool", Cst, 0.0)
            P.memset("pool", nst, 0.0)
            P.memset("pool", C_bf, 0.0)
            P.memset("pool", n_bf, 0.0)
        q4 = qk4
        cparts = [cacc, ctmp, og, hm.re("p h v -> p (h v)")]
        for j in range(4):
            P.tt("dve" if j % 2 == 0 else "pool", cparts[j], q4[:, j, :], cvw_bc[:, j, :], ALU.mult)
        for j in range(1, 4):
            P.tt("dve", cacc, cacc, cparts[j], ALU.add)
        P.tt("dve", cacc, cacc, cvb_bc, ALU.add)
        P.act(slu, cacc, AF.Silu)
        m = mr
        P.act(og, m[:, 512:1024], AF.Tanh, scale=0.5)
        yield
        P.tt("dve", g8[:, 0:8], m[:, 1024:1032], gb_bc, ALU.add)
        P.act(g8[:, 8:16], g8[:, 0:8], AF.Exp, scale=2.0 / 15.0)
        P.ts("dve", g8[:, 8:16], g8[:, 8:16], 1.0, ALU.add)
        recip("dve", g8[:, 8:16], g8[:, 8:16])
        P.ts("dve", g8[:, 8:16], g8[:, 8:16], -30.0, ALU.mult, 15.0, ALU.add)
        P.act(g8[:, 16:20], g8[:, 12:16], AF.Exp, scale=-1.0)
        P.act(g8[:, 20:24], g8[:, 16:20], AF.Ln, bias=1.0, scale=1.0)
        P.ts("dve", g8[:, 24:28], g8[:, 20:24], -1.0, ALU.mult)
        lf = g8[:, 24:28]
        yield
        psg = bankm()
        P.mm(psg[:, 0:4], tri, lf)
        P.mm(psg[:, 4:8], onesf, lf)
        P.copy("dve", g8[:, 28:36], psg[:, 0:8])
        bt = g8[:, 28:32]; bL = g8[:, 32:36]
        P.tt("dve", g8[:, 36:40], g8[:, 8:12], bt, ALU.subtract)
        P.act(g8[:, 40:44], bt, AF.Exp)
        P.tt("dve", g8[:, 44:48], g8[:, 36:40], bL, ALU.add)
        P.act(g8[:, 44:48], g8[:, 44:48], AF.Exp)
        P.act(g8[:, 48:52], bL, AF.Exp)
        yield
        P.copy("pool", lfb, lf.bc(2, 128))
        P.ts("dve", qq[:, 0:256], slu[:, 0:256], 0.125, ALU.mult)
        P.copy("pool", qq[:, 256:512], slu[:, 256:512])
        P.stt("dve", qq[:, 512:768].re(H4, h=4), slu[:, 0:256].re(H4, h=4), 0.125, g8[:, 40:44].bc(2, 64), ALU.mult, ALU.mult)
        yield
        for n in range(12):
            pb_ = ptbM[0] if n < 8 else ptbM[1]
            nn = n % 8
            P.tr(pb_[0:64, nn * 128:(nn + 1) * 128], qq[:, n * 64:(n + 1) * 64], ident)
        P.copy("act", qkT[0:64, 0:8, :], ptbM[0][0:64, :].re("p (n t) -> p n t", n=8))
        P.copy("dve", qkT[0:64, 8:12, :], ptbM[1][0:64, 0:512].re("p (n t) -> p n t", n=4))
        P.copy("act", Vb, m[:, 0:512].re("p (h v) -> p h v", h=4))
        P.tt("pool", Kw, slu[:, 256:512].re(H4, h=4), g8[:, 44:48].bc(2, 64), ALU.mult)
        yield
        pE = bankm(); pS_ = bankm(); pN = bankm(); pCm = bankm(); pdn = bankm()
        for h in range(4):
            P.mm(pE[:, h * 128:(h + 1) * 128], lfb[:, h, :], tri, start=True, stop=False)
            P.mm(pE[:, h * 128:(h + 1) * 128], identf, masknegf, start=False, stop=True)
        for h in range(4):
            P.mm(pS_[:, h * 128:(h + 1) * 128], qkT[0:64, 4 + h, :], qkT[0:64, h, :])
        for h in range(4):
            P.act(DTm[:, h, :], pE[:, h * 128:(h + 1) * 128], AF.Exp, bias=g8[:, 36 + h:37 + h], scale=1.0)
        P.tt("dve", PTm, pS_[:, :].re("p (h t) -> p h t", h=4), DTm, ALU.mult)
        for h in range(4):
            P.mm(pN[:, h * 128:(h + 1) * 128], PTm[:, h, :], Vb[:, h, :], start=True, stop=False)
            P.mm(pN[:, h * 128:(h + 1) * 128], qkT[0:64, 8 + h, :], C_bf[0:64, h, :], start=False, stop=True)
            P.mm(pdn[:, h:h + 1], PTm[:, h, :], ones_bf[:, 0:1], start=True, stop=False)
            P.mm(pdn[:, h:h + 1], qkT[0:64, 8 + h, :], n_bf[0:64, h:h + 1], start=False, stop=True)
            P.mm(pCm[0:64, h * 128:(h + 1) * 128], Kw[:, h, :], Vb[:, h, :])
            P.mm(pdn[0:64, 8 + h:9 + h], Kw[:, h, :], ones_bf[:, 0:1])
        P.copy("dve", g8[:, 52:56], pdn[:, 0:4])
        P.stt("dve", g8[:, 56:60], g8[:, 52:56], -1.0, g8[:, 52:56], ALU.mult, ALU.max)
        P.ts("dve", g8[:, 56:60], g8[:, 56:60], 1.0, ALU.max)
        recip("dve", g8[:, 60:64], g8[:, 56:60])
        P.tt("dve", hm, pN[:, :].re("p (h v) -> p h v", h=4), g8[:, 60:64].bc(2, 128), ALU.mult)
        if i <= 1:
            tap("h_mlstm%d" % i, hm, [128, 4, 128])
        P.tt("pool", Cst[0:64], Cst[0:64], g8[0:64, 48:52].bc(2, 128), ALU.mult)
        P.tt("dve", Cst[0:64], Cst[0:64], pCm[0:64, :].re("p (h v) -> p h v", h=4), ALU.add)
        P.tt("pool", nst[0:64], nst[0:64], g8[0:64, 48:52], ALU.mult)
        P.tt("dve", nst[0:64], nst[0:64], pdn[0:64, 8:12], ALU.add)
        P.copy("pool", C_bf[0:64], Cst[0:64])
        P.copy("pool", n_bf[0:64], nst[0:64])
        yield
        hflat = hm.re("p h v -> p (h v)")
        P.tt("pool", ctmp.re("p (h v) -> p h v", h=4), hm, hm, ALU.mult)
        P.red("dve", sm4[:, 0:4], ctmp.re("p (h v) -> p h v", h=4), ALU.add)
        P.ts("dve", sm4[:, 0:4], sm4[:, 0:4], 1.0 / 128, ALU.mult)
        P.act(sm4[:, 4:8], sm4[:, 0:4], AF.Ln, bias=epsM[:, 0:1], scale=1.0)
        P.act(sm4[:, 8:12], sm4[:, 4:8], AF.Exp, scale=-0.5)
        P.ts("dve", sm4[:, 8:12], sm4[:, 8:12], 0.5, ALU.mult)
        P.tt("dve", hm, hm, sm4[:, 8:12].bc(2, 128), ALU.mult)
        P.tt("pool", hflat, hflat, hnw_bc, ALU.mult)
        P.stt("dve", ym[i % 2], og, 1.0, hflat, ALU.add, ALU.mult)
        P.dma("sp", ycat_s[i * 128:(i + 1) * 128, 512:1024], ym[i % 2], "yms%d_%d" % (T["id"], i % 2))
        if i == 0:
            tap("ym", ym[0], [128, 512])
        if i == 1:
            tap("ym1", ym[1], [128, 512])

    run_streams(b2_tile, B2S, offset=2)
    P.end_phase()
    if stop_after <= 3:
        P.finish()
        return nc, tapd

    P.begin_phase()
    Wout = P.tile("Wout", [128, 8, D], BF16)
    Wgr = P.tile("Wgr", [128, 8, 36], BF16)
    brt_bc = P.tile("brt_bc", [128, 36])
    stg3 = [P.tile("stg3_%d" % i, [128, D]) for i in range(2)]
    for k in range(8):
        s = stg3[k % 2]
        P.dma("sp", s, dr["w_out"][k * 128:(k + 1) * 128, :], "stg3_%d" % (k % 2))
        P.copy(ceng[k % 3], Wout[:, k, :], s)
    s = P.tile("stg3r", [128, 288])
    P.dma("sp", s[:, 0:32].re("p (k g) -> p k g", k=8), dr["moe_w_group"].rearrange("(k p) g -> p k g", p=128), "stg3r")
    P.dma("sp", s[:, 32:288].re("p (k g) -> p k g", k=8), dr["moe_w_router"].rearrange("(k p) g -> p k g", p=128), "stg3r")
    P.copy("act", Wgr[:, :, 0:4], s[:, 0:32].re("p (k g) -> p k g", k=8))
    P.copy("act", Wgr[:, :, 4:36], s[:, 32:288].re("p (k g) -> p k g", k=8))
    bcload(brt_bc[:, 0:4], dr["moe_b_group"][0:1, :], 128, "c10")
    bcload(brt_bc[:, 4:36], dr["moe_b_router"][0:1, :], 128, "c10")
    def alloc_b3(q):
        sx = "_o%d" % q
        T = {"id": q}
        T["ycat"] = P.tile("ycat" + sx, [128, D], BF16)
        T["xB"] = P.tile("xB" + sx, [128, D])
        T["ycT"] = P.tile("ycT" + sx, [128, 8, 128], BF16)
        T["x1"] = P.tile("x1" + sx, [128, D])
        T["xn2"] = P.tile("xn2" + sx, [128, D], BF16)
        T["junkB"] = P.tile("junkB" + sx, [128, D], BF16)
        T["h2T"] = P.tile("h2T" + sx, [128, 8, 128], BF16)
        T["h2Tk"] = [T["h2T"][:, k, :].sub("h2T%s_%d" % (sx, k)) for k in range(8)]
        T["lg"] = P.tile("lg" + sx, [128, 36])
        T["r8"] = P.tile("r8" + sx, [128, 96])
        T["s16"] = P.tile("s16" + sx, [128, 16])
        T["cwt"] = P.tile("cwt" + sx, [128, 4, 8])
        T["gm_bc"] = P.tile("gm_bc" + sx, [128, D])
        return T

    B3S = [alloc_b3(q) for q in range(NSTR2)]
    ptb3 = [P.psum("ptb3_%d" % i, [128, 1024], BF16) for i in range(2)]
    pf3 = [P.psum("pf3_%d" % i, [128, 512]) for i in range(5)]
    p3i = [0]

    def bank3():
        b_ = pf3[p3i[0] % 5]
        p3i[0] += 1
        return b_

    def b3_tile(i, T):
        ycat = T["ycat"]
        xB = T["xB"]
        ycT = T["ycT"]
        x1 = T["x1"]
        xn2 = T["xn2"]
        junkB = T["junkB"]
        h2T = T["h2T"]
        h2Tk = T["h2Tk"]
        lg = T["lg"]
        r8 = T["r8"]
        s16 = T["s16"]
        cwt = T["cwt"]
        gm_bc = T["gm_bc"]
        b = i // NT
        it = i % NT
        P.dma("sp", ycat, ycat_s[i * 128:(i + 1) * 128, :], "ycl%d" % T["id"])
        P.dma("sp", xB, dr["x"][i * 128:(i + 1) * 128, :], "xB%d" % T["id"])
        if it == 0:
            P.dma("sp", gm_bc, mod_s[b:b + 1, 2048:3072].partition_broadcast(128), "gmbc%d" % T["id"])
        for k in range(8):
            P.tr(ptb3[0][:, k * 128:(k + 1) * 128], ycat[:, k * 128:(k + 1) * 128], ident)
        P.copy("dve", ycT, ptb3[0][:, :].re("p (k t) -> p k t", k=8))
        yield
        x1t = x1
        for cb in range(2):
            po_ = bank3()
            for k in range(8):
                P.mm(po_, ycT[:, k, :], Wout[:, k, cb * 512:(cb + 1) * 512], start=(k == 0), stop=(k == 7))
            P.tt("dve", x1t[:, cb * 512:(cb + 1) * 512], po_, gm_bc[:, cb * 512:(cb + 1) * 512], ALU.mult)
        P.tt("pool", x1t, x1t, xB, ALU.add)
        P.dma("sp", x1_s[i * 128:(i + 1) * 128, :], x1t, "x1s%d" % T["id"])
        if i == 0:
            tap("x1", x1t, [128, D])
        if i == 1:
            tap("x1b", x1t, [128, D])
        yield
        P.act(junkB, x1t, AF.Square, accum=s16[:, 0:1])
        P.ts("dve", s16[:, 1:2], s16[:, 0:1], 1.0 / D, ALU.mult)
        P.act(s16[:, 2:3], s16[:, 1:2], AF.Ln, bias=epsM[:, 0:1], scale=1.0)
        P.act(s16[:, 3:4], s16[:, 2:3], AF.Exp, scale=-0.5)
        P.act(xn2, x1t, AF.Copy, scale=s16[:, 3:4])
        for k in range(8):
            P.tr(ptb3[1][:, k * 128:(k + 1) * 128], xn2[:, k * 128:(k + 1) * 128], ident)
        h2 = h2T
        h2k = h2Tk
        for k in range(8):
            P.act(h2k[k], ptb3[1][:, k * 128:(k + 1) * 128], AF.Identity, bias=shF[:, b, k:k + 1], scale=gamF[:, b, k:k + 1])
        P.add("sp", (lambda o_, i_: (lambda e: e.dma_start(out=o_, in_=i_)))(h2T_s[:, :, i * 128:(i + 1) * 128], h2.ap), h2k, [], dma_key="h2s%d" % T["id"])
        yield
        pr = bank3()
        for k in range(8):
            P.mm(pr[:, 0:36], h2k[k], Wgr[:, k, :], start=(k == 0), stop=(k == 7))
        P.tt("dve", lg, pr[:, 0:36], brt_bc, ALU.add)
        yield
        P.red("dve", r8[:, 0:1], lg[:, 0:4], ALU.max)
        P.ts("dve", r8[:, 1:5], lg[:, 0:4], r8[:, 0:1], ALU.is_equal)
        P.ts("dve", r8[:, 5:6], r8[:, 0:1], -1.0, ALU.mult)
        P.act(r8[:, 6:10], lg[:, 0:4], AF.Exp, bias=r8[:, 5:6], scale=1.0, accum=r8[:, 10:11])
        recip("dve", r8[:, 11:12], r8[:, 10:11])
        P.tt("dve", r8[:, 16:48].re("p (g e) -> p g e", g=4), lg[:, 4:36].re("p (g e) -> p g e", g=4), r8[:, 1:5].bc(2, 8), ALU.mult)
        P.red("dve", r8[:, 48:56], r8[:, 16:48].re("p (g e) -> p e g", g=4), ALU.add)
        P.red("dve", r8[:, 56:57], r8[:, 48:56], ALU.max)
        P.ts("dve", r8[:, 64:72], r8[:, 48:56], r8[:, 56:57], ALU.is_equal)
        P.stt("dve", r8[:, 72:80], r8[:, 64:72], -1e30, r8[:, 48:56], ALU.mult, ALU.add)
        P.red("dve", r8[:, 57:58], r8[:, 72:80], ALU.max)
        P.ts("dve", r8[:, 80:88], r8[:, 72:80], r8[:, 57:58], ALU.is_equal)
        P.tt("dve", r8[:, 58:59], r8[:, 56:57], r8[:, 57:58], ALU.subtract)
        P.act(r8[:, 60:61], r8[:, 58:59], AF.Exp, scale=-1.0)
        P.ts("dve", r8[:, 59:60], r8[:, 60:61], 1.0, ALU.add)
        recip("dve", r8[:, 59:60], r8[:, 59:60])
        P.tt("dve", r8[:, 60:61], r8[:, 60:61], r8[:, 59:60], ALU.mult)
        P.ts("dve", r8[:, 64:72], r8[:, 64:72], r8[:, 59:60], ALU.mult)
        P.stt("dve", r8[:, 64:72], r8[:, 80:88], r8[:, 60:61], r8[:, 64:72], ALU.mult, ALU.add)
        P.ts("dve", r8[:, 64:72], r8[:, 64:72], r8[:, 11:12], ALU.mult)
        cw_t = cwt
        P.tt("dve", cw_t, r8[:, 1:5].bc(2, 8), r8[:, 64:72].bc(1, 4), ALU.mult)
        P.dma("sp", cw_s[i * 128:(i + 1) * 128, :], cw_t.re("p g e -> p (g e)"), "cws%d" % T["id"])
        if i == 0:
            tap("cw", cw_t, [128, 4, 8])

    run_streams(b3_tile, B3S, offset=0)
    P.end_phase()
    if stop_after <= 4:
        P.finish()
        return nc, tapd

    P.begin_phase()
    BLK = min(1024, SEQ)
    SUB = min(512, BLK)
    NB = NTOK // BLK
    TPB = BLK // 128
    fnw_bc = P.tile("fnw_bc", [128, D])
    bcload(fnw_bc, dr["final_norm_w"][0:1, :], 128, "c11")
    h2bs = [P.tile("h2b%d" % i, [128, 8, BLK], BF16) for i in range(2)]
    cwbs = [P.tile("cwb%d" % i, [128, TPB, NE]) for i in range(2)]
    yaccss = [[P.tile("yacc%d_%d" % (j, i), [128, TPB, 512]) for i in range(2)] for j in range(2)]
    gf_bcs = [P.tile("gf_bc%d" % i, [128, D]) for i in range(2)]
    wgb = [P.tile("wgb%d" % i, [128, 8, DE], BF16) for i in range(2)]
    wub = [P.tile("wub%d" % i, [128, 8, DE], BF16) for i in range(2)]
    wdb = [P.tile("wdb%d" % i, [128, 2, D], BF16) for i in range(2)]
    sg = [P.tile("sg%d" % i, [128, SUB]) for i in range(2)]
    actT = [[P.tile("actT%d_%d" % (s_, f), [128, SUB], BF16) for f in range(2)] for s_ in range(2)]
    x1c = [P.tile("x1c%d" % i, [128, D]) for i in range(2)]
    junkC = P.tile("junkC", [128, D], BF16)
    pG = [P.psum("pG%d" % i, [128, 512]) for i in range(2)]
    pUu = [P.psum("pU%d" % i, [128, 512]) for i in range(2)]
    pD = [P.psum("pD%d" % i, [128, 512]) for i in range(3)]
    pdi = [0]
    wcnt = [0]

    obig = P.tile("obig", [128, TPB, D])
    obt = [obig[:, ti, :].sub("obig_%d" % ti) for ti in range(TPB)]
    ssq = P.tile("ssqC", [128, 4, TPB])

    def epilogue(blk, slot):
        for ti in range(TPB):
            gi = blk * TPB + ti
            sl = gi % 2
            P.dma("sp", x1c[sl], x1_s[gi * 128:(gi + 1) * 128, :], "x1c%d" % sl)
            o = obt[ti]
            for cb in range(2):
                P.tt("dve", o[:, cb * 512:(cb + 1) * 512], yaccss[slot][cb][:, ti, :], gf_bcs[slot][:, cb * 512:(cb + 1) * 512], ALU.mult)
            P.tt("dve", o, o, x1c[sl], ALU.add)
            P.act(junkC, o, AF.Square, accum=ssq[:, 0, ti:ti + 1])
        P.ts("dve", ssq[:, 1, :], ssq[:, 0, :], 1.0 / D, ALU.mult)
        P.act(ssq[:, 2, :], ssq[:, 1, :], AF.Sqrt, bias=epsM[:, 0:1], scale=1.0)
        recip("dve", ssq[:, 3, :], ssq[:, 2, :])
        for ti in range(TPB):
            gi = blk * TPB + ti
            o = obt[ti]
            P.stt("dve", o, o, ssq[:, 3, ti:ti + 1], fnw_bc, ALU.mult, ALU.mult)
            P.dma("sp", out_d[gi * 128:(gi + 1) * 128, :], o, "outC%d" % (ti % 4))

    for blk in range(NB):
        t0 = blk * BLK
        b = t0 // SEQ
        slot = blk % 2
        h2b = h2bs[slot]
        cwb = cwbs[slot]
        yaccs = yaccss[slot]
        def blk_loads(bk):
            t0_ = bk * BLK
            sl_ = bk % 2
            P.dma("sp", h2bs[sl_], h2T_s[:, :, t0_:t0_ + BLK], "h2b%d" % sl_)
            P.dma("sp", cwbs[sl_], cw_s[t0_:t0_ + BLK, :].rearrange("(n p) e -> p n e", p=128), "cwb%d" % sl_)
            P.dma("sp", gf_bcs[sl_], mod_s[t0_ // SEQ:t0_ // SEQ + 1, 5120:6144].partition_broadcast(128), "gfbc%d" % sl_)

        if blk == 0:
            blk_loads(0)
        NSB = BLK // SUB
        units = [(e, sb, (e * NSB + sb) % 2) for e in range(NE) for sb in range(NSB)]

        def c_loads(e):
            ws = wcnt[0] % 2
            wcnt[0] += 1
            P.dma("sp", wgb[ws], wg_s[e].rearrange("p (k f) -> p k f", k=8), "wgb%d" % ws)
            P.dma("sp", wub[ws], wu_s[e].rearrange("p (k f) -> p k f", k=8), "wub%d" % ws)
            P.dma("sp", wdb[ws], wd_s[e].rearrange("p (c d) -> p c d", c=2), "wdb%d" % ws)
            return ws

        wslot = {}

        def c_gu(u, fc):
            e, sb, asl = u
            ws = wslot[e]
            for k in range(8):
                P.mm(pG[fc][:, 0:SUB], wgb[ws][:, k, fc * 128:(fc + 1) * 128], h2b[:, k, sb * SUB:(sb + 1) * SUB], start=(k == 0), stop=(k == 7))
            for k in range(8):
                P.mm(pUu[fc][:, 0:SUB], wub[ws][:, k, fc * 128:(fc + 1) * 128], h2b[:, k, sb * SUB:(sb + 1) * SUB], start=(k == 0), stop=(k == 7))
            P.act(sg[fc], pG[fc][:, 0:SUB], AF.Silu)
            P.tt("dve", actT[asl][fc], pUu[fc][:, 0:SUB], sg[fc], ALU.mult)

        def c_down(u):
            e, sb, asl = u
            ws = wslot[e]
            for tt_ in range(SUB // 128):
                ti = sb * (SUB // 128) + tt_
                for cb in range(2):
                    pd_ = pD[pdi[0] % 3]; pdi[0] += 1
                    for fc in range(2):
                        P.mm(pd_, actT[asl][fc][:, tt_ * 128:(tt_ + 1) * 128], wdb[ws][:, fc, cb * 512:(cb + 1) * 512], start=(fc == 0), stop=(fc == 1))
                    ysl = yaccs[cb][:, ti, :]
                    if e == 0:
                        P.ts("dve", ysl, pd_, cwb[:, ti, e:e + 1], ALU.mult)
                    else:
                        P.stt("dve", ysl, pd_, cwb[:, ti, e:e + 1], ysl, ALU.mult, ALU.add)

        wslot[0] = c_loads(0)
        c_gu(units[0], 0)
        c_gu(units[0], 1)
        for n in range(len(units)):
            if n + 1 < len(units):
                if units[n + 1][1] == 0:
                    wslot[units[n + 1][0]] = c_loads(units[n + 1][0])
                c_gu(units[n + 1], 0)
            c_down(units[n])
            if n + 1 < len(units):
                c_gu(units[n + 1], 1)
            if n == len(units) // 2 and blk + 1 < NB:
                blk_loads(blk + 1)
        epilogue(blk, slot)
    P.end_phase()
    P.finish()
    return nc, tapd


_NC_CACHE = {}


def kernel(**inputs):
    NCORES = 8
    x = np.asarray(inputs["x"], dtype=np.float32)
    B, S, _ = x.shape
    NSEQ = B // NCORES
    key = (NSEQ, S)
    if key not in _NC_CACHE:
        _NC_CACHE[key] = build(NSEQ, S)[0]
    nc = _NC_CACHE[key]
    shared = {}
    for k, shp in PARAM_SHAPES.items():
        shared[k] = np.ascontiguousarray(np.asarray(inputs[k], dtype=np.float32).reshape(shp))
    c = np.asarray(inputs["c"], dtype=np.float32)
    in_maps = []
    for i in range(NCORES):
        m = dict(shared)
        m["x"] = np.ascontiguousarray(x[i * NSEQ:(i + 1) * NSEQ].reshape(NSEQ * S, D))
        m["c"] = np.ascontiguousarray(c[i * NSEQ:(i + 1) * NSEQ])
        in_maps.append(m)
    res = run_bass_kernel_spmd(nc, in_maps, core_ids=list(range(NCORES)))
    outs = [np.asarray(r["out"]).reshape(NSEQ, S, D) for r in res.results]
    return np.concatenate(outs, axis=0).astype(np.float32)
```

```python
import numpy as np
from concourse.bass_utils import run_bass_kernel_spmd
import numpy as np
from contextlib import ExitStack
import concourse.bass as bass
import concourse.mybir as mybir

F32 = mybir.dt.float32
BF16 = mybir.dt.bfloat16
AF = mybir.ActivationFunctionType
ALU = mybir.AluOpType
AX = mybir.AxisListType

ENGS = ("pe", "act", "dve", "pool", "sp")
SEM_CAP = 30000


class Buf:
    __slots__ = ("name", "writers", "readers", "psum")

    def __init__(self, name, psum=False):
        self.name = name
        self.writers = {}
        self.readers = []
        self.psum = psum


class V:
    __slots__ = ("ap", "buf")

    def __init__(self, ap, buf):
        self.ap = ap
        self.buf = buf

    def __getitem__(self, k):
        return V(self.ap[k], self.buf)

    def re(self, pat, **kw):
        return V(self.ap.rearrange(pat, **kw), self.buf)

    def bc(self, axis, n):
        a = self.ap.unsqueeze(axis)
        shp = list(a.shape)
        shp[axis] = n
        return V(a.to_broadcast(shp), self.buf)

    def sub(self, name):
        return V(self.ap, Buf(name))


class Op:
    __slots__ = ("eng", "fn", "idx", "eidx", "dma_key", "deps", "signals", "sigval", "semi", "extra", "phase")

    def __init__(self, eng, fn, dma_key):
        self.eng = eng
        self.fn = fn
        self.dma_key = dma_key
        self.deps = []
        self.signals = False
        self.sigval = 0
        self.semi = 0
        self.extra = []


def _bufs(vs):
    out = []
    for v in vs:
        if isinstance(v, V):
            out.append(v.buf)
        elif isinstance(v, Buf):
            out.append(v)
    return out


class Prog:
    def __init__(self, nc):
        self.nc = nc
        self.ops = []
        self.eops = {e: [] for e in ENGS}
        self.gstack = ExitStack()
        self.pstack = None
        self.cnt = {e: 0 for e in ENGS}
        self.dcnt = {}
        self.esems = {e: [] for e in ENGS}
        self.dsems = {}
        self.free_dsems = []
        self.waited = {e: {} for e in ENGS}
        self.carry = []
        self.nops_total = 0
        self.phase = 0

    def _stack(self):
        return self.pstack if self.pstack is not None else self.gstack

    def tile(self, name, shape, dt=F32):
        t = self._stack().enter_context(self.nc.sbuf_tensor(name, list(shape), dt))
        return V(t[:], Buf(name))

    def psum(self, name, shape, dt=F32):
        t = self._stack().enter_context(self.nc.psum_tensor(name, list(shape), dt))
        return V(t[:], Buf(name, psum=True))

    def begin_phase(self):
        self.pstack = ExitStack()
        self.ops = []
        self.eops = {e: [] for e in ENGS}

    def add(self, eng, fn, reads=(), writes=(), dma_key=None):
        op = Op(eng, fn, dma_key)
        op.idx = len(self.ops)
        op.phase = self.phase
        op.eidx = len(self.eops[eng])
        deps = {}
        wkey = ("dma", dma_key) if dma_key is not None else eng
        rb = _bufs(reads)
        wb = _bufs(writes)
        for b in rb:
            for w in b.writers.values():
                deps[id(w)] = w
            if b.psum:
                for r in b.readers:
                    if r.eng != eng:
                        deps[id(r)] = r
        for b in wb:
            for w in b.writers.values():
                deps[id(w)] = w
            for r in b.readers:
                deps[id(r)] = r
        deps.pop(id(op), None)
        op.deps = list(deps.values())
        for b in rb:
            b.readers.append(op)
        for b in wb:
            b.writers[wkey] = op
            b.readers = []
        self.ops.append(op)
        self.eops[eng].append(op)
        return op

    def mm(self, out, lhsT, rhs, start=True, stop=True, extra_r=()):
        return self.add("pe", lambda e: e.matmul(out.ap, lhsT=lhsT.ap, rhs=rhs.ap, start=start, stop=stop),
                        [lhsT, rhs] + list(extra_r), [out])

    def tr(self, out, in_, ident):
        return self.add("pe", lambda e: e.transpose(out.ap, in_.ap, ident.ap), [in_, ident], [out])

    def act(self, out, in_, func, bias=None, scale=None, accum=None, eng="act"):
        kw = {}
        r = [in_]
        if bias is not None:
            kw["bias"] = bias.ap if isinstance(bias, V) else bias
            if isinstance(bias, V):
                r.append(bias)
        if scale is not None:
            kw["scale"] = scale.ap if isinstance(scale, V) else scale
            if isinstance(scale, V):
                r.append(scale)
        w = [out]
        if accum is not None:
            kw["accum_out"] = accum.ap
            w.append(accum)
        return self.add(eng, lambda e: e.activation(out=out.ap, in_=in_.ap, func=func, **kw), r, w)

    def tt(self, eng, out, in0, in1, op):
        return self.add(eng, lambda e: e.tensor_tensor(out=out.ap, in0=in0.ap, in1=in1.ap, op=op), [in0, in1], [out])

    def ts(self, eng, out, in0, s1, op0, s2=None, op1=None, accum=None):
        r = [in0]
        a1 = s1.ap if isinstance(s1, V) else s1
        a2 = s2.ap if isinstance(s2, V) else s2
        if isinstance(s1, V):
            r.append(s1)
        if isinstance(s2, V):
            r.append(s2)
        w = [out]
        kw = {}
        if op1 is not None:
            kw["op1"] = op1
        if accum is not None:
            kw["accum_out"] = accum.ap
            w.append(accum)
        return self.add(eng, lambda e: e.tensor_scalar(out=out.ap, in0=in0.ap, scalar1=a1, scalar2=a2, op0=op0, **kw), r, w)

    def stt(self, eng, out, in0, scalar, in1, op0, op1):
        eng = "dve"
        r = [in0, in1]
        a = scalar.ap if isinstance(scalar, V) else scalar
        if isinstance(scalar, V):
            r.append(scalar)
        return self.add(eng, lambda e: e.scalar_tensor_tensor(out=out.ap, in0=in0.ap, scalar=a, in1=in1.ap, op0=op0, op1=op1), r, [out])

    def copy(self, eng, out, in_):
        if eng == "act":
            return self.add(eng, lambda e: e.copy(out=out.ap, in_=in_.ap), [in_], [out])
        return self.add(eng, lambda e: e.tensor_copy(out=out.ap, in_=in_.ap), [in_], [out])

    def red(self, eng, out, in_, op, axis=AX.X):
        return self.add(eng, lambda e: e.tensor_reduce(out=out.ap, in_=in_.ap, axis=axis, op=op), [in_], [out])

    def memset(self, eng, out, val):
        return self.add(eng, lambda e: e.memset(out.ap, val), [], [out])

    def dma(self, eng, out, in_, key, **kw):
        r = [in_] if isinstance(in_, V) else []
        w = [out] if isinstance(out, V) else []
        oa = out.ap if isinstance(out, V) else out
        ia = in_.ap if isinstance(in_, V) else in_
        return self.add(eng, lambda e: e.dma_start(out=oa, in_=ia, **kw), r, w, dma_key=key)

    def end_phase(self):
        nc = self.nc
        ops = self.ops
        need = {}
        for op in ops:
            wl = []
            for p in op.deps:
                if p.phase != self.phase:
                    continue
                if p.dma_key is not None:
                    wl.append(p)
                elif p.eng != op.eng:
                    p.signals = True
                    wl.append(p)
                else:
                    if op.eng == "pe" and op.dma_key is None:
                        continue
                    if op.dma_key is not None or (op.eidx - p.eidx) <= 4:
                        p.signals = True
                        wl.append(p)
            need[id(op)] = wl
        lastc = {}
        for e in ENGS:
            for op in reversed(self.eops[e]):
                if op.dma_key is None:
                    op.signals = True
                    lastc[e] = op
                    break
        for op in ops:
            if op.dma_key is not None:
                if op.dma_key not in self.dsems:
                    if self.free_dsems:
                        sem_, c0_ = self.free_dsems.pop()
                        self.dsems[op.dma_key] = sem_
                        self.dcnt[op.dma_key] = c0_
                    else:
                        self.dsems[op.dma_key] = self.gstack.enter_context(nc.semaphore("d_%s" % (op.dma_key,)))
                        self.dcnt[op.dma_key] = 0
                self.dcnt[op.dma_key] += 16
                op.sigval = self.dcnt[op.dma_key]
            elif op.signals:
                c = self.cnt[op.eng]
                op.semi = c // SEM_CAP
                op.sigval = c % SEM_CAP + 1
                self.cnt[op.eng] = c + 1
                while len(self.esems[op.eng]) <= op.semi:
                    i = len(self.esems[op.eng])
                    self.esems[op.eng].append(self.gstack.enter_context(nc.semaphore("s_%s_%d" % (op.eng, i))))
        plans = {e: [] for e in ENGS}
        first = {e: True for e in ENGS}
        for op in ops:
            ws = {}
            if first[op.eng]:
                first[op.eng] = False
                for key, sem, v in self.carry:
                    if self.waited[op.eng].get(key, 0) < v:
                        ws[key] = (sem, v)
            for p in need[id(op)]:
                if p.dma_key is not None:
                    sem = self.dsems[p.dma_key]
                    key = ("dsem", id(sem))
                else:
                    key = (p.eng, p.semi)
                    sem = self.esems[p.eng][p.semi]
                v = p.sigval
                if self.waited[op.eng].get(key, 0) >= v:
                    continue
                if key not in ws or ws[key][1] < v:
                    ws[key] = (sem, v)
            for key, (sem, v) in ws.items():
                self.waited[op.eng][key] = v
            if op.dma_key is not None:
                inc = (self.dsems[op.dma_key], 16)
            elif op.signals:
                inc = (self.esems[op.eng][op.semi], 1)
            else:
                inc = None
            plans[op.eng].append((op, list(ws.values()), inc))
        carry = [(("dsem", id(self.dsems[k])), self.dsems[k], v) for k, v in self.dcnt.items()]
        for e, op in lastc.items():
            carry.append(((e, op.semi), self.esems[e][op.semi], op.sigval))
        self.carry = carry + [c for c in self.carry if c[0] not in {x[0] for x in carry}]
        final_waits = [(sem, v) for (_, sem, v) in self.carry]

        def run(engobj, plan, final=None):
            for op, ws, inc in plan:
                for sem, v in ws:
                    engobj.wait_ge(sem, v)
                ins = op.fn(engobj)
                if inc is not None:
                    ins.then_inc(inc[0], inc[1])
            if final:
                for sem, v in final:
                    engobj.wait_ge(sem, v)

        with nc.Block() as block:
            @block.tensor
            def _(e):
                run(e, plans["pe"])

            @block.scalar
            def _(e):
                run(e, plans["act"])

            @block.vector
            def _(e):
                run(e, plans["dve"])

            @block.gpsimd
            def _(e):
                run(e, plans["pool"])

            @block.sync
            def _(e):
                run(e, plans["sp"], final_waits)
        self.nops_total += len(ops)
        self.phase += 1
        for k_ in list(self.dsems):
            self.free_dsems.append((self.dsems.pop(k_), self.dcnt.pop(k_)))
        self.ops = []
        self.eops = {e: [] for e in ENGS}
        if self.pstack is not None:
            self.pstack.close()
            self.pstack = None

    def finish(self):
        self.gstack.close()

D = 1024
INC = 3336
RW = 1792
NE = 32
DE = 256
EPS = 1e-6
GN_EPS = 64e-5
DEC = 0.6065306597126334

PARAM_SHAPES = {
    "ada_w": [1024, 6144], "ada_b": [1, 6144], "mix_norm_w": [1, 1024], "w_in": [1024, 3336],
    "rwkv_mu": [1, 1792], "rwkv_w0": [1, 512], "rwkv_w_up": [64, 512], "rwkv_a0": [1, 512],
    "rwkv_a_up": [64, 512], "rwkv_g_up": [128, 512], "rwkv_k_k": [1, 512], "rwkv_k_a": [1, 512],
    "rwkv_r_k": [1, 512], "rwkv_gn_w": [1, 512], "rwkv_gn_b": [1, 512], "mlstm_conv_w": [4, 512],
    "mlstm_conv_b": [1, 512], "mlstm_i_b": [1, 4], "mlstm_f_b": [1, 4], "mlstm_hn_w": [1, 512],
    "w_out": [1024, 1024], "ffn_norm_w": [1, 1024], "moe_w_group": [1024, 4], "moe_b_group": [1, 4],
    "moe_w_router": [1024, 32], "moe_b_router": [1, 32], "moe_w_gate": [32, 1024, 256],
    "moe_w_up": [32, 1024, 256], "moe_w_down": [32, 256, 1024], "final_norm_w": [1, 1024],
}


def build(NSEQ, SEQ, taps=None, stop_after=99):
    nc = bass.Bass("TRN2", target_bir_lowering=False)
    NT = SEQ // 128
    NTOK = NSEQ * SEQ
    NTILES = NSEQ * NT
    PADR = SEQ + 3
    dr = {}
    dr["x"] = nc.dram_tensor("x", [NTOK, D], F32, kind="ExternalInput").ap()
    dr["c"] = nc.dram_tensor("c", [NSEQ, D], F32, kind="ExternalInput").ap()
    for k, shp in PARAM_SHAPES.items():
        dr[k] = nc.dram_tensor(k, shp, F32, kind="ExternalInput").ap()
    out_d = nc.dram_tensor("out", [NTOK, D], F32, kind="ExternalOutput").ap()
    proj_s = nc.dram_tensor("proj_s", [NSEQ * PADR, INC], F32).ap()
    x1_s = nc.dram_tensor("x1_s", [NTOK, D], F32).ap()
    h2T_s = nc.dram_tensor("h2T_s", [128, 8, NTOK], BF16).ap()
    cw_s = nc.dram_tensor("cw_s", [NTOK, NE], F32).ap()
    mod_s = nc.dram_tensor("mod_s", [NSEQ, 6144], F32).ap()
    wg_s = nc.dram_tensor("wg_s", [NE, 128, 2048], BF16).ap()
    wu_s = nc.dram_tensor("wu_s", [NE, 128, 2048], BF16).ap()
    wd_s = nc.dram_tensor("wd_s", [NE, 128, 2048], BF16).ap()
    tapd = {}

    P = Prog(nc)

    def tap(name, v, shape):
        if taps is None or name not in taps:
            return
        t = nc.dram_tensor("tap_" + name, list(shape), v.ap.dtype, kind="ExternalOutput").ap()
        tapd[name] = t
        P.dma("sp", t, v, "tap_" + name)

    identf = P.tile("identf", [128, 128])
    ident = P.tile("ident", [128, 128], BF16)
    tri = P.tile("tri", [128, 128])
    onesf = P.tile("onesf", [128, 128])
    trimid = P.tile("trimid", [128, 128])
    indmid = P.tile("indmid", [128, 2])
    masknegf = P.tile("masknegf", [128, 128])
    maskA = P.tile("maskA", [128, 4, 128], BF16)
    mSI = P.tile("mSI", [128, 2, 2, 128], BF16)
    epsM = P.tile("epsM", [128, 1])
    epsG = P.tile("epsG", [128, 1])
    gamM = P.tile("gamM", [128, NSEQ, 8])
    shM = P.tile("shM", [128, NSEQ, 8])
    gamF = P.tile("gamF", [128, NSEQ, 8])
    shF = P.tile("shF", [128, NSEQ, 8])

    P.begin_phase()
    P.memset("pool", identf, 1.0)
    P.add("pool", lambda e: e.affine_select(identf.ap, identf.ap, [[-1, 128]], ALU.is_equal, 0.0, base=0, channel_multiplier=1), [identf], [identf])
    P.copy("pool", ident, identf)
    P.memset("pool", tri, 1.0)
    P.add("pool", lambda e: e.affine_select(tri.ap, tri.ap, [[1, 128]], ALU.is_ge, 0.0, base=0, channel_multiplier=-1), [tri], [tri])
    P.memset("pool", onesf, 1.0)
    colm = P.tile("colm", [128, 128])
    P.memset("pool", colm, 1.0)
    P.add("pool", lambda e: e.affine_select(colm.ap, colm.ap, [[0, 128]], ALU.is_ge, 0.0, base=63, channel_multiplier=-1), [colm], [colm])
    P.tt("pool", trimid, tri, colm, ALU.subtract)
    P.ts("pool", trimid, trimid, -DEC, ALU.mult)
    P.ts("pool", indmid[:, 0:1], colm[:, 0:1], -DEC, ALU.mult)
    P.ts("pool", indmid[:, 1:2], colm[:, 0:1], DEC, ALU.mult, -DEC, ALU.add)
    P.memset("pool", masknegf, 0.0)
    P.add("pool", lambda e: e.affine_select(masknegf.ap, masknegf.ap, [[1, 128]], ALU.is_ge, -30000.0, base=0, channel_multiplier=-1), [masknegf], [masknegf])
    mstr = P.tile("mstr", [128, 128])
    P.memset("pool", mstr, 1.0)
    P.add("pool", lambda e: e.affine_select(mstr.ap, mstr.ap, [[1, 128]], ALU.is_ge, 0.0, base=-1, channel_multiplier=-1), [mstr], [mstr])
    mlow = P.tile("mlow", [128, 128])
    P.memset("pool", mlow, 1.0)
    P.add("pool", lambda e: e.affine_select(mlow.ap, mlow.ap, [[-1, 128]], ALU.is_ge, 0.0, base=-1, channel_multiplier=1), [mlow], [mlow])
    for hh in range(4):
        P.copy("pool", maskA[:, hh, :], mlow)
    for hh in range(2):
        P.copy("pool", mSI[:, hh, 0, :], mstr)
        P.copy("pool", mSI[:, hh, 1, :], tri)
    P.memset("pool", epsM, EPS)
    P.memset("pool", epsG, GN_EPS)

    def bcload(dst, src, n, key):
        P.dma("sp", dst, src.partition_broadcast(n), key)

    ct = P.tile("ct", [128, D])
    P.dma("sp", ct[0:NSEQ, :], dr["c"][:, :], "c12")
    sct = P.tile("sct", [128, D])
    P.act(sct[0:NSEQ, :], ct[0:NSEQ, :], AF.Silu)
    psA = P.psum("psA", [128, 512])
    psB = P.psum("psB", [128, 512])
    psC = P.psum("psC", [128, 512])
    scT = P.tile("scT", [128, 8, NSEQ])
    for k in range(8):
        P.mm(psA[:, k * NSEQ:(k + 1) * NSEQ], sct[0:NSEQ, k * 128:(k + 1) * 128], identf[0:NSEQ, 0:NSEQ])
    P.copy("dve", scT, psA[:, 0:8 * NSEQ].re("p (k b) -> p k b", k=8))
    adab = P.tile("adab", [128, 6144])
    P.dma("sp", adab[0:NSEQ, :], dr["ada_b"][0:1, :].partition_broadcast(NSEQ), "c13")
    nrm = P.tile("nrm", [128, 2 * D])
    P.dma("sp", nrm[0:1, 0:D], dr["mix_norm_w"][0:1, :], "c14")
    P.dma("sp", nrm[0:1, D:2 * D], dr["ffn_norm_w"][0:1, :], "c14")
    nrmT = P.tile("nrmT", [128, 16])
    for k in range(16):
        P.mm(psC[:, k:k + 1], nrm[0:1, k * 128:(k + 1) * 128], onesf[0:1, 0:1])
    P.copy("dve", nrmT, psC[:, 0:16])
    adw = [P.tile("adw%d" % i, [128, 8, 512]) for i in range(2)]
    modb = [P.tile("modb%d" % i, [128, 512]) for i in range(2)]
    modT = P.tile("modT", [128, 48, NSEQ])
    for cb in range(12):
        a = adw[cb % 2]
        P.dma("sp", a, dr["ada_w"][:, cb * 512:(cb + 1) * 512].rearrange("(k p) f -> p k f", p=128), "adw%d" % (cb % 2))
        pm = psA if cb % 2 == 0 else psB
        for k in range(8):
            P.mm(pm[0:NSEQ, :], scT[:, k, :], a[:, k, :], start=(k == 0), stop=(k == 7))
        mb = modb[cb % 2]
        P.tt("dve", mb[0:NSEQ, :], pm[0:NSEQ, :], adab[0:NSEQ, cb * 512:(cb + 1) * 512], ALU.add)
        P.dma("sp", mod_s[:, cb * 512:(cb + 1) * 512], mb[0:NSEQ, :], "mods%d" % (cb % 2))
        for q in range(4):
            P.mm(psC[:, 64 + q * NSEQ:64 + (q + 1) * NSEQ], mb[0:NSEQ, q * 128:(q + 1) * 128], identf[0:NSEQ, 0:NSEQ])
        P.copy("act", modT[:, cb * 4:(cb + 1) * 4, :], psC[:, 64:64 + 4 * NSEQ].re("p (q b) -> p q b", q=4))
    for b in range(NSEQ):
        P.stt("dve", gamM[:, b, :], modT[:, 8:16, b], 1.0, nrmT[:, 0:8], ALU.add, ALU.mult)
        P.copy("dve", shM[:, b, :], modT[:, 0:8, b])
        P.stt("dve", gamF[:, b, :], modT[:, 32:40, b], 1.0, nrmT[:, 8:16], ALU.add, ALU.mult)
        P.copy("dve", shF[:, b, :], modT[:, 24:32, b])
    zt = P.tile("zt", [128, 512])
    P.memset("pool", zt, 0.0)
    for b in range(NSEQ):
        P.dma("sp", proj_s[b * PADR:b * PADR + 3, :].rearrange("r (a f) -> (r a) f", f=417), zt[0:24, 0:417], "zpad")
    tap("gamM", gamM, [128, NSEQ, 8])
    tap("shM", shM, [128, NSEQ, 8])
    P.end_phase()
    if stop_after <= 0:
        P.finish()
        return nc, tapd

    ycat_s = nc.dram_tensor("ycat_s", [NTOK, D], BF16).ap()

    def recip(eng, o, a):
        P.add(eng, lambda e: e.reciprocal(out=o.ap, in_=a.ap), [a], [o])

    P.begin_phase()
    Win = P.tile("Win", [128, 8, INC], BF16)
    stg = [P.tile("stg%d" % i, [128, INC]) for i in range(2)]
    ceng = ["act", "dve", "pool"]
    for k in range(8):
        s = stg[k % 2]
        P.dma("sp", s, dr["w_in"][k * 128:(k + 1) * 128, :], "stg%d" % (k % 2))
        P.copy(ceng[k % 3], Win[:, k, :], s)
    xt = [P.tile("xt%d" % i, [128, D]) for i in range(3)]
    junk = P.tile("junkA", [128, D], BF16)
    xn = [P.tile("xn%d" % i, [128, D], BF16) for i in range(2)]
    hT = [P.tile("hT%d" % i, [128, 8, 128], BF16) for i in range(2)]
    hTk = [[hT[i][:, k, :].sub("hT%d_%d" % (i, k)) for k in range(8)] for i in range(2)]
    pj = [P.tile("pj%d" % i, [128, INC]) for i in range(3)]
    pjc = [[pj[i][:, cb * 512:min(INC, (cb + 1) * 512)].sub("pj%d_%d" % (i, cb)) for cb in range(7)] for i in range(3)]
    st = [P.tile("stA%d" % i, [128, 4]) for i in range(2)]
    ptb = [P.psum("ptbA%d" % i, [128, 8, 128], BF16) for i in range(2)]
    pp = [P.psum("ppA%d" % i, [128, 512]) for i in range(5)]
    wst = [P.tile("wst%d" % i, [128, 2048]) for i in range(4)]
    wbt = [P.tile("wbt%d" % i, [128, 2048], BF16) for i in range(4)]
    pc_list = []
    for e in range(NE):
        pc_list.append((dr["moe_w_gate"][e].rearrange("(k p) f -> p k f", p=128), wg_s[e], 8))
        pc_list.append((dr["moe_w_up"][e].rearrange("(k p) f -> p k f", p=128), wu_s[e], 8))
        pc_list.append((dr["moe_w_down"][e].rearrange("(c p) d -> p c d", p=128), wd_s[e], 2))

    def pc_load(n):
        src, dst, a_ = pc_list[n]
        P.dma("pool", wst[n % 4].re("p (a b) -> p a b", a=a_), src, "wst%d" % (n % 4))

    def precast_gen():
        for n in range(min(3, len(pc_list))):
            pc_load(n)
        for n in range(len(pc_list)):
            if n + 3 < len(pc_list):
                pc_load(n + 3)
            P.copy("pool" if n % 2 == 0 else "act", wbt[n % 4], wst[n % 4])
            P.dma("pool", pc_list[n][1], wbt[n % 4], "wbt%d" % (n % 4))
            yield

    pcg = precast_gen()

    def a_load(i):
        P.dma("sp", xt[i % 3], dr["x"][i * 128:(i + 1) * 128, :], "xA%d" % (i % 3))

    def a_front(i):
        b = i // NT
        sl = i % 2
        s4 = st[sl]
        P.act(junk, xt[i % 3], AF.Square, accum=s4[:, 0:1])
        P.ts("dve", s4[:, 1:2], s4[:, 0:1], 1.0 / D, ALU.mult)
        P.act(s4[:, 2:3], s4[:, 1:2], AF.Sqrt, bias=epsM[:, 0:1], scale=1.0)
        recip("dve", s4[:, 3:4], s4[:, 2:3])
        P.act(xn[sl], xt[i % 3], AF.Copy, scale=s4[:, 3:4])
        for k in range(8):
            P.tr(ptb[k // 4][:, k % 4, :], xn[sl][:, k * 128:(k + 1) * 128], ident)
        for k in range(8):
            if k < 4:
                P.ts("dve", hTk[sl][k], ptb[0][:, k % 4, :], gamM[:, b, k:k + 1], ALU.mult, shM[:, b, k:k + 1], ALU.add)
            else:
                P.act(hTk[sl][k], ptb[1][:, k % 4, :], AF.Identity, bias=shM[:, b, k:k + 1], scale=gamM[:, b, k:k + 1])

    ppi = [0]

    def a_back(i):
        b = i // NT
        it = i % NT
        sl = i % 2
        for cb in range(7):
            c0 = cb * 512
            cw_ = min(512, INC - c0)
            pq = pp[ppi[0] % 5]; ppi[0] += 1
            for k in range(8):
                P.mm(pq[:, 0:cw_], hTk[sl][k], Win[:, k, c0:c0 + cw_], start=(k == 0), stop=(k == 7))
            P.copy("act" if cb % 2 == 0 else "dve", pjc[i % 3][cb], pq[:, 0:cw_])
        r0 = b * PADR + 3 + it * 128
        P.add("sp", (lambda o_, i_: (lambda e: e.dma_start(out=o_, in_=i_)))(proj_s[r0:r0 + 128, :], pj[i % 3].ap), pjc[i % 3], [], dma_key="pjA%d" % (i % 3))

    a_load(0)
    if NTILES > 1:
        a_load(1)
    a_front(0)
    for i in range(NTILES):
        if i + 2 < NTILES:
            a_load(i + 2)
        if i + 1 < NTILES:
            a_front(i + 1)
        a_back(i)
        for _ in range(2):
            next(pcg, None)
    for _ in pcg:
        pass
    P.end_phase()
    if stop_after <= 1:
        P.finish()
        return nc, tapd

    P.begin_phase()
    Wup = P.tile("Wup", [128, 512], BF16)
    Aup = P.tile("Aup", [128, 512], BF16)
    Gup = P.tile("Gup", [128, 512], BF16)
    mu_bc = P.tile("mu_bc", [128, RW])
    kk_bc = P.tile("kk_bc", [128, 512])
    ka_bc = P.tile("ka_bc", [128, 512])
    rk_bc = P.tile("rk_bc", [128, 512])
    gnw_bc = P.tile("gnw_bc", [128, 512])
    gnb_bc = P.tile("gnb_bc", [128, 512])
    bcload(mu_bc, dr["rwkv_mu"][0:1, :], 128, "c0")
    bcload(kk_bc, dr["rwkv_k_k"][0:1, :], 128, "c1")
    bcload(ka_bc, dr["rwkv_k_a"][0:1, :], 128, "c2")
    bcload(rk_bc, dr["rwkv_r_k"][0:1, :], 128, "c3")
    bcload(gnw_bc, dr["rwkv_gn_w"][0:1, :], 128, "c4")
    bcload(gnb_bc, dr["rwkv_gn_b"][0:1, :], 128, "c5")
    s = P.tile("stgB1", [128, 1536])
    P.dma("sp", s[0:64, 0:512], dr["rwkv_w_up"][:, :], "stgb1")
    P.dma("sp", s[64:65, 0:512], dr["rwkv_w0"][0:1, :], "stgb1")
    P.dma("sp", s[0:64, 512:1024], dr["rwkv_a_up"][:, :], "stgb1")
    P.dma("sp", s[64:65, 512:1024], dr["rwkv_a0"][0:1, :], "stgb1")
    P.dma("sp", s[:, 1024:1536], dr["rwkv_g_up"][:, :], "stgb1")
    P.copy("act", Wup[0:64, :], s[0:64, 0:512])
    P.copy("act", Wup[64:65, :], s[64:65, 0:512])
    P.copy("dve", Aup[0:64, :], s[0:64, 512:1024])
    P.copy("dve", Aup[64:65, :], s[64:65, 512:1024])
    P.copy("pool", Gup, s[:, 1024:1536])
    NSTR = 2 if NSEQ % 2 == 0 else 1

    def alloc_b1(q):
        sx = "_s%d" % q
        T = {"id": q}
        T["rw"] = P.tile("rw" + sx, [128, RW])
        T["rwp"] = P.tile("rwp" + sx, [128, RW])
        T["li"] = P.tile("li" + sx, [128, 256], BF16)
        T["liT"] = P.tile("liT" + sx, [128, 384], BF16)
        for n in ("sgz", "av", "gv", "Ep", "Em", "Epv", "kkk", "tmpR", "tmpR2", "kkv", "knew", "y_sb"):
            T[n] = P.tile(n + sx, [128, 512])
        T["gmL"] = P.tile("gmL" + sx, [128, 4, 2])
        T["sm8"] = P.tile("sm8" + sx, [128, 64])
        T["tok4"] = P.tile("tok4" + sx, [128, 4, 512], BF16)
        T["v_bf"] = P.tile("v_bf" + sx, [128, 512], BF16)
        T["FT"] = P.tile("FT" + sx, [128, 4, 4, 128], BF16)
        T["A_sb"] = [[P.tile("A_sb%d_%d%s" % (g, i, sx), [128, 4, 128], BF16) for i in range(2)] for g in range(2)]
        T["MT"] = [[P.tile("MT%d_%d%s" % (g, i, sx), [128, 4, 2, 128], BF16) for i in range(2)] for g in range(2)]
        T["MRB"] = P.tile("MRB" + sx, [128, 8, 2, 128], BF16)
        T["AKRK"] = P.tile("AKRK" + sx, [128, 8, 2, 128], BF16)
        T["TT"] = P.tile("TT" + sx, [128, 8, 128], BF16)
        T["X_bf"] = P.tile("X_bf" + sx, [128, 512], BF16)
        T["U_bf"] = P.tile("U_bf" + sx, [128, 512], BF16)
        T["Hst"] = P.tile("Hst" + sx, [128, 4, 64])
        T["Ht"] = P.tile("Ht" + sx, [128, 4, 64])
        T["Hbd"] = P.tile("Hbd" + sx, [128, 4, 128], BF16)
        T["yr"] = [P.tile("yr%d%s" % (i, sx), [128, 512], BF16) for i in range(2)]
        P.memset("pool", T["liT"], 1.0)
        P.memset("pool", T["Hbd"], 0.0)
        return T

    B1S = [alloc_b1(q) for q in range(NSTR)]
    ptbB = [P.psum("ptbB%d" % i, [128, 1024], BF16) for i in range(2)]
    pf = [P.psum("pfB%d" % i, [128, 512]) for i in range(5)]
    pfi = [0]

    def bank():
        b_ = pf[pfi[0] % 5]
        pfi[0] += 1
        return b_

    H8 = "p (h c) -> p h c"
    def b1_tile(i, T):
        rw = T["rw"]
        rwp = T["rwp"]
        li = T["li"]
        liT = T["liT"]
        sgz = T["sgz"]
        av = T["av"]
        gv = T["gv"]
        Ep = T["Ep"]
        Em = T["Em"]
        Epv = T["Epv"]
        gmL = T["gmL"]
        kkk = T["kkk"]
        tmpR = T["tmpR"]
        tmpR2 = T["tmpR2"]
        kkv = T["kkv"]
        knew = T["knew"]
        sm8 = T["sm8"]
        tok4 = T["tok4"]
        v_bf = T["v_bf"]
        FT = T["FT"]
        A_sb = T["A_sb"]
        MT = T["MT"]
        MRB = T["MRB"]
        AKRK = T["AKRK"]
        TT = T["TT"]
        X_bf = T["X_bf"]
        U_bf = T["U_bf"]
        Hst = T["Hst"]
        Ht = T["Ht"]
        Hbd = T["Hbd"]
        y_sb = T["y_sb"]
        yr = T["yr"]
        b = i // NT
        it = i % NT
        r0 = b * PADR + 3 + it * 128
        P.dma("sp", rw, proj_s[r0:r0 + 128, 0:RW], "rw%d" % T["id"])
        P.dma("sp", rwp, proj_s[r0 - 1:r0 + 127, 0:RW], "rwp%d" % T["id"])
        if it == 0:
            P.memset("pool", Hst, 0.0)
        u = rwp
        P.tt("dve", u, u, rw, ALU.subtract)
        P.tt("dve", u, u, mu_bc, ALU.mult)
        P.tt("dve", u, u, rw, ALU.add)
        r_ = u[:, 0:512]; k_ = u[:, 512:1024]; v_ = u[:, 1024:1536]
        if i == 0:
            tap("u0", u, [128, RW])
        yield
        P.act(li[:, 0:64], u[:, 1536:1600], AF.Tanh)
        P.copy("pool", li[:, 64:128], u[:, 1600:1664])
        P.act(li[:, 128:256], u[:, 1664:1792], AF.Sigmoid)
        pb0 = ptbB[0]
        P.tr(pb0[0:64, 0:128], li[:, 0:64], ident)
        P.tr(pb0[0:64, 128:256], li[:, 64:128], ident)
        P.tr(pb0[:, 256:384], li[:, 128:256], ident)
        P.copy("act", liT[0:64, 0:256], pb0[0:64, 0:256])
        P.copy("act", liT[:, 256:384], pb0[:, 256:384])
        pz = bank(); pa = bank(); pg = bank()
        P.mm(pz, liT[0:65, 0:128], Wup[0:65, :])
        P.mm(pa, liT[0:65, 128:256], Aup[0:65, :])
        P.mm(pg, liT[:, 256:384], Gup)
        P.act(sgz, pz, AF.Sigmoid)
        P.act(av, pa, AF.Sigmoid)
        P.copy("act", gv, pg)
        yield
        pc = bank()
        P.mm(pc, trimid, sgz)
        P.act(Ep, pc, AF.Exp)
        P.act(Em, pc, AF.Exp, scale=-1.0)
        P.stt("dve", Epv, sgz, DEC, pc, ALU.mult, ALU.add)
        P.act(Epv, Epv, AF.Exp)
        psm = bank()
        for j in range(4):
            P.mm(psm[:, 2 * j:2 * j + 2], sgz[:, j * 128:(j + 1) * 128], indmid)
        P.act(gmL, psm[:, 0:8].re("p (j t) -> p j t", j=4), AF.Exp)
        yield
        P.tt("dve", kkk, k_, kk_bc, ALU.mult)
        P.tt("dve", tmpR, kkk, kkk, ALU.mult)
        P.red("dve", sm8[:, 0:8], tmpR.re(H8, h=8), ALU.add)
        P.act(sm8[:, 8:16], sm8[:, 0:8], AF.Sqrt)
        P.ts("dve", sm8[:, 8:16], sm8[:, 8:16], 1e-12, ALU.max)
        recip("dve", sm8[:, 16:24], sm8[:, 8:16])
        P.tt("dve", kkv.re(H8, h=8), kkk.re(H8, h=8), sm8[:, 16:24].bc(2, 64), ALU.mult)
        P.stt("pool", tmpR, av, -1.0, ka_bc, ALU.add, ALU.mult)
        P.stt("pool", knew, tmpR, 1.0, k_, ALU.add, ALU.mult)
        P.tt("pool", tmpR, r_, knew, ALU.mult)
        P.tt("pool", tmpR, tmpR, rk_bc, ALU.mult)
        P.red("dve", sm8[:, 24:32], tmpR.re(H8, h=8), ALU.add)
        yield
        P.stt("dve", tok4[:, 0, :], kkv, -1.0, Epv, ALU.mult, ALU.mult)
        P.tt("dve", tok4[:, 1, :], r_, Ep, ALU.mult)
        P.tt("dve", tok4[:, 2, :], knew, Em, ALU.mult)
        P.tt("dve", tmpR2, kkv, av, ALU.mult)
        P.tt("dve", tok4[:, 3, :], tmpR2, Em, ALU.mult)
        P.copy("act", v_bf, v_)
        if i == 0:
            tap("tok4", tok4, [128, 4, 512])
            tap("sgz", sgz, [128, 512])
        yield
        for half in range(2):
            pb_ = ptbB[half]
            for jj in range(2):
                j = half * 2 + jj
                for w in range(4):
                    n = jj * 4 + w
                    P.tr(pb_[:, n * 128:(n + 1) * 128], tok4[:, w, j * 128:(j + 1) * 128], ident)
            P.copy("act" if half == 0 else "dve",
                   FT[:, half * 2:half * 2 + 2, :, :], pb_[:, :].re("p (j w t) -> p j w t", j=2, w=4))
        yield
        for g in range(2):
            pA = bank()
            pMR = [bank(), bank()]
            pKR = [bank(), bank()]
            for hh in range(4):
                j = hh
                po = 64 * g
                aT = FT[po:po + 64, j, 0, :]
                arT = FT[po:po + 64, j, 0:2, :]
                kT = FT[po:po + 64, j, 2, :]
                bT = FT[po:po + 64, j, 3, :]
                P.mm(pA[:, hh * 128:(hh + 1) * 128], aT, bT)
                P.mm(pMR[hh // 2][:, (hh % 2) * 256:(hh % 2 + 1) * 256], bT, arT)
                P.mm(pKR[hh // 2][:, (hh % 2) * 256:(hh % 2 + 1) * 256], kT, arT)
            P.tt("dve", A_sb[g][0], pA[:, :].re("p (h t) -> p h t", h=4), maskA, ALU.mult)
            for q in range(2):
                P.tt("dve", MRB[:, g * 4 + q * 2:g * 4 + q * 2 + 2, :, :], pMR[q][:, :].re("p (h w t) -> p h w t", h=2, w=2), mSI, ALU.mult)
                P.tt("dve", AKRK[:, g * 4 + q * 2:g * 4 + q * 2 + 2, :, :], pKR[q][:, :].re("p (h w t) -> p h w t", h=2, w=2), mSI, ALU.mult)
            P.tt("pool", MT[g][0][:, :, 1, :], MRB[:, g * 4:g * 4 + 4, 0, :], ident.bc(1, 4), ALU.add)
            if g == 0:
                yield
        yield
        for g in range(2):
            pM = bank(); pA2 = bank()
            for hh in range(4):
                h = g * 4 + hh
                P.mm(pM[:, hh * 128:(hh + 1) * 128], A_sb[g][0][:, hh, :], MRB[:, h, 0, :])
                P.mm(pA2[:, hh * 128:(hh + 1) * 128], MRB[:, h, 0, :], A_sb[g][0][:, hh, :])
            P.copy("act", MT[g][0][:, :, 0, :], pM[:, :].re("p (h t) -> p h t", h=4))
            P.copy("dve", A_sb[g][1], pA2[:, :].re("p (h t) -> p h t", h=4))
            if g == 0:
                yield
        yield
        cur = 0
        for lev in range(1, 6):
            yield
            for g in range(2):
                Ak = A_sb[g][lev % 2]
                An = A_sb[g][(lev + 1) % 2]
                Mc = MT[g][cur]
                Mn = MT[g][1 - cur]
                pS = [bank(), bank()]
                pA2 = bank()
                for hh in range(4):
                    reg = pS[hh // 2][:, (hh % 2) * 256:(hh % 2 + 1) * 256]
                    P.mm(reg, Ak[:, hh, :], Mc[:, hh, :, :], start=True, stop=False)
                    P.mm(reg[:, 128:256], ident, Mc[:, hh, 1, :], start=False, stop=True)
                    P.mm(pA2[:, hh * 128:(hh + 1) * 128], Mc[:, hh, 0, :], Ak[:, hh, :])
                for q in range(2):
                    P.copy("act" if q == 0 else "dve", Mn[:, q * 2:q * 2 + 2, :, :], pS[q][:, :].re("p (h w t) -> p h w t", h=2, w=2))
                P.copy("act", An, pA2[:, :].re("p (h t) -> p h t", h=4))
                if g == 0:
                    yield
            cur = 1 - cur
        for g in range(2):
            Ak = A_sb[g][0]
            Mc = MT[g][cur]
            pT = bank()
            for hh in range(4):
                P.mm(pT[:, hh * 128:(hh + 1) * 128], Ak[:, hh, :], Mc[:, hh, 1, :], start=True, stop=False)
                P.mm(pT[:, hh * 128:(hh + 1) * 128], ident, Mc[:, hh, 1, :], start=False, stop=True)
            P.copy("act" if g == 0 else "dve", TT[:, g * 4:g * 4 + 4, :], pT[:, :].re("p (h t) -> p h t", h=4))
            if g == 0:
                yield
        if i == 0:
            tap("TT", TT, [128, 8, 128])
            tap("MRB", MRB, [128, 8, 2, 128])
        yield
        P.tt("pool", Ht, Hst, gmL[:, :, 0].bc(2, 64), ALU.mult)
        P.copy("pool", Hbd[0:64, :, 0:64], Ht[0:64, :, :])
        P.copy("pool", Hbd[64:128, :, 64:128], Ht[64:128, :, :])
        pX = bank()
        SI = [(h % 2) * 4 + h // 2 for h in range(8)]
        for j in range(4):
            P.mm(pX[:, j * 128:(j + 1) * 128], FT[:, j, 0, :], Hbd[:, j, :], start=True, stop=False)
            for h in (2 * j, 2 * j + 1):
                P.mm(pX[:, h * 64:(h + 1) * 64], AKRK[:, SI[h], 0, :], v_bf[:, h * 64:(h + 1) * 64], start=False, stop=(h == 2 * j + 1))
        P.copy("act", X_bf, pX)
        yield
        pU = bank()
        for h in range(8):
            P.mm(pU[:, h * 64:(h + 1) * 64], TT[:, SI[h], :], X_bf[:, h * 64:(h + 1) * 64])
        P.copy("act", U_bf, pU)
        yield
        pY = bank()
        for j in range(4):
            P.mm(pY[:, j * 128:(j + 1) * 128], FT[:, j, 1, :], Hbd[:, j, :], start=True, stop=False)
            for h in (2 * j, 2 * j + 1):
                P.mm(pY[:, h * 64:(h + 1) * 64], AKRK[:, SI[h], 1, :], v_bf[:, h * 64:(h + 1) * 64], start=False, stop=False)
                P.mm(pY[:, h * 64:(h + 1) * 64], MRB[:, SI[h], 1, :], U_bf[:, h * 64:(h + 1) * 64], start=False, stop=(h == 2 * j + 1))
        P.copy("act", y_sb, pY)
        yield
        pH = bank()
        for j in range(4):
            P.mm(pH[:, j * 128:(j + 1) * 128], tok4[:, 2, j * 128:(j + 1) * 128], v_bf[:, j * 128:(j + 1) * 128], start=True, stop=False)
            P.mm(pH[:, j * 128:(j + 1) * 128], tok4[:, 3, j * 128:(j + 1) * 128], U_bf[:, j * 128:(j + 1) * 128], start=False, stop=True)
        pHv = pH[:, :].re("p (j v) -> p j v", j=4)
        P.tt("dve", Hst[0:64, :, :], pHv[0:64, :, 0:64], Ht[0:64, :, :], ALU.add)
        P.tt("dve", Hst[64:128, :, :], pHv[64:128, :, 64:128], Ht[64:128, :, :], ALU.add)
        P.tt("pool", Hst, Hst, gmL[:, :, 1].bc(2, 64), ALU.mult)
        yield
        if i <= 1:
            tap("y_rwkv%d" % i, y_sb, [128, 512])
        y3 = y_sb.re(H8, h=8)
        P.red("dve", sm8[:, 32:40], y3, ALU.add)
        P.tt("pool", tmpR, y_sb, y_sb, ALU.mult)
        P.red("dve", sm8[:, 40:48], tmpR.re(H8, h=8), ALU.add)
        P.ts("dve", sm8[:, 32:40], sm8[:, 32:40], 1.0 / 64, ALU.mult)
        P.tt("dve", sm8[:, 48:56], sm8[:, 32:40], sm8[:, 32:40], ALU.mult)
        P.stt("dve", sm8[:, 40:48], sm8[:, 40:48], 1.0 / 64, sm8[:, 48:56], ALU.mult, ALU.subtract)
        P.act(sm8[:, 48:56], sm8[:, 40:48], AF.Sqrt, bias=epsG[:, 0:1], scale=1.0)
        recip("dve", sm8[:, 56:64], sm8[:, 48:56])
        P.tt("dve", tmpR.re(H8, h=8), y3, sm8[:, 32:40].bc(2, 64), ALU.subtract)
        P.tt("dve", tmpR.re(H8, h=8), tmpR.re(H8, h=8), sm8[:, 56:64].bc(2, 64), ALU.mult)
        P.tt("pool", tmpR, tmpR, gnw_bc, ALU.mult)
        P.tt("dve", tmpR, tmpR, gnb_bc, ALU.add)
        P.tt("pool", tmpR2.re(H8, h=8), v_.re(H8, h=8), sm8[:, 24:32].bc(2, 64), ALU.mult)
        P.tt("dve", tmpR, tmpR, tmpR2, ALU.add)
        P.tt("pool", yr[i % 2], tmpR, gv, ALU.mult)
        P.dma("sp", ycat_s[i * 128:(i + 1) * 128, 0:512], yr[i % 2], "yrs%d_%d" % (T["id"], i % 2))
        if i == 0:
            tap("yr", yr[0], [128, 512])
        if i == 1:
            tap("yr1", yr[1], [128, 512])

    def run_streams(tile_fn, streams, offset=0):
        ns = min(len(streams), NSEQ)

        def chain(q):
            for b_ in range(q, NSEQ, ns):
                for it_ in range(NT):
                    yield from tile_fn(b_ * NT + it_, streams[q])
                    yield

        gens = [chain(q) for q in range(ns)]
        for q, g_ in enumerate(gens):
            for _ in range(q * offset):
                next(g_, None)
        live = list(gens)
        while live:
            nxt = []
            for g_ in live:
                try:
                    next(g_)
                    nxt.append(g_)
                except StopIteration:
                    pass
            live = nxt

    run_streams(b1_tile, B1S, offset=0)
    P.end_phase()
    if stop_after <= 2:
        P.finish()
        return nc, tapd

    P.begin_phase()
    hnw_bc = P.tile("hnw_bc", [128, 512])
    cvw_bc = P.tile("cvw_bc", [128, 4, 512])
    cvb_bc = P.tile("cvb_bc", [128, 512])
    gb_bc = P.tile("gb_bc", [128, 8])
    bcload(hnw_bc, dr["mlstm_hn_w"][0:1, :], 128, "c6")
    for j in range(4):
        bcload(cvw_bc[:, j, :], dr["mlstm_conv_w"][j:j + 1, :], 128, "c7")
    bcload(cvb_bc, dr["mlstm_conv_b"][0:1, :], 128, "c8")
    bcload(gb_bc[:, 0:4], dr["mlstm_i_b"][0:1, :], 128, "c9")
    bcload(gb_bc[:, 4:8], dr["mlstm_f_b"][0:1, :], 128, "c9")
    ones_bf = P.tile("ones_bf", [128, 1], BF16)

    def alloc_b2(q):
        sx = "_m%d" % q
        T = {"id": q}
        T["qk4"] = P.tile("qk4" + sx, [128, 4, 512])
        T["mr"] = P.tile("mr" + sx, [128, 1032])
        for n in ("cacc", "ctmp", "slu", "og"):
            T[n] = P.tile(n + sx, [128, 512])
        T["qq"] = P.tile("qq" + sx, [128, 768], BF16)
        T["qkT"] = P.tile("qkT" + sx, [128, 12, 128], BF16)
        T["g8"] = P.tile("g8" + sx, [128, 64])
        T["sm4"] = P.tile("sm4" + sx, [128, 16])
        T["lfb"] = P.tile("lfb" + sx, [128, 4, 128])
        T["DTm"] = P.tile("DTm" + sx, [128, 4, 128])
        T["PTm"] = P.tile("PTm" + sx, [128, 4, 128], BF16)
        T["Vb"] = P.tile("Vb" + sx, [128, 4, 128], BF16)
        T["Kw"] = P.tile("Kw" + sx, [128, 4, 64], BF16)
        T["Cst"] = P.tile("Cst" + sx, [128, 4, 128])
        T["nst"] = P.tile("nst" + sx, [128, 4])
        T["C_bf"] = P.tile("C_bf" + sx, [128, 4, 128], BF16)
        T["n_bf"] = P.tile("n_bf" + sx, [128, 4], BF16)
        T["hm"] = P.tile("hm" + sx, [128, 4, 128])
        T["ym"] = [P.tile("ym%d%s" % (i, sx), [128, 512], BF16) for i in range(2)]
        return T

    NSTR2 = 4 if NSEQ % 4 == 0 else NSTR
    B2S = [alloc_b2(q) for q in range(NSTR2)]
    ptbM = [P.psum("ptbM%d" % i, [128, 1024], BF16) for i in range(2)]
    pfm = [P.psum("pfM%d" % i, [128, 512]) for i in range(5)]
    pmi = [0]

    def bankm():
        b_ = pfm[pmi[0] % 5]
        pmi[0] += 1
        return b_

    P.memset("pool", ones_bf, 1.0)
    H4 = "p (h c) -> p h c"
    def b2_tile(i, T):
        qk4 = T["qk4"]
        mr = T["mr"]
        cacc = T["cacc"]
        ctmp = T["ctmp"]
        slu = T["slu"]
        qq = T["qq"]
        qkT = T["qkT"]
        g8 = T["g8"]
        sm4 = T["sm4"]
        lfb = T["lfb"]
        DTm = T["DTm"]
        PTm = T["PTm"]
        Vb = T["Vb"]
        Kw = T["Kw"]
        Cst = T["Cst"]
        nst = T["nst"]
        C_bf = T["C_bf"]
        n_bf = T["n_bf"]
        hm = T["hm"]
        og = T["og"]
        ym = T["ym"]
        b = i // NT
        it = i % NT
        r0 = b * PADR + 3 + it * 128
        for j in range(4):
            P.dma("sp", qk4[:, j, :], proj_s[r0 - 3 + j:r0 + 125 + j, RW:RW + 512], "qk4%d" % T["id"])
        P.dma("sp", mr, proj_s[r0:r0 + 128, RW + 512:INC], "mr%d" % T["id"])
        if it == 0:
            P.memset("pool", Cst, 0.0)
            P.memset("pool", nst, 0.0)
            P.memset("pool", C_bf, 0.0)
            P.memset("pool", n_bf, 0.0)
        q4 = qk4
        cparts = [cacc, ctmp, og, hm.re("p h v -> p (h v)")]
        for j in range(4):
            P.tt("dve" if j % 2 == 0 else "pool", cparts[j], q4[:, j, :], cvw_bc[:, j, :], ALU.mult)
        for j in range(1, 4):
            P.tt("dve", cacc, cacc, cparts[j], ALU.add)
        P.tt("dve", cacc, cacc, cvb_bc, ALU.add)
        P.act(slu, cacc, AF.Silu)
        m = mr
        P.act(og, m[:, 512:1024], AF.Tanh, scale=0.5)
        yield
        P.tt("dve", g8[:, 0:8], m[:, 1024:1032], gb_bc, ALU.add)
        P.act(g8[:, 8:16], g8[:, 0:8], AF.Exp, scale=2.0 / 15.0)
        P.ts("dve", g8[:, 8:16], g8[:, 8:16], 1.0, ALU.add)
        recip("dve", g8[:, 8:16], g8[:, 8:16])
        P.ts("dve", g8[:, 8:16], g8[:, 8:16], -30.0, ALU.mult, 15.0, ALU.add)
        P.act(g8[:, 16:20], g8[:, 12:16], AF.Exp, scale=-1.0)
        P.act(g8[:, 20:24], g8[:, 16:20], AF.Ln, bias=1.0, scale=1.0)
        P.ts("dve", g8[:, 24:28], g8[:, 20:24], -1.0, ALU.mult)
        lf = g8[:, 24:28]
        yield
        psg = bankm()
        P.mm(psg[:, 0:4], tri, lf)
        P.mm(psg[:, 4:8], onesf, lf)
        P.copy("dve", g8[:, 28:36], psg[:, 0:8])
        bt = g8[:, 28:32]; bL = g8[:, 32:36]
        P.tt("dve", g8[:, 36:40], g8[:, 8:12], bt, ALU.subtract)
        P.act(g8[:, 40:44], bt, AF.Exp)
        P.tt("dve", g8[:, 44:48], g8[:, 36:40], bL, ALU.add)
        P.act(g8[:, 44:48], g8[:, 44:48], AF.Exp)
        P.act(g8[:, 48:52], bL, AF.Exp)
        yield
        P.copy("pool", lfb, lf.bc(2, 128))
        P.ts("dve", qq[:, 0:256], slu[:, 0:256], 0.125, ALU.mult)
        P.copy("pool", qq[:, 256:512], slu[:, 256:512])
        P.stt("dve", qq[:, 512:768].re(H4, h=4), slu[:, 0:256].re(H4, h=4), 0.125, g8[:, 40:44].bc(2, 64), ALU.mult, ALU.mult)
        yield
        for n in range(12):
            pb_ = ptbM[0] if n < 8 else ptbM[1]
            nn = n % 8
            P.tr(pb_[0:64, nn * 128:(nn + 1) * 128], qq[:, n * 64:(n + 1) * 64], ident)
        P.copy("act", qkT[0:64, 0:8, :], ptbM[0][0:64, :].re("p (n t) -> p n t", n=8))
        P.copy("dve", qkT[0:64, 8:12, :], ptbM[1][0:64, 0:512].re("p (n t) -> p n t", n=4))
        P.copy("act", Vb, m[:, 0:512].re("p (h v) -> p h v", h=4))
        P.tt("pool", Kw, slu[:, 256:512].re(H4, h=4), g8[:, 44:48].bc(2, 64), ALU.mult)
        yield
        pE = bankm(); pS_ = bankm(); pN = bankm(); pCm = bankm(); pdn = bankm()
        for h in range(4):
            P.mm(pE[:, h * 128:(h + 1) * 128], lfb[:, h, :], tri, start=True, stop=False)
            P.mm(pE[:, h * 128:(h + 1) * 128], identf, masknegf, start=False, stop=True)
        for h in range(4):
            P.mm(pS_[:, h * 128:(h + 1) * 128], qkT[0:64, 4 + h, :], qkT[0:64, h, :])
        for h in range(4):
            P.act(DTm[:, h, :], pE[:, h * 128:(h + 1) * 128], AF.Exp, bias=g8[:, 36 + h:37 + h], scale=1.0)
        P.tt("dve", PTm, pS_[:, :].re("p (h t) -> p h t", h=4), DTm, ALU.mult)
        for h in range(4):
            P.mm(pN[:, h * 128:(h + 1) * 128], PTm[:, h, :], Vb[:, h, :], start=True, stop=False)
            P.mm(pN[:, h * 128:(h + 1) * 128], qkT[0:64, 8 + h, :], C_bf[0:64, h, :], start=False, stop=True)
            P.mm(pdn[:, h:h + 1], PTm[:, h, :], ones_bf[:, 0:1], start=True, stop=False)
            P.mm(pdn[:, h:h + 1], qkT[0:64, 8 + h, :], n_bf[0:64, h:h + 1], start=False, stop=True)
            P.mm(pCm[0:64, h * 128:(h + 1) * 128], Kw[:, h, :], Vb[:, h, :])
            P.mm(pdn[0:64, 8 + h:9 + h], Kw[:, h, :], ones_bf[:, 0:1])
        P.copy("dve", g8[:, 52:56], pdn[:, 0:4])
        P.stt("dve", g8[:, 56:60], g8[:, 52:56], -1.0, g8[:, 52:56], ALU.mult, ALU.max)
        P.ts("dve", g8[:, 56:60], g8[:, 56:60], 1.0, ALU.max)
        recip("dve", g8[:, 60:64], g8[:, 56:60])
        P.tt("dve", hm, pN[:, :].re("p (h v) -> p h v", h=4), g8[:, 60:64].bc(2, 128), ALU.mult)
        if i <= 1:
            tap("h_mlstm%d" % i, hm, [128, 4, 128])
        P.tt("pool", Cst[0:64], Cst[0:64], g8[0:64, 48:52].bc(2, 128), ALU.mult)
        P.tt("dve", Cst[0:64], Cst[0:64], pCm[0:64, :].re("p (h v) -> p h v", h=4), ALU.add)
        P.tt("pool", nst[0:64], nst[0:64], g8[0:64, 48:52], ALU.mult)
        P.tt("dve", nst[0:64], nst[0:64], pdn[0:64, 8:12], ALU.add)
        P.copy("pool", C_bf[0:64], Cst[0:64])
        P.copy("pool", n_bf[0:64], nst[0:64])
        yield
        hflat = hm.re("p h v -> p (h v)")
        P.tt("pool", ctmp.re("p (h v) -> p h v", h=4), hm, hm, ALU.mult)
        P.red("dve", sm4[:, 0:4], ctmp.re("p (h v) -> p h v", h=4), ALU.add)
        P.ts("dve", sm4[:, 0:4], sm4[:, 0:4], 1.0 / 128, ALU.mult)
        P.act(sm4[:, 4:8], sm4[:, 0:4], AF.Ln, bias=epsM[:, 0:1], scale=1.0)
        P.act(sm4[:, 8:12], sm4[:, 4:8], AF.Exp, scale=-0.5)
        P.ts("dve", sm4[:, 8:12], sm4[:, 8:12], 0.5, ALU.mult)
        P.tt("dve", hm, hm, sm4[:, 8:12].bc(2, 128), ALU.mult)
        P.tt("pool", hflat, hflat, hnw_bc, ALU.mult)
        P.stt("dve", ym[i % 2], og, 1.0, hflat, ALU.add, ALU.mult)
        P.dma("sp", ycat_s[i * 128:(i + 1) * 128, 512:1024], ym[i % 2], "yms%d_%d" % (T["id"], i % 2))
        if i == 0:
            tap("ym", ym[0], [128, 512])
        if i == 1:
            tap("ym1", ym[1], [128, 512])

    run_streams(b2_tile, B2S, offset=2)
    P.end_phase()
    if stop_after <= 3:
        P.finish()
        return nc, tapd

    P.begin_phase()
    Wout = P.tile("Wout", [128, 8, D], BF16)
    Wgr = P.tile("Wgr", [128, 8, 36], BF16)
    brt_bc = P.tile("brt_bc", [128, 36])
    stg3 = [P.tile("stg3_%d" % i, [128, D]) for i in range(2)]
    for k in range(8):
        s = stg3[k % 2]
        P.dma("sp", s, dr["w_out"][k * 128:(k + 1) * 128, :], "stg3_%d" % (k % 2))
        P.copy(ceng[k % 3], Wout[:, k, :], s)
    s = P.tile("stg3r", [128, 288])
    P.dma("sp", s[:, 0:32].re("p (k g) -> p k g", k=8), dr["moe_w_group"].rearrange("(k p) g -> p k g", p=128), "stg3r")
    P.dma("sp", s[:, 32:288].re("p (k g) -> p k g", k=8), dr["moe_w_router"].rearrange("(k p) g -> p k g", p=128), "stg3r")
    P.copy("act", Wgr[:, :, 0:4], s[:, 0:32].re("p (k g) -> p k g", k=8))
    P.copy("act", Wgr[:, :, 4:36], s[:, 32:288].re("p (k g) -> p k g", k=8))
    bcload(brt_bc[:, 0:4], dr["moe_b_group"][0:1, :], 128, "c10")
    bcload(brt_bc[:, 4:36], dr["moe_b_router"][0:1, :], 128, "c10")
    def alloc_b3(q):
        sx = "_o%d" % q
        T = {"id": q}
        T["ycat"] = P.tile("ycat" + sx, [128, D], BF16)
        T["xB"] = P.tile("xB" + sx, [128, D])
        T["ycT"] = P.tile("ycT" + sx, [128, 8, 128], BF16)
        T["x1"] = P.tile("x1" + sx, [128, D])
        T["xn2"] = P.tile("xn2" + sx, [128, D], BF16)
        T["junkB"] = P.tile("junkB" + sx, [128, D], BF16)
        T["h2T"] = P.tile("h2T" + sx, [128, 8, 128], BF16)
        T["h2Tk"] = [T["h2T"][:, k, :].sub("h2T%s_%d" % (sx, k)) for k in range(8)]
        T["lg"] = P.tile("lg" + sx, [128, 36])
        T["r8"] = P.tile("r8" + sx, [128, 96])
        T["s16"] = P.tile("s16" + sx, [128, 16])
        T["cwt"] = P.tile("cwt" + sx, [128, 4, 8])
        T["gm_bc"] = P.tile("gm_bc" + sx, [128, D])
        return T

    B3S = [alloc_b3(q) for q in range(NSTR2)]
    ptb3 = [P.psum("ptb3_%d" % i, [128, 1024], BF16) for i in range(2)]
    pf3 = [P.psum("pf3_%d" % i, [128, 512]) for i in range(5)]
    p3i = [0]

    def bank3():
        b_ = pf3[p3i[0] % 5]
        p3i[0] += 1
        return b_

    def b3_tile(i, T):
        ycat = T["ycat"]
        xB = T["xB"]
        ycT = T["ycT"]
        x1 = T["x1"]
        xn2 = T["xn2"]
        junkB = T["junkB"]
        h2T = T["h2T"]
        h2Tk = T["h2Tk"]
        lg = T["lg"]
        r8 = T["r8"]
        s16 = T["s16"]
        cwt = T["cwt"]
        gm_bc = T["gm_bc"]
        b = i // NT
        it = i % NT
        P.dma("sp", ycat, ycat_s[i * 128:(i + 1) * 128, :], "ycl%d" % T["id"])
        P.dma("sp", xB, dr["x"][i * 128:(i + 1) * 128, :], "xB%d" % T["id"])
        if it == 0:
            P.dma("sp", gm_bc, mod_s[b:b + 1, 2048:3072].partition_broadcast(128), "gmbc%d" % T["id"])
        for k in range(8):
            P.tr(ptb3[0][:, k * 128:(k + 1) * 128], ycat[:, k * 128:(k + 1) * 128], ident)
        P.copy("dve", ycT, ptb3[0][:, :].re("p (k t) -> p k t", k=8))
        yield
        x1t = x1
        for cb in range(2):
            po_ = bank3()
            for k in range(8):
                P.mm(po_, ycT[:, k, :], Wout[:, k, cb * 512:(cb + 1) * 512], start=(k == 0), stop=(k == 7))
            P.tt("dve", x1t[:, cb * 512:(cb + 1) * 512], po_, gm_bc[:, cb * 512:(cb + 1) * 512], ALU.mult)
        P.tt("pool", x1t, x1t, xB, ALU.add)
        P.dma("sp", x1_s[i * 128:(i + 1) * 128, :], x1t, "x1s%d" % T["id"])
        if i == 0:
            tap("x1", x1t, [128, D])
        if i == 1:
            tap("x1b", x1t, [128, D])
        yield
        P.act(junkB, x1t, AF.Square, accum=s16[:, 0:1])
        P.ts("dve", s16[:, 1:2], s16[:, 0:1], 1.0 / D, ALU.mult)
        P.act(s16[:, 2:3], s16[:, 1:2], AF.Ln, bias=epsM[:, 0:1], scale=1.0)
        P.act(s16[:, 3:4], s16[:, 2:3], AF.Exp, scale=-0.5)
        P.act(xn2, x1t, AF.Copy, scale=s16[:, 3:4])
        for k in range(8):
            P.tr(ptb3[1][:, k * 128:(k + 1) * 128], xn2[:, k * 128:(k + 1) * 128], ident)
        h2 = h2T
        h2k = h2Tk
        for k in range(8):
            P.act(h2k[k], ptb3[1][:, k * 128:(k + 1) * 128], AF.Identity, bias=shF[:, b, k:k + 1], scale=gamF[:, b, k:k + 1])
        P.add("sp", (lambda o_, i_: (lambda e: e.dma_start(out=o_, in_=i_)))(h2T_s[:, :, i * 128:(i + 1) * 128], h2.ap), h2k, [], dma_key="h2s%d" % T["id"])
        yield
        pr = bank3()
        for k in range(8):
            P.mm(pr[:, 0:36], h2k[k], Wgr[:, k, :], start=(k == 0), stop=(k == 7))
        P.tt("dve", lg, pr[:, 0:36], brt_bc, ALU.add)
        yield
        P.red("dve", r8[:, 0:1], lg[:, 0:4], ALU.max)
        P.ts("dve", r8[:, 1:5], lg[:, 0:4], r8[:, 0:1], ALU.is_equal)
        P.ts("dve", r8[:, 5:6], r8[:, 0:1], -1.0, ALU.mult)
        P.act(r8[:, 6:10], lg[:, 0:4], AF.Exp, bias=r8[:, 5:6], scale=1.0, accum=r8[:, 10:11])
        recip("dve", r8[:, 11:12], r8[:, 10:11])
        P.tt("dve", r8[:, 16:48].re("p (g e) -> p g e", g=4), lg[:, 4:36].re("p (g e) -> p g e", g=4), r8[:, 1:5].bc(2, 8), ALU.mult)
        P.red("dve", r8[:, 48:56], r8[:, 16:48].re("p (g e) -> p e g", g=4), ALU.add)
        P.red("dve", r8[:, 56:57], r8[:, 48:56], ALU.max)
        P.ts("dve", r8[:, 64:72], r8[:, 48:56], r8[:, 56:57], ALU.is_equal)
        P.stt("dve", r8[:, 72:80], r8[:, 64:72], -1e30, r8[:, 48:56], ALU.mult, ALU.add)
        P.red("dve", r8[:, 57:58], r8[:, 72:80], ALU.max)
        P.ts("dve", r8[:, 80:88], r8[:, 72:80], r8[:, 57:58], ALU.is_equal)
        P.tt("dve", r8[:, 58:59], r8[:, 56:57], r8[:, 57:58], ALU.subtract)
        P.act(r8[:, 60:61], r8[:, 58:59], AF.Exp, scale=-1.0)
        P.ts("dve", r8[:, 59:60], r8[:, 60:61], 1.0, ALU.add)
        recip("dve", r8[:, 59:60], r8[:, 59:60])
        P.tt("dve", r8[:, 60:61], r8[:, 60:61], r8[:, 59:60], ALU.mult)
        P.ts("dve", r8[:, 64:72], r8[:, 64:72], r8[:, 59:60], ALU.mult)
        P.stt("dve", r8[:, 64:72], r8[:, 80:88], r8[:, 60:61], r8[:, 64:72], ALU.mult, ALU.add)
        P.ts("dve", r8[:, 64:72], r8[:, 64:72], r8[:, 11:12], ALU.mult)
        cw_t = cwt
        P.tt("dve", cw_t, r8[:, 1:5].bc(2, 8), r8[:, 64:72].bc(1, 4), ALU.mult)
        P.dma("sp", cw_s[i * 128:(i + 1) * 128, :], cw_t.re("p g e -> p (g e)"), "cws%d" % T["id"])
        if i == 0:
            tap("cw", cw_t, [128, 4, 8])

    run_streams(b3_tile, B3S, offset=0)
    P.end_phase()
    if stop_after <= 4:
        P.finish()
        return nc, tapd

    P.begin_phase()
    BLK = min(1024, SEQ)
    SUB = min(512, BLK)
    NB = NTOK // BLK
    TPB = BLK // 128
    fnw_bc = P.tile("fnw_bc", [128, D])
    bcload(fnw_bc, dr["final_norm_w"][0:1, :], 128, "c11")
    h2bs = [P.tile("h2b%d" % i, [128, 8, BLK], BF16) for i in range(2)]
    cwbs = [P.tile("cwb%d" % i, [128, TPB, NE]) for i in range(2)]
    yaccss = [[P.tile("yacc%d_%d" % (j, i), [128, TPB, 512]) for i in range(2)] for j in range(2)]
    gf_bcs = [P.tile("gf_bc%d" % i, [128, D]) for i in range(2)]
    wgb = [P.tile("wgb%d" % i, [128, 8, DE], BF16) for i in range(2)]
    wub = [P.tile("wub%d" % i, [128, 8, DE], BF16) for i in range(2)]
    wdb = [P.tile("wdb%d" % i, [128, 2, D], BF16) for i in range(2)]
    sg = [P.tile("sg%d" % i, [128, SUB]) for i in range(2)]
    actT = [[P.tile("actT%d_%d" % (s_, f), [128, SUB], BF16) for f in range(2)] for s_ in range(2)]
    x1c = [P.tile("x1c%d" % i, [128, D]) for i in range(2)]
    junkC = P.tile("junkC", [128, D], BF16)
    pG = [P.psum("pG%d" % i, [128, 512]) for i in range(2)]
    pUu = [P.psum("pU%d" % i, [128, 512]) for i in range(2)]
    pD = [P.psum("pD%d" % i, [128, 512]) for i in range(3)]
    pdi = [0]
    wcnt = [0]

    obig = P.tile("obig", [128, TPB, D])
    obt = [obig[:, ti, :].sub("obig_%d" % ti) for ti in range(TPB)]
    ssq = P.tile("ssqC", [128, 4, TPB])

    def epilogue(blk, slot):
        for ti in range(TPB):
            gi = blk * TPB + ti
            sl = gi % 2
            P.dma("sp", x1c[sl], x1_s[gi * 128:(gi + 1) * 128, :], "x1c%d" % sl)
            o = obt[ti]
            for cb in range(2):
                P.tt("dve", o[:, cb * 512:(cb + 1) * 512], yaccss[slot][cb][:, ti, :], gf_bcs[slot][:, cb * 512:(cb + 1) * 512], ALU.mult)
            P.tt("dve", o, o, x1c[sl], ALU.add)
            P.act(junkC, o, AF.Square, accum=ssq[:, 0, ti:ti + 1])
        P.ts("dve", ssq[:, 1, :], ssq[:, 0, :], 1.0 / D, ALU.mult)
        P.act(ssq[:, 2, :], ssq[:, 1, :], AF.Sqrt, bias=epsM[:, 0:1], scale=1.0)
        recip("dve", ssq[:, 3, :], ssq[:, 2, :])
        for ti in range(TPB):
            gi = blk * TPB + ti
            o = obt[ti]
            P.stt("dve", o, o, ssq[:, 3, ti:ti + 1], fnw_bc, ALU.mult, ALU.mult)
            P.dma("sp", out_d[gi * 128:(gi + 1) * 128, :], o, "outC%d" % ti)

    for blk in range(NB):
        t0 = blk * BLK
        b = t0 // SEQ
        slot = blk % 2
        h2b = h2bs[slot]
        cwb = cwbs[slot]
        yaccs = yaccss[slot]
        def blk_loads(bk):
            t0_ = bk * BLK
            sl_ = bk % 2
            P.dma("sp", h2bs[sl_], h2T_s[:, :, t0_:t0_ + BLK], "h2b%d" % sl_)
            P.dma("sp", cwbs[sl_], cw_s[t0_:t0_ + BLK, :].rearrange("(n p) e -> p n e", p=128), "cwb%d" % sl_)
            P.dma("sp", gf_bcs[sl_], mod_s[t0_ // SEQ:t0_ // SEQ + 1, 5120:6144].partition_broadcast(128), "gfbc%d" % sl_)

        if blk == 0:
            blk_loads(0)
        NSB = BLK // SUB
        units = [(e, sb, (e * NSB + sb) % 2) for e in range(NE) for sb in range(NSB)]

        def c_loads(e):
            ws = wcnt[0] % 2
            wcnt[0] += 1
            P.dma("sp", wgb[ws], wg_s[e].rearrange("p (k f) -> p k f", k=8), "wgb%d" % ws)
            P.dma("sp", wub[ws], wu_s[e].rearrange("p (k f) -> p k f", k=8), "wub%d" % ws)
            P.dma("sp", wdb[ws], wd_s[e].rearrange("p (c d) -> p c d", c=2), "wdb%d" % ws)
            return ws

        wslot = {}

        def c_gu(u, fc):
            e, sb, asl = u
            ws = wslot[e]
            for k in range(8):
                P.mm(pG[fc][:, 0:SUB], wgb[ws][:, k, fc * 128:(fc + 1) * 128], h2b[:, k, sb * SUB:(sb + 1) * SUB], start=(k == 0), stop=(k == 7))
            for k in range(8):
                P.mm(pUu[fc][:, 0:SUB], wub[ws][:, k, fc * 128:(fc + 1) * 128], h2b[:, k, sb * SUB:(sb + 1) * SUB], start=(k == 0), stop=(k == 7))
            P.act(sg[fc], pG[fc][:, 0:SUB], AF.Silu)
            P.tt("dve", actT[asl][fc], pUu[fc][:, 0:SUB], sg[fc], ALU.mult)

        def c_down(u):
            e, sb, asl = u
            ws = wslot[e]
            for tt_ in range(SUB // 128):
                ti = sb * (SUB // 128) + tt_
                for cb in range(2):
                    pd_ = pD[pdi[0] % 3]; pdi[0] += 1
                    for fc in range(2):
                        P.mm(pd_, actT[asl][fc][:, tt_ * 128:(tt_ + 1) * 128], wdb[ws][:, fc, cb * 512:(cb + 1) * 512], start=(fc == 0), stop=(fc == 1))
                    ysl = yaccs[cb][:, ti, :]
                    if e == 0:
                        P.ts("dve", ysl, pd_, cwb[:, ti, e:e + 1], ALU.mult)
                    else:
                        P.stt("dve", ysl, pd_, cwb[:, ti, e:e + 1], ysl, ALU.mult, ALU.add)

        wslot[0] = c_loads(0)
        c_gu(units[0], 0)
        c_gu(units[0], 1)
        for n in range(len(units)):
            if n + 1 < len(units):
                if units[n + 1][1] == 0:
                    wslot[units[n + 1][0]] = c_loads(units[n + 1][0])
                c_gu(units[n + 1], 0)
            c_down(units[n])
            if n + 1 < len(units):
                c_gu(units[n + 1], 1)
            if n == len(units) // 2 and blk + 1 < NB:
                blk_loads(blk + 1)
        epilogue(blk, slot)
    P.end_phase()
    P.finish()
    return nc, tapd


_NC_CACHE = {}


def kernel(**inputs):
    NCORES = 8
    x = np.asarray(inputs["x"], dtype=np.float32)
    B, S, _ = x.shape
    NSEQ = B // NCORES
    key = (NSEQ, S)
    if key not in _NC_CACHE:
        _NC_CACHE[key] = build(NSEQ, S)[0]
    nc = _NC_CACHE[key]
    shared = {}
    for k, shp in PARAM_SHAPES.items():
        shared[k] = np.ascontiguousarray(np.asarray(inputs[k], dtype=np.float32).reshape(shp))
    c = np.asarray(inputs["c"], dtype=np.float32)
    in_maps = []
    for i in range(NCORES):
        m = dict(shared)
        m["x"] = np.ascontiguousarray(x[i * NSEQ:(i + 1) * NSEQ].reshape(NSEQ * S, D))
        m["c"] = np.ascontiguousarray(c[i * NSEQ:(i + 1) * NSEQ])
        in_maps.append(m)
    res = run_bass_kernel_spmd(nc, in_maps, core_ids=list(range(NCORES)))
    outs = [np.asarray(r["out"]).reshape(NSEQ, S, D) for r in res.results]
    return np.concatenate(outs, axis=0).astype(np.float32)
```

```python
import numpy as np
from concourse.bass_utils import run_bass_kernel_spmd
import numpy as np
from contextlib import ExitStack
import concourse.bass as bass
import concourse.mybir as mybir

F32 = mybir.dt.float32
BF16 = mybir.dt.bfloat16
AF = mybir.ActivationFunctionType
ALU = mybir.AluOpType
AX = mybir.AxisListType

ENGS = ("pe", "act", "dve", "pool", "sp")
SEM_CAP = 30000


class Buf:
    __slots__ = ("name", "writers", "readers", "psum")

    def __init__(self, name, psum=False):
        self.name = name
        self.writers = {}
        self.readers = []
        self.psum = psum


class V:
    __slots__ = ("ap", "buf")

    def __init__(self, ap, buf):
        self.ap = ap
        self.buf = buf

    def __getitem__(self, k):
        return V(self.ap[k], self.buf)

    def re(self, pat, **kw):
        return V(self.ap.rearrange(pat, **kw), self.buf)

    def bc(self, axis, n):
        a = self.ap.unsqueeze(axis)
        shp = list(a.shape)
        shp[axis] = n
        return V(a.to_broadcast(shp), self.buf)

    def sub(self, name):
        return V(self.ap, Buf(name))


class Op:
    __slots__ = ("eng", "fn", "idx", "eidx", "dma_key", "deps", "signals", "sigval", "semi", "extra", "phase")

    def __init__(self, eng, fn, dma_key):
        self.eng = eng
        self.fn = fn
        self.dma_key = dma_key
        self.deps = []
        self.signals = False
        self.sigval = 0
        self.semi = 0
        self.extra = []


def _bufs(vs):
    out = []
    for v in vs:
        if isinstance(v, V):
            out.append(v.buf)
        elif isinstance(v, Buf):
            out.append(v)
    return out


class Prog:
    def __init__(self, nc):
        self.nc = nc
        self.ops = []
        self.eops = {e: [] for e in ENGS}
        self.gstack = ExitStack()
        self.pstack = None
        self.cnt = {e: 0 for e in ENGS}
        self.dcnt = {}
        self.esems = {e: [] for e in ENGS}
        self.dsems = {}
        self.free_dsems = []
        self.waited = {e: {} for e in ENGS}
        self.carry = []
        self.nops_total = 0
        self.phase = 0

    def _stack(self):
        return self.pstack if self.pstack is not None else self.gstack

    def tile(self, name, shape, dt=F32):
        t = self._stack().enter_context(self.nc.sbuf_tensor(name, list(shape), dt))
        return V(t[:], Buf(name))

    def psum(self, name, shape, dt=F32):
        t = self._stack().enter_context(self.nc.psum_tensor(name, list(shape), dt))
        return V(t[:], Buf(name, psum=True))

    def begin_phase(self):
        self.pstack = ExitStack()
        self.ops = []
        self.eops = {e: [] for e in ENGS}

    def add(self, eng, fn, reads=(), writes=(), dma_key=None):
        op = Op(eng, fn, dma_key)
        op.idx = len(self.ops)
        op.phase = self.phase
        op.eidx = len(self.eops[eng])
        deps = {}
        wkey = ("dma", dma_key) if dma_key is not None else eng
        rb = _bufs(reads)
        wb = _bufs(writes)
        for b in rb:
            for w in b.writers.values():
                deps[id(w)] = w
            if b.psum:
                for r in b.readers:
                    if r.eng != eng:
                        deps[id(r)] = r
        for b in wb:
            for w in b.writers.values():
                deps[id(w)] = w
            for r in b.readers:
                deps[id(r)] = r
        deps.pop(id(op), None)
        op.deps = list(deps.values())
        for b in rb:
            b.readers.append(op)
        for b in wb:
            b.writers[wkey] = op
            b.readers = []
        self.ops.append(op)
        self.eops[eng].append(op)
        return op

    def mm(self, out, lhsT, rhs, start=True, stop=True, extra_r=()):
        return self.add("pe", lambda e: e.matmul(out.ap, lhsT=lhsT.ap, rhs=rhs.ap, start=start, stop=stop),
                        [lhsT, rhs] + list(extra_r), [out])

    def tr(self, out, in_, ident):
        return self.add("pe", lambda e: e.transpose(out.ap, in_.ap, ident.ap), [in_, ident], [out])

    def act(self, out, in_, func, bias=None, scale=None, accum=None, eng="act"):
        kw = {}
        r = [in_]
        if bias is not None:
            kw["bias"] = bias.ap if isinstance(bias, V) else bias
            if isinstance(bias, V):
                r.append(bias)
        if scale is not None:
            kw["scale"] = scale.ap if isinstance(scale, V) else scale
            if isinstance(scale, V):
                r.append(scale)
        w = [out]
        if accum is not None:
            kw["accum_out"] = accum.ap
            w.append(accum)
        return self.add(eng, lambda e: e.activation(out=out.ap, in_=in_.ap, func=func, **kw), r, w)

    def tt(self, eng, out, in0, in1, op):
        return self.add(eng, lambda e: e.tensor_tensor(out=out.ap, in0=in0.ap, in1=in1.ap, op=op), [in0, in1], [out])

    def ts(self, eng, out, in0, s1, op0, s2=None, op1=None, accum=None):
        r = [in0]
        a1 = s1.ap if isinstance(s1, V) else s1
        a2 = s2.ap if isinstance(s2, V) else s2
        if isinstance(s1, V):
            r.append(s1)
        if isinstance(s2, V):
            r.append(s2)
        w = [out]
        kw = {}
        if op1 is not None:
            kw["op1"] = op1
        if accum is not None:
            kw["accum_out"] = accum.ap
            w.append(accum)
        return self.add(eng, lambda e: e.tensor_scalar(out=out.ap, in0=in0.ap, scalar1=a1, scalar2=a2, op0=op0, **kw), r, w)

    def stt(self, eng, out, in0, scalar, in1, op0, op1):
        eng = "dve"
        r = [in0, in1]
        a = scalar.ap if isinstance(scalar, V) else scalar
        if isinstance(scalar, V):
            r.append(scalar)
        return self.add(eng, lambda e: e.scalar_tensor_tensor(out=out.ap, in0=in0.ap, scalar=a, in1=in1.ap, op0=op0, op1=op1), r, [out])

    def copy(self, eng, out, in_):
        if eng == "act":
            return self.add(eng, lambda e: e.copy(out=out.ap, in_=in_.ap), [in_], [out])
        return self.add(eng, lambda e: e.tensor_copy(out=out.ap, in_=in_.ap), [in_], [out])

    def red(self, eng, out, in_, op, axis=AX.X):
        return self.add(eng, lambda e: e.tensor_reduce(out=out.ap, in_=in_.ap, axis=axis, op=op), [in_], [out])

    def memset(self, eng, out, val):
        return self.add(eng, lambda e: e.memset(out.ap, val), [], [out])

    def dma(self, eng, out, in_, key, **kw):
        r = [in_] if isinstance(in_, V) else []
        w = [out] if isinstance(out, V) else []
        oa = out.ap if isinstance(out, V) else out
        ia = in_.ap if isinstance(in_, V) else in_
        return self.add(eng, lambda e: e.dma_start(out=oa, in_=ia, **kw), r, w, dma_key=key)

    def end_phase(self):
        nc = self.nc
        ops = self.ops
        need = {}
        for op in ops:
            wl = []
            for p in op.deps:
                if p.phase != self.phase:
                    continue
                if p.dma_key is not None:
                    wl.append(p)
                elif p.eng != op.eng:
                    p.signals = True
                    wl.append(p)
                else:
                    if op.eng == "pe" and op.dma_key is None:
                        continue
                    if op.dma_key is not None or (op.eidx - p.eidx) <= 4:
                        p.signals = True
                        wl.append(p)
            need[id(op)] = wl
        lastc = {}
        for e in ENGS:
            for op in reversed(self.eops[e]):
                if op.dma_key is None:
                    op.signals = True
                    lastc[e] = op
                    break
        for op in ops:
            if op.dma_key is not None:
                if op.dma_key not in self.dsems:
                    if self.free_dsems and op.eng != "pool":
                        sem_, c0_ = self.free_dsems.pop()
                        self.dsems[op.dma_key] = sem_
                        self.dcnt[op.dma_key] = c0_
                    else:
                        self.dsems[op.dma_key] = self.gstack.enter_context(nc.semaphore("d_%s" % (op.dma_key,)))
                        self.dcnt[op.dma_key] = 0
                self.dcnt[op.dma_key] += 16
                op.sigval = self.dcnt[op.dma_key]
            elif op.signals:
                c = self.cnt[op.eng]
                op.semi = c // SEM_CAP
                op.sigval = c % SEM_CAP + 1
                self.cnt[op.eng] = c + 1
                while len(self.esems[op.eng]) <= op.semi:
                    i = len(self.esems[op.eng])
                    self.esems[op.eng].append(self.gstack.enter_context(nc.semaphore("s_%s_%d" % (op.eng, i))))
        plans = {e: [] for e in ENGS}
        first = {e: True for e in ENGS}
        for op in ops:
            ws = {}
            if first[op.eng]:
                first[op.eng] = False
                for key, sem, v in self.carry:
                    if self.waited[op.eng].get(key, 0) < v:
                        ws[key] = (sem, v)
            for p in need[id(op)]:
                if p.dma_key is not None:
                    sem = self.dsems[p.dma_key]
                    key = ("dsem", id(sem))
                else:
                    key = (p.eng, p.semi)
                    sem = self.esems[p.eng][p.semi]
                v = p.sigval
                if self.waited[op.eng].get(key, 0) >= v:
                    continue
                if key not in ws or ws[key][1] < v:
                    ws[key] = (sem, v)
            for key, (sem, v) in ws.items():
                self.waited[op.eng][key] = v
            if op.dma_key is not None:
                inc = (self.dsems[op.dma_key], 16)
            elif op.signals:
                inc = (self.esems[op.eng][op.semi], 1)
            else:
                inc = None
            plans[op.eng].append((op, list(ws.values()), inc))
        carry = [(("dsem", id(self.dsems[k])), self.dsems[k], v) for k, v in self.dcnt.items()]
        for e, op in lastc.items():
            carry.append(((e, op.semi), self.esems[e][op.semi], op.sigval))
        self.carry = carry + [c for c in self.carry if c[0] not in {x[0] for x in carry}]
        final_waits = [(sem, v) for (_, sem, v) in self.carry]

        def run(engobj, plan, final=None):
            for op, ws, inc in plan:
                for sem, v in ws:
                    engobj.wait_ge(sem, v)
                ins = op.fn(engobj)
                if inc is not None:
                    ins.then_inc(inc[0], inc[1])
            if final:
                for sem, v in final:
                    engobj.wait_ge(sem, v)

        with nc.Block() as block:
            @block.tensor
            def _(e):
                run(e, plans["pe"])

            @block.scalar
            def _(e):
                run(e, plans["act"])

            @block.vector
            def _(e):
                run(e, plans["dve"])

            @block.gpsimd
            def _(e):
                run(e, plans["pool"])

            @block.sync
            def _(e):
                run(e, plans["sp"], final_waits)
        self.nops_total += len(ops)
        self.phase += 1
        for k_ in list(self.dsems):
            self.free_dsems.append((self.dsems.pop(k_), self.dcnt.pop(k_)))
        self.ops = []
        self.eops = {e: [] for e in ENGS}
        if self.pstack is not None:
            self.pstack.close()
            self.pstack = None

    def finish(self):
        self.gstack.close()

D = 1024
INC = 3336
RW = 1792
NE = 32
DE = 256
EPS = 1e-6
GN_EPS = 64e-5
DEC = 0.6065306597126334

PARAM_SHAPES = {
    "ada_w": [1024, 6144], "ada_b": [1, 6144], "mix_norm_w": [1, 1024], "w_in": [1024, 3336],
    "rwkv_mu": [1, 1792], "rwkv_w0": [1, 512], "rwkv_w_up": [64, 512], "rwkv_a0": [1, 512],
    "rwkv_a_up": [64, 512], "rwkv_g_up": [128, 512], "rwkv_k_k": [1, 512], "rwkv_k_a": [1, 512],
    "rwkv_r_k": [1, 512], "rwkv_gn_w": [1, 512], "rwkv_gn_b": [1, 512], "mlstm_conv_w": [4, 512],
    "mlstm_conv_b": [1, 512], "mlstm_i_b": [1, 4], "mlstm_f_b": [1, 4], "mlstm_hn_w": [1, 512],
    "w_out": [1024, 1024], "ffn_norm_w": [1, 1024], "moe_w_group": [1024, 4], "moe_b_group": [1, 4],
    "moe_w_router": [1024, 32], "moe_b_router": [1, 32], "moe_w_gate": [32, 1024, 256],
    "moe_w_up": [32, 1024, 256], "moe_w_down": [32, 256, 1024], "final_norm_w": [1, 1024],
}


def build(NSEQ, SEQ, taps=None, stop_after=99):
    nc = bass.Bass("TRN2", target_bir_lowering=False)
    NT = SEQ // 128
    NTOK = NSEQ * SEQ
    NTILES = NSEQ * NT
    PADR = SEQ + 3
    dr = {}
    dr["x"] = nc.dram_tensor("x", [NTOK, D], F32, kind="ExternalInput").ap()
    dr["c"] = nc.dram_tensor("c", [NSEQ, D], F32, kind="ExternalInput").ap()
    for k, shp in PARAM_SHAPES.items():
        dr[k] = nc.dram_tensor(k, shp, F32, kind="ExternalInput").ap()
    out_d = nc.dram_tensor("out", [NTOK, D], F32, kind="ExternalOutput").ap()
    proj_s = nc.dram_tensor("proj_s", [NSEQ * PADR, INC], F32).ap()
    x1_s = nc.dram_tensor("x1_s", [NTOK, D], F32).ap()
    h2T_s = nc.dram_tensor("h2T_s", [128, 8, NTOK], BF16).ap()
    cw_s = nc.dram_tensor("cw_s", [NTOK, NE], F32).ap()
    mod_s = nc.dram_tensor("mod_s", [NSEQ, 6144], F32).ap()
    wg_s = nc.dram_tensor("wg_s", [NE, 128, 2048], BF16).ap()
    wu_s = nc.dram_tensor("wu_s", [NE, 128, 2048], BF16).ap()
    wd_s = nc.dram_tensor("wd_s", [NE, 128, 2048], BF16).ap()
    tapd = {}

    P = Prog(nc)

    def tap(name, v, shape):
        if taps is None or name not in taps:
            return
        t = nc.dram_tensor("tap_" + name, list(shape), v.ap.dtype, kind="ExternalOutput").ap()
        tapd[name] = t
        P.dma("sp", t, v, "tap_" + name)

    identf = P.tile("identf", [128, 128])
    ident = P.tile("ident", [128, 128], BF16)
    tri = P.tile("tri", [128, 128])
    onesf = P.tile("onesf", [128, 128])
    trimid = P.tile("trimid", [128, 128])
    indmid = P.tile("indmid", [128, 2])
    masknegf = P.tile("masknegf", [128, 128])
    maskA = P.tile("maskA", [128, 4, 128], BF16)
    mSI = P.tile("mSI", [128, 2, 2, 128], BF16)
    epsM = P.tile("epsM", [128, 1])
    epsG = P.tile("epsG", [128, 1])
    gamM = P.tile("gamM", [128, NSEQ, 8])
    shM = P.tile("shM", [128, NSEQ, 8])
    gamF = P.tile("gamF", [128, NSEQ, 8])
    shF = P.tile("shF", [128, NSEQ, 8])

    P.begin_phase()
    P.memset("pool", identf, 1.0)
    P.add("pool", lambda e: e.affine_select(identf.ap, identf.ap, [[-1, 128]], ALU.is_equal, 0.0, base=0, channel_multiplier=1), [identf], [identf])
    P.copy("pool", ident, identf)
    P.memset("pool", tri, 1.0)
    P.add("pool", lambda e: e.affine_select(tri.ap, tri.ap, [[1, 128]], ALU.is_ge, 0.0, base=0, channel_multiplier=-1), [tri], [tri])
    P.memset("pool", onesf, 1.0)
    colm = P.tile("colm", [128, 128])
    P.memset("pool", colm, 1.0)
    P.add("pool", lambda e: e.affine_select(colm.ap, colm.ap, [[0, 128]], ALU.is_ge, 0.0, base=63, channel_multiplier=-1), [colm], [colm])
    P.tt("pool", trimid, tri, colm, ALU.subtract)
    P.ts("pool", trimid, trimid, -DEC, ALU.mult)
    P.ts("pool", indmid[:, 0:1], colm[:, 0:1], -DEC, ALU.mult)
    P.ts("pool", indmid[:, 1:2], colm[:, 0:1], DEC, ALU.mult, -DEC, ALU.add)
    P.memset("pool", masknegf, 0.0)
    P.add("pool", lambda e: e.affine_select(masknegf.ap, masknegf.ap, [[1, 128]], ALU.is_ge, -30000.0, base=0, channel_multiplier=-1), [masknegf], [masknegf])
    mstr = P.tile("mstr", [128, 128])
    P.memset("pool", mstr, 1.0)
    P.add("pool", lambda e: e.affine_select(mstr.ap, mstr.ap, [[1, 128]], ALU.is_ge, 0.0, base=-1, channel_multiplier=-1), [mstr], [mstr])
    mlow = P.tile("mlow", [128, 128])
    P.memset("pool", mlow, 1.0)
    P.add("pool", lambda e: e.affine_select(mlow.ap, mlow.ap, [[-1, 128]], ALU.is_ge, 0.0, base=-1, channel_multiplier=1), [mlow], [mlow])
    for hh in range(4):
        P.copy("pool", maskA[:, hh, :], mlow)
    for hh in range(2):
        P.copy("pool", mSI[:, hh, 0, :], mstr)
        P.copy("pool", mSI[:, hh, 1, :], tri)
    P.memset("pool", epsM, EPS)
    P.memset("pool", epsG, GN_EPS)

    def bcload(dst, src, n, key):
        P.dma("sp", dst, src.partition_broadcast(n), key)

    ct = P.tile("ct", [128, D])
    P.dma("sp", ct[0:NSEQ, :], dr["c"][:, :], "c12")
    sct = P.tile("sct", [128, D])
    P.act(sct[0:NSEQ, :], ct[0:NSEQ, :], AF.Silu)
    psA = P.psum("psA", [128, 512])
    psB = P.psum("psB", [128, 512])
    psC = P.psum("psC", [128, 512])
    scT = P.tile("scT", [128, 8, NSEQ])
    for k in range(8):
        P.mm(psA[:, k * NSEQ:(k + 1) * NSEQ], sct[0:NSEQ, k * 128:(k + 1) * 128], identf[0:NSEQ, 0:NSEQ])
    P.copy("dve", scT, psA[:, 0:8 * NSEQ].re("p (k b) -> p k b", k=8))
    adab = P.tile("adab", [128, 6144])
    P.dma("sp", adab[0:NSEQ, :], dr["ada_b"][0:1, :].partition_broadcast(NSEQ), "c13")
    nrm = P.tile("nrm", [128, 2 * D])
    P.dma("sp", nrm[0:1, 0:D], dr["mix_norm_w"][0:1, :], "c14")
    P.dma("sp", nrm[0:1, D:2 * D], dr["ffn_norm_w"][0:1, :], "c14")
    nrmT = P.tile("nrmT", [128, 16])
    for k in range(16):
        P.mm(psC[:, k:k + 1], nrm[0:1, k * 128:(k + 1) * 128], onesf[0:1, 0:1])
    P.copy("dve", nrmT, psC[:, 0:16])
    adw = [P.tile("adw%d" % i, [128, 8, 512]) for i in range(2)]
    modb = [P.tile("modb%d" % i, [128, 512]) for i in range(2)]
    modT = P.tile("modT", [128, 48, NSEQ])
    for cb in range(12):
        a = adw[cb % 2]
        P.dma("sp", a, dr["ada_w"][:, cb * 512:(cb + 1) * 512].rearrange("(k p) f -> p k f", p=128), "adw%d" % (cb % 2))
        pm = psA if cb % 2 == 0 else psB
        for k in range(8):
            P.mm(pm[0:NSEQ, :], scT[:, k, :], a[:, k, :], start=(k == 0), stop=(k == 7))
        mb = modb[cb % 2]
        P.tt("dve", mb[0:NSEQ, :], pm[0:NSEQ, :], adab[0:NSEQ, cb * 512:(cb + 1) * 512], ALU.add)
        P.dma("sp", mod_s[:, cb * 512:(cb + 1) * 512], mb[0:NSEQ, :], "mods%d" % (cb % 2))
        for q in range(4):
            P.mm(psC[:, 64 + q * NSEQ:64 + (q + 1) * NSEQ], mb[0:NSEQ, q * 128:(q + 1) * 128], identf[0:NSEQ, 0:NSEQ])
        P.copy("act", modT[:, cb * 4:(cb + 1) * 4, :], psC[:, 64:64 + 4 * NSEQ].re("p (q b) -> p q b", q=4))
    for b in range(NSEQ):
        P.stt("dve", gamM[:, b, :], modT[:, 8:16, b], 1.0, nrmT[:, 0:8], ALU.add, ALU.mult)
        P.copy("dve", shM[:, b, :], modT[:, 0:8, b])
        P.stt("dve", gamF[:, b, :], modT[:, 32:40, b], 1.0, nrmT[:, 8:16], ALU.add, ALU.mult)
        P.copy("dve", shF[:, b, :], modT[:, 24:32, b])
    zt = P.tile("zt", [128, 512])
    P.memset("pool", zt, 0.0)
    for b in range(NSEQ):
        P.dma("sp", proj_s[b * PADR:b * PADR + 3, :].rearrange("r (a f) -> (r a) f", f=417), zt[0:24, 0:417], "zpad")
    tap("gamM", gamM, [128, NSEQ, 8])
    tap("shM", shM, [128, NSEQ, 8])
    P.end_phase()
    if stop_after <= 0:
        P.finish()
        return nc, tapd

    ycat_s = nc.dram_tensor("ycat_s", [NTOK, D], BF16).ap()

    def recip(eng, o, a):
        P.add(eng, lambda e: e.reciprocal(out=o.ap, in_=a.ap), [a], [o])

    P.begin_phase()
    Win = P.tile("Win", [128, 8, INC], BF16)
    stg = [P.tile("stg%d" % i, [128, INC]) for i in range(2)]
    ceng = ["act", "dve", "pool"]
    for k in range(8):
        s = stg[k % 2]
        P.dma("sp", s, dr["w_in"][k * 128:(k + 1) * 128, :], "stg%d" % (k % 2))
        P.copy(ceng[k % 3], Win[:, k, :], s)
    xt = [P.tile("xt%d" % i, [128, D]) for i in range(3)]
    junk = P.tile("junkA", [128, D], BF16)
    xn = [P.tile("xn%d" % i, [128, D], BF16) for i in range(2)]
    hT = [P.tile("hT%d" % i, [128, 8, 128], BF16) for i in range(2)]
    hTk = [[hT[i][:, k, :].sub("hT%d_%d" % (i, k)) for k in range(8)] for i in range(2)]
    pj = [P.tile("pj%d" % i, [128, INC]) for i in range(3)]
    pjc = [[pj[i][:, cb * 512:min(INC, (cb + 1) * 512)].sub("pj%d_%d" % (i, cb)) for cb in range(7)] for i in range(3)]
    st = [P.tile("stA%d" % i, [128, 4]) for i in range(2)]
    ptb = [P.psum("ptbA%d" % i, [128, 8, 128], BF16) for i in range(2)]
    pp = [P.psum("ppA%d" % i, [128, 512]) for i in range(5)]
    wst = [P.tile("wst%d" % i, [128, 2048]) for i in range(4)]
    wbt = [P.tile("wbt%d" % i, [128, 2048], BF16) for i in range(4)]
    pc_list = []
    for e in range(NE):
        pc_list.append((dr["moe_w_gate"][e].rearrange("(k p) f -> p k f", p=128), wg_s[e], 8))
        pc_list.append((dr["moe_w_up"][e].rearrange("(k p) f -> p k f", p=128), wu_s[e], 8))
        pc_list.append((dr["moe_w_down"][e].rearrange("(c p) d -> p c d", p=128), wd_s[e], 2))

    def pc_load(n):
        src, dst, a_ = pc_list[n]
        P.dma("pool", wst[n % 4].re("p (a b) -> p a b", a=a_), src, "wst%d" % (n % 4))

    def precast_gen():
        for n in range(min(3, len(pc_list))):
            pc_load(n)
        for n in range(len(pc_list)):
            if n + 3 < len(pc_list):
                pc_load(n + 3)
            P.copy("pool" if n % 2 == 0 else "act", wbt[n % 4], wst[n % 4])
            P.dma("pool", pc_list[n][1], wbt[n % 4], "wbt%d" % (n % 4))
            yield

    pcg = precast_gen()

    def a_load(i):
        P.dma("sp", xt[i % 3], dr["x"][i * 128:(i + 1) * 128, :], "xA%d" % (i % 3))

    def a_front(i):
        b = i // NT
        sl = i % 2
        s4 = st[sl]
        P.act(junk, xt[i % 3], AF.Square, accum=s4[:, 0:1])
        P.ts("dve", s4[:, 1:2], s4[:, 0:1], 1.0 / D, ALU.mult)
        P.act(s4[:, 2:3], s4[:, 1:2], AF.Sqrt, bias=epsM[:, 0:1], scale=1.0)
        recip("dve", s4[:, 3:4], s4[:, 2:3])
        P.act(xn[sl], xt[i % 3], AF.Copy, scale=s4[:, 3:4])
        for k in range(8):
            P.tr(ptb[k // 4][:, k % 4, :], xn[sl][:, k * 128:(k + 1) * 128], ident)
        for k in range(8):
            if k < 4:
                P.ts("dve", hTk[sl][k], ptb[0][:, k % 4, :], gamM[:, b, k:k + 1], ALU.mult, shM[:, b, k:k + 1], ALU.add)
            else:
                P.act(hTk[sl][k], ptb[1][:, k % 4, :], AF.Identity, bias=shM[:, b, k:k + 1], scale=gamM[:, b, k:k + 1])

    ppi = [0]

    def a_back(i):
        b = i // NT
        it = i % NT
        sl = i % 2
        for cb in range(7):
            c0 = cb * 512
            cw_ = min(512, INC - c0)
            pq = pp[ppi[0] % 5]; ppi[0] += 1
            for k in range(8):
                P.mm(pq[:, 0:cw_], hTk[sl][k], Win[:, k, c0:c0 + cw_], start=(k == 0), stop=(k == 7))
            P.copy("act" if cb % 2 == 0 else "dve", pjc[i % 3][cb], pq[:, 0:cw_])
        r0 = b * PADR + 3 + it * 128
        P.add("sp", (lambda o_, i_: (lambda e: e.dma_start(out=o_, in_=i_)))(proj_s[r0:r0 + 128, :], pj[i % 3].ap), pjc[i % 3], [], dma_key="pjA%d" % (i % 3))

    a_load(0)
    if NTILES > 1:
        a_load(1)
    a_front(0)
    for i in range(NTILES):
        if i + 2 < NTILES:
            a_load(i + 2)
        if i + 1 < NTILES:
            a_front(i + 1)
        a_back(i)
        for _ in range(2):
            next(pcg, None)
    for _ in pcg:
        pass
    P.end_phase()
    if stop_after <= 1:
        P.finish()
        return nc, tapd

    P.begin_phase()
    Wup = P.tile("Wup", [128, 512], BF16)
    Aup = P.tile("Aup", [128, 512], BF16)
    Gup = P.tile("Gup", [128, 512], BF16)
    mu_bc = P.tile("mu_bc", [128, RW])
    kk_bc = P.tile("kk_bc", [128, 512])
    ka_bc = P.tile("ka_bc", [128, 512])
    rk_bc = P.tile("rk_bc", [128, 512])
    gnw_bc = P.tile("gnw_bc", [128, 512])
    gnb_bc = P.tile("gnb_bc", [128, 512])
    bcload(mu_bc, dr["rwkv_mu"][0:1, :], 128, "c0")
    bcload(kk_bc, dr["rwkv_k_k"][0:1, :], 128, "c1")
    bcload(ka_bc, dr["rwkv_k_a"][0:1, :], 128, "c2")
    bcload(rk_bc, dr["rwkv_r_k"][0:1, :], 128, "c3")
    bcload(gnw_bc, dr["rwkv_gn_w"][0:1, :], 128, "c4")
    bcload(gnb_bc, dr["rwkv_gn_b"][0:1, :], 128, "c5")
    s = P.tile("stgB1", [128, 1536])
    P.dma("sp", s[0:64, 0:512], dr["rwkv_w_up"][:, :], "stgb1")
    P.dma("sp", s[64:65, 0:512], dr["rwkv_w0"][0:1, :], "stgb1")
    P.dma("sp", s[0:64, 512:1024], dr["rwkv_a_up"][:, :], "stgb1")
    P.dma("sp", s[64:65, 512:1024], dr["rwkv_a0"][0:1, :], "stgb1")
    P.dma("sp", s[:, 1024:1536], dr["rwkv_g_up"][:, :], "stgb1")
    P.copy("act", Wup[0:64, :], s[0:64, 0:512])
    P.copy("act", Wup[64:65, :], s[64:65, 0:512])
    P.copy("dve", Aup[0:64, :], s[0:64, 512:1024])
    P.copy("dve", Aup[64:65, :], s[64:65, 512:1024])
    P.copy("pool", Gup, s[:, 1024:1536])
    NSTR = 2 if NSEQ % 2 == 0 else 1

    def alloc_b1(q):
        sx = "_s%d" % q
        T = {"id": q}
        T["rw"] = P.tile("rw" + sx, [128, RW])
        T["rwp"] = P.tile("rwp" + sx, [128, RW])
        T["li"] = P.tile("li" + sx, [128, 256], BF16)
        T["liT"] = P.tile("liT" + sx, [128, 384], BF16)
        for n in ("sgz", "av", "gv", "Ep", "Em", "Epv", "kkk", "tmpR", "tmpR2", "kkv", "knew", "y_sb"):
            T[n] = P.tile(n + sx, [128, 512])
        T["gmL"] = P.tile("gmL" + sx, [128, 4, 2])
        T["sm8"] = P.tile("sm8" + sx, [128, 64])
        T["tok4"] = P.tile("tok4" + sx, [128, 4, 512], BF16)
        T["v_bf"] = P.tile("v_bf" + sx, [128, 512], BF16)
        T["FT"] = P.tile("FT" + sx, [128, 4, 4, 128], BF16)
        T["A_sb"] = [[P.tile("A_sb%d_%d%s" % (g, i, sx), [128, 4, 128], BF16) for i in range(2)] for g in range(2)]
        T["MT"] = [[P.tile("MT%d_%d%s" % (g, i, sx), [128, 4, 2, 128], BF16) for i in range(2)] for g in range(2)]
        T["MRB"] = P.tile("MRB" + sx, [128, 8, 2, 128], BF16)
        T["AKRK"] = P.tile("AKRK" + sx, [128, 8, 2, 128], BF16)
        T["TT"] = P.tile("TT" + sx, [128, 8, 128], BF16)
        T["X_bf"] = P.tile("X_bf" + sx, [128, 512], BF16)
        T["U_bf"] = P.tile("U_bf" + sx, [128, 512], BF16)
        T["Hst"] = P.tile("Hst" + sx, [128, 4, 64])
        T["Ht"] = P.tile("Ht" + sx, [128, 4, 64])
        T["Hbd"] = P.tile("Hbd" + sx, [128, 4, 128], BF16)
        T["yr"] = [P.tile("yr%d%s" % (i, sx), [128, 512], BF16) for i in range(2)]
        P.memset("pool", T["liT"], 1.0)
        P.memset("pool", T["Hbd"], 0.0)
        return T

    B1S = [alloc_b1(q) for q in range(NSTR)]
    ptbB = [P.psum("ptbB%d" % i, [128, 1024], BF16) for i in range(2)]
    pf = [P.psum("pfB%d" % i, [128, 512]) for i in range(5)]
    pfi = [0]

    def bank():
        b_ = pf[pfi[0] % 5]
        pfi[0] += 1
        return b_

    H8 = "p (h c) -> p h c"
    def b1_tile(i, T):
        rw = T["rw"]
        rwp = T["rwp"]
        li = T["li"]
        liT = T["liT"]
        sgz = T["sgz"]
        av = T["av"]
        gv = T["gv"]
        Ep = T["Ep"]
        Em = T["Em"]
        Epv = T["Epv"]
        gmL = T["gmL"]
        kkk = T["kkk"]
        tmpR = T["tmpR"]
        tmpR2 = T["tmpR2"]
        kkv = T["kkv"]
        knew = T["knew"]
        sm8 = T["sm8"]
        tok4 = T["tok4"]
        v_bf = T["v_bf"]
        FT = T["FT"]
        A_sb = T["A_sb"]
        MT = T["MT"]
        MRB = T["MRB"]
        AKRK = T["AKRK"]
        TT = T["TT"]
        X_bf = T["X_bf"]
        U_bf = T["U_bf"]
        Hst = T["Hst"]
        Ht = T["Ht"]
        Hbd = T["Hbd"]
        y_sb = T["y_sb"]
        yr = T["yr"]
        b = i // NT
        it = i % NT
        r0 = b * PADR + 3 + it * 128
        P.dma("sp", rw, proj_s[r0:r0 + 128, 0:RW], "rw%d" % T["id"])
        P.dma("sp", rwp, proj_s[r0 - 1:r0 + 127, 0:RW], "rwp%d" % T["id"])
        if it == 0:
            P.memset("pool", Hst, 0.0)
        u = rwp
        P.tt("dve", u, u, rw, ALU.subtract)
        P.tt("dve", u, u, mu_bc, ALU.mult)
        P.tt("dve", u, u, rw, ALU.add)
        r_ = u[:, 0:512]; k_ = u[:, 512:1024]; v_ = u[:, 1024:1536]
        if i == 0:
            tap("u0", u, [128, RW])
        yield
        P.act(li[:, 0:64], u[:, 1536:1600], AF.Tanh)
        P.copy("pool", li[:, 64:128], u[:, 1600:1664])
        P.act(li[:, 128:256], u[:, 1664:1792], AF.Sigmoid)
        pb0 = ptbB[0]
        P.tr(pb0[0:64, 0:128], li[:, 0:64], ident)
        P.tr(pb0[0:64, 128:256], li[:, 64:128], ident)
        P.tr(pb0[:, 256:384], li[:, 128:256], ident)
        P.copy("act", liT[0:64, 0:256], pb0[0:64, 0:256])
        P.copy("act", liT[:, 256:384], pb0[:, 256:384])
        pz = bank(); pa = bank(); pg = bank()
        P.mm(pz, liT[0:65, 0:128], Wup[0:65, :])
        P.mm(pa, liT[0:65, 128:256], Aup[0:65, :])
        P.mm(pg, liT[:, 256:384], Gup)
        P.act(sgz, pz, AF.Sigmoid)
        P.act(av, pa, AF.Sigmoid)
        P.copy("act", gv, pg)
        yield
        pc = bank()
        P.mm(pc, trimid, sgz)
        P.act(Ep, pc, AF.Exp)
        P.act(Em, pc, AF.Exp, scale=-1.0)
        P.stt("dve", Epv, sgz, DEC, pc, ALU.mult, ALU.add)
        P.act(Epv, Epv, AF.Exp)
        psm = bank()
        for j in range(4):
            P.mm(psm[:, 2 * j:2 * j + 2], sgz[:, j * 128:(j + 1) * 128], indmid)
        P.act(gmL, psm[:, 0:8].re("p (j t) -> p j t", j=4), AF.Exp)
        yield
        P.tt("dve", kkk, k_, kk_bc, ALU.mult)
        P.tt("dve", tmpR, kkk, kkk, ALU.mult)
        P.red("dve", sm8[:, 0:8], tmpR.re(H8, h=8), ALU.add)
        P.act(sm8[:, 8:16], sm8[:, 0:8], AF.Sqrt)
        P.ts("dve", sm8[:, 8:16], sm8[:, 8:16], 1e-12, ALU.max)
        recip("dve", sm8[:, 16:24], sm8[:, 8:16])
        P.tt("dve", kkv.re(H8, h=8), kkk.re(H8, h=8), sm8[:, 16:24].bc(2, 64), ALU.mult)
        P.stt("pool", tmpR, av, -1.0, ka_bc, ALU.add, ALU.mult)
        P.stt("pool", knew, tmpR, 1.0, k_, ALU.add, ALU.mult)
        P.tt("pool", tmpR, r_, knew, ALU.mult)
        P.tt("pool", tmpR, tmpR, rk_bc, ALU.mult)
        P.red("dve", sm8[:, 24:32], tmpR.re(H8, h=8), ALU.add)
        yield
        P.stt("dve", tok4[:, 0, :], kkv, -1.0, Epv, ALU.mult, ALU.mult)
        P.tt("dve", tok4[:, 1, :], r_, Ep, ALU.mult)
        P.tt("dve", tok4[:, 2, :], knew, Em, ALU.mult)
        P.tt("dve", tmpR2, kkv, av, ALU.mult)
        P.tt("dve", tok4[:, 3, :], tmpR2, Em, ALU.mult)
        P.copy("act", v_bf, v_)
        if i == 0:
            tap("tok4", tok4, [128, 4, 512])
            tap("sgz", sgz, [128, 512])
        yield
        for half in range(2):
            pb_ = ptbB[half]
            for jj in range(2):
                j = half * 2 + jj
                for w in range(4):
                    n = jj * 4 + w
                    P.tr(pb_[:, n * 128:(n + 1) * 128], tok4[:, w, j * 128:(j + 1) * 128], ident)
            P.copy("act" if half == 0 else "dve",
                   FT[:, half * 2:half * 2 + 2, :, :], pb_[:, :].re("p (j w t) -> p j w t", j=2, w=4))
        yield
        for g in range(2):
            pA = bank()
            pMR = [bank(), bank()]
            pKR = [bank(), bank()]
            for hh in range(4):
                j = hh
                po = 64 * g
                aT = FT[po:po + 64, j, 0, :]
                arT = FT[po:po + 64, j, 0:2, :]
                kT = FT[po:po + 64, j, 2, :]
                bT = FT[po:po + 64, j, 3, :]
                P.mm(pA[:, hh * 128:(hh + 1) * 128], aT, bT)
                P.mm(pMR[hh // 2][:, (hh % 2) * 256:(hh % 2 + 1) * 256], bT, arT)
                P.mm(pKR[hh // 2][:, (hh % 2) * 256:(hh % 2 + 1) * 256], kT, arT)
            P.tt("dve", A_sb[g][0], pA[:, :].re("p (h t) -> p h t", h=4), maskA, ALU.mult)
            for q in range(2):
                P.tt("dve", MRB[:, g * 4 + q * 2:g * 4 + q * 2 + 2, :, :], pMR[q][:, :].re("p (h w t) -> p h w t", h=2, w=2), mSI, ALU.mult)
                P.tt("dve", AKRK[:, g * 4 + q * 2:g * 4 + q * 2 + 2, :, :], pKR[q][:, :].re("p (h w t) -> p h w t", h=2, w=2), mSI, ALU.mult)
            P.tt("pool", MT[g][0][:, :, 1, :], MRB[:, g * 4:g * 4 + 4, 0, :], ident.bc(1, 4), ALU.add)
            if g == 0:
                yield
        yield
        for g in range(2):
            pM = bank(); pA2 = bank()
            for hh in range(4):
                h = g * 4 + hh
                P.mm(pM[:, hh * 128:(hh + 1) * 128], A_sb[g][0][:, hh, :], MRB[:, h, 0, :])
                P.mm(pA2[:, hh * 128:(hh + 1) * 128], MRB[:, h, 0, :], A_sb[g][0][:, hh, :])
            P.copy("act", MT[g][0][:, :, 0, :], pM[:, :].re("p (h t) -> p h t", h=4))
            P.copy("dve", A_sb[g][1], pA2[:, :].re("p (h t) -> p h t", h=4))
            if g == 0:
                yield
        yield
        cur = 0
        for lev in range(1, 6):
            yield
            for g in range(2):
                Ak = A_sb[g][lev % 2]
                An = A_sb[g][(lev + 1) % 2]
                Mc = MT[g][cur]
                Mn = MT[g][1 - cur]
                pS = [bank(), bank()]
                pA2 = bank()
                for hh in range(4):
                    reg = pS[hh // 2][:, (hh % 2) * 256:(hh % 2 + 1) * 256]
                    P.mm(reg, Ak[:, hh, :], Mc[:, hh, :, :], start=True, stop=False)
                    P.mm(reg[:, 128:256], ident, Mc[:, hh, 1, :], start=False, stop=True)
                    P.mm(pA2[:, hh * 128:(hh + 1) * 128], Mc[:, hh, 0, :], Ak[:, hh, :])
                for q in range(2):
                    P.copy("act" if q == 0 else "dve", Mn[:, q * 2:q * 2 + 2, :, :], pS[q][:, :].re("p (h w t) -> p h w t", h=2, w=2))
                P.copy("act", An, pA2[:, :].re("p (h t) -> p h t", h=4))
                if g == 0:
                    yield
            cur = 1 - cur
        for g in range(2):
            Ak = A_sb[g][0]
            Mc = MT[g][cur]
            pT = bank()
            for hh in range(4):
                P.mm(pT[:, hh * 128:(hh + 1) * 128], Ak[:, hh, :], Mc[:, hh, 1, :], start=True, stop=False)
                P.mm(pT[:, hh * 128:(hh + 1) * 128], ident, Mc[:, hh, 1, :], start=False, stop=True)
            P.copy("act" if g == 0 else "dve", TT[:, g * 4:g * 4 + 4, :], pT[:, :].re("p (h t) -> p h t", h=4))
            if g == 0:
                yield
        if i == 0:
            tap("TT", TT, [128, 8, 128])
            tap("MRB", MRB, [128, 8, 2, 128])
        yield
        P.tt("pool", Ht, Hst, gmL[:, :, 0].bc(2, 64), ALU.mult)
        P.copy("pool", Hbd[0:64, :, 0:64], Ht[0:64, :, :])
        P.copy("pool", Hbd[64:128, :, 64:128], Ht[64:128, :, :])
        pX = bank()
        SI = [(h % 2) * 4 + h // 2 for h in range(8)]
        for j in range(4):
            P.mm(pX[:, j * 128:(j + 1) * 128], FT[:, j, 0, :], Hbd[:, j, :], start=True, stop=False)
            for h in (2 * j, 2 * j + 1):
                P.mm(pX[:, h * 64:(h + 1) * 64], AKRK[:, SI[h], 0, :], v_bf[:, h * 64:(h + 1) * 64], start=False, stop=(h == 2 * j + 1))
        P.copy("act", X_bf, pX)
        yield
        pU = bank()
        for h in range(8):
            P.mm(pU[:, h * 64:(h + 1) * 64], TT[:, SI[h], :], X_bf[:, h * 64:(h + 1) * 64])
        P.copy("act", U_bf, pU)
        yield
        pY = bank()
        for j in range(4):
            P.mm(pY[:, j * 128:(j + 1) * 128], FT[:, j, 1, :], Hbd[:, j, :], start=True, stop=False)
            for h in (2 * j, 2 * j + 1):
                P.mm(pY[:, h * 64:(h + 1) * 64], AKRK[:, SI[h], 1, :], v_bf[:, h * 64:(h + 1) * 64], start=False, stop=False)
                P.mm(pY[:, h * 64:(h + 1) * 64], MRB[:, SI[h], 1, :], U_bf[:, h * 64:(h + 1) * 64], start=False, stop=(h == 2 * j + 1))
        P.copy("act", y_sb, pY)
        yield
        pH = bank()
        for j in range(4):
            P.mm(pH[:, j * 128:(j + 1) * 128], tok4[:, 2, j * 128:(j + 1) * 128], v_bf[:, j * 128:(j + 1) * 128], start=True, stop=False)
            P.mm(pH[:, j * 128:(j + 1) * 128], tok4[:, 3, j * 128:(j + 1) * 128], U_bf[:, j * 128:(j + 1) * 128], start=False, stop=True)
        pHv = pH[:, :].re("p (j v) -> p j v", j=4)
        P.tt("dve", Hst[0:64, :, :], pHv[0:64, :, 0:64], Ht[0:64, :, :], ALU.add)
        P.tt("dve", Hst[64:128, :, :], pHv[64:128, :, 64:128], Ht[64:128, :, :], ALU.add)
        P.tt("pool", Hst, Hst, gmL[:, :, 1].bc(2, 64), ALU.mult)
        yield
        if i <= 1:
            tap("y_rwkv%d" % i, y_sb, [128, 512])
        y3 = y_sb.re(H8, h=8)
        P.red("dve", sm8[:, 32:40], y3, ALU.add)
        P.tt("pool", tmpR, y_sb, y_sb, ALU.mult)
        P.red("dve", sm8[:, 40:48], tmpR.re(H8, h=8), ALU.add)
        P.ts("dve", sm8[:, 32:40], sm8[:, 32:40], 1.0 / 64, ALU.mult)
        P.tt("dve", sm8[:, 48:56], sm8[:, 32:40], sm8[:, 32:40], ALU.mult)
        P.stt("dve", sm8[:, 40:48], sm8[:, 40:48], 1.0 / 64, sm8[:, 48:56], ALU.mult, ALU.subtract)
        P.act(sm8[:, 48:56], sm8[:, 40:48], AF.Sqrt, bias=epsG[:, 0:1], scale=1.0)
        recip("dve", sm8[:, 56:64], sm8[:, 48:56])
        P.tt("dve", tmpR.re(H8, h=8), y3, sm8[:, 32:40].bc(2, 64), ALU.subtract)
        P.tt("dve", tmpR.re(H8, h=8), tmpR.re(H8, h=8), sm8[:, 56:64].bc(2, 64), ALU.mult)
        P.tt("pool", tmpR, tmpR, gnw_bc, ALU.mult)
        P.tt("dve", tmpR, tmpR, gnb_bc, ALU.add)
        P.tt("pool", tmpR2.re(H8, h=8), v_.re(H8, h=8), sm8[:, 24:32].bc(2, 64), ALU.mult)
        P.tt("dve", tmpR, tmpR, tmpR2, ALU.add)
        P.tt("pool", yr[i % 2], tmpR, gv, ALU.mult)
        P.dma("sp", ycat_s[i * 128:(i + 1) * 128, 0:512], yr[i % 2], "yrs%d_%d" % (T["id"], i % 2))
        if i == 0:
            tap("yr", yr[0], [128, 512])
        if i == 1:
            tap("yr1", yr[1], [128, 512])

    def run_streams(tile_fn, streams, offset=0):
        ns = min(len(streams), NSEQ)

        def chain(q):
            for b_ in range(q, NSEQ, ns):
                for it_ in range(NT):
                    yield from tile_fn(b_ * NT + it_, streams[q])
                    yield

        gens = [chain(q) for q in range(ns)]
        for q, g_ in enumerate(gens):
            for _ in range(q * offset):
                next(g_, None)
        live = list(gens)
        while live:
            nxt = []
            for g_ in live:
                try:
                    next(g_)
                    nxt.append(g_)
                except StopIteration:
                    pass
            live = nxt

    run_streams(b1_tile, B1S, offset=0)
    P.end_phase()
    if stop_after <= 2:
        P.finish()
        return nc, tapd

    P.begin_phase()
    hnw_bc = P.tile("hnw_bc", [128, 512])
    cvw_bc = P.tile("cvw_bc", [128, 4, 512])
    cvb_bc = P.tile("cvb_bc", [128, 512])
    gb_bc = P.tile("gb_bc", [128, 8])
    bcload(hnw_bc, dr["mlstm_hn_w"][0:1, :], 128, "c6")
    for j in range(4):
        bcload(cvw_bc[:, j, :], dr["mlstm_conv_w"][j:j + 1, :], 128, "c7")
    bcload(cvb_bc, dr["mlstm_conv_b"][0:1, :], 128, "c8")
    bcload(gb_bc[:, 0:4], dr["mlstm_i_b"][0:1, :], 128, "c9")
    bcload(gb_bc[:, 4:8], dr["mlstm_f_b"][0:1, :], 128, "c9")
    ones_bf = P.tile("ones_bf", [128, 1], BF16)

    def alloc_b2(q):
        sx = "_m%d" % q
        T = {"id": q}
        T["qk4"] = P.tile("qk4" + sx, [128, 4, 512])
        T["mr"] = P.tile("mr" + sx, [128, 1032])
        for n in ("cacc", "ctmp", "slu", "og"):
            T[n] = P.tile(n + sx, [128, 512])
        T["qq"] = P.tile("qq" + sx, [128, 768], BF16)
        T["qkT"] = P.tile("qkT" + sx, [128, 12, 128], BF16)
        T["g8"] = P.tile("g8" + sx, [128, 64])
        T["sm4"] = P.tile("sm4" + sx, [128, 16])
        T["lfb"] = P.tile("lfb" + sx, [128, 4, 128])
        T["DTm"] = P.tile("DTm" + sx, [128, 4, 128])
        T["PTm"] = P.tile("PTm" + sx, [128, 4, 128], BF16)
        T["Vb"] = P.tile("Vb" + sx, [128, 4, 128], BF16)
        T["Kw"] = P.tile("Kw" + sx, [128, 4, 64], BF16)
        T["Cst"] = P.tile("Cst" + sx, [128, 4, 128])
        T["nst"] = P.tile("nst" + sx, [128, 4])
        T["C_bf"] = P.tile("C_bf" + sx, [128, 4, 128], BF16)
        T["n_bf"] = P.tile("n_bf" + sx, [128, 4], BF16)
        T["hm"] = P.tile("hm" + sx, [128, 4, 128])
        T["ym"] = [P.tile("ym%d%s" % (i, sx), [128, 512], BF16) for i in range(2)]
        return T

    NSTR2 = 4 if NSEQ % 4 == 0 else NSTR
    B2S = [alloc_b2(q) for q in range(NSTR2)]
    ptbM = [P.psum("ptbM%d" % i, [128, 1024], BF16) for i in range(2)]
    pfm = [P.psum("pfM%d" % i, [128, 512]) for i in range(5)]
    pmi = [0]

    def bankm():
        b_ = pfm[pmi[0] % 5]
        pmi[0] += 1
        return b_

    P.memset("pool", ones_bf, 1.0)
    H4 = "p (h c) -> p h c"
    def b2_tile(i, T):
        qk4 = T["qk4"]
        mr = T["mr"]
        cacc = T["cacc"]
        ctmp = T["ctmp"]
        slu = T["slu"]
        qq = T["qq"]
        qkT = T["qkT"]
        g8 = T["g8"]
        sm4 = T["sm4"]
        lfb = T["lfb"]
        DTm = T["DTm"]
        PTm = T["PTm"]
        Vb = T["Vb"]
        Kw = T["Kw"]
        Cst = T["Cst"]
        nst = T["nst"]
        C_bf = T["C_bf"]
        n_bf = T["n_bf"]
        hm = T["hm"]
        og = T["og"]
        ym = T["ym"]
        b = i // NT
        it = i % NT
        r0 = b * PADR + 3 + it * 128
        for j in range(4):
            P.dma("sp", qk4[:, j, :], proj_s[r0 - 3 + j:r0 + 125 + j, RW:RW + 512], "qk4%d" % T["id"])
        P.dma("sp", mr, proj_s[r0:r0 + 128, RW + 512:INC], "mr%d" % T["id"])
        if it == 0:
            P.memset("pool", Cst, 0.0)
            P.memset("pool", nst, 0.0)
            P.memset("pool", C_bf, 0.0)
            P.memset("pool", n_bf, 0.0)
        q4 = qk4
        cparts = [cacc, ctmp, og, hm.re("p h v -> p (h v)")]
        for j in range(4):
            P.tt("dve" if j % 2 == 0 else "pool", cparts[j], q4[:, j, :], cvw_bc[:, j, :], ALU.mult)
        for j in range(1, 4):
            P.tt("dve", cacc, cacc, cparts[j], ALU.add)
        P.tt("dve", cacc, cacc, cvb_bc, ALU.add)
        P.act(slu, cacc, AF.Silu)
        m = mr
        P.act(og, m[:, 512:1024], AF.Tanh, scale=0.5)
        yield
        P.tt("dve", g8[:, 0:8], m[:, 1024:1032], gb_bc, ALU.add)
        P.act(g8[:, 8:16], g8[:, 0:8], AF.Exp, scale=2.0 / 15.0)
        P.ts("dve", g8[:, 8:16], g8[:, 8:16], 1.0, ALU.add)
        recip("dve", g8[:, 8:16], g8[:, 8:16])
        P.ts("dve", g8[:, 8:16], g8[:, 8:16], -30.0, ALU.mult, 15.0, ALU.add)
        P.act(g8[:, 16:20], g8[:, 12:16], AF.Exp, scale=-1.0)
        P.act(g8[:, 20:24], g8[:, 16:20], AF.Ln, bias=1.0, scale=1.0)
        P.ts("dve", g8[:, 24:28], g8[:, 20:24], -1.0, ALU.mult)
        lf = g8[:, 24:28]
        yield
        psg = bankm()
        P.mm(psg[:, 0:4], tri, lf)
        P.mm(psg[:, 4:8], onesf, lf)
        P.copy("dve", g8[:, 28:36], psg[:, 0:8])
        bt = g8[:, 28:32]; bL = g8[:, 32:36]
        P.tt("dve", g8[:, 36:40], g8[:, 8:12], bt, ALU.subtract)
        P.act(g8[:, 40:44], bt, AF.Exp)
        P.tt("dve", g8[:, 44:48], g8[:, 36:40], bL, ALU.add)
        P.act(g8[:, 44:48], g8[:, 44:48], AF.Exp)
        P.act(g8[:, 48:52], bL, AF.Exp)
        yield
        P.copy("pool", lfb, lf.bc(2, 128))
        P.ts("dve", qq[:, 0:256], slu[:, 0:256], 0.125, ALU.mult)
        P.copy("pool", qq[:, 256:512], slu[:, 256:512])
        P.stt("dve", qq[:, 512:768].re(H4, h=4), slu[:, 0:256].re(H4, h=4), 0.125, g8[:, 40:44].bc(2, 64), ALU.mult, ALU.mult)
        yield
        for n in range(12):
            pb_ = ptbM[0] if n < 8 else ptbM[1]
            nn = n % 8
            P.tr(pb_[0:64, nn * 128:(nn + 1) * 128], qq[:, n * 64:(n + 1) * 64], ident)
        P.copy("act", qkT[0:64, 0:8, :], ptbM[0][0:64, :].re("p (n t) -> p n t", n=8))
        P.copy("dve", qkT[0:64, 8:12, :], ptbM[1][0:64, 0:512].re("p (n t) -> p n t", n=4))
        P.copy("act", Vb, m[:, 0:512].re("p (h v) -> p h v", h=4))
        P.tt("pool", Kw, slu[:, 256:512].re(H4, h=4), g8[:, 44:48].bc(2, 64), ALU.mult)
        yield
        pE = bankm(); pS_ = bankm(); pN = bankm(); pCm = bankm(); pdn = bankm()
        for h in range(4):
            P.mm(pE[:, h * 128:(h + 1) * 128], lfb[:, h, :], tri, start=True, stop=False)
            P.mm(pE[:, h * 128:(h + 1) * 128], identf, masknegf, start=False, stop=True)
        for h in range(4):
            P.mm(pS_[:, h * 128:(h + 1) * 128], qkT[0:64, 4 + h, :], qkT[0:64, h, :])
        for h in range(4):
            P.act(DTm[:, h, :], pE[:, h * 128:(h + 1) * 128], AF.Exp, bias=g8[:, 36 + h:37 + h], scale=1.0)
        P.tt("dve", PTm, pS_[:, :].re("p (h t) -> p h t", h=4), DTm, ALU.mult)
        for h in range(4):
            P.mm(pN[:, h * 128:(h + 1) * 128], PTm[:, h, :], Vb[:, h, :], start=True, stop=False)
            P.mm(pN[:, h * 128:(h + 1) * 128], qkT[0:64, 8 + h, :], C_bf[0:64, h, :], start=False, stop=True)
            P.mm(pdn[:, h:h + 1], PTm[:, h, :], ones_bf[:, 0:1], start=True, stop=False)
            P.mm(pdn[:, h:h + 1], qkT[0:64, 8 + h, :], n_bf[0:64, h:h + 1], start=False, stop=True)
            P.mm(pCm[0:64, h * 128:(h + 1) * 128], Kw[:, h, :], Vb[:, h, :])
            P.mm(pdn[0:64, 8 + h:9 + h], Kw[:, h, :], ones_bf[:, 0:1])
        P.copy("dve", g8[:, 52:56], pdn[:, 0:4])
        P.stt("dve", g8[:, 56:60], g8[:, 52:56], -1.0, g8[:, 52:56], ALU.mult, ALU.max)
        P.ts("dve", g8[:, 56:60], g8[:, 56:60], 1.0, ALU.max)
        recip("dve", g8[:, 60:64], g8[:, 56:60])
        P.tt("dve", hm, pN[:, :].re("p (h v) -> p h v", h=4), g8[:, 60:64].bc(2, 128), ALU.mult)
        if i <= 1:
            tap("h_mlstm%d" % i, hm, [128, 4, 128])
        P.tt("pool", Cst[0:64], Cst[0:64], g8[0:64, 48:52].bc(2, 128), ALU.mult)
        P.tt("dve", Cst[0:64], Cst[0:64], pCm[0:64, :].re("p (h v) -> p h v", h=4), ALU.add)
        P.tt("pool", nst[0:64], nst[0:64], g8[0:64, 48:52], ALU.mult)
        P.tt("dve", nst[0:64], nst[0:64], pdn[0:64, 8:12], ALU.add)
        P.copy("pool", C_bf[0:64], Cst[0:64])
        P.copy("pool", n_bf[0:64], nst[0:64])
        yield
        hflat = hm.re("p h v -> p (h v)")
        P.tt("pool", ctmp.re("p (h v) -> p h v", h=4), hm, hm, ALU.mult)
        P.red("dve", sm4[:, 0:4], ctmp.re("p (h v) -> p h v", h=4), ALU.add)
        P.ts("dve", sm4[:, 0:4], sm4[:, 0:4], 1.0 / 128, ALU.mult)
        P.act(sm4[:, 4:8], sm4[:, 0:4], AF.Ln, bias=epsM[:, 0:1], scale=1.0)
        P.act(sm4[:, 8:12], sm4[:, 4:8], AF.Exp, scale=-0.5)
        P.ts("dve", sm4[:, 8:12], sm4[:, 8:12], 0.5, ALU.mult)
        P.tt("dve", hm, hm, sm4[:, 8:12].bc(2, 128), ALU.mult)
        P.tt("pool", hflat, hflat, hnw_bc, ALU.mult)
        P.stt("dve", ym[i % 2], og, 1.0, hflat, ALU.add, ALU.mult)
        P.dma("sp", ycat_s[i * 128:(i + 1) * 128, 512:1024], ym[i % 2], "yms%d_%d" % (T["id"], i % 2))
        if i == 0:
            tap("ym", ym[0], [128, 512])
        if i == 1:
            tap("ym1", ym[1], [128, 512])

    run_streams(b2_tile, B2S, offset=2)
    P.end_phase()
    if stop_after <= 3:
        P.finish()
        return nc, tapd

    P.begin_phase()
    Wout = P.tile("Wout", [128, 8, D], BF16)
    Wgr = P.tile("Wgr", [128, 8, 36], BF16)
    brt_bc = P.tile("brt_bc", [128, 36])
    stg3 = [P.tile("stg3_%d" % i, [128, D]) for i in range(2)]
    for k in range(8):
        s = stg3[k % 2]
        P.dma("sp", s, dr["w_out"][k * 128:(k + 1) * 128, :], "stg3_%d" % (k % 2))
        P.copy(ceng[k % 3], Wout[:, k, :], s)
    s = P.tile("stg3r", [128, 288])
    P.dma("sp", s[:, 0:32].re("p (k g) -> p k g", k=8), dr["moe_w_group"].rearrange("(k p) g -> p k g", p=128), "stg3r")
    P.dma("sp", s[:, 32:288].re("p (k g) -> p k g", k=8), dr["moe_w_router"].rearrange("(k p) g -> p k g", p=128), "stg3r")
    P.copy("act", Wgr[:, :, 0:4], s[:, 0:32].re("p (k g) -> p k g", k=8))
    P.copy("act", Wgr[:, :, 4:36], s[:, 32:288].re("p (k g) -> p k g", k=8))
    bcload(brt_bc[:, 0:4], dr["moe_b_group"][0:1, :], 128, "c10")
    bcload(brt_bc[:, 4:36], dr["moe_b_router"][0:1, :], 128, "c10")
    def alloc_b3(q):
        sx = "_o%d" % q
        T = {"id": q}
        T["ycat"] = P.tile("ycat" + sx, [128, D], BF16)
        T["xB"] = P.tile("xB" + sx, [128, D])
        T["ycT"] = P.tile("ycT" + sx, [128, 8, 128], BF16)
        T["x1"] = P.tile("x1" + sx, [128, D])
        T["xn2"] = P.tile("xn2" + sx, [128, D], BF16)
        T["junkB"] = P.tile("junkB" + sx, [128, D], BF16)
        T["h2T"] = P.tile("h2T" + sx, [128, 8, 128], BF16)
        T["h2Tk"] = [T["h2T"][:, k, :].sub("h2T%s_%d" % (sx, k)) for k in range(8)]
        T["lg"] = P.tile("lg" + sx, [128, 36])
        T["r8"] = P.tile("r8" + sx, [128, 96])
        T["s16"] = P.tile("s16" + sx, [128, 16])
        T["cwt"] = P.tile("cwt" + sx, [128, 4, 8])
        T["gm_bc"] = P.tile("gm_bc" + sx, [128, D])
        return T

    B3S = [alloc_b3(q) for q in range(NSTR2)]
    ptb3 = [P.psum("ptb3_%d" % i, [128, 1024], BF16) for i in range(2)]
    pf3 = [P.psum("pf3_%d" % i, [128, 512]) for i in range(5)]
    p3i = [0]

    def bank3():
        b_ = pf3[p3i[0] % 5]
        p3i[0] += 1
        return b_

    def b3_tile(i, T):
        ycat = T["ycat"]
        xB = T["xB"]
        ycT = T["ycT"]
        x1 = T["x1"]
        xn2 = T["xn2"]
        junkB = T["junkB"]
        h2T = T["h2T"]
        h2Tk = T["h2Tk"]
        lg = T["lg"]
        r8 = T["r8"]
        s16 = T["s16"]
        cwt = T["cwt"]
        gm_bc = T["gm_bc"]
        b = i // NT
        it = i % NT
        P.dma("sp", ycat, ycat_s[i * 128:(i + 1) * 128, :], "ycl%d" % T["id"])
        P.dma("sp", xB, dr["x"][i * 128:(i + 1) * 128, :], "xB%d" % T["id"])
        if it == 0:
            P.dma("sp", gm_bc, mod_s[b:b + 1, 2048:3072].partition_broadcast(128), "gmbc%d" % T["id"])
        for k in range(8):
            P.tr(ptb3[0][:, k * 128:(k + 1) * 128], ycat[:, k * 128:(k + 1) * 128], ident)
        P.copy("dve", ycT, ptb3[0][:, :].re("p (k t) -> p k t", k=8))
        yield
        x1t = x1
        for cb in range(2):
            po_ = bank3()
            for k in range(8):
                P.mm(po_, ycT[:, k, :], Wout[:, k, cb * 512:(cb + 1) * 512], start=(k == 0), stop=(k == 7))
            P.tt("dve", x1t[:, cb * 512:(cb + 1) * 512], po_, gm_bc[:, cb * 512:(cb + 1) * 512], ALU.mult)
        P.tt("pool", x1t, x1t, xB, ALU.add)
        P.dma("sp", x1_s[i * 128:(i + 1) * 128, :], x1t, "x1s%d" % T["id"])
        if i == 0:
            tap("x1", x1t, [128, D])
        if i == 1:
            tap("x1b", x1t, [128, D])
        yield
        P.act(junkB, x1t, AF.Square, accum=s16[:, 0:1])
        P.ts("dve", s16[:, 1:2], s16[:, 0:1], 1.0 / D, ALU.mult)
        P.act(s16[:, 2:3], s16[:, 1:2], AF.Ln, bias=epsM[:, 0:1], scale=1.0)
        P.act(s16[:, 3:4], s16[:, 2:3], AF.Exp, scale=-0.5)
        P.act(xn2, x1t, AF.Copy, scale=s16[:, 3:4])
        for k in range(8):
            P.tr(ptb3[1][:, k * 128:(k + 1) * 128], xn2[:, k * 128:(k + 1) * 128], ident)
        h2 = h2T
        h2k = h2Tk
        for k in range(8):
            P.act(h2k[k], ptb3[1][:, k * 128:(k + 1) * 128], AF.Identity, bias=shF[:, b, k:k + 1], scale=gamF[:, b, k:k + 1])
        P.add("sp", (lambda o_, i_: (lambda e: e.dma_start(out=o_, in_=i_)))(h2T_s[:, :, i * 128:(i + 1) * 128], h2.ap), h2k, [], dma_key="h2s%d" % T["id"])
        yield
        pr = bank3()
        for k in range(8):
            P.mm(pr[:, 0:36], h2k[k], Wgr[:, k, :], start=(k == 0), stop=(k == 7))
        P.tt("dve", lg, pr[:, 0:36], brt_bc, ALU.add)
        yield
        P.red("dve", r8[:, 0:1], lg[:, 0:4], ALU.max)
        P.ts("dve", r8[:, 1:5], lg[:, 0:4], r8[:, 0:1], ALU.is_equal)
        P.ts("dve", r8[:, 5:6], r8[:, 0:1], -1.0, ALU.mult)
        P.act(r8[:, 6:10], lg[:, 0:4], AF.Exp, bias=r8[:, 5:6], scale=1.0, accum=r8[:, 10:11])
        recip("dve", r8[:, 11:12], r8[:, 10:11])
        P.tt("dve", r8[:, 16:48].re("p (g e) -> p g e", g=4), lg[:, 4:36].re("p (g e) -> p g e", g=4), r8[:, 1:5].bc(2, 8), ALU.mult)
        P.red("dve", r8[:, 48:56], r8[:, 16:48].re("p (g e) -> p e g", g=4), ALU.add)
        P.red("dve", r8[:, 56:57], r8[:, 48:56], ALU.max)
        P.ts("dve", r8[:, 64:72], r8[:, 48:56], r8[:, 56:57], ALU.is_equal)
        P.stt("dve", r8[:, 72:80], r8[:, 64:72], -1e30, r8[:, 48:56], ALU.mult, ALU.add)
        P.red("dve", r8[:, 57:58], r8[:, 72:80], ALU.max)
        P.ts("dve", r8[:, 80:88], r8[:, 72:80], r8[:, 57:58], ALU.is_equal)
        P.tt("dve", r8[:, 58:59], r8[:, 56:57], r8[:, 57:58], ALU.subtract)
        P.act(r8[:, 60:61], r8[:, 58:59], AF.Exp, scale=-1.0)
        P.ts("dve", r8[:, 59:60], r8[:, 60:61], 1.0, ALU.add)
        recip("dve", r8[:, 59:60], r8[:, 59:60])
        P.tt("dve", r8[:, 60:61], r8[:, 60:61], r8[:, 59:60], ALU.mult)
        P.ts("dve", r8[:, 64:72], r8[:, 64:72], r8[:, 59:60], ALU.mult)
        P.stt("dve", r8[:, 64:72], r8[:, 80:88], r8[:, 60:61], r8[:, 64:72], ALU.mult, ALU.add)
        P.ts("dve", r8[:, 64:72], r8[:, 64:72], r8[:, 11:12], ALU.mult)
        cw_t = cwt
        P.tt("dve", cw_t, r8[:, 1:5].bc(2, 8), r8[:, 64:72].bc(1, 4), ALU.mult)
        P.dma("sp", cw_s[i * 128:(i + 1) * 128, :], cw_t.re("p g e -> p (g e)"), "cws%d" % T["id"])
        if i == 0:
            tap("cw", cw_t, [128, 4, 8])

    run_streams(b3_tile, B3S, offset=0)
    P.end_phase()
    if stop_after <= 4:
        P.finish()
        return nc, tapd

    P.begin_phase()
    BLK = min(1024, SEQ)
    SUB = min(512, BLK)
    NB = NTOK // BLK
    TPB = BLK // 128
    fnw_bc = P.tile("fnw_bc", [128, D])
    bcload(fnw_bc, dr["final_norm_w"][0:1, :], 128, "c11")
    h2bs = [P.tile("h2b%d" % i, [128, 8, BLK], BF16) for i in range(2)]
    cwbs = [P.tile("cwb%d" % i, [128, TPB, NE]) for i in range(2)]
    yaccss = [[P.tile("yacc%d_%d" % (j, i), [128, TPB, 512]) for i in range(2)] for j in range(2)]
    gf_bcs = [P.tile("gf_bc%d" % i, [128, D]) for i in range(2)]
    wgb = [P.tile("wgb%d" % i, [128, 8, DE], BF16) for i in range(2)]
    wub = [P.tile("wub%d" % i, [128, 8, DE], BF16) for i in range(2)]
    wdb = [P.tile("wdb%d" % i, [128, 2, D], BF16) for i in range(2)]
    sg = [P.tile("sg%d" % i, [128, SUB]) for i in range(2)]
    actT = [[P.tile("actT%d_%d" % (s_, f), [128, SUB], BF16) for f in range(2)] for s_ in range(2)]
    x1c = [P.tile("x1c%d" % i, [128, D]) for i in range(2)]
    junkC = P.tile("junkC", [128, D], BF16)
    pG = [P.psum("pG%d" % i, [128, 512]) for i in range(2)]
    pUu = [P.psum("pU%d" % i, [128, 512]) for i in range(2)]
    pD = [P.psum("pD%d" % i, [128, 512]) for i in range(3)]
    pdi = [0]
    wcnt = [0]

    obig = P.tile("obig", [128, TPB, D])
    obt = [obig[:, ti, :].sub("obig_%d" % ti) for ti in range(TPB)]
    ssq = P.tile("ssqC", [128, 4, TPB])

    def epilogue(blk, slot):
        for ti in range(TPB):
            gi = blk * TPB + ti
            sl = gi % 2
            P.dma("sp", x1c[sl], x1_s[gi * 128:(gi + 1) * 128, :], "x1c%d" % sl)
            o = obt[ti]
            for cb in range(2):
                P.tt("dve", o[:, cb * 512:(cb + 1) * 512], yaccss[slot][cb][:, ti, :], gf_bcs[slot][:, cb * 512:(cb + 1) * 512], ALU.mult)
            P.tt("dve", o, o, x1c[sl], ALU.add)
            P.act(junkC, o, AF.Square, accum=ssq[:, 0, ti:ti + 1])
        P.ts("dve", ssq[:, 1, :], ssq[:, 0, :], 1.0 / D, ALU.mult)
        P.act(ssq[:, 2, :], ssq[:, 1, :], AF.Sqrt, bias=epsM[:, 0:1], scale=1.0)
        recip("dve", ssq[:, 3, :], ssq[:, 2, :])
        for ti in range(TPB):
            gi = blk * TPB + ti
            o = obt[ti]
            P.stt("dve", o, o, ssq[:, 3, ti:ti + 1], fnw_bc, ALU.mult, ALU.mult)
            P.dma("sp", out_d[gi * 128:(gi + 1) * 128, :], o, "outC%d" % ti)

    for blk in range(NB):
        t0 = blk * BLK
        b = t0 // SEQ
        slot = blk % 2
        h2b = h2bs[slot]
        cwb = cwbs[slot]
        yaccs = yaccss[slot]
        def blk_loads(bk):
            t0_ = bk * BLK
            sl_ = bk % 2
            P.dma("sp", h2bs[sl_], h2T_s[:, :, t0_:t0_ + BLK], "h2b%d" % sl_)
            P.dma("sp", cwbs[sl_], cw_s[t0_:t0_ + BLK, :].rearrange("(n p) e -> p n e", p=128), "cwb%d" % sl_)
            P.dma("sp", gf_bcs[sl_], mod_s[t0_ // SEQ:t0_ // SEQ + 1, 5120:6144].partition_broadcast(128), "gfbc%d" % sl_)

        if blk == 0:
            blk_loads(0)
        NSB = BLK // SUB
        units = [(e, sb, (e * NSB + sb) % 2) for e in range(NE) for sb in range(NSB)]

        def c_loads(e):
            ws = wcnt[0] % 2
            wcnt[0] += 1
            P.dma("sp", wgb[ws], wg_s[e].rearrange("p (k f) -> p k f", k=8), "wgb%d" % ws)
            P.dma("sp", wub[ws], wu_s[e].rearrange("p (k f) -> p k f", k=8), "wub%d" % ws)
            P.dma("sp", wdb[ws], wd_s[e].rearrange("p (c d) -> p c d", c=2), "wdb%d" % ws)
            return ws

        wslot = {}

        def c_gu(u, fc):
            e, sb, asl = u
            ws = wslot[e]
            for k in range(8):
                P.mm(pG[fc][:, 0:SUB], wgb[ws][:, k, fc * 128:(fc + 1) * 128], h2b[:, k, sb * SUB:(sb + 1) * SUB], start=(k == 0), stop=(k == 7))
            for k in range(8):
                P.mm(pUu[fc][:, 0:SUB], wub[ws][:, k, fc * 128:(fc + 1) * 128], h2b[:, k, sb * SUB:(sb + 1) * SUB], start=(k == 0), stop=(k == 7))
            P.act(sg[fc], pG[fc][:, 0:SUB], AF.Silu)
            P.tt("dve", actT[asl][fc], pUu[fc][:, 0:SUB], sg[fc], ALU.mult)

        def c_down(u):
            e, sb, asl = u
            ws = wslot[e]
            for tt_ in range(SUB // 128):
                ti = sb * (SUB // 128) + tt_
                for cb in range(2):
                    pd_ = pD[pdi[0] % 3]; pdi[0] += 1
                    for fc in range(2):
                        P.mm(pd_, actT[asl][fc][:, tt_ * 128:(tt_ + 1) * 128], wdb[ws][:, fc, cb * 512:(cb + 1) * 512], start=(fc == 0), stop=(fc == 1))
                    ysl = yaccs[cb][:, ti, :]
                    if e == 0:
                        P.ts("dve", ysl, pd_, cwb[:, ti, e:e + 1], ALU.mult)
                    else:
                        P.stt("dve", ysl, pd_, cwb[:, ti, e:e + 1], ysl, ALU.mult, ALU.add)

        wslot[0] = c_loads(0)
        c_gu(units[0], 0)
        c_gu(units[0], 1)
        for n in range(len(units)):
            if n + 1 < len(units):
                if units[n + 1][1] == 0:
                    wslot[units[n + 1][0]] = c_loads(units[n + 1][0])
                c_gu(units[n + 1], 0)
            c_down(units[n])
            if n + 1 < len(units):
                c_gu(units[n + 1], 1)
            if n == len(units) // 2 and blk + 1 < NB:
                blk_loads(blk + 1)
        epilogue(blk, slot)
    P.end_phase()
    P.finish()
    return nc, tapd


_NC_CACHE = {}


def kernel(**inputs):
    NCORES = 8
    x = np.asarray(inputs["x"], dtype=np.float32)
    B, S, _ = x.shape
    NSEQ = B // NCORES
    key = (NSEQ, S)
    if key not in _NC_CACHE:
        _NC_CACHE[key] = build(NSEQ, S)[0]
    nc = _NC_CACHE[key]
    shared = {}
    for k, shp in PARAM_SHAPES.items():
        shared[k] = np.ascontiguousarray(np.asarray(inputs[k], dtype=np.float32).reshape(shp))
    c = np.asarray(inputs["c"], dtype=np.float32)
    in_maps = []
    for i in range(NCORES):
        m = dict(shared)
        m["x"] = np.ascontiguousarray(x[i * NSEQ:(i + 1) * NSEQ].reshape(NSEQ * S, D))
        m["c"] = np.ascontiguousarray(c[i * NSEQ:(i + 1) * NSEQ])
        in_maps.append(m)
    res = run_bass_kernel_spmd(nc, in_maps, core_ids=list(range(NCORES)))
    outs = [np.asarray(r["out"]).reshape(NSEQ, S, D) for r in res.results]
    return np.concatenate(outs, axis=0).astype(np.float32)
```
